# Optimizing a Trainium2 kernel written in Bass

```python
import math
import jax, jax.numpy as jnp
from jax import lax
import numpy as np

D_MODEL = 1024
BATCH = 16
SEQ = 4096
DEPTH = 1

ATT_HEADS = 16
ATT_KV_HEADS = 2
ATT_HEAD_DIM = 64
WINDOW = 128
ROPE_DIM = ATT_HEAD_DIM // 4
ROPE_THETA = 500000.0
ML_HEADS = 8
ML_QK_DIM = 64
ML_V_DIM = 128
ML_CHUNK = 64
GATE_CAP = 15.0
PEER_HEADS = 8
PEER_KEY_DIM = 128
N_KEYS = 128
N_EXPERTS = N_KEYS * N_KEYS
PEER_TOPK = 16
PEER_TOKEN_BLOCK = 128
EPS = 1e-6

ATT_Q_W = ATT_HEADS * ATT_HEAD_DIM
ATT_KV_W = ATT_KV_HEADS * ATT_HEAD_DIM
ML_QK_W = ML_HEADS * ML_QK_DIM
ML_V_W = ML_HEADS * ML_V_DIM
IN_SPLITS = (ATT_Q_W, ATT_KV_W, ATT_KV_W, ML_QK_W, ML_QK_W, ML_V_W, ML_HEADS, ML_HEADS, ML_V_W, D_MODEL, D_MODEL)
IN_WIDTH = ATT_Q_W + 2 * ATT_KV_W + 2 * ML_QK_W + 2 * ML_V_W + 2 * ML_HEADS + 2 * D_MODEL

kernel_name = "hybrid_mlstm_swa_sink_peer"


def rmsnorm(x, gain):
    xf = x.astype(jnp.float32)
    r = lax.rsqrt(jnp.mean(xf * xf, axis=-1, keepdims=True) + EPS)
    return (xf * r * gain.astype(jnp.float32)).astype(x.dtype)


def partial_rope(x, positions):
    half = ROPE_DIM // 2
    inv_freq = ROPE_THETA ** (-jnp.arange(0, ROPE_DIM, 2, dtype=jnp.float32) / ROPE_DIM)
    ang = positions.astype(jnp.float32)[..., None] * inv_freq
    cos = jnp.cos(ang)[:, :, None, :]
    sin = jnp.sin(ang)[:, :, None, :]
    xf = x.astype(jnp.float32)
    x1 = xf[..., :half]
    x2 = xf[..., half:ROPE_DIM]
    rot = jnp.concatenate([x1 * cos - x2 * sin, x2 * cos + x1 * sin], axis=-1)
    return jnp.concatenate([rot, xf[..., ROPE_DIM:]], axis=-1).astype(x.dtype)


def sliding_window_attention(q, k, v, sinks):
    B, S = q.shape[0], q.shape[1]
    nb = S // WINDOW
    G = ATT_HEADS // ATT_KV_HEADS
    qb = q.reshape(B, nb, WINDOW, ATT_KV_HEADS, G, ATT_HEAD_DIM)
    kb = k.reshape(B, nb, WINDOW, ATT_KV_HEADS, ATT_HEAD_DIM)
    vb = v.reshape(B, nb, WINDOW, ATT_KV_HEADS, ATT_HEAD_DIM)

    def with_prev(t):
        prev = jnp.pad(t, ((0, 0), (1, 0), (0, 0), (0, 0), (0, 0)))[:, :-1]
        return jnp.concatenate([prev, t], axis=2)

    kband = with_prev(kb)
    vband = with_prev(vb)
    a_idx = jnp.arange(WINDOW)[:, None]
    c_idx = jnp.arange(2 * WINDOW)[None, :]
    band = (c_idx > a_idx) & (c_idx <= a_idx + WINDOW)
    sink = sinks.astype(jnp.float32).reshape(ATT_KV_HEADS, G)[None, :, :, None, None]
    scale = ATT_HEAD_DIM ** -0.5

    def one_block(args):
        qj, kj, vj, j = args
        s = jnp.einsum('bqhgd,bkhd->bhgqk', qj, kj).astype(jnp.float32) * scale
        valid = band & ((j * WINDOW + c_idx - WINDOW) >= 0)
        s = jnp.where(valid, s, -jnp.inf)
        m = jnp.maximum(jnp.max(s, axis=-1, keepdims=True), sink)
        p = jnp.exp(s - m)
        denom = jnp.sum(p, axis=-1, keepdims=True) + jnp.exp(sink - m)
        return jnp.einsum('bhgqk,bkhd->bqhgd', (p / denom).astype(vj.dtype), vj)

    xs = (jnp.moveaxis(qb, 1, 0), jnp.moveaxis(kband, 1, 0), jnp.moveaxis(vband, 1, 0),
          jnp.arange(nb, dtype=jnp.int32))
    out = lax.map(one_block, xs)
    return jnp.moveaxis(out, 0, 1).reshape(B, S, ATT_Q_W)


def mlstm_chunkwise(q, k, v, i_pre, f_pre):
    B, S, H = q.shape[0], q.shape[1], q.shape[2]
    L = ML_CHUNK
    nc = S // L
    f32 = jnp.float32
    qc = q.astype(f32).reshape(B, nc, L, H, ML_QK_DIM).transpose(1, 0, 3, 2, 4)
    kc = (k.astype(f32) * (ML_QK_DIM ** -0.5)).reshape(B, nc, L, H, ML_QK_DIM).transpose(1, 0, 3, 2, 4)
    vc = v.astype(f32).reshape(B, nc, L, H, ML_V_DIM).transpose(1, 0, 3, 2, 4)
    ic = i_pre.reshape(B, nc, L, H).transpose(1, 0, 3, 2)
    lfc = jax.nn.log_sigmoid(f_pre).reshape(B, nc, L, H).transpose(1, 0, 3, 2)
    causal = jnp.tril(jnp.ones((L, L), dtype=bool))

    def step(carry, xs):
        C, n, m = carry
        qj, kj, vj, ij, lfj = xs
        b = jnp.cumsum(lfj, axis=-1)
        dmat = jnp.where(causal, b[..., :, None] - b[..., None, :] + ij[..., None, :], -jnp.inf)
        inter = b + m[..., None]
        m_t = jnp.maximum(inter, jnp.max(dmat, axis=-1))
        w_intra = jnp.exp(dmat - m_t[..., None])
        w_inter = jnp.exp(inter - m_t)
        qk = jnp.einsum('bhtd,bhsd->bhts', qj, kj) * w_intra
        num = jnp.einsum('bhts,bhsv->bhtv', qk, vj) + w_inter[..., None] * jnp.einsum('bhvd,bhtd->bhtv', C, qj)
        den = jnp.sum(qk, axis=-1) + w_inter * jnp.einsum('bhd,bhtd->bht', n, qj)
        h = num / jnp.maximum(jnp.abs(den), jnp.exp(-m_t))[..., None]
        b_last = b[..., -1]
        g = b_last[..., None] - b + ij
        m_new = jnp.maximum(b_last + m, jnp.max(g, axis=-1))
        w_s = jnp.exp(g - m_new[..., None])
        decay = jnp.exp(b_last + m - m_new)
        C_new = decay[..., None, None] * C + jnp.einsum('bhs,bhsv,bhsd->bhvd', w_s, vj, kj)
        n_new = decay[..., None] * n + jnp.einsum('bhs,bhsd->bhd', w_s, kj)
        return (C_new, n_new, m_new), h

    init = (jnp.zeros((B, H, ML_V_DIM, ML_QK_DIM), f32), jnp.zeros((B, H, ML_QK_DIM), f32),
            jnp.zeros((B, H), f32))
    _, h = lax.scan(step, init, (qc, kc, vc, ic, lfc))
    return h.transpose(1, 0, 3, 2, 4).reshape(B, S, H, ML_V_DIM).astype(q.dtype)


def peer_ffn(x, w_query, sub_keys, expert_down, expert_up):
    B, S, D = x.shape
    xt = x.reshape((B * S) // PEER_TOKEN_BLOCK, PEER_TOKEN_BLOCK, D)
    K = PEER_TOPK

    def one_block(xb):
        t = xb.shape[0]
        q = (xb @ w_query).reshape(t, PEER_HEADS, 2, PEER_KEY_DIM // 2)
        s = jnp.einsum('thpd,hpkd->thpk', q, sub_keys).astype(jnp.float32)
        top_s, top_i = lax.top_k(s, K)
        cand_s = (top_s[:, :, 0, :, None] + top_s[:, :, 1, None, :]).reshape(t, PEER_HEADS, K * K)
        cand_i = (top_i[:, :, 0, :, None] * N_KEYS + top_i[:, :, 1, None, :]).reshape(t, PEER_HEADS, K * K)
        best_s, best_pos = lax.top_k(cand_s, K)
        idx = jnp.take_along_axis(cand_i, best_pos, axis=-1)
        gate = jax.nn.softmax(best_s, axis=-1)
        u = expert_down[idx]
        act = jax.nn.gelu(jnp.einsum('td,thkd->thk', xb, u).astype(jnp.float32), approximate=False)
        vv = expert_up[idx]
        return jnp.einsum('thk,thkd->td', (gate * act).astype(xb.dtype), vv)

    return lax.map(one_block, xt).reshape(B, S, D)


def split_columns(proj):
    offs = np.cumsum(np.array(IN_SPLITS))[:-1].tolist()
    return jnp.split(proj, offs, axis=-1)


def setup_inputs(seed: int = 0) -> dict:
    key = jax.random.key(seed)
    ks = jax.random.split(key, 20)
    f32 = jnp.float32
    nrm = lambda k, shape, s: jax.random.normal(k, shape, f32) * s
    x = jax.random.normal(ks[0], (BATCH, SEQ, D_MODEL), f32)
    offset = jax.random.randint(ks[1], (BATCH, 1), 0, 1024, dtype=jnp.int32)
    positions = offset + jnp.arange(SEQ, dtype=jnp.int32)[None, :]
    return {
        "x": x,
        "positions": positions,
        "norm1_gain": 1.0 + nrm(ks[2], (DEPTH, D_MODEL), 0.02),
        "w_in": nrm(ks[3], (DEPTH, D_MODEL, IN_WIDTH), D_MODEL ** -0.5),
        "ml_i_bias": nrm(ks[4], (DEPTH, ML_HEADS), 0.1),
        "ml_f_bias": jnp.linspace(3.0, 6.0, ML_HEADS, dtype=f32)[None, :] + nrm(ks[5], (DEPTH, ML_HEADS), 0.1),
        "q_norm_gain": 1.0 + nrm(ks[6], (DEPTH, ATT_HEAD_DIM), 0.02),
        "k_norm_gain": 1.0 + nrm(ks[7], (DEPTH, ATT_HEAD_DIM), 0.02),
        "attn_sinks": nrm(ks[8], (DEPTH, ATT_HEADS), 0.5),
        "ml_out_norm_gain": 1.0 + nrm(ks[9], (DEPTH, ML_V_W), 0.02),
        "w_branch_attn": nrm(ks[10], (DEPTH, ATT_Q_W, D_MODEL), ATT_Q_W ** -0.5),
        "w_branch_mlstm": nrm(ks[11], (DEPTH, ML_V_W, D_MODEL), ML_V_W ** -0.5),
        "w_out": nrm(ks[12], (DEPTH, D_MODEL, D_MODEL), D_MODEL ** -0.5),
        "norm2_gain": 1.0 + nrm(ks[13], (DEPTH, D_MODEL), 0.02),
        "peer_w_query": nrm(ks[14], (DEPTH, D_MODEL, PEER_HEADS * PEER_KEY_DIM), D_MODEL ** -0.5),
        "peer_sub_keys": nrm(ks[15], (DEPTH, PEER_HEADS, 2, N_KEYS, PEER_KEY_DIM // 2), (PEER_KEY_DIM // 2) ** -0.5),
        "peer_down": nrm(ks[16], (DEPTH, N_EXPERTS, D_MODEL), D_MODEL ** -0.5),
        "peer_up": nrm(ks[17], (DEPTH, N_EXPERTS, D_MODEL), PEER_HEADS ** -0.5),
    }


def reference(x, positions, norm1_gain, w_in, ml_i_bias, ml_f_bias, q_norm_gain, k_norm_gain,
              attn_sinks, ml_out_norm_gain, w_branch_attn, w_branch_mlstm, w_out, norm2_gain,
              peer_w_query, peer_sub_keys, peer_down, peer_up):
    B, S, _ = x.shape
    for l in range(DEPTH):
        h = rmsnorm(x, norm1_gain[l])
        (aq, ak, av, mq, mk, mv, mi, mf, mo, gate_a, gate_m) = split_columns(h @ w_in[l])
        aq = partial_rope(rmsnorm(aq.reshape(B, S, ATT_HEADS, ATT_HEAD_DIM), q_norm_gain[l]), positions)
        ak = partial_rope(rmsnorm(ak.reshape(B, S, ATT_KV_HEADS, ATT_HEAD_DIM), k_norm_gain[l]), positions)
        av = av.reshape(B, S, ATT_KV_HEADS, ATT_HEAD_DIM)
        att = sliding_window_attention(aq, ak, av, attn_sinks[l])
        att_b = att @ w_branch_attn[l]
        i_pre = mi.astype(jnp.float32) + ml_i_bias[l].astype(jnp.float32)
        f_pre = mf.astype(jnp.float32) + ml_f_bias[l].astype(jnp.float32)
        i_pre = GATE_CAP * jnp.tanh(i_pre / GATE_CAP)
        f_pre = GATE_CAP * jnp.tanh(f_pre / GATE_CAP)
        hm = mlstm_chunkwise(mq.reshape(B, S, ML_HEADS, ML_QK_DIM), mk.reshape(B, S, ML_HEADS, ML_QK_DIM),
                             mv.reshape(B, S, ML_HEADS, ML_V_DIM), i_pre, f_pre)
        hm = rmsnorm(hm, ml_out_norm_gain[l].reshape(ML_HEADS, ML_V_DIM)).reshape(B, S, ML_V_W)
        hm = hm * jax.nn.sigmoid(mo)
        ml_b = hm @ w_branch_mlstm[l]
        mixed = jax.nn.sigmoid(gate_a) * att_b + jax.nn.sigmoid(gate_m) * ml_b
        x = x + mixed @ w_out[l]
        x = x + peer_ffn(rmsnorm(x, norm2_gain[l]), peer_w_query[l], peer_sub_keys[l], peer_down[l], peer_up[l])
    return x
```

```python
import numpy as np
import concourse.bass as bass
import concourse.mybir as mybir
from contextlib import ExitStack

F32 = mybir.dt.float32
BF16 = mybir.dt.bfloat16
I32 = mybir.dt.int32
U32 = mybir.dt.uint32
ALU = mybir.AluOpType
AF = mybir.ActivationFunctionType
AX = mybir.AxisListType

ENGS = ("pe", "act", "dve", "pool", "sp")


class T:
    __slots__ = ("name", "ap", "writers", "readers", "dsem", "dcount", "last_dma_read")

    def __init__(self, name, ap=None):
        self.name = name
        self.ap = ap
        self.writers = []
        self.readers = []
        self.dsem = None
        self.dcount = 0
        self.last_dma_read = None

    def __getitem__(self, k):
        return self.ap[k]


class Op:
    __slots__ = ("eng", "fn", "seq", "deps", "dma", "dsem", "dval", "signal", "sigval")

    def __init__(self, eng, fn):
        self.eng = eng
        self.fn = fn
        self.seq = None
        self.deps = []
        self.dma = False
        self.dsem = None
        self.dval = 0
        self.signal = False
        self.sigval = 0


class Prog:
    def __init__(self, nc):
        self.nc = nc
        self.stack = ExitStack()
        self.ops = []
        self.per_eng = {e: [] for e in ENGS}
        self.nsem = 0
        self.esem = {}
        for e in ENGS:
            if e != "sp":
                self.esem[e] = self.sem("s_" + e)
        self.stores = []
        self.scopes = []
        self.last_dma = {}

    def open_scope(self):
        self.scopes.append(ExitStack())

    def close_scope(self):
        self.barrier()
        self.scopes.pop().close()

    def barrier(self):
        last_c = []
        for e in ENGS:
            for o in reversed(self.per_eng[e]):
                if not o.dma and o.fn is not None:
                    last_c.append(o)
                    break
        deps = last_c + list(self.last_dma.values())
        for e in ENGS:
            op = Op(e, None)
            op.seq = len(self.per_eng[e])
            op.deps = list(deps)
            self.ops.append(op)
            self.per_eng[e].append(op)

    def sem(self, name):
        self.nsem += 1
        return self.stack.enter_context(self.nc.semaphore(name))

    def sb(self, name, shape, dt):
        stk = self.scopes[-1] if self.scopes else self.stack
        t = stk.enter_context(self.nc.sbuf_tensor(name, list(shape), dt))
        return T(name, t)

    def ps(self, name, shape, dt):
        t = self.stack.enter_context(self.nc.psum_tensor(name, list(shape), dt))
        return T(name, t)

    def dram(self, name, shape, dt, kind="Internal"):
        return self.nc.dram_tensor(name, list(shape), dt, kind=kind).ap()

    def _rec(self, eng, fn, reads, writes, parts=(), dma_tile=None, dma_is_read=False):
        op = Op(eng, fn)
        op.seq = len(self.per_eng[eng])
        deps = []
        for t in reads:
            deps.extend(t.writers)
        for t in writes:
            if t.readers:
                deps.extend(t.readers)
                deps.extend(t.writers)
                t.writers = [op]
                t.readers = []
            else:
                deps.extend(t.writers)
                t.writers = [op]
        for t in parts:
            if t.readers:
                deps.extend(t.readers)
                deps.extend(t.writers)
                t.writers = [op]
                t.readers = []
            else:
                t.writers = t.writers + [op]
        for t in reads:
            t.readers.append(op)
        if dma_tile is not None:
            op.dma = True
            if dma_tile.dsem is None:
                dma_tile.dsem = self.sem("d_" + dma_tile.name)
            if dma_is_read and dma_tile.last_dma_read is not None:
                deps.append(dma_tile.last_dma_read)
            dma_tile.dcount += 1
            op.dsem = dma_tile.dsem
            op.dval = 16 * dma_tile.dcount
            dma_tile.last_dma_read = op if dma_is_read else None
            self.last_dma[id(op.dsem)] = op
        op.deps = [d for d in deps if d is not op]
        self.ops.append(op)
        self.per_eng[eng].append(op)
        return op

    def op(self, eng, fn, reads=(), writes=(), parts=()):
        return self._rec(eng, fn, reads, writes, parts)

    def dma(self, out, in_, reads=(), writes=(), parts=(), sem_tile=None, is_read=False, eng="sp", **kw):
        def fn(e):
            return e.dma_start(out=out, in_=in_, **kw)
        return self._rec(eng, fn, reads, writes, parts, dma_tile=sem_tile, dma_is_read=is_read)

    def load(self, dst_tile, dst_ap, src_ap, part=False, dram_reads=(), eng="sp", **kw):
        if part:
            return self.dma(dst_ap, src_ap, reads=dram_reads, parts=(dst_tile,), sem_tile=dst_tile, eng=eng, **kw)
        return self.dma(dst_ap, src_ap, reads=dram_reads, writes=(dst_tile,), sem_tile=dst_tile, eng=eng, **kw)

    def store(self, dst_ap, src_tile, src_ap, dram_writes=(), dram_parts=(), final=False, eng="sp", **kw):
        o = self.dma(dst_ap, src_ap, reads=(src_tile,), writes=dram_writes, parts=dram_parts,
                     sem_tile=src_tile, is_read=True, eng=eng, **kw)
        if final:
            self.stores.append(o)
        return o

    def emit(self):
        nc = self.nc
        fin = Op("sp", None)
        fin.seq = len(self.per_eng["sp"])
        fin.deps = list(self.stores)
        self.ops.append(fin)
        self.per_eng["sp"].append(fin)

        clock = {e: {f: -1 for f in ENGS} for e in ENGS}
        dclock = {e: {} for e in ENGS}
        waits = {}
        for op in self.ops:
            X = op.eng
            need_e = {}
            need_d = {}
            for d in op.deps:
                if d.dma:
                    key = id(d.dsem)
                    if dclock[X].get(key, 0) >= d.dval:
                        continue
                    cur = need_d.get(key)
                    if cur is None or cur[1] < d.dval:
                        need_d[key] = (d.dsem, d.dval)
                else:
                    Y = d.eng
                    if Y == X and X == "pe":
                        continue
                    if clock[X][Y] >= d.seq:
                        continue
                    if need_e.get(Y, -1) < d.seq:
                        need_e[Y] = d.seq
            wl = []
            for Y, s in need_e.items():
                clock[X][Y] = s
                tgt = self.per_eng[Y][s]
                tgt.signal = True
                wl.append(("e", Y, tgt))
            for key, (sem, val) in need_d.items():
                dclock[X][key] = val
                wl.append(("d", sem, val))
            waits[id(op)] = wl
        for e in ENGS:
            c = 0
            for op in self.per_eng[e]:
                if op.signal and not op.dma:
                    c += 1
                    op.sigval = c
        self.sigmax = {e: max([o.sigval for o in self.per_eng[e]] + [0]) for e in ENGS}
        engobj = {"pe": "tensor", "act": "scalar", "dve": "vector", "pool": "gpsimd", "sp": "sync"}
        with nc.Block() as block:
            for e in ENGS:
                ops_e = self.per_eng[e]
                if not ops_e:
                    continue

                def body(eng, ops_e=ops_e, e=e):
                    for op in ops_e:
                        for w in waits[id(op)]:
                            if w[0] == "e":
                                eng.wait_ge(self.esem[w[1]], w[2].sigval)
                            else:
                                eng.wait_ge(w[1], w[2])
                        if op.fn is None:
                            continue
                        ins = op.fn(eng)
                        if op.dma:
                            ins.then_inc(op.dsem, 16)
                        elif op.signal:
                            ins.then_inc(self.esem[e], 1)

                getattr(block, engobj[e])(body)
        self.stack.close()


D = 1024
IN_W = 6416
EPS = 1e-6
NEG = -30000.0
TWO_PI = 6.283185


def host_consts():
    c = {}
    c["identf"] = np.eye(128, dtype=np.float32)
    k = np.arange(128)[:, None]
    q = np.arange(128)[None, :]
    m_prev = np.where(k > q, 0.0, NEG).astype(np.float32)
    m_cur = np.where(k <= q, 0.0, NEG).astype(np.float32)
    c["amask"] = np.stack([np.tile(m_prev, (1, 4)), np.tile(m_cur, (1, 4))], axis=1).astype(np.float32)
    c["cmask"] = np.tile((k <= q).astype(np.float32), (1, 4))
    invf = (500000.0 ** (-np.arange(0, 16, 2, dtype=np.float32) / 16.0)).astype(np.float32)
    c["invf"] = np.tile((invf / (2 * np.pi)).astype(np.float32)[None, :], (128, 1))
    onesab = np.zeros((128, 2, 128), np.float32)
    onesab[:, 0, 0:64] = 1.0
    onesab[:, 1, 64:128] = 1.0
    c["onesab"] = onesab
    c["iota128"] = np.tile(np.arange(128, dtype=np.float32)[None, :], (128, 1))
    return c


CONST_SHAPES = {"identf": [128, 128], "amask": [128, 2, 512], "cmask": [128, 512], "invf": [128, 8],
                "onesab": [128, 2, 128], "iota128": [128, 128]}


class Ctx:
    pass


def build_program(NB, NT, phases="ABC", dbg=False):
    NTT = NB * NT
    TOK = NTT * 128
    nc = bass.Bass("TRN2", target_bir_lowering=False)
    P = Prog(nc)
    c = Ctx()
    c.nc, c.P, c.NB, c.NT, c.NTT, c.TOK = nc, P, NB, NT, NTT, TOK
    c.phases = phases

    def din(name, shape, dt=F32):
        return nc.dram_tensor(name, list(shape), dt, kind="ExternalInput").ap()

    c.x = din("x", [TOK, D])
    c.pos = din("positions", [TOK], I32)
    c.norm1_gain = din("norm1_gain", [1, D])
    c.w_in = din("w_in", [D, IN_W])
    c.ml_i_bias = din("ml_i_bias", [1, 8])
    c.ml_f_bias = din("ml_f_bias", [1, 8])
    c.q_norm_gain = din("q_norm_gain", [1, 64])
    c.k_norm_gain = din("k_norm_gain", [1, 64])
    c.attn_sinks = din("attn_sinks", [1, 16])
    c.ml_out_norm_gain = din("ml_out_norm_gain", [1, D])
    c.w_branch_attn = din("w_branch_attn", [D, D])
    c.w_branch_mlstm = din("w_branch_mlstm", [D, D])
    c.w_out = din("w_out", [D, D])
    if "C" in phases:
        c.norm2_gain = din("norm2_gain", [1, D])
        c.peer_w_query = din("peer_w_query", [D, D])
        c.peer_sub_keys = din("peer_sub_keys", [8, 2, 128, 64])
        c.peer_down = din("peer_down", [16384, D])
        c.peer_up = din("peer_up", [16384, D])
    c.cst = {k: din("c_" + k, s) for k, s in CONST_SHAPES.items()}
    c.out = nc.dram_tensor("out", [TOK, D], F32, kind="ExternalOutput").ap()
    if dbg:
        c.mixa = nc.dram_tensor("mixa_s", [TOK, D], F32, kind="ExternalOutput").ap()
    else:
        c.mixa = P.dram("mixa_s", [TOK, D], F32)
    c.dbg = dbg
    c.dbg_outs = {}
    c.mixa_t = [T("mixa%d" % i) for i in range(NTT)]
    c.x1_t = [T("x1_%d" % i) for i in range(NTT)]

    c.identf = P.sb("identf", [128, 128], F32)
    c.identb = P.sb("identb", [128, 128], BF16)
    P.load(c.identf, c.identf[:], c.cst["identf"])
    P.op("dve", lambda e: e.tensor_copy(out=c.identb[:], in_=c.identf[:]), reads=[c.identf], writes=[c.identb])
    c.bank = [P.ps("bank%d" % i, [128, 512], F32) for i in range(8)]

    for ph, fn in (("A", phase_a), ("B", phase_b), ("C", phase_c)):
        if ph in phases:
            P.open_scope()
            fn(c)
            P.close_scope()
    P.emit()
    return nc, P


def rms_rstd(c, pfx, ssq, n, rstd, tmp):
    P = c.P
    P.op("dve", lambda e: e.tensor_scalar(out=tmp[:], in0=ssq[:], scalar1=1.0 / n, scalar2=EPS, op0=ALU.mult, op1=ALU.add),
         reads=[ssq], writes=[tmp])
    P.op("act", lambda e: e.activation(out=tmp[:], in_=tmp[:], func=AF.Sqrt), reads=[tmp], writes=[tmp])
    P.op("dve", lambda e: e.reciprocal(out=rstd[:], in_=tmp[:]), reads=[tmp], writes=[rstd])


def norm_and_transpose(c, xs, gain, hb, hT, junk, st, ptr_bank):
    P = c.P
    ssq, tmp, rstd = st
    P.op("act", lambda e: e.activation(out=junk[:], in_=xs[:], func=AF.Square, accum_out=ssq[:]),
         reads=[xs], writes=[junk, ssq])
    rms_rstd(c, "n", ssq, D, rstd, tmp)
    P.op("dve", lambda e: e.scalar_tensor_tensor(out=hb[:], in0=xs[:], scalar=rstd[:], in1=gain[:], op0=ALU.mult, op1=ALU.mult),
         reads=[xs, rstd, gain], writes=[hb])
    transpose8(c, hb, hT, ptr_bank)


def transpose8(c, src, dst, ptr_bank, eng="act"):
    P = c.P
    pv = ptr_bank[:].bitcast(BF16)
    for k in range(8):
        P.op("pe", lambda e, k=k: e.transpose(out=pv[:, k * 128:(k + 1) * 128], in_=src[:, k * 128:(k + 1) * 128], identity=c.identb[:]),
             reads=[src, c.identb], writes=[ptr_bank] if k == 0 else [], parts=[] if k == 0 else [ptr_bank])
    if eng == "act":
        P.op("act", lambda e: e.copy(out=dst[:].rearrange("p k t -> p (k t)"), in_=pv), reads=[ptr_bank], writes=[dst])
    else:
        P.op(eng, lambda e: e.tensor_copy(out=dst[:].rearrange("p k t -> p (k t)"), in_=pv), reads=[ptr_bank], writes=[dst])


def load_w_bf16(c, dst, col0, ncols, src, dcol0=0):
    P = c.P
    for k in range(8):
        P.load(dst, dst[:, k, dcol0:dcol0 + ncols], src[k * 128:(k + 1) * 128, col0:col0 + ncols], part=True, eng="pool",
               max_dma_last_dim=4096)


def rope_tables(c):
    P = c.P
    NTT = c.NTT
    posi = P.sb("posi", [128, NTT], I32)
    P.load(posi, posi[:], c.pos.rearrange("(n p) -> p n", p=128), allow_slow_non_contiguous=True)
    posf = P.sb("posf", [128, NTT], F32)
    P.op("dve", lambda e: e.tensor_copy(out=posf[:], in_=posi[:]), reads=[posi], writes=[posf])
    invf = P.sb("invf", [128, 8], F32)
    P.load(invf, invf[:], c.cst["invf"])
    y = P.sb("rope_y", [128, NTT, 16], F32)
    yi = P.sb("rope_yi", [128, NTT, 16], I32)
    yf = P.sb("rope_yf", [128, NTT, 16], F32)
    cs = P.sb("rope_cs", [128, NTT, 16], F32)
    pb = posf[:].unsqueeze(2).to_broadcast([128, NTT, 8])
    ib = invf[:].unsqueeze(1).to_broadcast([128, NTT, 8])
    P.op("dve", lambda e: e.tensor_tensor(out=y[:, :, 8:16], in0=pb, in1=ib, op=ALU.mult), reads=[posf, invf], writes=[y])
    P.op("dve", lambda e: e.tensor_scalar(out=y[:, :, 0:8], in0=y[:, :, 8:16], scalar1=0.25, scalar2=None, op0=ALU.add),
         reads=[y], writes=[y])
    P.op("dve", lambda e: e.tensor_copy(out=yi[:], in_=y[:]), reads=[y], writes=[yi])
    P.op("dve", lambda e: e.tensor_copy(out=yf[:], in_=yi[:]), reads=[yi], writes=[yf])
    P.op("dve", lambda e: e.tensor_tensor(out=y[:], in0=y[:], in1=yf[:], op=ALU.subtract), reads=[y, yf], writes=[y])
    P.op("act", lambda e: e.activation(out=cs[:], in_=y[:], func=AF.Sin, scale=TWO_PI), reads=[y], writes=[cs])
    return cs


def qk_norm_rope(c, pfx, src, nh, gain, cs_n, outb, tmps):
    P = c.P
    sq, ssq, tmp, rstd, qn, r1, r2 = tmps
    W = nh * 64
    s3 = src[:, 0:W].rearrange("p (h d) -> p h d", d=64)
    P.op("pool", lambda e: e.tensor_tensor(out=sq[:, 0:W], in0=src[:, 0:W], in1=src[:, 0:W], op=ALU.mult), reads=[src], writes=[sq])
    P.op("dve", lambda e: e.tensor_reduce(out=ssq[:, 0:nh], in_=sq[:, 0:W].rearrange("p (h d) -> p h d", d=64), axis=AX.X, op=ALU.add),
         reads=[sq], writes=[ssq])
    P.op("dve", lambda e: e.tensor_scalar(out=tmp[:, 0:nh], in0=ssq[:, 0:nh], scalar1=1.0 / 64, scalar2=EPS, op0=ALU.mult, op1=ALU.add),
         reads=[ssq], writes=[tmp])
    P.op("act", lambda e: e.activation(out=tmp[:, 0:nh], in_=tmp[:, 0:nh], func=AF.Sqrt), reads=[tmp], writes=[tmp])
    P.op("dve", lambda e: e.reciprocal(out=rstd[:, 0:nh], in_=tmp[:, 0:nh]), reads=[tmp], writes=[rstd])
    q3 = qn[:, 0:W].rearrange("p (h d) -> p h d", d=64)
    P.op("dve", lambda e: e.tensor_tensor(out=q3, in0=s3, in1=rstd[:, 0:nh].unsqueeze(2).to_broadcast([128, nh, 64]), op=ALU.mult),
         reads=[src, rstd], writes=[qn])
    P.op("pool", lambda e: e.tensor_tensor(out=q3, in0=q3, in1=gain[:].unsqueeze(1).to_broadcast([128, nh, 64]), op=ALU.mult),
         reads=[qn, gain], writes=[qn])
    P.op("act", lambda e: e.copy(out=outb[:], in_=q3), reads=[qn], writes=[outb])
    cosb = cs_n[:, 0:8].unsqueeze(1).to_broadcast([128, nh, 8])
    sinb = cs_n[:, 8:16].unsqueeze(1).to_broadcast([128, nh, 8])
    a3 = r1[:, 0:nh * 8].rearrange("p (h d) -> p h d", d=8)
    b3 = r2[:, 0:nh * 8].rearrange("p (h d) -> p h d", d=8)
    cst = c.cs
    P.op("dve", lambda e: e.tensor_tensor(out=a3, in0=q3[:, :, 0:8], in1=cosb, op=ALU.mult), reads=[qn, cst], writes=[r1])
    P.op("dve", lambda e: e.tensor_tensor(out=b3, in0=q3[:, :, 8:16], in1=sinb, op=ALU.mult), reads=[qn, cst], writes=[r2])
    P.op("dve", lambda e: e.tensor_tensor(out=outb[:, :, 0:8], in0=a3, in1=b3, op=ALU.subtract), reads=[r1, r2, outb], writes=[outb])
    P.op("dve", lambda e: e.tensor_tensor(out=a3, in0=q3[:, :, 8:16], in1=cosb, op=ALU.mult), reads=[qn, cst], writes=[r1])
    P.op("dve", lambda e: e.tensor_tensor(out=b3, in0=q3[:, :, 0:8], in1=sinb, op=ALU.mult), reads=[qn, cst], writes=[r2])
    P.op("dve", lambda e: e.tensor_tensor(out=outb[:, :, 8:16], in0=a3, in1=b3, op=ALU.add), reads=[r1, r2, outb], writes=[outb])


def phase_a(c):
    P, NB, NT, NTT = c.P, c.NB, c.NT, c.NTT
    bank = c.bank
    c.g1 = P.sb("a_g1", [128, D], F32)
    P.load(c.g1, c.g1[:], c.norm1_gain[0].partition_broadcast(128))
    wA = P.sb("wA", [128, 8, 2304], BF16)
    load_w_bf16(c, wA, 0, 1280, c.w_in, 0)
    load_w_bf16(c, wA, 4368, 1024, c.w_in, 1280)
    wba = P.sb("wba", [128, 8, D], BF16)
    load_w_bf16(c, wba, 0, D, c.w_branch_attn)
    gq = P.sb("gq", [128, 64], F32)
    gk = P.sb("gk", [128, 64], F32)
    P.load(gq, gq[:], c.q_norm_gain[0].partition_broadcast(128))
    P.load(gk, gk[:], c.k_norm_gain[0].partition_broadcast(128))
    sk = P.sb("sk", [128, 16], F32)
    P.load(sk, sk[:], c.attn_sinks[0].partition_broadcast(128))
    sinkp = P.sb("sinkp", [128, 8], F32)
    sk3 = sk[:].rearrange("p (i two) -> p i two", two=2)
    P.op("dve", lambda e: e.tensor_copy(out=sinkp[0:64, :], in_=sk3[0:64, :, 0]), reads=[sk], writes=[sinkp])
    P.op("dve", lambda e: e.tensor_copy(out=sinkp[64:128, :], in_=sk3[64:128, :, 1]), reads=[sk], parts=[sinkp])
    P.op("act", lambda e: e.activation(out=sinkp[:], in_=sinkp[:], func=AF.Exp), reads=[sinkp], writes=[sinkp])
    amaskf = P.sb("amaskf", [128, 2, 512], F32)
    P.load(amaskf, amaskf[:], c.cst["amask"])
    amask = P.sb("amask", [128, 2, 512], BF16)
    P.op("dve", lambda e: e.tensor_copy(out=amask[:], in_=amaskf[:]), reads=[amaskf], writes=[amask])
    onesf = P.sb("onesf", [128, 2, 128], F32)
    P.load(onesf, onesf[:], c.cst["onesab"])
    onesab = P.sb("onesab", [128, 2, 128], BF16)
    P.op("dve", lambda e: e.tensor_copy(out=onesab[:], in_=onesf[:]), reads=[onesf], writes=[onesab])
    c.cs = rope_tables(c)

    xs = [P.sb("a_xs%d" % i, [128, D], F32) for i in range(2)]
    junk = P.sb("a_junk", [128, D], BF16)
    st = (P.sb("a_ssq", [128, 1], F32), P.sb("a_tmp", [128, 1], F32), P.sb("a_rstd", [128, 1], F32))
    hb = P.sb("a_hb", [128, D], BF16)
    hT = P.sb("a_hT", [128, 8, 128], BF16)
    qf = P.sb("a_qf", [128, D], F32)
    kvf = P.sb("a_kvf", [128, 256], F32)
    sga = P.sb("a_sga", [128, D], F32)
    tmps = (P.sb("a_sq", [128, D], F32), P.sb("a_ssq16", [128, 16], F32), P.sb("a_tmp16", [128, 16], F32),
            P.sb("a_rstd16", [128, 16], F32), P.sb("a_qn", [128, D], F32), P.sb("a_r1", [128, 128], F32), P.sb("a_r2", [128, 128], F32))
    qb = P.sb("a_qb", [128, 16, 64], BF16)
    kb = P.sb("a_kb", [128, 2, 64], BF16)
    kdup = P.sb("a_kdup", [128, 2, 2, 64], BF16)
    qT = P.sb("a_qT", [128, 8, 128], BF16)
    kT = [P.sb("a_kT%d" % i, [128, 2, 128], BF16) for i in range(2)]
    vA = [P.sb("a_vA%d" % i, [128, 2, 128], BF16) for i in range(2)]
    vB = [P.sb("a_vB%d" % i, [128, 2, 128], BF16) for i in range(2)]
    for i in range(2):
        P.op("pool", lambda e, i=i: e.memset(vA[i][:], 0.0), writes=[vA[i]])
        P.op("pool", lambda e, i=i: e.memset(vB[i][:], 0.0), writes=[vB[i]])
    PT = [[[P.sb("a_PT%d%d%d" % (g, k, h), [128, 4, 128], BF16) for h in range(2)] for k in range(2)] for g in range(2)]
    rden = P.sb("a_rden", [128, 4, 128], F32)
    attT = P.sb("a_attT", [128, 8, 128], BF16)
    mo = [P.sb("a_mo%d" % i, [128, D], F32) for i in range(2)]

    def load_x(n):
        P.load(xs[n % 2], xs[n % 2][:], c.x[n * 128:(n + 1) * 128, :])

    load_x(0)

    def tile(n):
        j = n % NT
        cur = n % 2
        prv = 1 - cur
        if n + 1 < NTT:
            load_x(n + 1)
        x_t = xs[cur]
        norm_and_transpose(c, x_t, c.g1, hb, hT, junk, st, bank[7])
        def proj(bk, col0, ncols):
            for k in range(8):
                P.op("pe", lambda e, k=k: e.matmul(out=bk[:, 0:ncols], lhsT=hT[:, k, :], rhs=wA[:, k, col0:col0 + ncols],
                                                   start=(k == 0), stop=(k == 7)),
                     reads=[hT, wA], writes=[bk] if k == 0 else [], parts=[] if k == 0 else [bk])
        proj(bank[0], 0, 512)
        P.op("dve", lambda e: e.tensor_copy(out=qf[:, 0:512], in_=bank[0][:]), reads=[bank[0]], writes=[qf])
        proj(bank[1], 512, 512)
        P.op("act", lambda e: e.copy(out=qf[:, 512:1024], in_=bank[1][:]), reads=[bank[1]], parts=[qf])
        proj(bank[2], 1024, 256)
        P.op("dve", lambda e: e.tensor_copy(out=kvf[:], in_=bank[2][:, 0:256]), reads=[bank[2]], writes=[kvf])
        proj(bank[3], 1280, 512)
        P.op("act", lambda e: e.activation(out=sga[:, 0:512], in_=bank[3][:], func=AF.Sigmoid), reads=[bank[3]], writes=[sga])
        proj(bank[4], 1792, 512)
        P.op("act", lambda e: e.activation(out=sga[:, 512:1024], in_=bank[4][:], func=AF.Sigmoid), reads=[bank[4]], parts=[sga])
        cs_n = c.cs[:, n, :]
        qk_norm_rope(c, "q", qf, 16, gq, cs_n, qb, tmps)
        qk_norm_rope(c, "k", kvf, 2, gk, cs_n, kb, tmps)
        P.op("pool", lambda e: e.tensor_copy(out=kdup[:, :, 0, :], in_=kb[:]), reads=[kb], writes=[kdup])
        P.op("pool", lambda e: e.tensor_copy(out=kdup[:, :, 1, :], in_=kb[:]), reads=[kb], parts=[kdup])
        v3 = kvf[:, 128:256].rearrange("p (g d) -> p g d", d=64)
        P.op("pool", lambda e: e.tensor_copy(out=vA[cur][:, :, 0:64], in_=v3), reads=[kvf], writes=[vA[cur]])
        P.op("pool", lambda e: e.tensor_copy(out=vB[cur][:, :, 64:128], in_=v3), reads=[kvf], writes=[vB[cur]])
        pv = bank[7][:].bitcast(BF16)
        qflat = qb[:].rearrange("p h d -> p (h d)")
        for k in range(8):
            P.op("pe", lambda e, k=k: e.transpose(out=pv[:, k * 128:(k + 1) * 128], in_=qflat[:, k * 128:(k + 1) * 128], identity=c.identb[:]),
                 reads=[qb, c.identb], writes=[bank[7]] if k == 0 else [], parts=[] if k == 0 else [bank[7]])
        P.op("act", lambda e: e.copy(out=qT[:].rearrange("p k t -> p (k t)"), in_=pv), reads=[bank[7]], writes=[qT])
        pv6 = bank[6][:].bitcast(BF16)
        kflat = kdup[:].rearrange("p g u d -> p (g u d)")
        for g in range(2):
            P.op("pe", lambda e, g=g: e.transpose(out=pv6[:, g * 128:(g + 1) * 128], in_=kflat[:, g * 128:(g + 1) * 128], identity=c.identb[:]),
                 reads=[kdup, c.identb], writes=[bank[6]] if g == 0 else [], parts=[] if g == 0 else [bank[6]])
        P.op("dve", lambda e: e.tensor_copy(out=kT[cur][:].rearrange("p g t -> p (g t)"), in_=pv6[:, 0:256]), reads=[bank[6]], writes=[kT[cur]])
        kbs = [1] if j == 0 else [0, 1]
        bi = 0
        for g in range(2):
            for kk in kbs:
                slot = cur if kk == 1 else prv
                for hh in range(2):
                    bk = bank[bi % 6]
                    bi += 1
                    P.op("pe", lambda e, bk=bk, slot=slot, g=g, hh=hh: e.matmul(
                        out=bk[:], lhsT=kT[slot][hh * 64:(hh + 1) * 64, g, :],
                        rhs=qT[hh * 64:(hh + 1) * 64, 4 * g:4 * g + 4, :], start=True, stop=False),
                        reads=[kT[slot], qT], writes=[bk])
                    P.op("pe", lambda e, bk=bk, kk=kk: e.matmul(out=bk[:], lhsT=c.identb[:], rhs=amask[:, kk, :], start=False, stop=True),
                         reads=[c.identb, amask], parts=[bk])
                    pt = PT[g][kk][hh]
                    P.op("act", lambda e, bk=bk, pt=pt: e.activation(out=pt[:].rearrange("p i t -> p (i t)"), in_=bk[:], func=AF.Exp, scale=0.125),
                         reads=[bk], writes=[pt])
        for g in range(2):
            pav = bank[6]
            pden = bank[7]
            first_av = True
            for p in range(4):
                combos = [(kk, hh) for kk in kbs for hh in range(2)]
                for ci, (kk, hh) in enumerate(combos):
                    slot = cur if kk == 1 else prv
                    vt = vA[slot] if hh == 0 else vB[slot]
                    pt = PT[g][kk][hh]
                    w_first = first_av
                    P.op("pe", lambda e, vt=vt, pt=pt, p=p, g=g, ci=ci, ncmb=len(combos): e.matmul(
                        out=pav[:, p * 128:(p + 1) * 128], lhsT=vt[:, g, :], rhs=pt[:, p, :], start=(ci == 0), stop=(ci == ncmb - 1)),
                        reads=[vt, pt], writes=[pav] if w_first else [], parts=[] if w_first else [pav])
                    P.op("pe", lambda e, pt=pt, p=p, hh=hh, ci=ci, ncmb=len(combos): e.matmul(
                        out=pden[:, p * 128:(p + 1) * 128], lhsT=onesab[:, hh, :], rhs=pt[:, p, :], start=(ci == 0), stop=(ci == ncmb - 1)),
                        reads=[onesab, pt], writes=[pden] if w_first else [], parts=[] if w_first else [pden])
                    first_av = False
            P.op("dve", lambda e, g=g: e.tensor_tensor(out=rden[:], in0=pden[:].rearrange("p (i t) -> p i t", t=128),
                                                       in1=sinkp[:, 4 * g:4 * g + 4].unsqueeze(2).to_broadcast([128, 4, 128]), op=ALU.add),
                 reads=[pden, sinkp], writes=[rden])
            P.op("dve", lambda e: e.reciprocal(out=rden[:], in_=rden[:]), reads=[rden], writes=[rden])
            P.op("dve", lambda e, g=g: e.tensor_tensor(out=attT[:, 4 * g:4 * g + 4, :], in0=pav[:].rearrange("p (i t) -> p i t", t=128),
                                                       in1=rden[:], op=ALU.mult),
                 reads=[pav, rden], writes=[attT] if g == 0 else [], parts=[] if g == 0 else [attT])
        m_t = mo[n % 2]
        for hn in range(2):
            bk = bank[hn]
            for i in range(8):
                P.op("pe", lambda e, bk=bk, i=i, hn=hn: e.matmul(out=bk[:], lhsT=attT[:, i, :], rhs=wba[:, i, hn * 512:(hn + 1) * 512],
                                                               start=(i == 0), stop=(i == 7)),
                     reads=[attT, wba], writes=[bk] if i == 0 else [], parts=[] if i == 0 else [bk])
            P.op("dve", lambda e, bk=bk, hn=hn: e.tensor_tensor(out=m_t[:, hn * 512:(hn + 1) * 512], in0=bk[:], in1=sga[:, hn * 512:(hn + 1) * 512], op=ALU.mult),
                 reads=[bk, sga], writes=[m_t] if hn == 0 else [], parts=[] if hn == 0 else [m_t])
        P.store(c.mixa[n * 128:(n + 1) * 128, :], m_t, m_t[:], dram_writes=[c.mixa_t[n]], final=c.dbg)
        if c.dbg and n == c.dbg - 1:
            for nm, tl, shp, dt in (("qb", qb, [128, 1024], BF16), ("kb", kb, [128, 128], BF16), ("attT", attT, [128, 1024], BF16),
                                    ("qf", qf, [128, 1024], F32), ("sga", sga, [128, 1024], F32), ("hT", hT, [128, 1024], BF16),
                                    ("PT", PT[0][1][0], [128, 512], BF16), ("rden", rden, [128, 512], F32), ("qT", qT, [128, 1024], BF16),
                                    ("kT", kT[cur], [128, 256], BF16), ("kdup", kdup, [128, 256], BF16), ("vA", vA[cur], [128, 256], BF16)):
                d_ap = c.nc.dram_tensor("dbg_" + nm, shp, dt, kind="ExternalOutput").ap()
                flat = tl[:]
                if len(flat.shape) == 3:
                    flat = flat.rearrange("p a b -> p (a b)")
                if len(flat.shape) == 4:
                    flat = flat.rearrange("p a b c -> p (a b c)")
                P.store(d_ap, tl, flat, final=True)

    for n in range(NTT):
        tile(n)


def phase_b(c):
    P, NB, NT, NTT = c.P, c.NB, c.NT, c.NTT
    bank = c.bank
    c.g1 = P.sb("b_g1", [128, D], F32)
    P.load(c.g1, c.g1[:], c.norm1_gain[0].partition_broadcast(128))
    wB = P.sb("wB", [128, 8, 4112], BF16)
    load_w_bf16(c, wB, 1280, 3088, c.w_in, 0)
    load_w_bf16(c, wB, 5392, 1024, c.w_in, 3088)
    wbm = P.sb("wbm", [128, 8, D], BF16)
    load_w_bf16(c, wbm, 0, D, c.w_branch_mlstm)
    wout = P.sb("wout", [128, 8, D], BF16)
    load_w_bf16(c, wout, 0, D, c.w_out)
    mlg = P.sb("mlg", [128, D], F32)
    P.load(mlg, mlg[:], c.ml_out_norm_gain[0].partition_broadcast(128))
    cmask = P.sb("cmask", [128, 512], F32)
    P.load(cmask, cmask[:], c.cst["cmask"])
    bif = P.sb("b_bif", [8, 2], F32)
    P.load(bif, bif[:, 0:1], c.ml_i_bias.rearrange("o h -> h o"), allow_slow_non_contiguous=True)
    P.load(bif, bif[:, 1:2], c.ml_f_bias.rearrange("o h -> h o"), part=True, allow_slow_non_contiguous=True)
    P.op("dve", lambda e: e.tensor_scalar(out=bif[:], in0=bif[:], scalar1=1.0 / 15.0, scalar2=None, op0=ALU.mult), reads=[bif], writes=[bif])

    xs = [P.sb("b_xs%d" % i, [128, D], F32) for i in range(2)]
    ma = [P.sb("b_ma%d" % i, [128, D], F32) for i in range(2)]
    junk = P.sb("b_junk", [128, D], BF16)
    st = (P.sb("b_ssq", [128, 1], F32), P.sb("b_tmp", [128, 1], F32), P.sb("b_rstd", [128, 1], F32))
    hb = P.sb("b_hb", [128, D], BF16)
    hT = P.sb("b_hT", [128, 8, 128], BF16)
    g_ti = P.sb("g_ti", [8, 128], F32)
    g_tf = P.sb("g_tf", [8, 128], F32)
    g_nl = P.sb("g_nl", [8, 128], F32)
    g_cum = P.sb("g_cum", [8, 128], F32)
    g_a = P.sb("g_a", [8, 128], F32)
    g_M = P.sb("g_M", [8, 128], F32)
    g_d2 = P.sb("g_d2", [8, 128], F32)
    g_ones = P.sb("g_ones", [8, 128], F32)
    P.op("pool", lambda e: e.memset(g_ones[:], 1.0), writes=[g_ones])
    g_out = [P.sb("g_out%d" % i, [8, 128], F32) for i in range(5)]
    cumc = [P.sb("g_cumc%d" % b, [8, 1], F32) for b in range(NB)]
    Mc = [P.sb("g_Mc%d" % b, [8, 1], F32) for b in range(NB)]
    g_nM0 = P.sb("g_nM0", [8, 1], F32)
    g_nMe = P.sb("g_nMe", [8, 1], F32)
    g_dd = P.sb("g_dd", [8, 1], F32)
    gtok = P.sb("b_gtok", [128, 5, 8], F32)
    qt = P.sb("b_qt", [128, 512], BF16)
    kt = P.sb("b_kt", [128, 512], BF16)
    khat = P.sb("b_khat", [128, 512], BF16)
    vaug = P.sb("b_vaug", [128, 8, 129], BF16)
    P.op("pool", lambda e: e.memset(vaug[:], 1.0), writes=[vaug])
    sgo = P.sb("b_sgo", [128, D], F32)
    sgm = P.sb("b_sgm", [128, D], F32)
    qtT = P.sb("b_qtT", [64, 8, 128], BF16)
    ktT = P.sb("b_ktT", [64, 8, 128], BF16)
    PTm = P.sb("b_PT", [128, 8, 128], BF16)
    C32 = [P.sb("b_C32_%d" % b, [64, 8, 129], F32) for b in range(NB)]
    Cb = [P.sb("b_Cb_%d" % b, [64, 8, 129], BF16) for b in range(NB)]
    dmax = P.sb("b_dmax", [128, 8], F32)
    rc = P.sb("b_rc", [128, 8], F32)
    hm = P.sb("b_hm", [128, 8, 128], F32)
    hsq = P.sb("b_hsq", [128, 8, 128], F32)
    hs8 = (P.sb("b_hssq", [128, 8], F32), P.sb("b_htmp", [128, 8], F32), P.sb("b_hrstd", [128, 8], F32))
    hn = P.sb("b_hn", [128, D], BF16)
    hnT = P.sb("b_hnT", [128, 8, 128], BF16)
    mx = P.sb("b_mx", [128, D], F32)
    mxb = P.sb("b_mxb", [128, D], BF16)
    mxT = P.sb("b_mxT", [128, 8, 128], BF16)
    xo = [P.sb("b_xo%d" % i, [128, D], F32) for i in range(2)]
    HG = [(0, 3), (3, 6), (6, 8)]

    def load_in(n):
        P.load(xs[n % 2], xs[n % 2][:], c.x[n * 128:(n + 1) * 128, :])
        P.load(ma[n % 2], ma[n % 2][:], c.mixa[n * 128:(n + 1) * 128, :], dram_reads=[c.mixa_t[n]])

    load_in(0)

    def tile(n):
        b = n // NT
        j = n % NT
        if n + 1 < NTT:
            load_in(n + 1)
        x_t = xs[n % 2]
        ma_t = ma[n % 2]
        norm_and_transpose(c, x_t, c.g1, hb, hT, junk, st, bank[7])

        def proj(bk, col0, ncols, M=128, ocol=0):
            for k in range(8):
                P.op("pe", lambda e, k=k: e.matmul(out=bk[0:M, ocol:ocol + ncols], lhsT=hT[:, k, :], rhs=wB[:, k, col0:col0 + ncols],
                                                   start=(k == 0), stop=(k == 7)),
                     reads=[hT, wB], writes=[bk] if (k == 0 and ocol == 0) else [], parts=[] if (k == 0 and ocol == 0) else [bk])
        for gi, col in enumerate((2048, 2056)):
            for k in range(8):
                P.op("pe", lambda e, k=k, gi=gi, col=col: e.matmul(out=bank[6][0:8, gi * 128:(gi + 1) * 128], lhsT=wB[:, k, col:col + 8], rhs=hT[:, k, :],
                                                                    start=(k == 0), stop=(k == 7)),
                     reads=[hT, wB], writes=[bank[6]] if (k == 0 and gi == 0) else [], parts=[] if (k == 0 and gi == 0) else [bank[6]])
        P.op("act", lambda e: e.activation(out=g_ti[:], in_=bank[6][0:8, 0:128], func=AF.Tanh, bias=bif[:, 0:1], scale=1.0 / 15.0),
             reads=[bank[6], bif], writes=[g_ti])
        P.op("act", lambda e: e.activation(out=g_tf[:], in_=bank[6][0:8, 128:256], func=AF.Tanh, bias=bif[:, 1:2], scale=1.0 / 15.0),
             reads=[bank[6], bif], writes=[g_tf])
        P.op("act", lambda e: e.activation(out=g_nl[:], in_=g_tf[:], func=AF.Exp, scale=-15.0), reads=[g_tf], writes=[g_nl])
        P.op("act", lambda e: e.activation(out=g_nl[:], in_=g_nl[:], func=AF.Ln, bias=1.0), reads=[g_nl], writes=[g_nl])
        if j == 0:
            P.op("dve", lambda e: e.tensor_tensor_scan(out=g_cum[:], data0=g_ones[:], data1=g_nl[:], initial=0.0, op0=ALU.mult, op1=ALU.add),
                 reads=[g_ones, g_nl], writes=[g_cum])
        else:
            P.op("dve", lambda e: e.tensor_tensor_scan(out=g_cum[:], data0=g_ones[:], data1=g_nl[:], initial=cumc[b][:], op0=ALU.mult, op1=ALU.add),
                 reads=[g_ones, g_nl, cumc[b]], writes=[g_cum])
        P.op("dve", lambda e: e.scalar_tensor_tensor(out=g_a[:], in0=g_ti[:], scalar=15.0, in1=g_cum[:], op0=ALU.mult, op1=ALU.add),
             reads=[g_ti, g_cum], writes=[g_a])
        if j == 0:
            P.op("dve", lambda e: e.memset(Mc[b][:], 0.0), writes=[Mc[b]])
        P.op("dve", lambda e: e.tensor_tensor_scan(out=g_M[:], data0=g_a[:], data1=g_a[:], initial=Mc[b][:], op0=ALU.max, op1=ALU.max),
             reads=[g_a, Mc[b]], writes=[g_M])
        P.op("dve", lambda e: e.tensor_scalar(out=g_nM0[:], in0=Mc[b][:], scalar1=-1.0, scalar2=None, op0=ALU.mult), reads=[Mc[b]], writes=[g_nM0])
        P.op("dve", lambda e: e.tensor_scalar(out=g_nMe[:], in0=g_M[:, 127:128], scalar1=-1.0, scalar2=None, op0=ALU.mult), reads=[g_M], writes=[g_nMe])
        P.op("dve", lambda e: e.tensor_tensor(out=g_dd[:], in0=Mc[b][:], in1=g_nMe[:], op=ALU.add), reads=[Mc[b], g_nMe], writes=[g_dd])
        P.op("dve", lambda e: e.tensor_tensor(out=g_d2[:], in0=g_cum[:], in1=g_M[:], op=ALU.subtract), reads=[g_cum, g_M], writes=[g_d2])
        P.op("act", lambda e: e.activation(out=g_out[0][:], in_=g_M[:], func=AF.Exp, bias=Mc[b][:], scale=-1.0), reads=[g_M, Mc[b]], writes=[g_out[0]])
        P.op("act", lambda e: e.activation(out=g_out[1][:], in_=g_a[:], func=AF.Exp, bias=g_nM0[:], scale=1.0), reads=[g_a, g_nM0], writes=[g_out[1]])
        P.op("act", lambda e: e.activation(out=g_out[2][:], in_=g_a[:], func=AF.Exp, bias=g_nMe[:], scale=1.0), reads=[g_a, g_nMe], writes=[g_out[2]])
        P.op("act", lambda e: e.activation(out=g_out[3][:], in_=g_a[:], func=AF.Exp, bias=g_dd[:], scale=0.0), reads=[g_a, g_dd], writes=[g_out[3]])
        P.op("act", lambda e: e.activation(out=g_out[4][:], in_=g_d2[:], func=AF.Exp), reads=[g_d2], writes=[g_out[4]])
        P.op("dve", lambda e: e.tensor_copy(out=cumc[b][:], in_=g_cum[:, 127:128]), reads=[g_cum], writes=[cumc[b]])
        P.op("dve", lambda e: e.tensor_copy(out=Mc[b][:], in_=g_M[:, 127:128]), reads=[g_M], writes=[Mc[b]])
        for qi in range(5):
            P.op("pe", lambda e, qi=qi: e.transpose(out=bank[6][:, 256 + qi * 8:256 + (qi + 1) * 8], in_=g_out[qi][:], identity=c.identf[0:8, 0:8]),
                 reads=[g_out[qi], c.identf], writes=[bank[6]] if qi == 0 else [], parts=[] if qi == 0 else [bank[6]])
        P.op("dve", lambda e: e.tensor_copy(out=gtok[:].rearrange("p q h -> p (q h)"), in_=bank[6][:, 256:296]), reads=[bank[6]], writes=[gtok])
        proj(bank[0], 0, 512)
        proj(bank[1], 512, 512)
        P.op("dve", lambda e: e.tensor_tensor(out=qt[:].rearrange("p (h d) -> p h d", d=64), in0=bank[0][:].rearrange("p (h d) -> p h d", d=64),
                                              in1=gtok[:, 0, :].unsqueeze(2).to_broadcast([128, 8, 64]), op=ALU.mult),
             reads=[bank[0], gtok], writes=[qt])
        P.op("dve", lambda e: e.scalar_tensor_tensor(out=kt[:].rearrange("p (h d) -> p h d", d=64), in0=bank[1][:].rearrange("p (h d) -> p h d", d=64),
                                                     scalar=0.125, in1=gtok[:, 1, :].unsqueeze(2).to_broadcast([128, 8, 64]), op0=ALU.mult, op1=ALU.mult),
             reads=[bank[1], gtok], writes=[kt])
        P.op("dve", lambda e: e.scalar_tensor_tensor(out=khat[:].rearrange("p (h d) -> p h d", d=64), in0=bank[1][:].rearrange("p (h d) -> p h d", d=64),
                                                     scalar=0.125, in1=gtok[:, 2, :].unsqueeze(2).to_broadcast([128, 8, 64]), op0=ALU.mult, op1=ALU.mult),
             reads=[bank[1], gtok], writes=[khat])
        for hv in range(2):
            proj(bank[2 + hv], 1024 + hv * 512, 512)
            P.op("act", lambda e, hv=hv: e.copy(out=vaug[:, 4 * hv:4 * hv + 4, 0:128], in_=bank[2 + hv][:].rearrange("p (h d) -> p h d", d=128)),
                 reads=[bank[2 + hv]], writes=[vaug] if hv == 0 else [], parts=[] if hv == 0 else [vaug])
        for hv in range(2):
            proj(bank[4 + hv], 2064 + hv * 512, 512)
            P.op("act", lambda e, hv=hv: e.activation(out=sgo[:, hv * 512:(hv + 1) * 512], in_=bank[4 + hv][:], func=AF.Sigmoid),
                 reads=[bank[4 + hv]], writes=[sgo] if hv == 0 else [], parts=[] if hv == 0 else [sgo])
        for hv in range(2):
            proj(bank[2 + hv], 3088 + hv * 512, 512)
            P.op("act", lambda e, hv=hv: e.activation(out=sgm[:, hv * 512:(hv + 1) * 512], in_=bank[2 + hv][:], func=AF.Sigmoid),
                 reads=[bank[2 + hv]], writes=[sgm] if hv == 0 else [], parts=[] if hv == 0 else [sgm])
        for (src, dstT, bk, eng) in ((qt, qtT, bank[7], "act"), (kt, ktT, bank[6], "dve")):
            pv = bk[:].bitcast(BF16)
            for h in range(8):
                P.op("pe", lambda e, h=h, src=src, pv=pv: e.transpose(out=pv[0:64, h * 128:(h + 1) * 128], in_=src[:, h * 64:(h + 1) * 64], identity=c.identb[:]),
                     reads=[src, c.identb], writes=[bk] if h == 0 else [], parts=[] if h == 0 else [bk])
            if eng == "act":
                P.op("act", lambda e, pv=pv, dstT=dstT: e.copy(out=dstT[:].rearrange("p h t -> p (h t)"), in_=pv[0:64, :]), reads=[bk], writes=[dstT])
            else:
                P.op("dve", lambda e, pv=pv, dstT=dstT: e.tensor_copy(out=dstT[:].rearrange("p h t -> p (h t)"), in_=pv[0:64, :]), reads=[bk], writes=[dstT])
        for hb4 in range(2):
            bk = bank[hb4]
            for hh in range(4):
                h = hb4 * 4 + hh
                P.op("pe", lambda e, h=h, hh=hh, bk=bk: e.matmul(out=bk[:, hh * 128:(hh + 1) * 128], lhsT=ktT[:, h, :], rhs=qtT[:, h, :], start=True, stop=True),
                     reads=[ktT, qtT], writes=[bk] if hh == 0 else [], parts=[] if hh == 0 else [bk])
            P.op("dve", lambda e, bk=bk, hb4=hb4: e.tensor_tensor(out=PTm[:, 4 * hb4:4 * hb4 + 4, :].rearrange("p h t -> p (h t)"), in0=bk[:], in1=cmask[:], op=ALU.mult),
                 reads=[bk, cmask], writes=[PTm] if hb4 == 0 else [], parts=[] if hb4 == 0 else [PTm])
        for gi, (h0, h1) in enumerate(HG):
            bk = bank[2 + gi]
            for h in range(h0, h1):
                o = (h - h0) * 129
                P.op("pe", lambda e, h=h, o=o, bk=bk: e.matmul(out=bk[:, o:o + 129], lhsT=PTm[:, h, :], rhs=vaug[:, h, :], start=True, stop=(j == 0)),
                     reads=[PTm, vaug], writes=[bk] if h == h0 else [], parts=[] if h == h0 else [bk])
                if j > 0:
                    P.op("pe", lambda e, h=h, o=o, bk=bk: e.matmul(out=bk[:, o:o + 129], lhsT=qtT[:, h, :], rhs=Cb[b][:, h, :], start=False, stop=True),
                         reads=[qtT, Cb[b]], parts=[bk])
        for gi, (h0, h1) in enumerate(HG):
            bk = bank[5 + gi]
            for h in range(h0, h1):
                o = (h - h0) * 129
                P.op("pe", lambda e, h=h, o=o, bk=bk: e.matmul(out=bk[0:64, o:o + 129], lhsT=khat[:, h * 64:(h + 1) * 64], rhs=vaug[:, h, :], start=True, stop=True),
                     reads=[khat, vaug], writes=[bk] if h == h0 else [], parts=[] if h == h0 else [bk])
        if j > 0:
            P.op("dve", lambda e: e.tensor_tensor(out=C32[b][:], in0=C32[b][:], in1=gtok[0:64, 3, :].unsqueeze(2).to_broadcast([64, 8, 129]), op=ALU.mult),
                 reads=[C32[b], gtok], writes=[C32[b]])
        for gi, (h0, h1) in enumerate(HG):
            bk = bank[5 + gi]
            nh = h1 - h0
            if j > 0:
                P.op("dve", lambda e, bk=bk, h0=h0, h1=h1, nh=nh: e.tensor_tensor(out=C32[b][:, h0:h1, :], in0=C32[b][:, h0:h1, :],
                                                                                  in1=bk[0:64, 0:nh * 129].rearrange("p (h v) -> p h v", v=129), op=ALU.add),
                     reads=[bk, C32[b]], writes=[C32[b]])
            else:
                P.op("dve", lambda e, bk=bk, h0=h0, h1=h1, nh=nh: e.tensor_copy(out=C32[b][:, h0:h1, :], in_=bk[0:64, 0:nh * 129].rearrange("p (h v) -> p h v", v=129)),
                     reads=[bk], writes=[C32[b]] if gi == 0 else [], parts=[] if gi == 0 else [C32[b]])
        P.op("act", lambda e: e.copy(out=Cb[b][:], in_=C32[b][:]), reads=[C32[b]], writes=[Cb[b]])
        for gi, (h0, h1) in enumerate(HG):
            bk = bank[2 + gi]
            nh = h1 - h0
            v3 = bk[:, 0:nh * 129].rearrange("p (h v) -> p h v", v=129)
            P.op("dve", lambda e, v3=v3, h0=h0, h1=h1: e.tensor_tensor(out=dmax[:, h0:h1].unsqueeze(2), in0=v3[:, :, 128:129], in1=gtok[:, 4, h0:h1].unsqueeze(2), op=ALU.max),
                 reads=[bk, gtok], writes=[dmax])
            P.op("dve", lambda e, v3=v3, h0=h0, h1=h1: e.scalar_tensor_tensor(out=dmax[:, h0:h1].unsqueeze(2), in0=v3[:, :, 128:129], scalar=-1.0, in1=dmax[:, h0:h1].unsqueeze(2), op0=ALU.mult, op1=ALU.max),
                 reads=[bk, dmax], writes=[dmax])
        P.op("dve", lambda e: e.reciprocal(out=rc[:], in_=dmax[:]), reads=[dmax], writes=[rc])
        for gi, (h0, h1) in enumerate(HG):
            bk = bank[2 + gi]
            nh = h1 - h0
            v3 = bk[:, 0:nh * 129].rearrange("p (h v) -> p h v", v=129)
            P.op("dve", lambda e, v3=v3, h0=h0, h1=h1, nh=nh: e.tensor_tensor(out=hm[:, h0:h1, :], in0=v3[:, :, 0:128],
                                                                              in1=rc[:, h0:h1].unsqueeze(2).to_broadcast([128, nh, 128]), op=ALU.mult),
                 reads=[bk, rc], writes=[hm] if gi == 0 else [], parts=[] if gi == 0 else [hm])
        P.op("pool", lambda e: e.tensor_tensor(out=hsq[:], in0=hm[:], in1=hm[:], op=ALU.mult), reads=[hm], writes=[hsq])
        P.op("dve", lambda e: e.tensor_reduce(out=hs8[0][:], in_=hsq[:], axis=AX.X, op=ALU.add), reads=[hsq], writes=[hs8[0]])
        rms_rstd(c, "h", hs8[0], 128, hs8[2], hs8[1])
        P.op("dve", lambda e: e.tensor_tensor(out=hm[:], in0=hm[:], in1=hs8[2][:].unsqueeze(2).to_broadcast([128, 8, 128]), op=ALU.mult),
             reads=[hm, hs8[2]], writes=[hm])
        hmf = hm[:].rearrange("p h v -> p (h v)")
        P.op("pool", lambda e: e.tensor_tensor(out=hmf, in0=hmf, in1=mlg[:], op=ALU.mult), reads=[hm, mlg], writes=[hm])
        P.op("dve", lambda e: e.tensor_tensor(out=hn[:], in0=hmf, in1=sgo[:], op=ALU.mult), reads=[hm, sgo], writes=[hn])
        transpose8(c, hn, hnT, bank[7])
        for hv in range(2):
            bk = bank[hv]
            for i in range(8):
                P.op("pe", lambda e, bk=bk, i=i, hv=hv: e.matmul(out=bk[:], lhsT=hnT[:, i, :], rhs=wbm[:, i, hv * 512:(hv + 1) * 512], start=(i == 0), stop=(i == 7)),
                     reads=[hnT, wbm], writes=[bk] if i == 0 else [], parts=[] if i == 0 else [bk])
            P.op("dve", lambda e, bk=bk, hv=hv: e.tensor_tensor(out=mx[:, hv * 512:(hv + 1) * 512], in0=bk[:], in1=sgm[:, hv * 512:(hv + 1) * 512], op=ALU.mult),
                 reads=[bk, sgm], writes=[mx] if hv == 0 else [], parts=[] if hv == 0 else [mx])
        P.op("pool", lambda e: e.tensor_tensor(out=mxb[:], in0=mx[:], in1=ma_t[:], op=ALU.add), reads=[mx, ma_t], writes=[mxb])
        transpose8(c, mxb, mxT, bank[6], eng="dve")
        xo_t = xo[n % 2]
        for hv in range(2):
            bk = bank[2 + hv]
            for i in range(8):
                P.op("pe", lambda e, bk=bk, i=i, hv=hv: e.matmul(out=bk[:], lhsT=mxT[:, i, :], rhs=wout[:, i, hv * 512:(hv + 1) * 512], start=(i == 0), stop=(i == 7)),
                     reads=[mxT, wout], writes=[bk] if i == 0 else [], parts=[] if i == 0 else [bk])
            P.op("dve", lambda e, bk=bk, hv=hv: e.tensor_tensor(out=xo_t[:, hv * 512:(hv + 1) * 512], in0=bk[:], in1=x_t[:, hv * 512:(hv + 1) * 512], op=ALU.add),
                 reads=[bk, x_t], writes=[xo_t] if hv == 0 else [], parts=[] if hv == 0 else [xo_t])
        P.store(c.out[n * 128:(n + 1) * 128, :], xo_t, xo_t[:], dram_writes=[c.x1_t[n]], final=("C" not in c.phases))

    for n in range(NTT):
        tile(n)


def phase_c(c):
    P, NB, NT, NTT = c.P, c.NB, c.NT, c.NTT
    nc = c.nc
    bank = c.bank
    GT = 3
    ex_s = P.dram("ex_s", [128, 128, 2 * D], BF16)
    downT_s = ex_s[:, :, 0:D]
    up_s = ex_s[:, :, D:2 * D]
    downT_t = [T("downT_t%d" % i) for i in range(128)]
    up_t = [T("up_t%d" % i) for i in range(16)]
    up_v = c.peer_up.rearrange("(i c) d -> c i d", c=128)
    dn_v = c.peer_down.rearrange("(i c) d -> c i d", c=128)
    dummy = T("up_dma_sem")
    for u in range(16):
        P.dma(up_s[u * 8:(u + 1) * 8], up_v[u * 8:(u + 1) * 8], writes=[up_t[u]], sem_tile=dummy, eng="pool", max_dma_last_dim=4096)
    P.open_scope()
    dsrc = [P.sb("c_dsrc%d" % i, [128, D], BF16) for i in range(2)]
    dtr = [P.sb("c_dtr%d" % i, [128, D], BF16) for i in range(2)]
    for cb in range(128):
        sl = cb % 2
        P.load(dsrc[sl], dsrc[sl][:], dn_v[cb], eng="pool", max_dma_last_dim=4096)
        pv = bank[6 + sl][:].bitcast(BF16)
        for k in range(8):
            P.op("pe", lambda e, k=k, sl=sl, pv=pv: e.transpose(out=pv[:, k * 128:(k + 1) * 128], in_=dsrc[sl][:, k * 128:(k + 1) * 128], identity=c.identb[:]),
                 reads=[dsrc[sl], c.identb], writes=[bank[6 + sl]] if k == 0 else [], parts=[] if k == 0 else [bank[6 + sl]])
        if sl == 0:
            P.op("act", lambda e, sl=sl, pv=pv: e.copy(out=dtr[sl][:], in_=pv), reads=[bank[6 + sl]], writes=[dtr[sl]])
        else:
            P.op("dve", lambda e, sl=sl, pv=pv: e.tensor_copy(out=dtr[sl][:], in_=pv), reads=[bank[6 + sl]], writes=[dtr[sl]])
        P.store(downT_s[cb], dtr[sl], dtr[sl][:], dram_writes=[downT_t[cb]])
    P.close_scope()
    cstage = c.dbg if (c.dbg and c.phases == "C") else 99
    if cstage == 1:
        return

    wq = P.sb("c_wq", [128, 8, D], BF16)
    load_w_bf16(c, wq, 0, D, c.peer_w_query)
    g2 = P.sb("c_g2", [128, D], F32)
    P.load(g2, g2[:], c.norm2_gain[0].partition_broadcast(128))
    iof = P.sb("c_iof", [128, 128], F32)
    P.load(iof, iof[:], c.cst["iota128"])
    iob = P.sb("c_iob", [128, 128], BF16)
    P.op("dve", lambda e: e.tensor_copy(out=iob[:], in_=iof[:]), reads=[iof], writes=[iob])
    skT = P.sb("c_skT", [128, 8, 128], BF16)
    P.open_scope()
    skf = P.sb("c_skf", [128, 16, 64], F32)
    P.load(skf, skf[:], c.peer_sub_keys.rearrange("h p k d -> k (h p) d"))
    skb = P.sb("c_skb", [128, 16 * 64], BF16)
    P.op("dve", lambda e: e.tensor_copy(out=skb[:], in_=skf[:].rearrange("k a d -> k (a d)")), reads=[skf], writes=[skb])
    transpose8(c, skb, skT, bank[7])
    P.close_scope()

    if cstage == 2:
        return
    x1s = [P.sb("c_x1s%d" % i, [128, D], F32) for i in range(GT)]
    st = (P.sb("c_ssq", [128, 1], F32), P.sb("c_tmp", [128, 1], F32), P.sb("c_rstd", [128, 1], F32))
    xb = P.sb("c_xb", [128, D], BF16)
    xnT = P.sb("c_xnT", [128, 8, GT * 128], BF16)
    qT = P.sb("c_qT", [128, 8, 128], BF16)
    sc = P.sb("c_sc", [128, 16, 128], F32)
    sc_t = [T("sc_t%d" % i) for i in range(16)]
    v16 = P.sb("c_v16", [128, 16, 16], F32)
    ix = P.sb("c_ix", [128, 16, 16], U32)
    ixf = P.sb("c_ixf", [128, 16, 16], F32)
    cand = P.sb("c_cand", [128, 8, 256], F32)
    cand_t = [T("cand_t%d" % i) for i in range(8)]
    v16b, ixb, c16b, posb = T("v16b"), T("ixb"), T("c16b"), T("posb")
    c16 = P.sb("c_c16", [128, 8, 16], F32)
    pos = P.sb("c_pos", [128, 8, 16], U32)
    posf = P.sb("c_posf", [128, 8, 16], F32)
    pki = P.sb("c_pki", [128, 8, 16], I32)
    pkf = P.sb("c_pkf", [128, 8, 16], F32)
    qkf = P.sb("c_qkf", [128, 8, 16], F32)
    decs = [P.sb("c_dec%d" % i, [128, 2, 16, 16], F32) for i in range(4)]
    abg = [P.sb("c_abg%d" % i, [128, 8, 16], F32) for i in range(3)]
    z8 = P.sb("c_z8", [128, 8], F32)
    abgT = [P.sb("c_abgT%d" % i, [128, GT * 128], F32) for i in range(3)]
    CH = 8
    Ach = [P.sb("c_Ach%d" % i, [128, CH, 128], BF16) for i in range(2)]
    Bch = [P.sb("c_Bch%d" % i, [128, CH, 128], BF16) for i in range(2)]
    Wg = P.sb("c_Wg", [128, 128, GT * 128], BF16)
    NS = 4
    esl = [P.sb("c_esl%d" % i, [128, 2 * D], BF16) for i in range(NS)]
    actb = [P.sb("c_act%d" % i, [128, GT * 128], BF16) for i in range(2)]
    wab = [P.sb("c_wa%d" % i, [128, GT * 128], BF16) for i in range(2)]

    groups = []
    n0 = 0
    while n0 < NTT:
        g = min(GT, NTT - n0)
        groups.append((n0, g))
        n0 += g
    blk_ctr = [0]

    def route_tile(n, ti):
        x_t = x1s[ti]
        src_x1 = c.x if c.phases == "C" else c.out
        P.load(x_t, x_t[:], src_x1[n * 128:(n + 1) * 128, :], dram_reads=[c.x1_t[n]])
        ssq, tmp, rstd = st
        P.op("act", lambda e: e.activation(out=xb[:], in_=x_t[:], func=AF.Square, accum_out=ssq[:]), reads=[x_t], writes=[xb, ssq])
        rms_rstd(c, "n", ssq, D, rstd, tmp)
        P.op("dve", lambda e: e.scalar_tensor_tensor(out=xb[:], in0=x_t[:], scalar=rstd[:], in1=g2[:], op0=ALU.mult, op1=ALU.mult),
             reads=[x_t, rstd, g2], writes=[xb])
        pvx = bank[7][:].bitcast(BF16)
        for k in range(8):
            P.op("pe", lambda e, k=k: e.transpose(out=pvx[:, k * 128:(k + 1) * 128], in_=xb[:, k * 128:(k + 1) * 128], identity=c.identb[:]),
                 reads=[xb, c.identb], writes=[bank[7]] if k == 0 else [], parts=[] if k == 0 else [bank[7]])
        xt1 = xnT[:, :, ti * 128:(ti + 1) * 128]
        P.op("act", lambda e: e.copy(out=xt1, in_=pvx.rearrange("p (k t) -> p k t", t=128)), reads=[bank[7]], writes=[xnT] if ti == 0 else [], parts=[] if ti == 0 else [xnT])
        if cstage == 29:
            return
        for hd in range(8):
            bk = bank[hd % 2]
            for k in range(8):
                P.op("pe", lambda e, k=k, hd=hd, bk=bk: e.matmul(out=bk[:, 0:128], lhsT=wq[:, k, hd * 128:(hd + 1) * 128], rhs=xt1[:, k, :],
                                                                start=(k == 0), stop=(k == 7)),
                     reads=[wq, xnT], writes=[bk] if k == 0 else [], parts=[] if k == 0 else [bk])
            if hd % 2 == 0:
                P.op("act", lambda e, hd=hd, bk=bk: e.copy(out=qT[:, hd, :], in_=bk[:, 0:128]), reads=[bk], writes=[qT] if hd == 0 else [], parts=[] if hd == 0 else [qT])
            else:
                P.op("dve", lambda e, hd=hd, bk=bk: e.tensor_copy(out=qT[:, hd, :], in_=bk[:, 0:128]), reads=[bk], parts=[qT])
        if cstage == 30:
            return
        sc4 = sc[:].rearrange("p (h two) k -> p h two k", two=2)
        for par in range(2):
            for hf in range(2):
                bk = bank[2 + par * 2 + hf]
                for u in range(4):
                    hd = hf * 4 + u
                    P.op("pe", lambda e, u=u, hd=hd, par=par, bk=bk: e.matmul(out=bk[:, u * 128:(u + 1) * 128], lhsT=qT[par * 64:(par + 1) * 64, hd, :],
                                                                          rhs=skT[par * 64:(par + 1) * 64, hd, :], start=True, stop=True),
                         reads=[qT, skT], writes=[bk] if u == 0 else [], parts=[] if u == 0 else [bk])
        for par in range(2):
            for hf in range(2):
                bk = bank[2 + par * 2 + hf]
                first = (par == 0 and hf == 0)
                if hf == 0:
                    P.op("act", lambda e, par=par, hf=hf, bk=bk: e.copy(out=sc4[:, hf * 4:hf * 4 + 4, par, :], in_=bk[:].rearrange("p (a k) -> p a k", k=128)),
                         reads=[bk], writes=[sc_t[(hf * 4 + u_) * 2 + par] for u_ in range(4)])
                else:
                    P.op("dve", lambda e, par=par, hf=hf, bk=bk: e.tensor_copy(out=sc4[:, hf * 4:hf * 4 + 4, par, :], in_=bk[:].rearrange("p (a k) -> p a k", k=128)),
                         reads=[bk], writes=[sc_t[(hf * 4 + u_) * 2 + par] for u_ in range(4)])
        if cstage in (31, 305, 306, 307):
            return
        for hp in range(16):
            P.op("dve", lambda e, hp=hp: e.max(out=v16[:, hp, 0:8], in_=sc[:, hp, :]), reads=[sc_t[hp]], writes=[v16] if hp == 0 else [], parts=[] if hp == 0 else [v16])
        for hp in range(16):
            P.op("dve", lambda e, hp=hp: e.max_index(out=ix[:, hp, 0:8], in_max=v16[:, hp, 0:8], in_values=sc[:, hp, :]), reads=[sc_t[hp], v16],
                 writes=[ix] if hp == 0 else [], parts=[] if hp == 0 else [ix])
        for hp in range(16):
            P.op("dve", lambda e, hp=hp: e.match_replace(out=sc[:, hp, :], in_to_replace=v16[:, hp, 0:8], in_values=sc[:, hp, :], imm_value=-1e30),
                 reads=[sc_t[hp], v16], writes=[sc_t[hp]])
        for hp in range(16):
            P.op("dve", lambda e, hp=hp: e.max(out=v16[:, hp, 8:16], in_=sc[:, hp, :]), reads=[sc_t[hp]], writes=[v16b] if hp == 0 else [], parts=[] if hp == 0 else [v16b])
        for hp in range(16):
            P.op("dve", lambda e, hp=hp: e.max_index(out=ix[:, hp, 8:16], in_max=v16[:, hp, 8:16], in_values=sc[:, hp, :]), reads=[sc_t[hp], v16b],
                 writes=[ixb] if hp == 0 else [], parts=[] if hp == 0 else [ixb])
        if cstage == 32:
            return
        P.op("dve", lambda e: e.tensor_copy(out=ixf[:], in_=ix[:]), reads=[ix, ixb], writes=[ixf])
        vv = v16[:].rearrange("p (h two) k -> p h two k", two=2)
        iv = ixf[:].rearrange("p (h two) k -> p h two k", two=2)
        P.op("dve", lambda e: e.tensor_tensor(out=cand[:].rearrange("p h (a b) -> p h a b", b=16),
                                              in0=vv[:, :, 0, :].unsqueeze(3).to_broadcast([128, 8, 16, 16]),
                                              in1=vv[:, :, 1, :].unsqueeze(2).to_broadcast([128, 8, 16, 16]), op=ALU.add),
             reads=[v16, v16b], writes=cand_t)
        for h in range(8):
            P.op("dve", lambda e, h=h: e.max(out=c16[:, h, 0:8], in_=cand[:, h, :]), reads=[cand_t[h]], writes=[c16] if h == 0 else [], parts=[] if h == 0 else [c16])
        for h in range(8):
            P.op("dve", lambda e, h=h: e.max_index(out=pos[:, h, 0:8], in_max=c16[:, h, 0:8], in_values=cand[:, h, :]), reads=[cand_t[h], c16],
                 writes=[pos] if h == 0 else [], parts=[] if h == 0 else [pos])
        for h in range(8):
            P.op("dve", lambda e, h=h: e.match_replace(out=cand[:, h, :], in_to_replace=c16[:, h, 0:8], in_values=cand[:, h, :], imm_value=-1e30),
                 reads=[cand_t[h], c16], writes=[cand_t[h]])
        for h in range(8):
            P.op("dve", lambda e, h=h: e.max(out=c16[:, h, 8:16], in_=cand[:, h, :]), reads=[cand_t[h]], writes=[c16b] if h == 0 else [], parts=[] if h == 0 else [c16b])
        for h in range(8):
            P.op("dve", lambda e, h=h: e.max_index(out=pos[:, h, 8:16], in_max=c16[:, h, 8:16], in_values=cand[:, h, :]), reads=[cand_t[h], c16b],
                 writes=[posb] if h == 0 else [], parts=[] if h == 0 else [posb])
        if cstage == 33:
            return
        P.op("dve", lambda e: e.tensor_copy(out=posf[:], in_=pos[:]), reads=[pos, posb], writes=[posf])
        P.op("dve", lambda e: e.tensor_scalar(out=pkf[:], in0=posf[:], scalar1=-7.5, scalar2=0.0625, op0=ALU.add, op1=ALU.mult), reads=[posf], writes=[pkf])
        P.op("dve", lambda e: e.tensor_copy(out=pki[:], in_=pkf[:]), reads=[pkf], writes=[pki])
        P.op("dve", lambda e: e.tensor_copy(out=pkf[:], in_=pki[:]), reads=[pki], writes=[pkf])
        P.op("dve", lambda e: e.scalar_tensor_tensor(out=qkf[:], in0=pkf[:], scalar=-16.0, in1=posf[:], op0=ALU.mult, op1=ALU.add), reads=[pkf, posf], writes=[qkf])
        combos = [(hh, which, sel, dst) for hh in range(4) for (which, sel, dst) in ((0, pkf, abg[0]), (1, qkf, abg[1]))]
        for half in range(2):
            sub = combos[half * 4:(half + 1) * 4]
            for di, (hh, which, sel, dst) in enumerate(sub):
                hs = slice(hh * 2, hh * 2 + 2)
                dec = decs[di]
                P.op("dve", lambda e, sel=sel, hs=hs, dec=dec: e.tensor_tensor(out=dec[:], in0=iof[:, 0:16].unsqueeze(1).unsqueeze(1).to_broadcast([128, 2, 16, 16]),
                                                                            in1=sel[:, hs, :].unsqueeze(3).to_broadcast([128, 2, 16, 16]), op=ALU.is_equal),
                     reads=[iof, sel], writes=[dec])
            for di, (hh, which, sel, dst) in enumerate(sub):
                hs = slice(hh * 2, hh * 2 + 2)
                dec = decs[di]
                P.op("pool", lambda e, which=which, hs=hs, dec=dec: e.tensor_tensor(out=dec[:], in0=dec[:], in1=iv[:, hs, which, :].unsqueeze(2).to_broadcast([128, 2, 16, 16]), op=ALU.mult),
                     reads=[dec, ixf], writes=[dec])
            for di, (hh, which, sel, dst) in enumerate(sub):
                hs = slice(hh * 2, hh * 2 + 2)
                dec = decs[di]
                firstw = (hh == 0)
                P.op("dve", lambda e, dst=dst, hs=hs, dec=dec: e.tensor_reduce(out=dst[:, hs, :], in_=dec[:], axis=AX.X, op=ALU.add),
                     reads=[dec], writes=[dst] if firstw else [], parts=[] if firstw else [dst])
        if cstage == 34:
            return
        P.op("dve", lambda e: e.tensor_tensor(out=abg[2][:], in0=c16[:], in1=c16[:, :, 0:1].to_broadcast([128, 8, 16]), op=ALU.subtract), reads=[c16, c16b], writes=[abg[2]])
        P.op("act", lambda e: e.activation(out=abg[2][:], in_=abg[2][:], func=AF.Exp), reads=[abg[2]], writes=[abg[2]])
        P.op("dve", lambda e: e.tensor_reduce(out=z8[:], in_=abg[2][:], axis=AX.X, op=ALU.add), reads=[abg[2]], writes=[z8])
        P.op("dve", lambda e: e.reciprocal(out=z8[:], in_=z8[:]), reads=[z8], writes=[z8])
        P.op("dve", lambda e: e.tensor_tensor(out=abg[2][:], in0=abg[2][:], in1=z8[:].unsqueeze(2).to_broadcast([128, 8, 16]), op=ALU.mult), reads=[abg[2], z8], writes=[abg[2]])
        if cstage == 35:
            return
        for i3 in range(3):
            P.op("pe", lambda e, i3=i3: e.transpose(out=bank[6][:, i3 * 128:(i3 + 1) * 128], in_=abg[i3][:].rearrange("p h k -> p (h k)"), identity=c.identf[:]),
                 reads=[abg[i3], c.identf], writes=[bank[6]] if i3 == 0 else [], parts=[] if i3 == 0 else [bank[6]])
        for i3 in range(3):
            P.op("act", lambda e, i3=i3: e.copy(out=abgT[i3][:, ti * 128:(ti + 1) * 128], in_=bank[6][:, i3 * 128:(i3 + 1) * 128]), reads=[bank[6]], parts=[abgT[i3]])
        if cstage == 36:
            return
        for tq in range(0, 128, 4):
            chn = (tq // CH) % 2
            bk = bank[(tq // 4) % 2]
            for t in range(tq, tq + 4):
                tl = t % CH
                tg = ti * 128 + t
                first = (tl == 0)
                P.op("dve", lambda e, chn=chn, tl=tl, tg=tg: e.tensor_scalar(out=Ach[chn][:, tl, :], in0=iob[:], scalar1=abgT[0][:, tg:tg + 1], scalar2=abgT[2][:, tg:tg + 1],
                                                                       op0=ALU.is_equal, op1=ALU.mult),
                     reads=[iob, abgT[0], abgT[2]], writes=[Ach[chn]] if first else [], parts=[] if first else [Ach[chn]])
                P.op("pool", lambda e, chn=chn, tl=tl, tg=tg: e.tensor_scalar(out=Bch[chn][:, tl, :], in0=iob[:], scalar1=abgT[1][:, tg:tg + 1], scalar2=None, op0=ALU.is_equal),
                     reads=[iob, abgT[1]], writes=[Bch[chn]] if first else [], parts=[] if first else [Bch[chn]])
            for t in range(tq, tq + 4):
                tl = t % CH
                u = t - tq
                P.op("pe", lambda e, chn=chn, tl=tl, u=u, bk=bk: e.matmul(out=bk[:, u * 128:(u + 1) * 128], lhsT=Ach[chn][:, tl, :], rhs=Bch[chn][:, tl, :], start=True, stop=True),
                     reads=[Ach[chn], Bch[chn]], writes=[bk] if u == 0 else [], parts=[] if u == 0 else [bk])
            tg0 = ti * 128 + tq
            P.op("act", lambda e, bk=bk, tg0=tg0: e.copy(out=Wg[:, :, tg0:tg0 + 4], in_=bk[:].rearrange("p (t c) -> p c t", c=128)),
                 reads=[bk], parts=[Wg])

    def expert_loop(n0, g):
        W = g * 128
        for cb in range(128):
            sl = blk_ctr[0] % NS
            blk_ctr[0] += 1
            P.load(esl[sl], esl[sl][:], ex_s[cb], dram_reads=[downT_t[cb], up_t[cb // 8]])
            sb_ = bank[6 + cb % 2]
            for k in range(8):
                P.op("pe", lambda e, k=k, sl=sl, sb_=sb_: e.matmul(out=sb_[:, 0:W], lhsT=esl[sl][:, k * 128:(k + 1) * 128], rhs=xnT[:, k, 0:W], start=(k == 0), stop=(k == 7)),
                     reads=[esl[sl], xnT], writes=[sb_] if k == 0 else [], parts=[] if k == 0 else [sb_])
            ab = actb[cb % 2]
            wb = wab[cb % 2]
            P.op("act", lambda e, ab=ab, sb_=sb_: e.activation(out=ab[:, 0:W], in_=sb_[:, 0:W], func=AF.Gelu), reads=[sb_], writes=[ab])
            P.op("dve", lambda e, ab=ab, wb=wb, cb=cb: e.tensor_tensor(out=wb[:, 0:W], in0=ab[:, 0:W], in1=Wg[:, cb, 0:W], op=ALU.mult), reads=[ab, Wg], writes=[wb])
            for tt in range(g):
                for hf in range(2):
                    bk = bank[tt * 2 + hf]
                    P.op("pe", lambda e, tt=tt, hf=hf, bk=bk, wb=wb, sl=sl, cb=cb: e.matmul(out=bk[:], lhsT=wb[:, tt * 128:(tt + 1) * 128], rhs=esl[sl][:, D + hf * 512:D + (hf + 1) * 512],
                                                                                      start=(cb == 0), stop=(cb == 127)),
                         reads=[wb, esl[sl]], writes=[bk] if cb == 0 else [], parts=[] if cb == 0 else [bk])
        for tt in range(g):
            n = n0 + tt
            x_t = x1s[tt]
            for hf in range(2):
                bk = bank[tt * 2 + hf]
                P.op("dve", lambda e, bk=bk, hf=hf, x_t=x_t: e.tensor_tensor(out=x_t[:, hf * 512:(hf + 1) * 512], in0=bk[:], in1=x_t[:, hf * 512:(hf + 1) * 512], op=ALU.add),
                     reads=[bk, x_t], writes=[x_t])
            P.store(c.out[n * 128:(n + 1) * 128, :], x_t, x_t[:], dram_writes=[c.x1_t[n]], final=True)

    for (n0, g) in groups:
        for ti in range(g):
            route_tile(n0 + ti, ti)
        if 3 <= cstage <= 40:
            return
        if cstage == 50:
            continue
        expert_loop(n0, g)


from concourse.bass_utils import run_bass_kernel_spmd

W_NAMES = ["norm1_gain", "w_in", "ml_i_bias", "ml_f_bias", "q_norm_gain", "k_norm_gain", "attn_sinks", "ml_out_norm_gain",
           "w_branch_attn", "w_branch_mlstm", "w_out", "norm2_gain", "peer_w_query", "peer_sub_keys", "peer_down", "peer_up"]
PEER_NAMES = ["norm2_gain", "peer_w_query", "peer_sub_keys", "peer_down", "peer_up"]


def make_in_maps(inputs, NB, NT, ncores, phases="ABC"):
    consts = host_consts()
    S = NT * 128
    maps = []
    shared = {}
    for k in W_NAMES:
        if "C" not in phases and k in PEER_NAMES:
            continue
        shared[k] = np.ascontiguousarray(inputs[k][0])
    for k, v in consts.items():
        shared["c_" + k] = v
    for ci in range(ncores):
        m = dict(shared)
        m["x"] = np.ascontiguousarray(inputs["x"][ci * NB:(ci + 1) * NB, :S]).reshape(NB * S, D)
        m["positions"] = np.ascontiguousarray(inputs["positions"][ci * NB:(ci + 1) * NB, :S]).reshape(NB * S).astype(np.int32)
        maps.append(m)
    return maps


def kernel(**inputs):
    NB, NT, ncores = 2, 32, 8
    nc, _ = build_program(NB, NT, "ABC")
    maps = make_in_maps(inputs, NB, NT, ncores, "ABC")
    res = run_bass_kernel_spmd(nc, maps, core_ids=list(range(ncores)))
    outs = [r["out"].reshape(NB, NT * 128, D) for r in res.results]
    return np.concatenate(outs, axis=0).astype(np.float32)
```

```python
import numpy as np
import concourse.bass as bass
import concourse.mybir as mybir
from contextlib import ExitStack

F32 = mybir.dt.float32
BF16 = mybir.dt.bfloat16
I32 = mybir.dt.int32
U32 = mybir.dt.uint32
ALU = mybir.AluOpType
AF = mybir.ActivationFunctionType
AX = mybir.AxisListType

ENGS = ("pe", "act", "dve", "pool", "sp")


class T:
    __slots__ = ("name", "ap", "writers", "readers", "dsem", "dcount", "last_dma_read")

    def __init__(self, name, ap=None):
        self.name = name
        self.ap = ap
        self.writers = []
        self.readers = []
        self.dsem = None
        self.dcount = 0
        self.last_dma_read = None

    def __getitem__(self, k):
        return self.ap[k]


class Op:
    __slots__ = ("eng", "fn", "seq", "deps", "dma", "dsem", "dval", "signal", "sigval")

    def __init__(self, eng, fn):
        self.eng = eng
        self.fn = fn
        self.seq = None
        self.deps = []
        self.dma = False
        self.dsem = None
        self.dval = 0
        self.signal = False
        self.sigval = 0


class Prog:
    def __init__(self, nc):
        self.nc = nc
        self.stack = ExitStack()
        self.ops = []
        self.per_eng = {e: [] for e in ENGS}
        self.nsem = 0
        self.esem = {}
        for e in ENGS:
            if e != "sp":
                self.esem[e] = self.sem("s_" + e)
        self.stores = []
        self.scopes = []
        self.last_dma = {}

    def open_scope(self):
        self.scopes.append(ExitStack())

    def close_scope(self):
        self.barrier()
        self.scopes.pop().close()

    def barrier(self):
        last_c = []
        for e in ENGS:
            for o in reversed(self.per_eng[e]):
                if not o.dma and o.fn is not None:
                    last_c.append(o)
                    break
        deps = last_c + list(self.last_dma.values())
        for e in ENGS:
            op = Op(e, None)
            op.seq = len(self.per_eng[e])
            op.deps = list(deps)
            self.ops.append(op)
            self.per_eng[e].append(op)

    def sem(self, name):
        self.nsem += 1
        return self.stack.enter_context(self.nc.semaphore(name))

    def sb(self, name, shape, dt):
        stk = self.scopes[-1] if self.scopes else self.stack
        t = stk.enter_context(self.nc.sbuf_tensor(name, list(shape), dt))
        return T(name, t)

    def ps(self, name, shape, dt):
        t = self.stack.enter_context(self.nc.psum_tensor(name, list(shape), dt))
        return T(name, t)

    def dram(self, name, shape, dt, kind="Internal"):
        return self.nc.dram_tensor(name, list(shape), dt, kind=kind).ap()

    def _rec(self, eng, fn, reads, writes, parts=(), dma_tile=None, dma_is_read=False):
        op = Op(eng, fn)
        op.seq = len(self.per_eng[eng])
        deps = []
        for t in reads:
            deps.extend(t.writers)
        for t in writes:
            if t.readers:
                deps.extend(t.readers)
                deps.extend(t.writers)
                t.writers = [op]
                t.readers = []
            else:
                deps.extend(t.writers)
                t.writers = [op]
        for t in parts:
            if t.readers:
                deps.extend(t.readers)
                deps.extend(t.writers)
                t.writers = [op]
                t.readers = []
            else:
                t.writers = t.writers + [op]
        for t in reads:
            t.readers.append(op)
        if dma_tile is not None:
            op.dma = True
            if dma_tile.dsem is None:
                dma_tile.dsem = self.sem("d_" + dma_tile.name)
            if dma_is_read and dma_tile.last_dma_read is not None:
                deps.append(dma_tile.last_dma_read)
            dma_tile.dcount += 1
            op.dsem = dma_tile.dsem
            op.dval = 16 * dma_tile.dcount
            dma_tile.last_dma_read = op if dma_is_read else None
            self.last_dma[id(op.dsem)] = op
        op.deps = [d for d in deps if d is not op]
        self.ops.append(op)
        self.per_eng[eng].append(op)
        return op

    def op(self, eng, fn, reads=(), writes=(), parts=()):
        return self._rec(eng, fn, reads, writes, parts)

    def dma(self, out, in_, reads=(), writes=(), parts=(), sem_tile=None, is_read=False, eng="sp", **kw):
        def fn(e):
            return e.dma_start(out=out, in_=in_, **kw)
        return self._rec(eng, fn, reads, writes, parts, dma_tile=sem_tile, dma_is_read=is_read)

    def load(self, dst_tile, dst_ap, src_ap, part=False, dram_reads=(), eng="sp", **kw):
        if part:
            return self.dma(dst_ap, src_ap, reads=dram_reads, parts=(dst_tile,), sem_tile=dst_tile, eng=eng, **kw)
        return self.dma(dst_ap, src_ap, reads=dram_reads, writes=(dst_tile,), sem_tile=dst_tile, eng=eng, **kw)

    def store(self, dst_ap, src_tile, src_ap, dram_writes=(), dram_parts=(), final=False, eng="sp", **kw):
        o = self.dma(dst_ap, src_ap, reads=(src_tile,), writes=dram_writes, parts=dram_parts,
                     sem_tile=src_tile, is_read=True, eng=eng, **kw)
        if final:
            self.stores.append(o)
        return o

    def emit(self):
        nc = self.nc
        fin = Op("sp", None)
        fin.seq = len(self.per_eng["sp"])
        fin.deps = list(self.stores)
        self.ops.append(fin)
        self.per_eng["sp"].append(fin)

        clock = {e: {f: -1 for f in ENGS} for e in ENGS}
        dclock = {e: {} for e in ENGS}
        waits = {}
        for op in self.ops:
            X = op.eng
            need_e = {}
            need_d = {}
            for d in op.deps:
                if d.dma:
                    key = id(d.dsem)
                    if dclock[X].get(key, 0) >= d.dval:
                        continue
                    cur = need_d.get(key)
                    if cur is None or cur[1] < d.dval:
                        need_d[key] = (d.dsem, d.dval)
                else:
                    Y = d.eng
                    if Y == X and X == "pe":
                        continue
                    if clock[X][Y] >= d.seq:
                        continue
                    if need_e.get(Y, -1) < d.seq:
                        need_e[Y] = d.seq
            wl = []
            for Y, s in need_e.items():
                clock[X][Y] = s
                tgt = self.per_eng[Y][s]
                tgt.signal = True
                wl.append(("e", Y, tgt))
            for key, (sem, val) in need_d.items():
                dclock[X][key] = val
                wl.append(("d", sem, val))
            waits[id(op)] = wl
        for e in ENGS:
            c = 0
            for op in self.per_eng[e]:
                if op.signal and not op.dma:
                    c += 1
                    op.sigval = c
        self.sigmax = {e: max([o.sigval for o in self.per_eng[e]] + [0]) for e in ENGS}
        engobj = {"pe": "tensor", "act": "scalar", "dve": "vector", "pool": "gpsimd", "sp": "sync"}
        with nc.Block() as block:
            for e in ENGS:
                ops_e = self.per_eng[e]
                if not ops_e:
                    continue

                def body(eng, ops_e=ops_e, e=e):
                    for op in ops_e:
                        for w in waits[id(op)]:
                            if w[0] == "e":
                                eng.wait_ge(self.esem[w[1]], w[2].sigval)
                            else:
                                eng.wait_ge(w[1], w[2])
                        if op.fn is None:
                            continue
                        ins = op.fn(eng)
                        if op.dma:
                            ins.then_inc(op.dsem, 16)
                        elif op.signal:
                            ins.then_inc(self.esem[e], 1)

                getattr(block, engobj[e])(body)
        self.stack.close()


D = 1024
IN_W = 6416
EPS = 1e-6
NEG = -30000.0
TWO_PI = 6.283185


def host_consts():
    c = {}
    c["identf"] = np.eye(128, dtype=np.float32)
    k = np.arange(128)[:, None]
    q = np.arange(128)[None, :]
    m_prev = np.where(k > q, 0.0, NEG).astype(np.float32)
    m_cur = np.where(k <= q, 0.0, NEG).astype(np.float32)
    c["amask"] = np.stack([np.tile(m_prev, (1, 4)), np.tile(m_cur, (1, 4))], axis=1).astype(np.float32)
    c["cmask"] = np.tile((k <= q).astype(np.float32), (1, 4))
    invf = (500000.0 ** (-np.arange(0, 16, 2, dtype=np.float32) / 16.0)).astype(np.float32)
    c["invf"] = np.tile((invf / (2 * np.pi)).astype(np.float32)[None, :], (128, 1))
    onesab = np.zeros((128, 2, 128), np.float32)
    onesab[:, 0, 0:64] = 1.0
    onesab[:, 1, 64:128] = 1.0
    c["onesab"] = onesab
    c["iota128"] = np.tile(np.arange(128, dtype=np.float32)[None, :], (128, 1))
    return c


CONST_SHAPES = {"identf": [128, 128], "amask": [128, 2, 512], "cmask": [128, 512], "invf": [128, 8],
                "onesab": [128, 2, 128], "iota128": [128, 128]}


class Ctx:
    pass


def build_program(NB, NT, phases="ABC", dbg=False):
    NTT = NB * NT
    TOK = NTT * 128
    nc = bass.Bass("TRN2", target_bir_lowering=False)
    P = Prog(nc)
    c = Ctx()
    c.nc, c.P, c.NB, c.NT, c.NTT, c.TOK = nc, P, NB, NT, NTT, TOK
    c.phases = phases

    def din(name, shape, dt=F32):
        return nc.dram_tensor(name, list(shape), dt, kind="ExternalInput").ap()

    c.x = din("x", [TOK, D])
    c.pos = din("positions", [TOK], I32)
    c.norm1_gain = din("norm1_gain", [1, D])
    c.w_in = din("w_in", [D, IN_W])
    c.ml_i_bias = din("ml_i_bias", [1, 8])
    c.ml_f_bias = din("ml_f_bias", [1, 8])
    c.q_norm_gain = din("q_norm_gain", [1, 64])
    c.k_norm_gain = din("k_norm_gain", [1, 64])
    c.attn_sinks = din("attn_sinks", [1, 16])
    c.ml_out_norm_gain = din("ml_out_norm_gain", [1, D])
    c.w_branch_attn = din("w_branch_attn", [D, D])
    c.w_branch_mlstm = din("w_branch_mlstm", [D, D])
    c.w_out = din("w_out", [D, D])
    if "C" in phases:
        c.norm2_gain = din("norm2_gain", [1, D])
        c.peer_w_query = din("peer_w_query", [D, D])
        c.peer_sub_keys = din("peer_sub_keys", [8, 2, 128, 64])
        c.peer_down = din("peer_down", [16384, D])
        c.peer_up = din("peer_up", [16384, D])
    c.cst = {k: din("c_" + k, s) for k, s in CONST_SHAPES.items()}
    c.out = nc.dram_tensor("out", [TOK, D], F32, kind="ExternalOutput").ap()
    if dbg:
        c.mixa = nc.dram_tensor("mixa_s", [TOK, D], F32, kind="ExternalOutput").ap()
    else:
        c.mixa = P.dram("mixa_s", [TOK, D], F32)
    c.dbg = dbg
    c.dbg_outs = {}
    c.mixa_t = [T("mixa%d" % i) for i in range(NTT)]
    c.x1_t = [T("x1_%d" % i) for i in range(NTT)]

    c.identf = P.sb("identf", [128, 128], F32)
    c.identb = P.sb("identb", [128, 128], BF16)
    P.load(c.identf, c.identf[:], c.cst["identf"])
    P.op("dve", lambda e: e.tensor_copy(out=c.identb[:], in_=c.identf[:]), reads=[c.identf], writes=[c.identb])
    c.bank = [P.ps("bank%d" % i, [128, 512], F32) for i in range(8)]

    for ph, fn in (("A", phase_a), ("B", phase_b), ("C", phase_c)):
        if ph in phases:
            P.open_scope()
            fn(c)
            P.close_scope()
    P.emit()
    return nc, P


def rms_rstd(c, pfx, ssq, n, rstd, tmp):
    P = c.P
    P.op("dve", lambda e: e.tensor_scalar(out=tmp[:], in0=ssq[:], scalar1=1.0 / n, scalar2=EPS, op0=ALU.mult, op1=ALU.add),
         reads=[ssq], writes=[tmp])
    P.op("act", lambda e: e.activation(out=tmp[:], in_=tmp[:], func=AF.Sqrt), reads=[tmp], writes=[tmp])
    P.op("dve", lambda e: e.reciprocal(out=rstd[:], in_=tmp[:]), reads=[tmp], writes=[rstd])


def norm_and_transpose(c, xs, gain, hb, hT, junk, st, ptr_bank):
    P = c.P
    ssq, tmp, rstd = st
    P.op("act", lambda e: e.activation(out=junk[:], in_=xs[:], func=AF.Square, accum_out=ssq[:]),
         reads=[xs], writes=[junk, ssq])
    rms_rstd(c, "n", ssq, D, rstd, tmp)
    P.op("dve", lambda e: e.scalar_tensor_tensor(out=hb[:], in0=xs[:], scalar=rstd[:], in1=gain[:], op0=ALU.mult, op1=ALU.mult),
         reads=[xs, rstd, gain], writes=[hb])
    transpose8(c, hb, hT, ptr_bank)


def transpose8(c, src, dst, ptr_bank, eng="act"):
    P = c.P
    pv = ptr_bank[:].bitcast(BF16)
    for k in range(8):
        P.op("pe", lambda e, k=k: e.transpose(out=pv[:, k * 128:(k + 1) * 128], in_=src[:, k * 128:(k + 1) * 128], identity=c.identb[:]),
             reads=[src, c.identb], writes=[ptr_bank] if k == 0 else [], parts=[] if k == 0 else [ptr_bank])
    if eng == "act":
        P.op("act", lambda e: e.copy(out=dst[:].rearrange("p k t -> p (k t)"), in_=pv), reads=[ptr_bank], writes=[dst])
    else:
        P.op(eng, lambda e: e.tensor_copy(out=dst[:].rearrange("p k t -> p (k t)"), in_=pv), reads=[ptr_bank], writes=[dst])


def load_w_bf16(c, dst, col0, ncols, src, dcol0=0):
    P = c.P
    for k in range(8):
        P.load(dst, dst[:, k, dcol0:dcol0 + ncols], src[k * 128:(k + 1) * 128, col0:col0 + ncols], part=True, eng="pool",
               max_dma_last_dim=4096)


def rope_tables(c):
    P = c.P
    NTT = c.NTT
    posi = P.sb("posi", [128, NTT], I32)
    P.load(posi, posi[:], c.pos.rearrange("(n p) -> p n", p=128), allow_slow_non_contiguous=True)
    posf = P.sb("posf", [128, NTT], F32)
    P.op("dve", lambda e: e.tensor_copy(out=posf[:], in_=posi[:]), reads=[posi], writes=[posf])
    invf = P.sb("invf", [128, 8], F32)
    P.load(invf, invf[:], c.cst["invf"])
    y = P.sb("rope_y", [128, NTT, 16], F32)
    yi = P.sb("rope_yi", [128, NTT, 16], I32)
    yf = P.sb("rope_yf", [128, NTT, 16], F32)
    cs = P.sb("rope_cs", [128, NTT, 16], F32)
    pb = posf[:].unsqueeze(2).to_broadcast([128, NTT, 8])
    ib = invf[:].unsqueeze(1).to_broadcast([128, NTT, 8])
    P.op("dve", lambda e: e.tensor_tensor(out=y[:, :, 8:16], in0=pb, in1=ib, op=ALU.mult), reads=[posf, invf], writes=[y])
    P.op("dve", lambda e: e.tensor_scalar(out=y[:, :, 0:8], in0=y[:, :, 8:16], scalar1=0.25, scalar2=None, op0=ALU.add),
         reads=[y], writes=[y])
    P.op("dve", lambda e: e.tensor_copy(out=yi[:], in_=y[:]), reads=[y], writes=[yi])
    P.op("dve", lambda e: e.tensor_copy(out=yf[:], in_=yi[:]), reads=[yi], writes=[yf])
    P.op("dve", lambda e: e.tensor_tensor(out=y[:], in0=y[:], in1=yf[:], op=ALU.subtract), reads=[y, yf], writes=[y])
    P.op("act", lambda e: e.activation(out=cs[:], in_=y[:], func=AF.Sin, scale=TWO_PI), reads=[y], writes=[cs])
    return cs


def qk_norm_rope(c, pfx, src, nh, gain, cs_n, outb, tmps):
    P = c.P
    sq, ssq, tmp, rstd, qn, r1, r2 = tmps
    W = nh * 64
    s3 = src[:, 0:W].rearrange("p (h d) -> p h d", d=64)
    P.op("pool", lambda e: e.tensor_tensor(out=sq[:, 0:W], in0=src[:, 0:W], in1=src[:, 0:W], op=ALU.mult), reads=[src], writes=[sq])
    P.op("dve", lambda e: e.tensor_reduce(out=ssq[:, 0:nh], in_=sq[:, 0:W].rearrange("p (h d) -> p h d", d=64), axis=AX.X, op=ALU.add),
         reads=[sq], writes=[ssq])
    P.op("dve", lambda e: e.tensor_scalar(out=tmp[:, 0:nh], in0=ssq[:, 0:nh], scalar1=1.0 / 64, scalar2=EPS, op0=ALU.mult, op1=ALU.add),
         reads=[ssq], writes=[tmp])
    P.op("act", lambda e: e.activation(out=tmp[:, 0:nh], in_=tmp[:, 0:nh], func=AF.Sqrt), reads=[tmp], writes=[tmp])
    P.op("dve", lambda e: e.reciprocal(out=rstd[:, 0:nh], in_=tmp[:, 0:nh]), reads=[tmp], writes=[rstd])
    q3 = qn[:, 0:W].rearrange("p (h d) -> p h d", d=64)
    P.op("dve", lambda e: e.tensor_tensor(out=q3, in0=s3, in1=rstd[:, 0:nh].unsqueeze(2).to_broadcast([128, nh, 64]), op=ALU.mult),
         reads=[src, rstd], writes=[qn])
    P.op("pool", lambda e: e.tensor_tensor(out=q3, in0=q3, in1=gain[:].unsqueeze(1).to_broadcast([128, nh, 64]), op=ALU.mult),
         reads=[qn, gain], writes=[qn])
    P.op("act", lambda e: e.copy(out=outb[:], in_=q3), reads=[qn], writes=[outb])
    cosb = cs_n[:, 0:8].unsqueeze(1).to_broadcast([128, nh, 8])
    sinb = cs_n[:, 8:16].unsqueeze(1).to_broadcast([128, nh, 8])
    a3 = r1[:, 0:nh * 8].rearrange("p (h d) -> p h d", d=8)
    b3 = r2[:, 0:nh * 8].rearrange("p (h d) -> p h d", d=8)
    cst = c.cs
    P.op("dve", lambda e: e.tensor_tensor(out=a3, in0=q3[:, :, 0:8], in1=cosb, op=ALU.mult), reads=[qn, cst], writes=[r1])
    P.op("dve", lambda e: e.tensor_tensor(out=b3, in0=q3[:, :, 8:16], in1=sinb, op=ALU.mult), reads=[qn, cst], writes=[r2])
    P.op("dve", lambda e: e.tensor_tensor(out=outb[:, :, 0:8], in0=a3, in1=b3, op=ALU.subtract), reads=[r1, r2, outb], writes=[outb])
    P.op("dve", lambda e: e.tensor_tensor(out=a3, in0=q3[:, :, 8:16], in1=cosb, op=ALU.mult), reads=[qn, cst], writes=[r1])
    P.op("dve", lambda e: e.tensor_tensor(out=b3, in0=q3[:, :, 0:8], in1=sinb, op=ALU.mult), reads=[qn, cst], writes=[r2])
    P.op("dve", lambda e: e.tensor_tensor(out=outb[:, :, 8:16], in0=a3, in1=b3, op=ALU.add), reads=[r1, r2, outb], writes=[outb])


def phase_a(c):
    P, NB, NT, NTT = c.P, c.NB, c.NT, c.NTT
    bank = c.bank
    c.g1 = P.sb("a_g1", [128, D], F32)
    P.load(c.g1, c.g1[:], c.norm1_gain[0].partition_broadcast(128))
    wA = P.sb("wA", [128, 8, 2304], BF16)
    load_w_bf16(c, wA, 0, 1280, c.w_in, 0)
    load_w_bf16(c, wA, 4368, 1024, c.w_in, 1280)
    wba = P.sb("wba", [128, 8, D], BF16)
    load_w_bf16(c, wba, 0, D, c.w_branch_attn)
    gq = P.sb("gq", [128, 64], F32)
    gk = P.sb("gk", [128, 64], F32)
    P.load(gq, gq[:], c.q_norm_gain[0].partition_broadcast(128))
    P.load(gk, gk[:], c.k_norm_gain[0].partition_broadcast(128))
    sk = P.sb("sk", [128, 16], F32)
    P.load(sk, sk[:], c.attn_sinks[0].partition_broadcast(128))
    sinkp = P.sb("sinkp", [128, 8], F32)
    sk3 = sk[:].rearrange("p (i two) -> p i two", two=2)
    P.op("dve", lambda e: e.tensor_copy(out=sinkp[0:64, :], in_=sk3[0:64, :, 0]), reads=[sk], writes=[sinkp])
    P.op("dve", lambda e: e.tensor_copy(out=sinkp[64:128, :], in_=sk3[64:128, :, 1]), reads=[sk], parts=[sinkp])
    P.op("act", lambda e: e.activation(out=sinkp[:], in_=sinkp[:], func=AF.Exp), reads=[sinkp], writes=[sinkp])
    amaskf = P.sb("amaskf", [128, 2, 512], F32)
    P.load(amaskf, amaskf[:], c.cst["amask"])
    amask = P.sb("amask", [128, 2, 512], BF16)
    P.op("dve", lambda e: e.tensor_copy(out=amask[:], in_=amaskf[:]), reads=[amaskf], writes=[amask])
    onesf = P.sb("onesf", [128, 2, 128], F32)
    P.load(onesf, onesf[:], c.cst["onesab"])
    onesab = P.sb("onesab", [128, 2, 128], BF16)
    P.op("dve", lambda e: e.tensor_copy(out=onesab[:], in_=onesf[:]), reads=[onesf], writes=[onesab])
    c.cs = rope_tables(c)

    xs = [P.sb("a_xs%d" % i, [128, D], F32) for i in range(2)]
    junk = P.sb("a_junk", [128, D], BF16)
    st = (P.sb("a_ssq", [128, 1], F32), P.sb("a_tmp", [128, 1], F32), P.sb("a_rstd", [128, 1], F32))
    hb = P.sb("a_hb", [128, D], BF16)
    hT = P.sb("a_hT", [128, 8, 128], BF16)
    qf = P.sb("a_qf", [128, D], F32)
    kvf = P.sb("a_kvf", [128, 256], F32)
    sga = P.sb("a_sga", [128, D], F32)
    tmps = (P.sb("a_sq", [128, D], F32), P.sb("a_ssq16", [128, 16], F32), P.sb("a_tmp16", [128, 16], F32),
            P.sb("a_rstd16", [128, 16], F32), P.sb("a_qn", [128, D], F32), P.sb("a_r1", [128, 128], F32), P.sb("a_r2", [128, 128], F32))
    qb = P.sb("a_qb", [128, 16, 64], BF16)
    kb = P.sb("a_kb", [128, 2, 64], BF16)
    kdup = P.sb("a_kdup", [128, 2, 2, 64], BF16)
    qT = P.sb("a_qT", [128, 8, 128], BF16)
    kT = [P.sb("a_kT%d" % i, [128, 2, 128], BF16) for i in range(2)]
    vA = [P.sb("a_vA%d" % i, [128, 2, 128], BF16) for i in range(2)]
    vB = [P.sb("a_vB%d" % i, [128, 2, 128], BF16) for i in range(2)]
    for i in range(2):
        P.op("pool", lambda e, i=i: e.memset(vA[i][:], 0.0), writes=[vA[i]])
        P.op("pool", lambda e, i=i: e.memset(vB[i][:], 0.0), writes=[vB[i]])
    PT = [[[P.sb("a_PT%d%d%d" % (g, k, h), [128, 4, 128], BF16) for h in range(2)] for k in range(2)] for g in range(2)]
    rden = P.sb("a_rden", [128, 4, 128], F32)
    attT = P.sb("a_attT", [128, 8, 128], BF16)
    mo = [P.sb("a_mo%d" % i, [128, D], F32) for i in range(2)]

    def load_x(n):
        P.load(xs[n % 2], xs[n % 2][:], c.x[n * 128:(n + 1) * 128, :])

    load_x(0)

    def tile(n):
        j = n % NT
        cur = n % 2
        prv = 1 - cur
        if n + 1 < NTT:
            load_x(n + 1)
        x_t = xs[cur]
        norm_and_transpose(c, x_t, c.g1, hb, hT, junk, st, bank[7])
        def proj(bk, col0, ncols):
            for k in range(8):
                P.op("pe", lambda e, k=k: e.matmul(out=bk[:, 0:ncols], lhsT=hT[:, k, :], rhs=wA[:, k, col0:col0 + ncols],
                                                   start=(k == 0), stop=(k == 7)),
                     reads=[hT, wA], writes=[bk] if k == 0 else [], parts=[] if k == 0 else [bk])
        proj(bank[0], 0, 512)
        P.op("dve", lambda e: e.tensor_copy(out=qf[:, 0:512], in_=bank[0][:]), reads=[bank[0]], writes=[qf])
        proj(bank[1], 512, 512)
        P.op("act", lambda e: e.copy(out=qf[:, 512:1024], in_=bank[1][:]), reads=[bank[1]], parts=[qf])
        proj(bank[2], 1024, 256)
        P.op("dve", lambda e: e.tensor_copy(out=kvf[:], in_=bank[2][:, 0:256]), reads=[bank[2]], writes=[kvf])
        proj(bank[3], 1280, 512)
        P.op("act", lambda e: e.activation(out=sga[:, 0:512], in_=bank[3][:], func=AF.Sigmoid), reads=[bank[3]], writes=[sga])
        proj(bank[4], 1792, 512)
        P.op("act", lambda e: e.activation(out=sga[:, 512:1024], in_=bank[4][:], func=AF.Sigmoid), reads=[bank[4]], parts=[sga])
        cs_n = c.cs[:, n, :]
        qk_norm_rope(c, "q", qf, 16, gq, cs_n, qb, tmps)
        qk_norm_rope(c, "k", kvf, 2, gk, cs_n, kb, tmps)
        P.op("pool", lambda e: e.tensor_copy(out=kdup[:, :, 0, :], in_=kb[:]), reads=[kb], writes=[kdup])
        P.op("pool", lambda e: e.tensor_copy(out=kdup[:, :, 1, :], in_=kb[:]), reads=[kb], parts=[kdup])
        v3 = kvf[:, 128:256].rearrange("p (g d) -> p g d", d=64)
        P.op("pool", lambda e: e.tensor_copy(out=vA[cur][:, :, 0:64], in_=v3), reads=[kvf], writes=[vA[cur]])
        P.op("pool", lambda e: e.tensor_copy(out=vB[cur][:, :, 64:128], in_=v3), reads=[kvf], writes=[vB[cur]])
        pv = bank[7][:].bitcast(BF16)
        qflat = qb[:].rearrange("p h d -> p (h d)")
        for k in range(8):
            P.op("pe", lambda e, k=k: e.transpose(out=pv[:, k * 128:(k + 1) * 128], in_=qflat[:, k * 128:(k + 1) * 128], identity=c.identb[:]),
                 reads=[qb, c.identb], writes=[bank[7]] if k == 0 else [], parts=[] if k == 0 else [bank[7]])
        P.op("act", lambda e: e.copy(out=qT[:].rearrange("p k t -> p (k t)"), in_=pv), reads=[bank[7]], writes=[qT])
        pv6 = bank[6][:].bitcast(BF16)
        kflat = kdup[:].rearrange("p g u d -> p (g u d)")
        for g in range(2):
            P.op("pe", lambda e, g=g: e.transpose(out=pv6[:, g * 128:(g + 1) * 128], in_=kflat[:, g * 128:(g + 1) * 128], identity=c.identb[:]),
                 reads=[kdup, c.identb], writes=[bank[6]] if g == 0 else [], parts=[] if g == 0 else [bank[6]])
        P.op("dve", lambda e: e.tensor_copy(out=kT[cur][:].rearrange("p g t -> p (g t)"), in_=pv6[:, 0:256]), reads=[bank[6]], writes=[kT[cur]])
        kbs = [1] if j == 0 else [0, 1]
        bi = 0
        for g in range(2):
            for kk in kbs:
                slot = cur if kk == 1 else prv
                for hh in range(2):
                    bk = bank[bi % 6]
                    bi += 1
                    P.op("pe", lambda e, bk=bk, slot=slot, g=g, hh=hh: e.matmul(
                        out=bk[:], lhsT=kT[slot][hh * 64:(hh + 1) * 64, g, :],
                        rhs=qT[hh * 64:(hh + 1) * 64, 4 * g:4 * g + 4, :], start=True, stop=False),
                        reads=[kT[slot], qT], writes=[bk])
                    P.op("pe", lambda e, bk=bk, kk=kk: e.matmul(out=bk[:], lhsT=c.identb[:], rhs=amask[:, kk, :], start=False, stop=True),
                         reads=[c.identb, amask], parts=[bk])
                    pt = PT[g][kk][hh]
                    P.op("act", lambda e, bk=bk, pt=pt: e.activation(out=pt[:].rearrange("p i t -> p (i t)"), in_=bk[:], func=AF.Exp, scale=0.125),
                         reads=[bk], writes=[pt])
        for g in range(2):
            pav = bank[6]
            pden = bank[7]
            first_av = True
            for p in range(4):
                combos = [(kk, hh) for kk in kbs for hh in range(2)]
                for ci, (kk, hh) in enumerate(combos):
                    slot = cur if kk == 1 else prv
                    vt = vA[slot] if hh == 0 else vB[slot]
                    pt = PT[g][kk][hh]
                    w_first = first_av
                    P.op("pe", lambda e, vt=vt, pt=pt, p=p, g=g, ci=ci, ncmb=len(combos): e.matmul(
                        out=pav[:, p * 128:(p + 1) * 128], lhsT=vt[:, g, :], rhs=pt[:, p, :], start=(ci == 0), stop=(ci == ncmb - 1)),
                        reads=[vt, pt], writes=[pav] if w_first else [], parts=[] if w_first else [pav])
                    P.op("pe", lambda e, pt=pt, p=p, hh=hh, ci=ci, ncmb=len(combos): e.matmul(
                        out=pden[:, p * 128:(p + 1) * 128], lhsT=onesab[:, hh, :], rhs=pt[:, p, :], start=(ci == 0), stop=(ci == ncmb - 1)),
                        reads=[onesab, pt], writes=[pden] if w_first else [], parts=[] if w_first else [pden])
                    first_av = False
            P.op("dve", lambda e, g=g: e.tensor_tensor(out=rden[:], in0=pden[:].rearrange("p (i t) -> p i t", t=128),
                                                       in1=sinkp[:, 4 * g:4 * g + 4].unsqueeze(2).to_broadcast([128, 4, 128]), op=ALU.add),
                 reads=[pden, sinkp], writes=[rden])
            P.op("dve", lambda e: e.reciprocal(out=rden[:], in_=rden[:]), reads=[rden], writes=[rden])
            P.op("dve", lambda e, g=g: e.tensor_tensor(out=attT[:, 4 * g:4 * g + 4, :], in0=pav[:].rearrange("p (i t) -> p i t", t=128),
                                                       in1=rden[:], op=ALU.mult),
                 reads=[pav, rden], writes=[attT] if g == 0 else [], parts=[] if g == 0 else [attT])
        m_t = mo[n % 2]
        for hn in range(2):
            bk = bank[hn]
            for i in range(8):
                P.op("pe", lambda e, bk=bk, i=i, hn=hn: e.matmul(out=bk[:], lhsT=attT[:, i, :], rhs=wba[:, i, hn * 512:(hn + 1) * 512],
                                                               start=(i == 0), stop=(i == 7)),
                     reads=[attT, wba], writes=[bk] if i == 0 else [], parts=[] if i == 0 else [bk])
            P.op("dve", lambda e, bk=bk, hn=hn: e.tensor_tensor(out=m_t[:, hn * 512:(hn + 1) * 512], in0=bk[:], in1=sga[:, hn * 512:(hn + 1) * 512], op=ALU.mult),
                 reads=[bk, sga], writes=[m_t] if hn == 0 else [], parts=[] if hn == 0 else [m_t])
        P.store(c.mixa[n * 128:(n + 1) * 128, :], m_t, m_t[:], dram_writes=[c.mixa_t[n]], final=c.dbg)
        if c.dbg and n == c.dbg - 1:
            for nm, tl, shp, dt in (("qb", qb, [128, 1024], BF16), ("kb", kb, [128, 128], BF16), ("attT", attT, [128, 1024], BF16),
                                    ("qf", qf, [128, 1024], F32), ("sga", sga, [128, 1024], F32), ("hT", hT, [128, 1024], BF16),
                                    ("PT", PT[0][1][0], [128, 512], BF16), ("rden", rden, [128, 512], F32), ("qT", qT, [128, 1024], BF16),
                                    ("kT", kT[cur], [128, 256], BF16), ("kdup", kdup, [128, 256], BF16), ("vA", vA[cur], [128, 256], BF16)):
                d_ap = c.nc.dram_tensor("dbg_" + nm, shp, dt, kind="ExternalOutput").ap()
                flat = tl[:]
                if len(flat.shape) == 3:
                    flat = flat.rearrange("p a b -> p (a b)")
                if len(flat.shape) == 4:
                    flat = flat.rearrange("p a b c -> p (a b c)")
                P.store(d_ap, tl, flat, final=True)

    for n in range(NTT):
        tile(n)


def phase_b(c):
    P, NB, NT, NTT = c.P, c.NB, c.NT, c.NTT
    bank = c.bank
    c.g1 = P.sb("b_g1", [128, D], F32)
    P.load(c.g1, c.g1[:], c.norm1_gain[0].partition_broadcast(128))
    wB = P.sb("wB", [128, 8, 4112], BF16)
    load_w_bf16(c, wB, 1280, 3088, c.w_in, 0)
    load_w_bf16(c, wB, 5392, 1024, c.w_in, 3088)
    wbm = P.sb("wbm", [128, 8, D], BF16)
    load_w_bf16(c, wbm, 0, D, c.w_branch_mlstm)
    wout = P.sb("wout", [128, 8, D], BF16)
    load_w_bf16(c, wout, 0, D, c.w_out)
    mlg = P.sb("mlg", [128, D], F32)
    P.load(mlg, mlg[:], c.ml_out_norm_gain[0].partition_broadcast(128))
    cmask = P.sb("cmask", [128, 512], F32)
    P.load(cmask, cmask[:], c.cst["cmask"])
    bif = P.sb("b_bif", [8, 2], F32)
    P.load(bif, bif[:, 0:1], c.ml_i_bias.rearrange("o h -> h o"), allow_slow_non_contiguous=True)
    P.load(bif, bif[:, 1:2], c.ml_f_bias.rearrange("o h -> h o"), part=True, allow_slow_non_contiguous=True)
    P.op("dve", lambda e: e.tensor_scalar(out=bif[:], in0=bif[:], scalar1=1.0 / 15.0, scalar2=None, op0=ALU.mult), reads=[bif], writes=[bif])

    xs = [P.sb("b_xs%d" % i, [128, D], F32) for i in range(2)]
    ma = [P.sb("b_ma%d" % i, [128, D], F32) for i in range(2)]
    junk = P.sb("b_junk", [128, D], BF16)
    st = (P.sb("b_ssq", [128, 1], F32), P.sb("b_tmp", [128, 1], F32), P.sb("b_rstd", [128, 1], F32))
    hb = P.sb("b_hb", [128, D], BF16)
    hT = P.sb("b_hT", [128, 8, 128], BF16)
    g_ti = P.sb("g_ti", [8, 128], F32)
    g_tf = P.sb("g_tf", [8, 128], F32)
    g_nl = P.sb("g_nl", [8, 128], F32)
    g_cum = P.sb("g_cum", [8, 128], F32)
    g_a = P.sb("g_a", [8, 128], F32)
    g_M = P.sb("g_M", [8, 128], F32)
    g_d2 = P.sb("g_d2", [8, 128], F32)
    g_ones = P.sb("g_ones", [8, 128], F32)
    P.op("pool", lambda e: e.memset(g_ones[:], 1.0), writes=[g_ones])
    g_out = [P.sb("g_out%d" % i, [8, 128], F32) for i in range(5)]
    cumc = [P.sb("g_cumc%d" % b, [8, 1], F32) for b in range(NB)]
    Mc = [P.sb("g_Mc%d" % b, [8, 1], F32) for b in range(NB)]
    g_nM0 = P.sb("g_nM0", [8, 1], F32)
    g_nMe = P.sb("g_nMe", [8, 1], F32)
    g_dd = P.sb("g_dd", [8, 1], F32)
    gtok = P.sb("b_gtok", [128, 5, 8], F32)
    qt = P.sb("b_qt", [128, 512], BF16)
    kt = P.sb("b_kt", [128, 512], BF16)
    khat = P.sb("b_khat", [128, 512], BF16)
    vaug = P.sb("b_vaug", [128, 8, 129], BF16)
    P.op("pool", lambda e: e.memset(vaug[:], 1.0), writes=[vaug])
    sgo = P.sb("b_sgo", [128, D], F32)
    sgm = P.sb("b_sgm", [128, D], F32)
    qtT = P.sb("b_qtT", [64, 8, 128], BF16)
    ktT = P.sb("b_ktT", [64, 8, 128], BF16)
    PTm = P.sb("b_PT", [128, 8, 128], BF16)
    C32 = [P.sb("b_C32_%d" % b, [64, 8, 129], F32) for b in range(NB)]
    Cb = [P.sb("b_Cb_%d" % b, [64, 8, 129], BF16) for b in range(NB)]
    dmax = P.sb("b_dmax", [128, 8], F32)
    rc = P.sb("b_rc", [128, 8], F32)
    hm = P.sb("b_hm", [128, 8, 128], F32)
    hsq = P.sb("b_hsq", [128, 8, 128], F32)
    hs8 = (P.sb("b_hssq", [128, 8], F32), P.sb("b_htmp", [128, 8], F32), P.sb("b_hrstd", [128, 8], F32))
    hn = P.sb("b_hn", [128, D], BF16)
    hnT = P.sb("b_hnT", [128, 8, 128], BF16)
    mx = P.sb("b_mx", [128, D], F32)
    mxb = P.sb("b_mxb", [128, D], BF16)
    mxT = P.sb("b_mxT", [128, 8, 128], BF16)
    xo = [P.sb("b_xo%d" % i, [128, D], F32) for i in range(2)]
    HG = [(0, 3), (3, 6), (6, 8)]

    def load_in(n):
        P.load(xs[n % 2], xs[n % 2][:], c.x[n * 128:(n + 1) * 128, :])
        P.load(ma[n % 2], ma[n % 2][:], c.mixa[n * 128:(n + 1) * 128, :], dram_reads=[c.mixa_t[n]])

    load_in(0)

    def tile(n):
        b = n // NT
        j = n % NT
        if n + 1 < NTT:
            load_in(n + 1)
        x_t = xs[n % 2]
        ma_t = ma[n % 2]
        norm_and_transpose(c, x_t, c.g1, hb, hT, junk, st, bank[7])

        def proj(bk, col0, ncols, M=128, ocol=0):
            for k in range(8):
                P.op("pe", lambda e, k=k: e.matmul(out=bk[0:M, ocol:ocol + ncols], lhsT=hT[:, k, :], rhs=wB[:, k, col0:col0 + ncols],
                                                   start=(k == 0), stop=(k == 7)),
                     reads=[hT, wB], writes=[bk] if (k == 0 and ocol == 0) else [], parts=[] if (k == 0 and ocol == 0) else [bk])
        for gi, col in enumerate((2048, 2056)):
            for k in range(8):
                P.op("pe", lambda e, k=k, gi=gi, col=col: e.matmul(out=bank[6][0:8, gi * 128:(gi + 1) * 128], lhsT=wB[:, k, col:col + 8], rhs=hT[:, k, :],
                                                                    start=(k == 0), stop=(k == 7)),
                     reads=[hT, wB], writes=[bank[6]] if (k == 0 and gi == 0) else [], parts=[] if (k == 0 and gi == 0) else [bank[6]])
        P.op("act", lambda e: e.activation(out=g_ti[:], in_=bank[6][0:8, 0:128], func=AF.Tanh, bias=bif[:, 0:1], scale=1.0 / 15.0),
             reads=[bank[6], bif], writes=[g_ti])
        P.op("act", lambda e: e.activation(out=g_tf[:], in_=bank[6][0:8, 128:256], func=AF.Tanh, bias=bif[:, 1:2], scale=1.0 / 15.0),
             reads=[bank[6], bif], writes=[g_tf])
        P.op("act", lambda e: e.activation(out=g_nl[:], in_=g_tf[:], func=AF.Exp, scale=-15.0), reads=[g_tf], writes=[g_nl])
        P.op("act", lambda e: e.activation(out=g_nl[:], in_=g_nl[:], func=AF.Ln, bias=1.0), reads=[g_nl], writes=[g_nl])
        if j == 0:
            P.op("dve", lambda e: e.tensor_tensor_scan(out=g_cum[:], data0=g_ones[:], data1=g_nl[:], initial=0.0, op0=ALU.mult, op1=ALU.add),
                 reads=[g_ones, g_nl], writes=[g_cum])
        else:
            P.op("dve", lambda e: e.tensor_tensor_scan(out=g_cum[:], data0=g_ones[:], data1=g_nl[:], initial=cumc[b][:], op0=ALU.mult, op1=ALU.add),
                 reads=[g_ones, g_nl, cumc[b]], writes=[g_cum])
        P.op("dve", lambda e: e.scalar_tensor_tensor(out=g_a[:], in0=g_ti[:], scalar=15.0, in1=g_cum[:], op0=ALU.mult, op1=ALU.add),
             reads=[g_ti, g_cum], writes=[g_a])
        if j == 0:
            P.op("dve", lambda e: e.memset(Mc[b][:], 0.0), writes=[Mc[b]])
        P.op("dve", lambda e: e.tensor_tensor_scan(out=g_M[:], data0=g_a[:], data1=g_a[:], initial=Mc[b][:], op0=ALU.max, op1=ALU.max),
             reads=[g_a, Mc[b]], writes=[g_M])
        P.op("dve", lambda e: e.tensor_scalar(out=g_nM0[:], in0=Mc[b][:], scalar1=-1.0, scalar2=None, op0=ALU.mult), reads=[Mc[b]], writes=[g_nM0])
        P.op("dve", lambda e: e.tensor_scalar(out=g_nMe[:], in0=g_M[:, 127:128], scalar1=-1.0, scalar2=None, op0=ALU.mult), reads=[g_M], writes=[g_nMe])
        P.op("dve", lambda e: e.tensor_tensor(out=g_dd[:], in0=Mc[b][:], in1=g_nMe[:], op=ALU.add), reads=[Mc[b], g_nMe], writes=[g_dd])
        P.op("dve", lambda e: e.tensor_tensor(out=g_d2[:], in0=g_cum[:], in1=g_M[:], op=ALU.subtract), reads=[g_cum, g_M], writes=[g_d2])
        P.op("act", lambda e: e.activation(out=g_out[0][:], in_=g_M[:], func=AF.Exp, bias=Mc[b][:], scale=-1.0), reads=[g_M, Mc[b]], writes=[g_out[0]])
        P.op("act", lambda e: e.activation(out=g_out[1][:], in_=g_a[:], func=AF.Exp, bias=g_nM0[:], scale=1.0), reads=[g_a, g_nM0], writes=[g_out[1]])
        P.op("act", lambda e: e.activation(out=g_out[2][:], in_=g_a[:], func=AF.Exp, bias=g_nMe[:], scale=1.0), reads=[g_a, g_nMe], writes=[g_out[2]])
        P.op("act", lambda e: e.activation(out=g_out[3][:], in_=g_a[:], func=AF.Exp, bias=g_dd[:], scale=0.0), reads=[g_a, g_dd], writes=[g_out[3]])
        P.op("act", lambda e: e.activation(out=g_out[4][:], in_=g_d2[:], func=AF.Exp), reads=[g_d2], writes=[g_out[4]])
        P.op("dve", lambda e: e.tensor_copy(out=cumc[b][:], in_=g_cum[:, 127:128]), reads=[g_cum], writes=[cumc[b]])
        P.op("dve", lambda e: e.tensor_copy(out=Mc[b][:], in_=g_M[:, 127:128]), reads=[g_M], writes=[Mc[b]])
        for qi in range(5):
            P.op("pe", lambda e, qi=qi: e.transpose(out=bank[6][:, 256 + qi * 8:256 + (qi + 1) * 8], in_=g_out[qi][:], identity=c.identf[0:8, 0:8]),
                 reads=[g_out[qi], c.identf], writes=[bank[6]] if qi == 0 else [], parts=[] if qi == 0 else [bank[6]])
        P.op("dve", lambda e: e.tensor_copy(out=gtok[:].rearrange("p q h -> p (q h)"), in_=bank[6][:, 256:296]), reads=[bank[6]], writes=[gtok])
        proj(bank[0], 0, 512)
        proj(bank[1], 512, 512)
        P.op("dve", lambda e: e.tensor_tensor(out=qt[:].rearrange("p (h d) -> p h d", d=64), in0=bank[0][:].rearrange("p (h d) -> p h d", d=64),
                                              in1=gtok[:, 0, :].unsqueeze(2).to_broadcast([128, 8, 64]), op=ALU.mult),
             reads=[bank[0], gtok], writes=[qt])
        P.op("dve", lambda e: e.scalar_tensor_tensor(out=kt[:].rearrange("p (h d) -> p h d", d=64), in0=bank[1][:].rearrange("p (h d) -> p h d", d=64),
                                                     scalar=0.125, in1=gtok[:, 1, :].unsqueeze(2).to_broadcast([128, 8, 64]), op0=ALU.mult, op1=ALU.mult),
             reads=[bank[1], gtok], writes=[kt])
        P.op("dve", lambda e: e.scalar_tensor_tensor(out=khat[:].rearrange("p (h d) -> p h d", d=64), in0=bank[1][:].rearrange("p (h d) -> p h d", d=64),
                                                     scalar=0.125, in1=gtok[:, 2, :].unsqueeze(2).to_broadcast([128, 8, 64]), op0=ALU.mult, op1=ALU.mult),
             reads=[bank[1], gtok], writes=[khat])
        for hv in range(2):
            proj(bank[2 + hv], 1024 + hv * 512, 512)
            P.op("act", lambda e, hv=hv: e.copy(out=vaug[:, 4 * hv:4 * hv + 4, 0:128], in_=bank[2 + hv][:].rearrange("p (h d) -> p h d", d=128)),
                 reads=[bank[2 + hv]], writes=[vaug] if hv == 0 else [], parts=[] if hv == 0 else [vaug])
        for hv in range(2):
            proj(bank[4 + hv], 2064 + hv * 512, 512)
            P.op("act", lambda e, hv=hv: e.activation(out=sgo[:, hv * 512:(hv + 1) * 512], in_=bank[4 + hv][:], func=AF.Sigmoid),
                 reads=[bank[4 + hv]], writes=[sgo] if hv == 0 else [], parts=[] if hv == 0 else [sgo])
        for hv in range(2):
            proj(bank[2 + hv], 3088 + hv * 512, 512)
            P.op("act", lambda e, hv=hv: e.activation(out=sgm[:, hv * 512:(hv + 1) * 512], in_=bank[2 + hv][:], func=AF.Sigmoid),
                 reads=[bank[2 + hv]], writes=[sgm] if hv == 0 else [], parts=[] if hv == 0 else [sgm])
        for (src, dstT, bk, eng) in ((qt, qtT, bank[7], "act"), (kt, ktT, bank[6], "dve")):
            pv = bk[:].bitcast(BF16)
            for h in range(8):
                P.op("pe", lambda e, h=h, src=src, pv=pv: e.transpose(out=pv[0:64, h * 128:(h + 1) * 128], in_=src[:, h * 64:(h + 1) * 64], identity=c.identb[:]),
                     reads=[src, c.identb], writes=[bk] if h == 0 else [], parts=[] if h == 0 else [bk])
            if eng == "act":
                P.op("act", lambda e, pv=pv, dstT=dstT: e.copy(out=dstT[:].rearrange("p h t -> p (h t)"), in_=pv[0:64, :]), reads=[bk], writes=[dstT])
            else:
                P.op("dve", lambda e, pv=pv, dstT=dstT: e.tensor_copy(out=dstT[:].rearrange("p h t -> p (h t)"), in_=pv[0:64, :]), reads=[bk], writes=[dstT])
        for hb4 in range(2):
            bk = bank[hb4]
            for hh in range(4):
                h = hb4 * 4 + hh
                P.op("pe", lambda e, h=h, hh=hh, bk=bk: e.matmul(out=bk[:, hh * 128:(hh + 1) * 128], lhsT=ktT[:, h, :], rhs=qtT[:, h, :], start=True, stop=True),
                     reads=[ktT, qtT], writes=[bk] if hh == 0 else [], parts=[] if hh == 0 else [bk])
            P.op("dve", lambda e, bk=bk, hb4=hb4: e.tensor_tensor(out=PTm[:, 4 * hb4:4 * hb4 + 4, :].rearrange("p h t -> p (h t)"), in0=bk[:], in1=cmask[:], op=ALU.mult),
                 reads=[bk, cmask], writes=[PTm] if hb4 == 0 else [], parts=[] if hb4 == 0 else [PTm])
        for gi, (h0, h1) in enumerate(HG):
            bk = bank[2 + gi]
            for h in range(h0, h1):
                o = (h - h0) * 129
                P.op("pe", lambda e, h=h, o=o, bk=bk: e.matmul(out=bk[:, o:o + 129], lhsT=PTm[:, h, :], rhs=vaug[:, h, :], start=True, stop=(j == 0)),
                     reads=[PTm, vaug], writes=[bk] if h == h0 else [], parts=[] if h == h0 else [bk])
                if j > 0:
                    P.op("pe", lambda e, h=h, o=o, bk=bk: e.matmul(out=bk[:, o:o + 129], lhsT=qtT[:, h, :], rhs=Cb[b][:, h, :], start=False, stop=True),
                         reads=[qtT, Cb[b]], parts=[bk])
        for gi, (h0, h1) in enumerate(HG):
            bk = bank[5 + gi]
            for h in range(h0, h1):
                o = (h - h0) * 129
                P.op("pe", lambda e, h=h, o=o, bk=bk: e.matmul(out=bk[0:64, o:o + 129], lhsT=khat[:, h * 64:(h + 1) * 64], rhs=vaug[:, h, :], start=True, stop=True),
                     reads=[khat, vaug], writes=[bk] if h == h0 else [], parts=[] if h == h0 else [bk])
        if j > 0:
            P.op("dve", lambda e: e.tensor_tensor(out=C32[b][:], in0=C32[b][:], in1=gtok[0:64, 3, :].unsqueeze(2).to_broadcast([64, 8, 129]), op=ALU.mult),
                 reads=[C32[b], gtok], writes=[C32[b]])
        for gi, (h0, h1) in enumerate(HG):
            bk = bank[5 + gi]
            nh = h1 - h0
            if j > 0:
                P.op("dve", lambda e, bk=bk, h0=h0, h1=h1, nh=nh: e.tensor_tensor(out=C32[b][:, h0:h1, :], in0=C32[b][:, h0:h1, :],
                                                                                  in1=bk[0:64, 0:nh * 129].rearrange("p (h v) -> p h v", v=129), op=ALU.add),
                     reads=[bk, C32[b]], writes=[C32[b]])
            else:
                P.op("dve", lambda e, bk=bk, h0=h0, h1=h1, nh=nh: e.tensor_copy(out=C32[b][:, h0:h1, :], in_=bk[0:64, 0:nh * 129].rearrange("p (h v) -> p h v", v=129)),
                     reads=[bk], writes=[C32[b]] if gi == 0 else [], parts=[] if gi == 0 else [C32[b]])
        P.op("act", lambda e: e.copy(out=Cb[b][:], in_=C32[b][:]), reads=[C32[b]], writes=[Cb[b]])
        for gi, (h0, h1) in enumerate(HG):
            bk = bank[2 + gi]
            nh = h1 - h0
            v3 = bk[:, 0:nh * 129].rearrange("p (h v) -> p h v", v=129)
            P.op("dve", lambda e, v3=v3, h0=h0, h1=h1: e.tensor_tensor(out=dmax[:, h0:h1].unsqueeze(2), in0=v3[:, :, 128:129], in1=gtok[:, 4, h0:h1].unsqueeze(2), op=ALU.max),
                 reads=[bk, gtok], writes=[dmax])
            P.op("dve", lambda e, v3=v3, h0=h0, h1=h1: e.scalar_tensor_tensor(out=dmax[:, h0:h1].unsqueeze(2), in0=v3[:, :, 128:129], scalar=-1.0, in1=dmax[:, h0:h1].unsqueeze(2), op0=ALU.mult, op1=ALU.max),
                 reads=[bk, dmax], writes=[dmax])
        P.op("dve", lambda e: e.reciprocal(out=rc[:], in_=dmax[:]), reads=[dmax], writes=[rc])
        for gi, (h0, h1) in enumerate(HG):
            bk = bank[2 + gi]
            nh = h1 - h0
            v3 = bk[:, 0:nh * 129].rearrange("p (h v) -> p h v", v=129)
            P.op("dve", lambda e, v3=v3, h0=h0, h1=h1, nh=nh: e.tensor_tensor(out=hm[:, h0:h1, :], in0=v3[:, :, 0:128],
                                                                              in1=rc[:, h0:h1].unsqueeze(2).to_broadcast([128, nh, 128]), op=ALU.mult),
                 reads=[bk, rc], writes=[hm] if gi == 0 else [], parts=[] if gi == 0 else [hm])
        P.op("pool", lambda e: e.tensor_tensor(out=hsq[:], in0=hm[:], in1=hm[:], op=ALU.mult), reads=[hm], writes=[hsq])
        P.op("dve", lambda e: e.tensor_reduce(out=hs8[0][:], in_=hsq[:], axis=AX.X, op=ALU.add), reads=[hsq], writes=[hs8[0]])
        rms_rstd(c, "h", hs8[0], 128, hs8[2], hs8[1])
        P.op("dve", lambda e: e.tensor_tensor(out=hm[:], in0=hm[:], in1=hs8[2][:].unsqueeze(2).to_broadcast([128, 8, 128]), op=ALU.mult),
             reads=[hm, hs8[2]], writes=[hm])
        hmf = hm[:].rearrange("p h v -> p (h v)")
        P.op("pool", lambda e: e.tensor_tensor(out=hmf, in0=hmf, in1=mlg[:], op=ALU.mult), reads=[hm, mlg], writes=[hm])
        P.op("dve", lambda e: e.tensor_tensor(out=hn[:], in0=hmf, in1=sgo[:], op=ALU.mult), reads=[hm, sgo], writes=[hn])
        transpose8(c, hn, hnT, bank[7])
        for hv in range(2):
            bk = bank[hv]
            for i in range(8):
                P.op("pe", lambda e, bk=bk, i=i, hv=hv: e.matmul(out=bk[:], lhsT=hnT[:, i, :], rhs=wbm[:, i, hv * 512:(hv + 1) * 512], start=(i == 0), stop=(i == 7)),
                     reads=[hnT, wbm], writes=[bk] if i == 0 else [], parts=[] if i == 0 else [bk])
            P.op("dve", lambda e, bk=bk, hv=hv: e.tensor_tensor(out=mx[:, hv * 512:(hv + 1) * 512], in0=bk[:], in1=sgm[:, hv * 512:(hv + 1) * 512], op=ALU.mult),
                 reads=[bk, sgm], writes=[mx] if hv == 0 else [], parts=[] if hv == 0 else [mx])
        P.op("pool", lambda e: e.tensor_tensor(out=mxb[:], in0=mx[:], in1=ma_t[:], op=ALU.add), reads=[mx, ma_t], writes=[mxb])
        transpose8(c, mxb, mxT, bank[6], eng="dve")
        xo_t = xo[n % 2]
        for hv in range(2):
            bk = bank[2 + hv]
            for i in range(8):
                P.op("pe", lambda e, bk=bk, i=i, hv=hv: e.matmul(out=bk[:], lhsT=mxT[:, i, :], rhs=wout[:, i, hv * 512:(hv + 1) * 512], start=(i == 0), stop=(i == 7)),
                     reads=[mxT, wout], writes=[bk] if i == 0 else [], parts=[] if i == 0 else [bk])
            P.op("dve", lambda e, bk=bk, hv=hv: e.tensor_tensor(out=xo_t[:, hv * 512:(hv + 1) * 512], in0=bk[:], in1=x_t[:, hv * 512:(hv + 1) * 512], op=ALU.add),
                 reads=[bk, x_t], writes=[xo_t] if hv == 0 else [], parts=[] if hv == 0 else [xo_t])
        P.store(c.out[n * 128:(n + 1) * 128, :], xo_t, xo_t[:], dram_writes=[c.x1_t[n]], final=("C" not in c.phases))

    for n in range(NTT):
        tile(n)


def phase_c(c):
    P, NB, NT, NTT = c.P, c.NB, c.NT, c.NTT
    nc = c.nc
    bank = c.bank
    GT = 3
    ex_s = P.dram("ex_s", [128, 128, 2 * D], BF16)
    downT_s = ex_s[:, :, 0:D]
    up_s = ex_s[:, :, D:2 * D]
    downT_t = [T("downT_t%d" % i) for i in range(128)]
    up_t = [T("up_t%d" % i) for i in range(16)]
    up_v = c.peer_up.rearrange("(i c) d -> c i d", c=128)
    dn_v = c.peer_down.rearrange("(i c) d -> c i d", c=128)
    dummy = T("up_dma_sem")
    for u in range(16):
        P.dma(up_s[u * 8:(u + 1) * 8], up_v[u * 8:(u + 1) * 8], writes=[up_t[u]], sem_tile=dummy, eng="pool", max_dma_last_dim=4096)
    P.open_scope()
    dsrc = [P.sb("c_dsrc%d" % i, [128, D], BF16) for i in range(2)]
    dtr = [P.sb("c_dtr%d" % i, [128, D], BF16) for i in range(2)]
    for cb in range(128):
        sl = cb % 2
        P.load(dsrc[sl], dsrc[sl][:], dn_v[cb], eng="pool", max_dma_last_dim=4096)
        pv = bank[6 + sl][:].bitcast(BF16)
        for k in range(8):
            P.op("pe", lambda e, k=k, sl=sl, pv=pv: e.transpose(out=pv[:, k * 128:(k + 1) * 128], in_=dsrc[sl][:, k * 128:(k + 1) * 128], identity=c.identb[:]),
                 reads=[dsrc[sl], c.identb], writes=[bank[6 + sl]] if k == 0 else [], parts=[] if k == 0 else [bank[6 + sl]])
        if sl == 0:
            P.op("act", lambda e, sl=sl, pv=pv: e.copy(out=dtr[sl][:], in_=pv), reads=[bank[6 + sl]], writes=[dtr[sl]])
        else:
            P.op("dve", lambda e, sl=sl, pv=pv: e.tensor_copy(out=dtr[sl][:], in_=pv), reads=[bank[6 + sl]], writes=[dtr[sl]])
        P.store(downT_s[cb], dtr[sl], dtr[sl][:], dram_writes=[downT_t[cb]])
    P.close_scope()
    cstage = c.dbg if (c.dbg and c.phases == "C") else 99
    if cstage == 1:
        return

    wq = P.sb("c_wq", [128, 8, D], BF16)
    load_w_bf16(c, wq, 0, D, c.peer_w_query)
    g2 = P.sb("c_g2", [128, D], F32)
    P.load(g2, g2[:], c.norm2_gain[0].partition_broadcast(128))
    iof = P.sb("c_iof", [128, 128], F32)
    P.load(iof, iof[:], c.cst["iota128"])
    iob = P.sb("c_iob", [128, 128], BF16)
    P.op("dve", lambda e: e.tensor_copy(out=iob[:], in_=iof[:]), reads=[iof], writes=[iob])
    skT = P.sb("c_skT", [128, 8, 128], BF16)
    P.open_scope()
    skf = P.sb("c_skf", [128, 16, 64], F32)
    P.load(skf, skf[:], c.peer_sub_keys.rearrange("h p k d -> k (h p) d"))
    skb = P.sb("c_skb", [128, 16 * 64], BF16)
    P.op("dve", lambda e: e.tensor_copy(out=skb[:], in_=skf[:].rearrange("k a d -> k (a d)")), reads=[skf], writes=[skb])
    transpose8(c, skb, skT, bank[7])
    P.close_scope()

    if cstage == 2:
        return
    x1s = [P.sb("c_x1s%d" % i, [128, D], F32) for i in range(GT)]
    st = (P.sb("c_ssq", [128, 1], F32), P.sb("c_tmp", [128, 1], F32), P.sb("c_rstd", [128, 1], F32))
    xb = P.sb("c_xb", [128, D], BF16)
    xnT = P.sb("c_xnT", [128, 8, GT * 128], BF16)
    qT = P.sb("c_qT", [128, 8, 128], BF16)
    sc = P.sb("c_sc", [128, 16, 128], F32)
    sc_t = [T("sc_t%d" % i) for i in range(16)]
    v16 = P.sb("c_v16", [128, 16, 16], F32)
    ix = P.sb("c_ix", [128, 16, 16], U32)
    ixf = P.sb("c_ixf", [128, 16, 16], F32)
    cand = P.sb("c_cand", [128, 8, 256], F32)
    cand_t = [T("cand_t%d" % i) for i in range(8)]
    v16b, ixb, c16b, posb = T("v16b"), T("ixb"), T("c16b"), T("posb")
    c16 = P.sb("c_c16", [128, 8, 16], F32)
    pos = P.sb("c_pos", [128, 8, 16], U32)
    posf = P.sb("c_posf", [128, 8, 16], F32)
    pki = P.sb("c_pki", [128, 8, 16], I32)
    pkf = P.sb("c_pkf", [128, 8, 16], F32)
    qkf = P.sb("c_qkf", [128, 8, 16], F32)
    decs = [P.sb("c_dec%d" % i, [128, 2, 16, 16], F32) for i in range(4)]
    abg = [P.sb("c_abg%d" % i, [128, 8, 16], F32) for i in range(3)]
    z8 = P.sb("c_z8", [128, 8], F32)
    abgT = [P.sb("c_abgT%d" % i, [128, GT * 128], F32) for i in range(3)]
    CH = 8
    Ach = [P.sb("c_Ach%d" % i, [128, CH, 128], BF16) for i in range(2)]
    Bch = [P.sb("c_Bch%d" % i, [128, CH, 128], BF16) for i in range(2)]
    Wg = P.sb("c_Wg", [128, 128, GT * 128], BF16)
    NS = 4
    esl = [P.sb("c_esl%d" % i, [128, 2 * D], BF16) for i in range(NS)]
    actb = [P.sb("c_act%d" % i, [128, GT * 128], BF16) for i in range(2)]
    wab = [P.sb("c_wa%d" % i, [128, GT * 128], BF16) for i in range(2)]

    groups = []
    n0 = 0
    while n0 < NTT:
        g = min(GT, NTT - n0)
        groups.append((n0, g))
        n0 += g
    blk_ctr = [0]

    def route_tile(n, ti):
        x_t = x1s[ti]
        src_x1 = c.x if c.phases == "C" else c.out
        P.load(x_t, x_t[:], src_x1[n * 128:(n + 1) * 128, :], dram_reads=[c.x1_t[n]])
        ssq, tmp, rstd = st
        P.op("act", lambda e: e.activation(out=xb[:], in_=x_t[:], func=AF.Square, accum_out=ssq[:]), reads=[x_t], writes=[xb, ssq])
        rms_rstd(c, "n", ssq, D, rstd, tmp)
        P.op("dve", lambda e: e.scalar_tensor_tensor(out=xb[:], in0=x_t[:], scalar=rstd[:], in1=g2[:], op0=ALU.mult, op1=ALU.mult),
             reads=[x_t, rstd, g2], writes=[xb])
        pvx = bank[7][:].bitcast(BF16)
        for k in range(8):
            P.op("pe", lambda e, k=k: e.transpose(out=pvx[:, k * 128:(k + 1) * 128], in_=xb[:, k * 128:(k + 1) * 128], identity=c.identb[:]),
                 reads=[xb, c.identb], writes=[bank[7]] if k == 0 else [], parts=[] if k == 0 else [bank[7]])
        xt1 = xnT[:, :, ti * 128:(ti + 1) * 128]
        P.op("act", lambda e: e.copy(out=xt1, in_=pvx.rearrange("p (k t) -> p k t", t=128)), reads=[bank[7]], writes=[xnT] if ti == 0 else [], parts=[] if ti == 0 else [xnT])
        if cstage == 29:
            return
        for hd in range(8):
            bk = bank[hd % 2]
            for k in range(8):
                P.op("pe", lambda e, k=k, hd=hd, bk=bk: e.matmul(out=bk[:, 0:128], lhsT=wq[:, k, hd * 128:(hd + 1) * 128], rhs=xt1[:, k, :],
                                                                start=(k == 0), stop=(k == 7)),
                     reads=[wq, xnT], writes=[bk] if k == 0 else [], parts=[] if k == 0 else [bk])
            if hd % 2 == 0:
                P.op("act", lambda e, hd=hd, bk=bk: e.copy(out=qT[:, hd, :], in_=bk[:, 0:128]), reads=[bk], writes=[qT] if hd == 0 else [], parts=[] if hd == 0 else [qT])
            else:
                P.op("dve", lambda e, hd=hd, bk=bk: e.tensor_copy(out=qT[:, hd, :], in_=bk[:, 0:128]), reads=[bk], parts=[qT])
        if cstage == 30:
            return
        sc4 = sc[:].rearrange("p (h two) k -> p h two k", two=2)
        for par in range(2):
            for hf in range(2):
                bk = bank[2 + par * 2 + hf]
                for u in range(4):
                    hd = hf * 4 + u
                    P.op("pe", lambda e, u=u, hd=hd, par=par, bk=bk: e.matmul(out=bk[:, u * 128:(u + 1) * 128], lhsT=qT[par * 64:(par + 1) * 64, hd, :],
                                                                          rhs=skT[par * 64:(par + 1) * 64, hd, :], start=True, stop=True),
                         reads=[qT, skT], writes=[bk] if u == 0 else [], parts=[] if u == 0 else [bk])
        for par in range(2):
            for hf in range(2):
                bk = bank[2 + par * 2 + hf]
                first = (par == 0 and hf == 0)
                if hf == 0:
                    P.op("act", lambda e, par=par, hf=hf, bk=bk: e.copy(out=sc4[:, hf * 4:hf * 4 + 4, par, :], in_=bk[:].rearrange("p (a k) -> p a k", k=128)),
                         reads=[bk], writes=[sc_t[(hf * 4 + u_) * 2 + par] for u_ in range(4)])
                else:
                    P.op("dve", lambda e, par=par, hf=hf, bk=bk: e.tensor_copy(out=sc4[:, hf * 4:hf * 4 + 4, par, :], in_=bk[:].rearrange("p (a k) -> p a k", k=128)),
                         reads=[bk], writes=[sc_t[(hf * 4 + u_) * 2 + par] for u_ in range(4)])
        if cstage in (31, 305, 306, 307):
            return
        for hp in range(16):
            P.op("dve", lambda e, hp=hp: e.max(out=v16[:, hp, 0:8], in_=sc[:, hp, :]), reads=[sc_t[hp]], writes=[v16] if hp == 0 else [], parts=[] if hp == 0 else [v16])
        for hp in range(16):
            P.op("dve", lambda e, hp=hp: e.max_index(out=ix[:, hp, 0:8], in_max=v16[:, hp, 0:8], in_values=sc[:, hp, :]), reads=[sc_t[hp], v16],
                 writes=[ix] if hp == 0 else [], parts=[] if hp == 0 else [ix])
        for hp in range(16):
            P.op("dve", lambda e, hp=hp: e.match_replace(out=sc[:, hp, :], in_to_replace=v16[:, hp, 0:8], in_values=sc[:, hp, :], imm_value=-1e30),
                 reads=[sc_t[hp], v16], writes=[sc_t[hp]])
        for hp in range(16):
            P.op("dve", lambda e, hp=hp: e.max(out=v16[:, hp, 8:16], in_=sc[:, hp, :]), reads=[sc_t[hp]], writes=[v16b] if hp == 0 else [], parts=[] if hp == 0 else [v16b])
        for hp in range(16):
            P.op("dve", lambda e, hp=hp: e.max_index(out=ix[:, hp, 8:16], in_max=v16[:, hp, 8:16], in_values=sc[:, hp, :]), reads=[sc_t[hp], v16b],
                 writes=[ixb] if hp == 0 else [], parts=[] if hp == 0 else [ixb])
        if cstage == 32:
            return
        P.op("dve", lambda e: e.tensor_copy(out=ixf[:], in_=ix[:]), reads=[ix, ixb], writes=[ixf])
        vv = v16[:].rearrange("p (h two) k -> p h two k", two=2)
        iv = ixf[:].rearrange("p (h two) k -> p h two k", two=2)
        P.op("dve", lambda e: e.tensor_tensor(out=cand[:].rearrange("p h (a b) -> p h a b", b=16),
                                              in0=vv[:, :, 0, :].unsqueeze(3).to_broadcast([128, 8, 16, 16]),
                                              in1=vv[:, :, 1, :].unsqueeze(2).to_broadcast([128, 8, 16, 16]), op=ALU.add),
             reads=[v16, v16b], writes=cand_t)
        for h in range(8):
            P.op("dve", lambda e, h=h: e.max(out=c16[:, h, 0:8], in_=cand[:, h, :]), reads=[cand_t[h]], writes=[c16] if h == 0 else [], parts=[] if h == 0 else [c16])
        for h in range(8):
            P.op("dve", lambda e, h=h: e.max_index(out=pos[:, h, 0:8], in_max=c16[:, h, 0:8], in_values=cand[:, h, :]), reads=[cand_t[h], c16],
                 writes=[pos] if h == 0 else [], parts=[] if h == 0 else [pos])
        for h in range(8):
            P.op("dve", lambda e, h=h: e.match_replace(out=cand[:, h, :], in_to_replace=c16[:, h, 0:8], in_values=cand[:, h, :], imm_value=-1e30),
                 reads=[cand_t[h], c16], writes=[cand_t[h]])
        for h in range(8):
            P.op("dve", lambda e, h=h: e.max(out=c16[:, h, 8:16], in_=cand[:, h, :]), reads=[cand_t[h]], writes=[c16b] if h == 0 else [], parts=[] if h == 0 else [c16b])
        for h in range(8):
            P.op("dve", lambda e, h=h: e.max_index(out=pos[:, h, 8:16], in_max=c16[:, h, 8:16], in_values=cand[:, h, :]), reads=[cand_t[h], c16b],
                 writes=[posb] if h == 0 else [], parts=[] if h == 0 else [posb])
        if cstage == 33:
            return
        P.op("dve", lambda e: e.tensor_copy(out=posf[:], in_=pos[:]), reads=[pos, posb], writes=[posf])
        P.op("dve", lambda e: e.tensor_scalar(out=pkf[:], in0=posf[:], scalar1=-7.5, scalar2=0.0625, op0=ALU.add, op1=ALU.mult), reads=[posf], writes=[pkf])
        P.op("dve", lambda e: e.tensor_copy(out=pki[:], in_=pkf[:]), reads=[pkf], writes=[pki])
        P.op("dve", lambda e: e.tensor_copy(out=pkf[:], in_=pki[:]), reads=[pki], writes=[pkf])
        P.op("dve", lambda e: e.scalar_tensor_tensor(out=qkf[:], in0=pkf[:], scalar=-16.0, in1=posf[:], op0=ALU.mult, op1=ALU.add), reads=[pkf, posf], writes=[qkf])
        combos = [(hh, which, sel, dst) for hh in range(4) for (which, sel, dst) in ((0, pkf, abg[0]), (1, qkf, abg[1]))]
        for half in range(2):
            sub = combos[half * 4:(half + 1) * 4]
            for di, (hh, which, sel, dst) in enumerate(sub):
                hs = slice(hh * 2, hh * 2 + 2)
                dec = decs[di]
                P.op("dve", lambda e, sel=sel, hs=hs, dec=dec: e.tensor_tensor(out=dec[:], in0=iof[:, 0:16].unsqueeze(1).unsqueeze(1).to_broadcast([128, 2, 16, 16]),
                                                                            in1=sel[:, hs, :].unsqueeze(3).to_broadcast([128, 2, 16, 16]), op=ALU.is_equal),
                     reads=[iof, sel], writes=[dec])
            for di, (hh, which, sel, dst) in enumerate(sub):
                hs = slice(hh * 2, hh * 2 + 2)
                dec = decs[di]
                P.op("pool", lambda e, which=which, hs=hs, dec=dec: e.tensor_tensor(out=dec[:], in0=dec[:], in1=iv[:, hs, which, :].unsqueeze(2).to_broadcast([128, 2, 16, 16]), op=ALU.mult),
                     reads=[dec, ixf], writes=[dec])
            for di, (hh, which, sel, dst) in enumerate(sub):
                hs = slice(hh * 2, hh * 2 + 2)
                dec = decs[di]
                firstw = (hh == 0)
                P.op("dve", lambda e, dst=dst, hs=hs, dec=dec: e.tensor_reduce(out=dst[:, hs, :], in_=dec[:], axis=AX.X, op=ALU.add),
                     reads=[dec], writes=[dst] if firstw else [], parts=[] if firstw else [dst])
        if cstage == 34:
            return
        P.op("dve", lambda e: e.tensor_tensor(out=abg[2][:], in0=c16[:], in1=c16[:, :, 0:1].to_broadcast([128, 8, 16]), op=ALU.subtract), reads=[c16, c16b], writes=[abg[2]])
        P.op("act", lambda e: e.activation(out=abg[2][:], in_=abg[2][:], func=AF.Exp), reads=[abg[2]], writes=[abg[2]])
        P.op("dve", lambda e: e.tensor_reduce(out=z8[:], in_=abg[2][:], axis=AX.X, op=ALU.add), reads=[abg[2]], writes=[z8])
        P.op("dve", lambda e: e.reciprocal(out=z8[:], in_=z8[:]), reads=[z8], writes=[z8])
        P.op("dve", lambda e: e.tensor_tensor(out=abg[2][:], in0=abg[2][:], in1=z8[:].unsqueeze(2).to_broadcast([128, 8, 16]), op=ALU.mult), reads=[abg[2], z8], writes=[abg[2]])
        if cstage == 35:
            return
        for i3 in range(3):
            P.op("pe", lambda e, i3=i3: e.transpose(out=bank[6][:, i3 * 128:(i3 + 1) * 128], in_=abg[i3][:].rearrange("p h k -> p (h k)"), identity=c.identf[:]),
                 reads=[abg[i3], c.identf], writes=[bank[6]] if i3 == 0 else [], parts=[] if i3 == 0 else [bank[6]])
        for i3 in range(3):
            P.op("act", lambda e, i3=i3: e.copy(out=abgT[i3][:, ti * 128:(ti + 1) * 128], in_=bank[6][:, i3 * 128:(i3 + 1) * 128]), reads=[bank[6]], parts=[abgT[i3]])
        if cstage == 36:
            return
        for q8 in range(0, 128, CH):
            chn = (q8 // CH) % 2
            tg8 = ti * 128 + q8
            io_bc = iof[:].unsqueeze(1).to_broadcast([128, CH, 128])
            P.op("dve", lambda e, chn=chn, tg8=tg8, io_bc=io_bc: e.tensor_tensor(out=Ach[chn][:], in0=io_bc,
                                                                             in1=abgT[0][:, tg8:tg8 + CH].unsqueeze(2).to_broadcast([128, CH, 128]), op=ALU.is_equal),
                 reads=[iof, abgT[0]], writes=[Ach[chn]])
            P.op("dve", lambda e, chn=chn, tg8=tg8, io_bc=io_bc: e.tensor_tensor(out=Bch[chn][:], in0=io_bc,
                                                                             in1=abgT[1][:, tg8:tg8 + CH].unsqueeze(2).to_broadcast([128, CH, 128]), op=ALU.is_equal),
                 reads=[iof, abgT[1]], writes=[Bch[chn]])
            P.op("pool", lambda e, chn=chn, tg8=tg8: e.tensor_tensor(out=Bch[chn][:], in0=Bch[chn][:],
                                                                   in1=abgT[2][:, tg8:tg8 + CH].unsqueeze(2).to_broadcast([128, CH, 128]), op=ALU.mult),
                 reads=[Bch[chn], abgT[2]], writes=[Bch[chn]])
            for tq in range(q8, q8 + CH, 4):
                bk = bank[(tq // 4) % 2]
                for t in range(tq, tq + 4):
                    tl = t % CH
                    u = t - tq
                    P.op("pe", lambda e, chn=chn, tl=tl, u=u, bk=bk: e.matmul(out=bk[:, u * 128:(u + 1) * 128], lhsT=Ach[chn][:, tl, :], rhs=Bch[chn][:, tl, :], start=True, stop=True),
                         reads=[Ach[chn], Bch[chn]], writes=[bk] if u == 0 else [], parts=[] if u == 0 else [bk])
                tg0 = ti * 128 + tq
                P.op("act", lambda e, bk=bk, tg0=tg0: e.copy(out=Wg[:, :, tg0:tg0 + 4], in_=bk[:].rearrange("p (t c) -> p c t", c=128)),
                     reads=[bk], parts=[Wg])

    def expert_loop(n0, g):
        W = g * 128
        for cb in range(128):
            sl = blk_ctr[0] % NS
            blk_ctr[0] += 1
            P.load(esl[sl], esl[sl][:], ex_s[cb], dram_reads=[downT_t[cb], up_t[cb // 8]])
            sb_ = bank[6 + cb % 2]
            for k in range(8):
                P.op("pe", lambda e, k=k, sl=sl, sb_=sb_: e.matmul(out=sb_[:, 0:W], lhsT=esl[sl][:, k * 128:(k + 1) * 128], rhs=xnT[:, k, 0:W], start=(k == 0), stop=(k == 7)),
                     reads=[esl[sl], xnT], writes=[sb_] if k == 0 else [], parts=[] if k == 0 else [sb_])
            ab = actb[cb % 2]
            wb = wab[cb % 2]
            P.op("act", lambda e, ab=ab, sb_=sb_: e.activation(out=ab[:, 0:W], in_=sb_[:, 0:W], func=AF.Gelu), reads=[sb_], writes=[ab])
            P.op("dve", lambda e, ab=ab, wb=wb, cb=cb: e.tensor_tensor(out=wb[:, 0:W], in0=ab[:, 0:W], in1=Wg[:, cb, 0:W], op=ALU.mult), reads=[ab, Wg], writes=[wb])
            for tt in range(g):
                for hf in range(2):
                    bk = bank[tt * 2 + hf]
                    P.op("pe", lambda e, tt=tt, hf=hf, bk=bk, wb=wb, sl=sl, cb=cb: e.matmul(out=bk[:], lhsT=wb[:, tt * 128:(tt + 1) * 128], rhs=esl[sl][:, D + hf * 512:D + (hf + 1) * 512],
                                                                                      start=(cb == 0), stop=(cb == 127)),
                         reads=[wb, esl[sl]], writes=[bk] if cb == 0 else [], parts=[] if cb == 0 else [bk])
        for tt in range(g):
            n = n0 + tt
            x_t = x1s[tt]
            for hf in range(2):
                bk = bank[tt * 2 + hf]
                P.op("dve", lambda e, bk=bk, hf=hf, x_t=x_t: e.tensor_tensor(out=x_t[:, hf * 512:(hf + 1) * 512], in0=bk[:], in1=x_t[:, hf * 512:(hf + 1) * 512], op=ALU.add),
                     reads=[bk, x_t], writes=[x_t])
            P.store(c.out[n * 128:(n + 1) * 128, :], x_t, x_t[:], dram_writes=[c.x1_t[n]], final=True)

    for (n0, g) in groups:
        for ti in range(g):
            route_tile(n0 + ti, ti)
        if 3 <= cstage <= 40:
            return
        if cstage == 50:
            continue
        expert_loop(n0, g)


from concourse.bass_utils import run_bass_kernel_spmd

W_NAMES = ["norm1_gain", "w_in", "ml_i_bias", "ml_f_bias", "q_norm_gain", "k_norm_gain", "attn_sinks", "ml_out_norm_gain",
           "w_branch_attn", "w_branch_mlstm", "w_out", "norm2_gain", "peer_w_query", "peer_sub_keys", "peer_down", "peer_up"]
PEER_NAMES = ["norm2_gain", "peer_w_query", "peer_sub_keys", "peer_down", "peer_up"]


def make_in_maps(inputs, NB, NT, ncores, phases="ABC"):
    consts = host_consts()
    S = NT * 128
    maps = []
    shared = {}
    for k in W_NAMES:
        if "C" not in phases and k in PEER_NAMES:
            continue
        shared[k] = np.ascontiguousarray(inputs[k][0])
    for k, v in consts.items():
        shared["c_" + k] = v
    for ci in range(ncores):
        m = dict(shared)
        m["x"] = np.ascontiguousarray(inputs["x"][ci * NB:(ci + 1) * NB, :S]).reshape(NB * S, D)
        m["positions"] = np.ascontiguousarray(inputs["positions"][ci * NB:(ci + 1) * NB, :S]).reshape(NB * S).astype(np.int32)
        maps.append(m)
    return maps


def kernel(**inputs):
    NB, NT, ncores = 2, 32, 8
    nc, _ = build_program(NB, NT, "ABC")
    maps = make_in_maps(inputs, NB, NT, ncores, "ABC")
    res = run_bass_kernel_spmd(nc, maps, core_ids=list(range(ncores)))
    outs = [r["out"].reshape(NB, NT * 128, D) for r in res.results]
    return np.concatenate(outs, axis=0).astype(np.float32)
```

```python
import numpy as np
import concourse.bass as bass
import concourse.mybir as mybir
from contextlib import ExitStack

F32 = mybir.dt.float32
BF16 = mybir.dt.bfloat16
I32 = mybir.dt.int32
U32 = mybir.dt.uint32
ALU = mybir.AluOpType
AF = mybir.ActivationFunctionType
AX = mybir.AxisListType

ENGS = ("pe", "act", "dve", "pool", "sp")


class T:
    __slots__ = ("name", "ap", "writers", "readers", "dsem", "dcount", "last_dma_read")

    def __init__(self, name, ap=None):
        self.name = name
        self.ap = ap
        self.writers = []
        self.readers = []
        self.dsem = None
        self.dcount = 0
        self.last_dma_read = None

    def __getitem__(self, k):
        return self.ap[k]


class Op:
    __slots__ = ("eng", "fn", "seq", "deps", "dma", "dsem", "dval", "signal", "sigval")

    def __init__(self, eng, fn):
        self.eng = eng
        self.fn = fn
        self.seq = None
        self.deps = []
        self.dma = False
        self.dsem = None
        self.dval = 0
        self.signal = False
        self.sigval = 0


class Prog:
    def __init__(self, nc):
        self.nc = nc
        self.stack = ExitStack()
        self.ops = []
        self.per_eng = {e: [] for e in ENGS}
        self.nsem = 0
        self.esem = {}
        for e in ENGS:
            if e != "sp":
                self.esem[e] = self.sem("s_" + e)
        self.stores = []
        self.scopes = []
        self.last_dma = {}

    def open_scope(self):
        self.scopes.append(ExitStack())

    def close_scope(self):
        self.barrier()
        self.scopes.pop().close()

    def barrier(self):
        last_c = []
        for e in ENGS:
            for o in reversed(self.per_eng[e]):
                if not o.dma and o.fn is not None:
                    last_c.append(o)
                    break
        deps = last_c + list(self.last_dma.values())
        for e in ENGS:
            op = Op(e, None)
            op.seq = len(self.per_eng[e])
            op.deps = list(deps)
            self.ops.append(op)
            self.per_eng[e].append(op)

    def sem(self, name):
        self.nsem += 1
        return self.stack.enter_context(self.nc.semaphore(name))

    def sb(self, name, shape, dt):
        stk = self.scopes[-1] if self.scopes else self.stack
        t = stk.enter_context(self.nc.sbuf_tensor(name, list(shape), dt))
        return T(name, t)

    def ps(self, name, shape, dt):
        t = self.stack.enter_context(self.nc.psum_tensor(name, list(shape), dt))
        return T(name, t)

    def dram(self, name, shape, dt, kind="Internal"):
        return self.nc.dram_tensor(name, list(shape), dt, kind=kind).ap()

    def _rec(self, eng, fn, reads, writes, parts=(), dma_tile=None, dma_is_read=False):
        op = Op(eng, fn)
        op.seq = len(self.per_eng[eng])
        deps = []
        for t in reads:
            deps.extend(t.writers)
        for t in writes:
            if t.readers:
                deps.extend(t.readers)
                deps.extend(t.writers)
                t.writers = [op]
                t.readers = []
            else:
                deps.extend(t.writers)
                t.writers = [op]
        for t in parts:
            if t.readers:
                deps.extend(t.readers)
                deps.extend(t.writers)
                t.writers = [op]
                t.readers = []
            else:
                t.writers = t.writers + [op]
        for t in reads:
            t.readers.append(op)
        if dma_tile is not None:
            op.dma = True
            if dma_tile.dsem is None:
                dma_tile.dsem = self.sem("d_" + dma_tile.name)
            if dma_is_read and dma_tile.last_dma_read is not None:
                deps.append(dma_tile.last_dma_read)
            dma_tile.dcount += 1
            op.dsem = dma_tile.dsem
            op.dval = 16 * dma_tile.dcount
            dma_tile.last_dma_read = op if dma_is_read else None
            self.last_dma[id(op.dsem)] = op
        op.deps = [d for d in deps if d is not op]
        self.ops.append(op)
        self.per_eng[eng].append(op)
        return op

    def op(self, eng, fn, reads=(), writes=(), parts=()):
        return self._rec(eng, fn, reads, writes, parts)

    def dma(self, out, in_, reads=(), writes=(), parts=(), sem_tile=None, is_read=False, eng="sp", **kw):
        def fn(e):
            return e.dma_start(out=out, in_=in_, **kw)
        return self._rec(eng, fn, reads, writes, parts, dma_tile=sem_tile, dma_is_read=is_read)

    def load(self, dst_tile, dst_ap, src_ap, part=False, dram_reads=(), eng="sp", **kw):
        if part:
            return self.dma(dst_ap, src_ap, reads=dram_reads, parts=(dst_tile,), sem_tile=dst_tile, eng=eng, **kw)
        return self.dma(dst_ap, src_ap, reads=dram_reads, writes=(dst_tile,), sem_tile=dst_tile, eng=eng, **kw)

    def store(self, dst_ap, src_tile, src_ap, dram_writes=(), dram_parts=(), final=False, eng="sp", **kw):
        o = self.dma(dst_ap, src_ap, reads=(src_tile,), writes=dram_writes, parts=dram_parts,
                     sem_tile=src_tile, is_read=True, eng=eng, **kw)
        if final:
            self.stores.append(o)
        return o

    def emit(self):
        nc = self.nc
        fin = Op("sp", None)
        fin.seq = len(self.per_eng["sp"])
        fin.deps = list(self.stores)
        self.ops.append(fin)
        self.per_eng["sp"].append(fin)

        clock = {e: {f: -1 for f in ENGS} for e in ENGS}
        dclock = {e: {} for e in ENGS}
        waits = {}
        for op in self.ops:
            X = op.eng
            need_e = {}
            need_d = {}
            for d in op.deps:
                if d.dma:
                    key = id(d.dsem)
                    if dclock[X].get(key, 0) >= d.dval:
                        continue
                    cur = need_d.get(key)
                    if cur is None or cur[1] < d.dval:
                        need_d[key] = (d.dsem, d.dval)
                else:
                    Y = d.eng
                    if Y == X and X == "pe":
                        continue
                    if clock[X][Y] >= d.seq:
                        continue
                    if need_e.get(Y, -1) < d.seq:
                        need_e[Y] = d.seq
            wl = []
            for Y, s in need_e.items():
                clock[X][Y] = s
                tgt = self.per_eng[Y][s]
                tgt.signal = True
                wl.append(("e", Y, tgt))
            for key, (sem, val) in need_d.items():
                dclock[X][key] = val
                wl.append(("d", sem, val))
            waits[id(op)] = wl
        for e in ENGS:
            c = 0
            for op in self.per_eng[e]:
                if op.signal and not op.dma:
                    c += 1
                    op.sigval = c
        self.sigmax = {e: max([o.sigval for o in self.per_eng[e]] + [0]) for e in ENGS}
        engobj = {"pe": "tensor", "act": "scalar", "dve": "vector", "pool": "gpsimd", "sp": "sync"}
        with nc.Block() as block:
            for e in ENGS:
                ops_e = self.per_eng[e]
                if not ops_e:
                    continue

                def body(eng, ops_e=ops_e, e=e):
                    for op in ops_e:
                        for w in waits[id(op)]:
                            if w[0] == "e":
                                eng.wait_ge(self.esem[w[1]], w[2].sigval)
                            else:
                                eng.wait_ge(w[1], w[2])
                        if op.fn is None:
                            continue
                        ins = op.fn(eng)
                        if op.dma:
                            ins.then_inc(op.dsem, 16)
                        elif op.signal:
                            ins.then_inc(self.esem[e], 1)

                getattr(block, engobj[e])(body)
        self.stack.close()


D = 1024
IN_W = 6416
EPS = 1e-6
NEG = -30000.0
TWO_PI = 6.283185


def host_consts():
    c = {}
    c["identf"] = np.eye(128, dtype=np.float32)
    k = np.arange(128)[:, None]
    q = np.arange(128)[None, :]
    m_prev = np.where(k > q, 0.0, NEG).astype(np.float32)
    m_cur = np.where(k <= q, 0.0, NEG).astype(np.float32)
    c["amask"] = np.stack([np.tile(m_prev, (1, 4)), np.tile(m_cur, (1, 4))], axis=1).astype(np.float32)
    c["cmask"] = np.tile((k <= q).astype(np.float32), (1, 4))
    invf = (500000.0 ** (-np.arange(0, 16, 2, dtype=np.float32) / 16.0)).astype(np.float32)
    c["invf"] = np.tile((invf / (2 * np.pi)).astype(np.float32)[None, :], (128, 1))
    onesab = np.zeros((128, 2, 128), np.float32)
    onesab[:, 0, 0:64] = 1.0
    onesab[:, 1, 64:128] = 1.0
    c["onesab"] = onesab
    c["iota128"] = np.tile(np.arange(128, dtype=np.float32)[None, :], (128, 1))
    return c


CONST_SHAPES = {"identf": [128, 128], "amask": [128, 2, 512], "cmask": [128, 512], "invf": [128, 8],
                "onesab": [128, 2, 128], "iota128": [128, 128]}


class Ctx:
    pass


def build_program(NB, NT, phases="ABC", dbg=False):
    NTT = NB * NT
    TOK = NTT * 128
    nc = bass.Bass("TRN2", target_bir_lowering=False)
    P = Prog(nc)
    c = Ctx()
    c.nc, c.P, c.NB, c.NT, c.NTT, c.TOK = nc, P, NB, NT, NTT, TOK
    c.phases = phases

    def din(name, shape, dt=F32):
        return nc.dram_tensor(name, list(shape), dt, kind="ExternalInput").ap()

    c.x = din("x", [TOK, D])
    c.pos = din("positions", [TOK], I32)
    c.norm1_gain = din("norm1_gain", [1, D])
    c.w_in = din("w_in", [D, IN_W])
    c.ml_i_bias = din("ml_i_bias", [1, 8])
    c.ml_f_bias = din("ml_f_bias", [1, 8])
    c.q_norm_gain = din("q_norm_gain", [1, 64])
    c.k_norm_gain = din("k_norm_gain", [1, 64])
    c.attn_sinks = din("attn_sinks", [1, 16])
    c.ml_out_norm_gain = din("ml_out_norm_gain", [1, D])
    c.w_branch_attn = din("w_branch_attn", [D, D])
    c.w_branch_mlstm = din("w_branch_mlstm", [D, D])
    c.w_out = din("w_out", [D, D])
    if "C" in phases:
        c.norm2_gain = din("norm2_gain", [1, D])
        c.peer_w_query = din("peer_w_query", [D, D])
        c.peer_sub_keys = din("peer_sub_keys", [8, 2, 128, 64])
        c.peer_down = din("peer_down", [16384, D])
        c.peer_up = din("peer_up", [16384, D])
    c.cst = {k: din("c_" + k, s) for k, s in CONST_SHAPES.items()}
    c.out = nc.dram_tensor("out", [TOK, D], F32, kind="ExternalOutput").ap()
    if dbg:
        c.mixa = nc.dram_tensor("mixa_s", [TOK, D], F32, kind="ExternalOutput").ap()
    else:
        c.mixa = P.dram("mixa_s", [TOK, D], F32)
    c.dbg = dbg
    c.dbg_outs = {}
    c.mixa_t = [T("mixa%d" % i) for i in range(NTT)]
    c.x1_t = [T("x1_%d" % i) for i in range(NTT)]

    c.identf = P.sb("identf", [128, 128], F32)
    c.identb = P.sb("identb", [128, 128], BF16)
    P.load(c.identf, c.identf[:], c.cst["identf"])
    P.op("dve", lambda e: e.tensor_copy(out=c.identb[:], in_=c.identf[:]), reads=[c.identf], writes=[c.identb])
    c.bank = [P.ps("bank%d" % i, [128, 512], F32) for i in range(8)]

    for ph, fn in (("A", phase_a), ("B", phase_b), ("C", phase_c)):
        if ph in phases:
            P.open_scope()
            fn(c)
            P.close_scope()
    P.emit()
    return nc, P


def rms_rstd(c, pfx, ssq, n, rstd, tmp):
    P = c.P
    P.op("dve", lambda e: e.tensor_scalar(out=tmp[:], in0=ssq[:], scalar1=1.0 / n, scalar2=EPS, op0=ALU.mult, op1=ALU.add),
         reads=[ssq], writes=[tmp])
    P.op("act", lambda e: e.activation(out=tmp[:], in_=tmp[:], func=AF.Sqrt), reads=[tmp], writes=[tmp])
    P.op("dve", lambda e: e.reciprocal(out=rstd[:], in_=tmp[:]), reads=[tmp], writes=[rstd])


def norm_and_transpose(c, xs, gain, hb, hT, junk, st, ptr_bank):
    P = c.P
    ssq, tmp, rstd = st
    P.op("act", lambda e: e.activation(out=junk[:], in_=xs[:], func=AF.Square, accum_out=ssq[:]),
         reads=[xs], writes=[junk, ssq])
    rms_rstd(c, "n", ssq, D, rstd, tmp)
    P.op("dve", lambda e: e.scalar_tensor_tensor(out=hb[:], in0=xs[:], scalar=rstd[:], in1=gain[:], op0=ALU.mult, op1=ALU.mult),
         reads=[xs, rstd, gain], writes=[hb])
    transpose8(c, hb, hT, ptr_bank)


def transpose8(c, src, dst, ptr_bank, eng="act"):
    P = c.P
    pv = ptr_bank[:].bitcast(BF16)
    for k in range(8):
        P.op("pe", lambda e, k=k: e.transpose(out=pv[:, k * 128:(k + 1) * 128], in_=src[:, k * 128:(k + 1) * 128], identity=c.identb[:]),
             reads=[src, c.identb], writes=[ptr_bank] if k == 0 else [], parts=[] if k == 0 else [ptr_bank])
    if eng == "act":
        P.op("act", lambda e: e.copy(out=dst[:].rearrange("p k t -> p (k t)"), in_=pv), reads=[ptr_bank], writes=[dst])
    else:
        P.op(eng, lambda e: e.tensor_copy(out=dst[:].rearrange("p k t -> p (k t)"), in_=pv), reads=[ptr_bank], writes=[dst])


def load_w_bf16(c, dst, col0, ncols, src, dcol0=0):
    P = c.P
    for k in range(8):
        P.load(dst, dst[:, k, dcol0:dcol0 + ncols], src[k * 128:(k + 1) * 128, col0:col0 + ncols], part=True, eng="pool",
               max_dma_last_dim=4096)


def rope_tables(c):
    P = c.P
    NTT = c.NTT
    posi = P.sb("posi", [128, NTT], I32)
    P.load(posi, posi[:], c.pos.rearrange("(n p) -> p n", p=128), allow_slow_non_contiguous=True)
    posf = P.sb("posf", [128, NTT], F32)
    P.op("dve", lambda e: e.tensor_copy(out=posf[:], in_=posi[:]), reads=[posi], writes=[posf])
    invf = P.sb("invf", [128, 8], F32)
    P.load(invf, invf[:], c.cst["invf"])
    y = P.sb("rope_y", [128, NTT, 16], F32)
    yi = P.sb("rope_yi", [128, NTT, 16], I32)
    yf = P.sb("rope_yf", [128, NTT, 16], F32)
    cs = P.sb("rope_cs", [128, NTT, 16], F32)
    pb = posf[:].unsqueeze(2).to_broadcast([128, NTT, 8])
    ib = invf[:].unsqueeze(1).to_broadcast([128, NTT, 8])
    P.op("dve", lambda e: e.tensor_tensor(out=y[:, :, 8:16], in0=pb, in1=ib, op=ALU.mult), reads=[posf, invf], writes=[y])
    P.op("dve", lambda e: e.tensor_scalar(out=y[:, :, 0:8], in0=y[:, :, 8:16], scalar1=0.25, scalar2=None, op0=ALU.add),
         reads=[y], writes=[y])
    P.op("dve", lambda e: e.tensor_copy(out=yi[:], in_=y[:]), reads=[y], writes=[yi])
    P.op("dve", lambda e: e.tensor_copy(out=yf[:], in_=yi[:]), reads=[yi], writes=[yf])
    P.op("dve", lambda e: e.tensor_tensor(out=y[:], in0=y[:], in1=yf[:], op=ALU.subtract), reads=[y, yf], writes=[y])
    P.op("act", lambda e: e.activation(out=cs[:], in_=y[:], func=AF.Sin, scale=TWO_PI), reads=[y], writes=[cs])
    return cs


def qk_norm_rope(c, pfx, src, nh, gain, cs_n, outb, tmps):
    P = c.P
    sq, ssq, tmp, rstd, qn, r1, r2 = tmps
    W = nh * 64
    s3 = src[:, 0:W].rearrange("p (h d) -> p h d", d=64)
    P.op("pool", lambda e: e.tensor_tensor(out=sq[:, 0:W], in0=src[:, 0:W], in1=src[:, 0:W], op=ALU.mult), reads=[src], writes=[sq])
    P.op("dve", lambda e: e.tensor_reduce(out=ssq[:, 0:nh], in_=sq[:, 0:W].rearrange("p (h d) -> p h d", d=64), axis=AX.X, op=ALU.add),
         reads=[sq], writes=[ssq])
    P.op("dve", lambda e: e.tensor_scalar(out=tmp[:, 0:nh], in0=ssq[:, 0:nh], scalar1=1.0 / 64, scalar2=EPS, op0=ALU.mult, op1=ALU.add),
         reads=[ssq], writes=[tmp])
    P.op("act", lambda e: e.activation(out=tmp[:, 0:nh], in_=tmp[:, 0:nh], func=AF.Sqrt), reads=[tmp], writes=[tmp])
    P.op("dve", lambda e: e.reciprocal(out=rstd[:, 0:nh], in_=tmp[:, 0:nh]), reads=[tmp], writes=[rstd])
    q3 = qn[:, 0:W].rearrange("p (h d) -> p h d", d=64)
    P.op("dve", lambda e: e.tensor_tensor(out=q3, in0=s3, in1=rstd[:, 0:nh].unsqueeze(2).to_broadcast([128, nh, 64]), op=ALU.mult),
         reads=[src, rstd], writes=[qn])
    P.op("pool", lambda e: e.tensor_tensor(out=q3, in0=q3, in1=gain[:].unsqueeze(1).to_broadcast([128, nh, 64]), op=ALU.mult),
         reads=[qn, gain], writes=[qn])
    P.op("act", lambda e: e.copy(out=outb[:], in_=q3), reads=[qn], writes=[outb])
    cosb = cs_n[:, 0:8].unsqueeze(1).to_broadcast([128, nh, 8])
    sinb = cs_n[:, 8:16].unsqueeze(1).to_broadcast([128, nh, 8])
    a3 = r1[:, 0:nh * 8].rearrange("p (h d) -> p h d", d=8)
    b3 = r2[:, 0:nh * 8].rearrange("p (h d) -> p h d", d=8)
    cst = c.cs
    P.op("dve", lambda e: e.tensor_tensor(out=a3, in0=q3[:, :, 0:8], in1=cosb, op=ALU.mult), reads=[qn, cst], writes=[r1])
    P.op("dve", lambda e: e.tensor_tensor(out=b3, in0=q3[:, :, 8:16], in1=sinb, op=ALU.mult), reads=[qn, cst], writes=[r2])
    P.op("dve", lambda e: e.tensor_tensor(out=outb[:, :, 0:8], in0=a3, in1=b3, op=ALU.subtract), reads=[r1, r2, outb], writes=[outb])
    P.op("dve", lambda e: e.tensor_tensor(out=a3, in0=q3[:, :, 8:16], in1=cosb, op=ALU.mult), reads=[qn, cst], writes=[r1])
    P.op("dve", lambda e: e.tensor_tensor(out=b3, in0=q3[:, :, 0:8], in1=sinb, op=ALU.mult), reads=[qn, cst], writes=[r2])
    P.op("dve", lambda e: e.tensor_tensor(out=outb[:, :, 8:16], in0=a3, in1=b3, op=ALU.add), reads=[r1, r2, outb], writes=[outb])


def phase_a(c):
    P, NB, NT, NTT = c.P, c.NB, c.NT, c.NTT
    bank = c.bank
    c.g1 = P.sb("a_g1", [128, D], F32)
    P.load(c.g1, c.g1[:], c.norm1_gain[0].partition_broadcast(128))
    wA = P.sb("wA", [128, 8, 2304], BF16)
    load_w_bf16(c, wA, 0, 1280, c.w_in, 0)
    load_w_bf16(c, wA, 4368, 1024, c.w_in, 1280)
    wba = P.sb("wba", [128, 8, D], BF16)
    load_w_bf16(c, wba, 0, D, c.w_branch_attn)
    gq = P.sb("gq", [128, 64], F32)
    gk = P.sb("gk", [128, 64], F32)
    P.load(gq, gq[:], c.q_norm_gain[0].partition_broadcast(128))
    P.load(gk, gk[:], c.k_norm_gain[0].partition_broadcast(128))
    sk = P.sb("sk", [128, 16], F32)
    P.load(sk, sk[:], c.attn_sinks[0].partition_broadcast(128))
    sinkp = P.sb("sinkp", [128, 8], F32)
    sk3 = sk[:].rearrange("p (i two) -> p i two", two=2)
    P.op("dve", lambda e: e.tensor_copy(out=sinkp[0:64, :], in_=sk3[0:64, :, 0]), reads=[sk], writes=[sinkp])
    P.op("dve", lambda e: e.tensor_copy(out=sinkp[64:128, :], in_=sk3[64:128, :, 1]), reads=[sk], parts=[sinkp])
    P.op("act", lambda e: e.activation(out=sinkp[:], in_=sinkp[:], func=AF.Exp), reads=[sinkp], writes=[sinkp])
    amaskf = P.sb("amaskf", [128, 2, 512], F32)
    P.load(amaskf, amaskf[:], c.cst["amask"])
    amask = P.sb("amask", [128, 2, 512], BF16)
    P.op("dve", lambda e: e.tensor_copy(out=amask[:], in_=amaskf[:]), reads=[amaskf], writes=[amask])
    onesf = P.sb("onesf", [128, 2, 128], F32)
    P.load(onesf, onesf[:], c.cst["onesab"])
    onesab = P.sb("onesab", [128, 2, 128], BF16)
    P.op("dve", lambda e: e.tensor_copy(out=onesab[:], in_=onesf[:]), reads=[onesf], writes=[onesab])
    c.cs = rope_tables(c)

    xs = [P.sb("a_xs%d" % i, [128, D], F32) for i in range(2)]
    kT = [P.sb("a_kT%d" % i, [128, 2, 128], BF16) for i in range(2)]
    vA = [P.sb("a_vA%d" % i, [128, 2, 128], BF16) for i in range(2)]
    vB = [P.sb("a_vB%d" % i, [128, 2, 128], BF16) for i in range(2)]
    for i in range(2):
        P.op("pool", lambda e, i=i: e.memset(vA[i][:], 0.0), writes=[vA[i]])
        P.op("pool", lambda e, i=i: e.memset(vB[i][:], 0.0), writes=[vB[i]])
    SETS = []
    for si in range(2):
        sx = "s%d_" % si
        junk = P.sb(sx + "a_junk", [128, D], BF16)
        st = (P.sb(sx + "a_ssq", [128, 1], F32), P.sb(sx + "a_tmp", [128, 1], F32), P.sb(sx + "a_rstd", [128, 1], F32))
        hb = P.sb(sx + "a_hb", [128, D], BF16)
        hT = P.sb(sx + "a_hT", [128, 8, 128], BF16)
        qf = P.sb(sx + "a_qf", [128, D], F32)
        kvf = P.sb(sx + "a_kvf", [128, 256], F32)
        sga = P.sb(sx + "a_sga", [128, D], F32)
        tmps = (P.sb(sx + "a_sq", [128, D], F32), P.sb(sx + "a_ssq16", [128, 16], F32), P.sb(sx + "a_tmp16", [128, 16], F32),
                P.sb(sx + "a_rstd16", [128, 16], F32), P.sb(sx + "a_qn", [128, D], F32), P.sb(sx + "a_r1", [128, 128], F32), P.sb(sx + "a_r2", [128, 128], F32))
        qb = P.sb(sx + "a_qb", [128, 16, 64], BF16)
        kb = P.sb(sx + "a_kb", [128, 2, 64], BF16)
        kdup = P.sb(sx + "a_kdup", [128, 2, 2, 64], BF16)
        qT = P.sb(sx + "a_qT", [128, 8, 128], BF16)
        PT = [[[P.sb(sx + "a_PT%d%d%d" % (g, k, h), [128, 4, 128], BF16) for h in range(2)] for k in range(2)] for g in range(2)]
        rden = P.sb(sx + "a_rden", [128, 4, 128], F32)
        attT = P.sb(sx + "a_attT", [128, 8, 128], BF16)

        SETS.append((junk, st, hb, hT, qf, kvf, sga, tmps, qb, kb, kdup, qT, PT, rden, attT))
    mo = [P.sb("a_mo%d" % i, [128, D], F32) for i in range(2)]

    def load_x(n):
        P.load(xs[n % 2], xs[n % 2][:], c.x[n * 128:(n + 1) * 128, :])

    load_x(0)

    def tile(n):
        j = n % NT
        cur = n % 2
        (junk, st, hb, hT, qf, kvf, sga, tmps, qb, kb, kdup, qT, PT, rden, attT) = SETS[n % 2]
        prv = 1 - cur
        if n + 1 < NTT:
            load_x(n + 1)
        x_t = xs[cur]
        norm_and_transpose(c, x_t, c.g1, hb, hT, junk, st, bank[7])
        def proj(bk, col0, ncols):
            for k in range(8):
                P.op("pe", lambda e, k=k: e.matmul(out=bk[:, 0:ncols], lhsT=hT[:, k, :], rhs=wA[:, k, col0:col0 + ncols],
                                                   start=(k == 0), stop=(k == 7)),
                     reads=[hT, wA], writes=[bk] if k == 0 else [], parts=[] if k == 0 else [bk])
        proj(bank[0], 0, 512)
        P.op("dve", lambda e: e.tensor_copy(out=qf[:, 0:512], in_=bank[0][:]), reads=[bank[0]], writes=[qf])
        proj(bank[1], 512, 512)
        P.op("act", lambda e: e.copy(out=qf[:, 512:1024], in_=bank[1][:]), reads=[bank[1]], parts=[qf])
        proj(bank[2], 1024, 256)
        P.op("dve", lambda e: e.tensor_copy(out=kvf[:], in_=bank[2][:, 0:256]), reads=[bank[2]], writes=[kvf])
        proj(bank[3], 1280, 512)
        P.op("act", lambda e: e.activation(out=sga[:, 0:512], in_=bank[3][:], func=AF.Sigmoid), reads=[bank[3]], writes=[sga])
        proj(bank[4], 1792, 512)
        P.op("act", lambda e: e.activation(out=sga[:, 512:1024], in_=bank[4][:], func=AF.Sigmoid), reads=[bank[4]], parts=[sga])
        cs_n = c.cs[:, n, :]
        qk_norm_rope(c, "q", qf, 16, gq, cs_n, qb, tmps)
        qk_norm_rope(c, "k", kvf, 2, gk, cs_n, kb, tmps)
        P.op("pool", lambda e: e.tensor_copy(out=kdup[:, :, 0, :], in_=kb[:]), reads=[kb], writes=[kdup])
        P.op("pool", lambda e: e.tensor_copy(out=kdup[:, :, 1, :], in_=kb[:]), reads=[kb], parts=[kdup])
        v3 = kvf[:, 128:256].rearrange("p (g d) -> p g d", d=64)
        P.op("pool", lambda e: e.tensor_copy(out=vA[cur][:, :, 0:64], in_=v3), reads=[kvf], writes=[vA[cur]])
        P.op("pool", lambda e: e.tensor_copy(out=vB[cur][:, :, 64:128], in_=v3), reads=[kvf], writes=[vB[cur]])
        pv = bank[7][:].bitcast(BF16)
        qflat = qb[:].rearrange("p h d -> p (h d)")
        for k in range(8):
            P.op("pe", lambda e, k=k: e.transpose(out=pv[:, k * 128:(k + 1) * 128], in_=qflat[:, k * 128:(k + 1) * 128], identity=c.identb[:]),
                 reads=[qb, c.identb], writes=[bank[7]] if k == 0 else [], parts=[] if k == 0 else [bank[7]])
        P.op("act", lambda e: e.copy(out=qT[:].rearrange("p k t -> p (k t)"), in_=pv), reads=[bank[7]], writes=[qT])
        pv6 = bank[6][:].bitcast(BF16)
        kflat = kdup[:].rearrange("p g u d -> p (g u d)")
        for g in range(2):
            P.op("pe", lambda e, g=g: e.transpose(out=pv6[:, g * 128:(g + 1) * 128], in_=kflat[:, g * 128:(g + 1) * 128], identity=c.identb[:]),
                 reads=[kdup, c.identb], writes=[bank[6]] if g == 0 else [], parts=[] if g == 0 else [bank[6]])
        P.op("dve", lambda e: e.tensor_copy(out=kT[cur][:].rearrange("p g t -> p (g t)"), in_=pv6[:, 0:256]), reads=[bank[6]], writes=[kT[cur]])
        kbs = [1] if j == 0 else [0, 1]
        bi = 0
        for g in range(2):
            for kk in kbs:
                slot = cur if kk == 1 else prv
                for hh in range(2):
                    bk = bank[bi % 6]
                    bi += 1
                    P.op("pe", lambda e, bk=bk, slot=slot, g=g, hh=hh: e.matmul(
                        out=bk[:], lhsT=kT[slot][hh * 64:(hh + 1) * 64, g, :],
                        rhs=qT[hh * 64:(hh + 1) * 64, 4 * g:4 * g + 4, :], start=True, stop=False),
                        reads=[kT[slot], qT], writes=[bk])
                    P.op("pe", lambda e, bk=bk, kk=kk: e.matmul(out=bk[:], lhsT=c.identb[:], rhs=amask[:, kk, :], start=False, stop=True),
                         reads=[c.identb, amask], parts=[bk])
                    pt = PT[g][kk][hh]
                    P.op("act", lambda e, bk=bk, pt=pt: e.activation(out=pt[:].rearrange("p i t -> p (i t)"), in_=bk[:], func=AF.Exp, scale=0.125),
                         reads=[bk], writes=[pt])
        for g in range(2):
            pav = bank[6]
            pden = bank[7]
            first_av = True
            for p in range(4):
                combos = [(kk, hh) for kk in kbs for hh in range(2)]
                for ci, (kk, hh) in enumerate(combos):
                    slot = cur if kk == 1 else prv
                    vt = vA[slot] if hh == 0 else vB[slot]
                    pt = PT[g][kk][hh]
                    w_first = first_av
                    P.op("pe", lambda e, vt=vt, pt=pt, p=p, g=g, ci=ci, ncmb=len(combos): e.matmul(
                        out=pav[:, p * 128:(p + 1) * 128], lhsT=vt[:, g, :], rhs=pt[:, p, :], start=(ci == 0), stop=(ci == ncmb - 1)),
                        reads=[vt, pt], writes=[pav] if w_first else [], parts=[] if w_first else [pav])
                    P.op("pe", lambda e, pt=pt, p=p, hh=hh, ci=ci, ncmb=len(combos): e.matmul(
                        out=pden[:, p * 128:(p + 1) * 128], lhsT=onesab[:, hh, :], rhs=pt[:, p, :], start=(ci == 0), stop=(ci == ncmb - 1)),
                        reads=[onesab, pt], writes=[pden] if w_first else [], parts=[] if w_first else [pden])
                    first_av = False
            P.op("dve", lambda e, g=g: e.tensor_tensor(out=rden[:], in0=pden[:].rearrange("p (i t) -> p i t", t=128),
                                                       in1=sinkp[:, 4 * g:4 * g + 4].unsqueeze(2).to_broadcast([128, 4, 128]), op=ALU.add),
                 reads=[pden, sinkp], writes=[rden])
            P.op("dve", lambda e: e.reciprocal(out=rden[:], in_=rden[:]), reads=[rden], writes=[rden])
            P.op("dve", lambda e, g=g: e.tensor_tensor(out=attT[:, 4 * g:4 * g + 4, :], in0=pav[:].rearrange("p (i t) -> p i t", t=128),
                                                       in1=rden[:], op=ALU.mult),
                 reads=[pav, rden], writes=[attT] if g == 0 else [], parts=[] if g == 0 else [attT])
        m_t = mo[n % 2]
        for hn in range(2):
            bk = bank[hn]
            for i in range(8):
                P.op("pe", lambda e, bk=bk, i=i, hn=hn: e.matmul(out=bk[:], lhsT=attT[:, i, :], rhs=wba[:, i, hn * 512:(hn + 1) * 512],
                                                               start=(i == 0), stop=(i == 7)),
                     reads=[attT, wba], writes=[bk] if i == 0 else [], parts=[] if i == 0 else [bk])
            P.op("dve", lambda e, bk=bk, hn=hn: e.tensor_tensor(out=m_t[:, hn * 512:(hn + 1) * 512], in0=bk[:], in1=sga[:, hn * 512:(hn + 1) * 512], op=ALU.mult),
                 reads=[bk, sga], writes=[m_t] if hn == 0 else [], parts=[] if hn == 0 else [m_t])
        P.store(c.mixa[n * 128:(n + 1) * 128, :], m_t, m_t[:], dram_writes=[c.mixa_t[n]], final=c.dbg)
        if c.dbg and n == c.dbg - 1:
            for nm, tl, shp, dt in (("qb", qb, [128, 1024], BF16), ("kb", kb, [128, 128], BF16), ("attT", attT, [128, 1024], BF16),
                                    ("qf", qf, [128, 1024], F32), ("sga", sga, [128, 1024], F32), ("hT", hT, [128, 1024], BF16),
                                    ("PT", PT[0][1][0], [128, 512], BF16), ("rden", rden, [128, 512], F32), ("qT", qT, [128, 1024], BF16),
                                    ("kT", kT[cur], [128, 256], BF16), ("kdup", kdup, [128, 256], BF16), ("vA", vA[cur], [128, 256], BF16)):
                d_ap = c.nc.dram_tensor("dbg_" + nm, shp, dt, kind="ExternalOutput").ap()
                flat = tl[:]
                if len(flat.shape) == 3:
                    flat = flat.rearrange("p a b -> p (a b)")
                if len(flat.shape) == 4:
                    flat = flat.rearrange("p a b c -> p (a b c)")
                P.store(d_ap, tl, flat, final=True)

    for n in range(NTT):
        tile(n)


def phase_b(c):
    P, NB, NT, NTT = c.P, c.NB, c.NT, c.NTT
    bank = c.bank
    c.g1 = P.sb("b_g1", [128, D], F32)
    P.load(c.g1, c.g1[:], c.norm1_gain[0].partition_broadcast(128))
    wB = P.sb("wB", [128, 8, 4112], BF16)
    load_w_bf16(c, wB, 1280, 3088, c.w_in, 0)
    load_w_bf16(c, wB, 5392, 1024, c.w_in, 3088)
    wbm = P.sb("wbm", [128, 8, D], BF16)
    load_w_bf16(c, wbm, 0, D, c.w_branch_mlstm)
    wout = P.sb("wout", [128, 8, D], BF16)
    load_w_bf16(c, wout, 0, D, c.w_out)
    mlg = P.sb("mlg", [128, D], F32)
    P.load(mlg, mlg[:], c.ml_out_norm_gain[0].partition_broadcast(128))
    cmask = P.sb("cmask", [128, 512], F32)
    P.load(cmask, cmask[:], c.cst["cmask"])
    bif = P.sb("b_bif", [8, 2], F32)
    P.load(bif, bif[:, 0:1], c.ml_i_bias.rearrange("o h -> h o"), allow_slow_non_contiguous=True)
    P.load(bif, bif[:, 1:2], c.ml_f_bias.rearrange("o h -> h o"), part=True, allow_slow_non_contiguous=True)
    P.op("dve", lambda e: e.tensor_scalar(out=bif[:], in0=bif[:], scalar1=1.0 / 15.0, scalar2=None, op0=ALU.mult), reads=[bif], writes=[bif])

    xs = [P.sb("b_xs%d" % i, [128, D], F32) for i in range(2)]
    ma = [P.sb("b_ma%d" % i, [128, D], F32) for i in range(2)]
    junk = P.sb("b_junk", [128, D], BF16)
    st = (P.sb("b_ssq", [128, 1], F32), P.sb("b_tmp", [128, 1], F32), P.sb("b_rstd", [128, 1], F32))
    hb = P.sb("b_hb", [128, D], BF16)
    hT = P.sb("b_hT", [128, 8, 128], BF16)
    g_ti = P.sb("g_ti", [8, 128], F32)
    g_tf = P.sb("g_tf", [8, 128], F32)
    g_nl = P.sb("g_nl", [8, 128], F32)
    g_cum = P.sb("g_cum", [8, 128], F32)
    g_a = P.sb("g_a", [8, 128], F32)
    g_M = P.sb("g_M", [8, 128], F32)
    g_d2 = P.sb("g_d2", [8, 128], F32)
    g_ones = P.sb("g_ones", [8, 128], F32)
    P.op("pool", lambda e: e.memset(g_ones[:], 1.0), writes=[g_ones])
    g_out = [P.sb("g_out%d" % i, [8, 128], F32) for i in range(5)]
    cumc = [P.sb("g_cumc%d" % b, [8, 1], F32) for b in range(NB)]
    Mc = [P.sb("g_Mc%d" % b, [8, 1], F32) for b in range(NB)]
    g_nM0 = P.sb("g_nM0", [8, 1], F32)
    g_nMe = P.sb("g_nMe", [8, 1], F32)
    g_dd = P.sb("g_dd", [8, 1], F32)
    gtok = P.sb("b_gtok", [128, 5, 8], F32)
    qt = P.sb("b_qt", [128, 512], BF16)
    kt = P.sb("b_kt", [128, 512], BF16)
    khat = P.sb("b_khat", [128, 512], BF16)
    vaug = P.sb("b_vaug", [128, 8, 129], BF16)
    P.op("pool", lambda e: e.memset(vaug[:], 1.0), writes=[vaug])
    sgo = P.sb("b_sgo", [128, D], F32)
    sgm = P.sb("b_sgm", [128, D], F32)
    qtT = P.sb("b_qtT", [64, 8, 128], BF16)
    ktT = P.sb("b_ktT", [64, 8, 128], BF16)
    PTm = P.sb("b_PT", [128, 8, 128], BF16)
    C32 = [P.sb("b_C32_%d" % b, [64, 8, 129], F32) for b in range(NB)]
    Cb = [P.sb("b_Cb_%d" % b, [64, 8, 129], BF16) for b in range(NB)]
    dmax = P.sb("b_dmax", [128, 8], F32)
    rc = P.sb("b_rc", [128, 8], F32)
    hm = P.sb("b_hm", [128, 8, 128], F32)
    hsq = P.sb("b_hsq", [128, 8, 128], F32)
    hs8 = (P.sb("b_hssq", [128, 8], F32), P.sb("b_htmp", [128, 8], F32), P.sb("b_hrstd", [128, 8], F32))
    hn = P.sb("b_hn", [128, D], BF16)
    hnT = P.sb("b_hnT", [128, 8, 128], BF16)
    mx = P.sb("b_mx", [128, D], F32)
    mxb = P.sb("b_mxb", [128, D], BF16)
    mxT = P.sb("b_mxT", [128, 8, 128], BF16)
    xo = [P.sb("b_xo%d" % i, [128, D], F32) for i in range(2)]
    HG = [(0, 3), (3, 6), (6, 8)]

    def load_in(n):
        P.load(xs[n % 2], xs[n % 2][:], c.x[n * 128:(n + 1) * 128, :])
        P.load(ma[n % 2], ma[n % 2][:], c.mixa[n * 128:(n + 1) * 128, :], dram_reads=[c.mixa_t[n]])

    load_in(0)

    def tile(n):
        b = n // NT
        j = n % NT
        if n + 1 < NTT:
            load_in(n + 1)
        x_t = xs[n % 2]
        ma_t = ma[n % 2]
        norm_and_transpose(c, x_t, c.g1, hb, hT, junk, st, bank[7])

        def proj(bk, col0, ncols, M=128, ocol=0):
            for k in range(8):
                P.op("pe", lambda e, k=k: e.matmul(out=bk[0:M, ocol:ocol + ncols], lhsT=hT[:, k, :], rhs=wB[:, k, col0:col0 + ncols],
                                                   start=(k == 0), stop=(k == 7)),
                     reads=[hT, wB], writes=[bk] if (k == 0 and ocol == 0) else [], parts=[] if (k == 0 and ocol == 0) else [bk])
        for gi, col in enumerate((2048, 2056)):
            for k in range(8):
                P.op("pe", lambda e, k=k, gi=gi, col=col: e.matmul(out=bank[6][0:8, gi * 128:(gi + 1) * 128], lhsT=wB[:, k, col:col + 8], rhs=hT[:, k, :],
                                                                    start=(k == 0), stop=(k == 7)),
                     reads=[hT, wB], writes=[bank[6]] if (k == 0 and gi == 0) else [], parts=[] if (k == 0 and gi == 0) else [bank[6]])
        P.op("act", lambda e: e.activation(out=g_ti[:], in_=bank[6][0:8, 0:128], func=AF.Tanh, bias=bif[:, 0:1], scale=1.0 / 15.0),
             reads=[bank[6], bif], writes=[g_ti])
        P.op("act", lambda e: e.activation(out=g_tf[:], in_=bank[6][0:8, 128:256], func=AF.Tanh, bias=bif[:, 1:2], scale=1.0 / 15.0),
             reads=[bank[6], bif], writes=[g_tf])
        P.op("act", lambda e: e.activation(out=g_nl[:], in_=g_tf[:], func=AF.Exp, scale=-15.0), reads=[g_tf], writes=[g_nl])
        P.op("act", lambda e: e.activation(out=g_nl[:], in_=g_nl[:], func=AF.Ln, bias=1.0), reads=[g_nl], writes=[g_nl])
        if j == 0:
            P.op("dve", lambda e: e.tensor_tensor_scan(out=g_cum[:], data0=g_ones[:], data1=g_nl[:], initial=0.0, op0=ALU.mult, op1=ALU.add),
                 reads=[g_ones, g_nl], writes=[g_cum])
        else:
            P.op("dve", lambda e: e.tensor_tensor_scan(out=g_cum[:], data0=g_ones[:], data1=g_nl[:], initial=cumc[b][:], op0=ALU.mult, op1=ALU.add),
                 reads=[g_ones, g_nl, cumc[b]], writes=[g_cum])
        P.op("dve", lambda e: e.scalar_tensor_tensor(out=g_a[:], in0=g_ti[:], scalar=15.0, in1=g_cum[:], op0=ALU.mult, op1=ALU.add),
             reads=[g_ti, g_cum], writes=[g_a])
        if j == 0:
            P.op("dve", lambda e: e.memset(Mc[b][:], 0.0), writes=[Mc[b]])
        P.op("dve", lambda e: e.tensor_tensor_scan(out=g_M[:], data0=g_a[:], data1=g_a[:], initial=Mc[b][:], op0=ALU.max, op1=ALU.max),
             reads=[g_a, Mc[b]], writes=[g_M])
        P.op("dve", lambda e: e.tensor_scalar(out=g_nM0[:], in0=Mc[b][:], scalar1=-1.0, scalar2=None, op0=ALU.mult), reads=[Mc[b]], writes=[g_nM0])
        P.op("dve", lambda e: e.tensor_scalar(out=g_nMe[:], in0=g_M[:, 127:128], scalar1=-1.0, scalar2=None, op0=ALU.mult), reads=[g_M], writes=[g_nMe])
        P.op("dve", lambda e: e.tensor_tensor(out=g_dd[:], in0=Mc[b][:], in1=g_nMe[:], op=ALU.add), reads=[Mc[b], g_nMe], writes=[g_dd])
        P.op("dve", lambda e: e.tensor_tensor(out=g_d2[:], in0=g_cum[:], in1=g_M[:], op=ALU.subtract), reads=[g_cum, g_M], writes=[g_d2])
        P.op("act", lambda e: e.activation(out=g_out[0][:], in_=g_M[:], func=AF.Exp, bias=Mc[b][:], scale=-1.0), reads=[g_M, Mc[b]], writes=[g_out[0]])
        P.op("act", lambda e: e.activation(out=g_out[1][:], in_=g_a[:], func=AF.Exp, bias=g_nM0[:], scale=1.0), reads=[g_a, g_nM0], writes=[g_out[1]])
        P.op("act", lambda e: e.activation(out=g_out[2][:], in_=g_a[:], func=AF.Exp, bias=g_nMe[:], scale=1.0), reads=[g_a, g_nMe], writes=[g_out[2]])
        P.op("act", lambda e: e.activation(out=g_out[3][:], in_=g_a[:], func=AF.Exp, bias=g_dd[:], scale=0.0), reads=[g_a, g_dd], writes=[g_out[3]])
        P.op("act", lambda e: e.activation(out=g_out[4][:], in_=g_d2[:], func=AF.Exp), reads=[g_d2], writes=[g_out[4]])
        P.op("dve", lambda e: e.tensor_copy(out=cumc[b][:], in_=g_cum[:, 127:128]), reads=[g_cum], writes=[cumc[b]])
        P.op("dve", lambda e: e.tensor_copy(out=Mc[b][:], in_=g_M[:, 127:128]), reads=[g_M], writes=[Mc[b]])
        for qi in range(5):
            P.op("pe", lambda e, qi=qi: e.transpose(out=bank[6][:, 256 + qi * 8:256 + (qi + 1) * 8], in_=g_out[qi][:], identity=c.identf[0:8, 0:8]),
                 reads=[g_out[qi], c.identf], writes=[bank[6]] if qi == 0 else [], parts=[] if qi == 0 else [bank[6]])
        P.op("dve", lambda e: e.tensor_copy(out=gtok[:].rearrange("p q h -> p (q h)"), in_=bank[6][:, 256:296]), reads=[bank[6]], writes=[gtok])
        proj(bank[0], 0, 512)
        proj(bank[1], 512, 512)
        P.op("dve", lambda e: e.tensor_tensor(out=qt[:].rearrange("p (h d) -> p h d", d=64), in0=bank[0][:].rearrange("p (h d) -> p h d", d=64),
                                              in1=gtok[:, 0, :].unsqueeze(2).to_broadcast([128, 8, 64]), op=ALU.mult),
             reads=[bank[0], gtok], writes=[qt])
        P.op("dve", lambda e: e.scalar_tensor_tensor(out=kt[:].rearrange("p (h d) -> p h d", d=64), in0=bank[1][:].rearrange("p (h d) -> p h d", d=64),
                                                     scalar=0.125, in1=gtok[:, 1, :].unsqueeze(2).to_broadcast([128, 8, 64]), op0=ALU.mult, op1=ALU.mult),
             reads=[bank[1], gtok], writes=[kt])
        P.op("dve", lambda e: e.scalar_tensor_tensor(out=khat[:].rearrange("p (h d) -> p h d", d=64), in0=bank[1][:].rearrange("p (h d) -> p h d", d=64),
                                                     scalar=0.125, in1=gtok[:, 2, :].unsqueeze(2).to_broadcast([128, 8, 64]), op0=ALU.mult, op1=ALU.mult),
             reads=[bank[1], gtok], writes=[khat])
        for hv in range(2):
            proj(bank[2 + hv], 1024 + hv * 512, 512)
            P.op("act", lambda e, hv=hv: e.copy(out=vaug[:, 4 * hv:4 * hv + 4, 0:128], in_=bank[2 + hv][:].rearrange("p (h d) -> p h d", d=128)),
                 reads=[bank[2 + hv]], writes=[vaug] if hv == 0 else [], parts=[] if hv == 0 else [vaug])
        for hv in range(2):
            proj(bank[4 + hv], 2064 + hv * 512, 512)
            P.op("act", lambda e, hv=hv: e.activation(out=sgo[:, hv * 512:(hv + 1) * 512], in_=bank[4 + hv][:], func=AF.Sigmoid),
                 reads=[bank[4 + hv]], writes=[sgo] if hv == 0 else [], parts=[] if hv == 0 else [sgo])
        for hv in range(2):
            proj(bank[2 + hv], 3088 + hv * 512, 512)
            P.op("act", lambda e, hv=hv: e.activation(out=sgm[:, hv * 512:(hv + 1) * 512], in_=bank[2 + hv][:], func=AF.Sigmoid),
                 reads=[bank[2 + hv]], writes=[sgm] if hv == 0 else [], parts=[] if hv == 0 else [sgm])
        for (src, dstT, bk, eng) in ((qt, qtT, bank[7], "act"), (kt, ktT, bank[6], "dve")):
            pv = bk[:].bitcast(BF16)
            for h in range(8):
                P.op("pe", lambda e, h=h, src=src, pv=pv: e.transpose(out=pv[0:64, h * 128:(h + 1) * 128], in_=src[:, h * 64:(h + 1) * 64], identity=c.identb[:]),
                     reads=[src, c.identb], writes=[bk] if h == 0 else [], parts=[] if h == 0 else [bk])
            if eng == "act":
                P.op("act", lambda e, pv=pv, dstT=dstT: e.copy(out=dstT[:].rearrange("p h t -> p (h t)"), in_=pv[0:64, :]), reads=[bk], writes=[dstT])
            else:
                P.op("dve", lambda e, pv=pv, dstT=dstT: e.tensor_copy(out=dstT[:].rearrange("p h t -> p (h t)"), in_=pv[0:64, :]), reads=[bk], writes=[dstT])
        for hb4 in range(2):
            bk = bank[hb4]
            for hh in range(4):
                h = hb4 * 4 + hh
                P.op("pe", lambda e, h=h, hh=hh, bk=bk: e.matmul(out=bk[:, hh * 128:(hh + 1) * 128], lhsT=ktT[:, h, :], rhs=qtT[:, h, :], start=True, stop=True),
                     reads=[ktT, qtT], writes=[bk] if hh == 0 else [], parts=[] if hh == 0 else [bk])
            P.op("dve", lambda e, bk=bk, hb4=hb4: e.tensor_tensor(out=PTm[:, 4 * hb4:4 * hb4 + 4, :].rearrange("p h t -> p (h t)"), in0=bk[:], in1=cmask[:], op=ALU.mult),
                 reads=[bk, cmask], writes=[PTm] if hb4 == 0 else [], parts=[] if hb4 == 0 else [PTm])
        for gi, (h0, h1) in enumerate(HG):
            bk = bank[2 + gi]
            for h in range(h0, h1):
                o = (h - h0) * 129
                P.op("pe", lambda e, h=h, o=o, bk=bk: e.matmul(out=bk[:, o:o + 129], lhsT=PTm[:, h, :], rhs=vaug[:, h, :], start=True, stop=(j == 0)),
                     reads=[PTm, vaug], writes=[bk] if h == h0 else [], parts=[] if h == h0 else [bk])
                if j > 0:
                    P.op("pe", lambda e, h=h, o=o, bk=bk: e.matmul(out=bk[:, o:o + 129], lhsT=qtT[:, h, :], rhs=Cb[b][:, h, :], start=False, stop=True),
                         reads=[qtT, Cb[b]], parts=[bk])
        for gi, (h0, h1) in enumerate(HG):
            bk = bank[5 + gi]
            for h in range(h0, h1):
                o = (h - h0) * 129
                P.op("pe", lambda e, h=h, o=o, bk=bk: e.matmul(out=bk[0:64, o:o + 129], lhsT=khat[:, h * 64:(h + 1) * 64], rhs=vaug[:, h, :], start=True, stop=True),
                     reads=[khat, vaug], writes=[bk] if h == h0 else [], parts=[] if h == h0 else [bk])
        if j > 0:
            P.op("dve", lambda e: e.tensor_tensor(out=C32[b][:], in0=C32[b][:], in1=gtok[0:64, 3, :].unsqueeze(2).to_broadcast([64, 8, 129]), op=ALU.mult),
                 reads=[C32[b], gtok], writes=[C32[b]])
        for gi, (h0, h1) in enumerate(HG):
            bk = bank[5 + gi]
            nh = h1 - h0
            if j > 0:
                P.op("dve", lambda e, bk=bk, h0=h0, h1=h1, nh=nh: e.tensor_tensor(out=C32[b][:, h0:h1, :], in0=C32[b][:, h0:h1, :],
                                                                                  in1=bk[0:64, 0:nh * 129].rearrange("p (h v) -> p h v", v=129), op=ALU.add),
                     reads=[bk, C32[b]], writes=[C32[b]])
            else:
                P.op("dve", lambda e, bk=bk, h0=h0, h1=h1, nh=nh: e.tensor_copy(out=C32[b][:, h0:h1, :], in_=bk[0:64, 0:nh * 129].rearrange("p (h v) -> p h v", v=129)),
                     reads=[bk], writes=[C32[b]] if gi == 0 else [], parts=[] if gi == 0 else [C32[b]])
        P.op("act", lambda e: e.copy(out=Cb[b][:], in_=C32[b][:]), reads=[C32[b]], writes=[Cb[b]])
        for gi, (h0, h1) in enumerate(HG):
            bk = bank[2 + gi]
            nh = h1 - h0
            v3 = bk[:, 0:nh * 129].rearrange("p (h v) -> p h v", v=129)
            P.op("dve", lambda e, v3=v3, h0=h0, h1=h1: e.tensor_tensor(out=dmax[:, h0:h1].unsqueeze(2), in0=v3[:, :, 128:129], in1=gtok[:, 4, h0:h1].unsqueeze(2), op=ALU.max),
                 reads=[bk, gtok], writes=[dmax])
            P.op("dve", lambda e, v3=v3, h0=h0, h1=h1: e.scalar_tensor_tensor(out=dmax[:, h0:h1].unsqueeze(2), in0=v3[:, :, 128:129], scalar=-1.0, in1=dmax[:, h0:h1].unsqueeze(2), op0=ALU.mult, op1=ALU.max),
                 reads=[bk, dmax], writes=[dmax])
        P.op("dve", lambda e: e.reciprocal(out=rc[:], in_=dmax[:]), reads=[dmax], writes=[rc])
        for gi, (h0, h1) in enumerate(HG):
            bk = bank[2 + gi]
            nh = h1 - h0
            v3 = bk[:, 0:nh * 129].rearrange("p (h v) -> p h v", v=129)
            P.op("dve", lambda e, v3=v3, h0=h0, h1=h1, nh=nh: e.tensor_tensor(out=hm[:, h0:h1, :], in0=v3[:, :, 0:128],
                                                                              in1=rc[:, h0:h1].unsqueeze(2).to_broadcast([128, nh, 128]), op=ALU.mult),
                 reads=[bk, rc], writes=[hm] if gi == 0 else [], parts=[] if gi == 0 else [hm])
        P.op("pool", lambda e: e.tensor_tensor(out=hsq[:], in0=hm[:], in1=hm[:], op=ALU.mult), reads=[hm], writes=[hsq])
        P.op("dve", lambda e: e.tensor_reduce(out=hs8[0][:], in_=hsq[:], axis=AX.X, op=ALU.add), reads=[hsq], writes=[hs8[0]])
        rms_rstd(c, "h", hs8[0], 128, hs8[2], hs8[1])
        P.op("dve", lambda e: e.tensor_tensor(out=hm[:], in0=hm[:], in1=hs8[2][:].unsqueeze(2).to_broadcast([128, 8, 128]), op=ALU.mult),
             reads=[hm, hs8[2]], writes=[hm])
        hmf = hm[:].rearrange("p h v -> p (h v)")
        P.op("pool", lambda e: e.tensor_tensor(out=hmf, in0=hmf, in1=mlg[:], op=ALU.mult), reads=[hm, mlg], writes=[hm])
        P.op("dve", lambda e: e.tensor_tensor(out=hn[:], in0=hmf, in1=sgo[:], op=ALU.mult), reads=[hm, sgo], writes=[hn])
        transpose8(c, hn, hnT, bank[7])
        for hv in range(2):
            bk = bank[hv]
            for i in range(8):
                P.op("pe", lambda e, bk=bk, i=i, hv=hv: e.matmul(out=bk[:], lhsT=hnT[:, i, :], rhs=wbm[:, i, hv * 512:(hv + 1) * 512], start=(i == 0), stop=(i == 7)),
                     reads=[hnT, wbm], writes=[bk] if i == 0 else [], parts=[] if i == 0 else [bk])
            P.op("dve", lambda e, bk=bk, hv=hv: e.tensor_tensor(out=mx[:, hv * 512:(hv + 1) * 512], in0=bk[:], in1=sgm[:, hv * 512:(hv + 1) * 512], op=ALU.mult),
                 reads=[bk, sgm], writes=[mx] if hv == 0 else [], parts=[] if hv == 0 else [mx])
        P.op("pool", lambda e: e.tensor_tensor(out=mxb[:], in0=mx[:], in1=ma_t[:], op=ALU.add), reads=[mx, ma_t], writes=[mxb])
        transpose8(c, mxb, mxT, bank[6], eng="dve")
        xo_t = xo[n % 2]
        for hv in range(2):
            bk = bank[2 + hv]
            for i in range(8):
                P.op("pe", lambda e, bk=bk, i=i, hv=hv: e.matmul(out=bk[:], lhsT=mxT[:, i, :], rhs=wout[:, i, hv * 512:(hv + 1) * 512], start=(i == 0), stop=(i == 7)),
                     reads=[mxT, wout], writes=[bk] if i == 0 else [], parts=[] if i == 0 else [bk])
            P.op("dve", lambda e, bk=bk, hv=hv: e.tensor_tensor(out=xo_t[:, hv * 512:(hv + 1) * 512], in0=bk[:], in1=x_t[:, hv * 512:(hv + 1) * 512], op=ALU.add),
                 reads=[bk, x_t], writes=[xo_t] if hv == 0 else [], parts=[] if hv == 0 else [xo_t])
        P.store(c.out[n * 128:(n + 1) * 128, :], xo_t, xo_t[:], dram_writes=[c.x1_t[n]], final=("C" not in c.phases))

    for n in range(NTT):
        tile(n)


def phase_c(c):
    P, NB, NT, NTT = c.P, c.NB, c.NT, c.NTT
    nc = c.nc
    bank = c.bank
    GT = 3
    ex_s = P.dram("ex_s", [128, 128, 2 * D], BF16)
    downT_s = ex_s[:, :, 0:D]
    up_s = ex_s[:, :, D:2 * D]
    downT_t = [T("downT_t%d" % i) for i in range(128)]
    up_t = [T("up_t%d" % i) for i in range(16)]
    up_v = c.peer_up.rearrange("(i c) d -> c i d", c=128)
    dn_v = c.peer_down.rearrange("(i c) d -> c i d", c=128)
    dummy = T("up_dma_sem")
    for u in range(16):
        P.dma(up_s[u * 8:(u + 1) * 8], up_v[u * 8:(u + 1) * 8], writes=[up_t[u]], sem_tile=dummy, eng="pool", max_dma_last_dim=4096)
    P.open_scope()
    dsrc = [P.sb("c_dsrc%d" % i, [128, D], BF16) for i in range(2)]
    dtr = [P.sb("c_dtr%d" % i, [128, D], BF16) for i in range(2)]
    for cb in range(128):
        sl = cb % 2
        P.load(dsrc[sl], dsrc[sl][:], dn_v[cb], eng="pool", max_dma_last_dim=4096)
        pv = bank[6 + sl][:].bitcast(BF16)
        for k in range(8):
            P.op("pe", lambda e, k=k, sl=sl, pv=pv: e.transpose(out=pv[:, k * 128:(k + 1) * 128], in_=dsrc[sl][:, k * 128:(k + 1) * 128], identity=c.identb[:]),
                 reads=[dsrc[sl], c.identb], writes=[bank[6 + sl]] if k == 0 else [], parts=[] if k == 0 else [bank[6 + sl]])
        if sl == 0:
            P.op("act", lambda e, sl=sl, pv=pv: e.copy(out=dtr[sl][:], in_=pv), reads=[bank[6 + sl]], writes=[dtr[sl]])
        else:
            P.op("dve", lambda e, sl=sl, pv=pv: e.tensor_copy(out=dtr[sl][:], in_=pv), reads=[bank[6 + sl]], writes=[dtr[sl]])
        P.store(downT_s[cb], dtr[sl], dtr[sl][:], dram_writes=[downT_t[cb]])
    P.close_scope()
    cstage = c.dbg if (c.dbg and c.phases == "C") else 99
    if cstage == 1:
        return

    wq = P.sb("c_wq", [128, 8, D], BF16)
    load_w_bf16(c, wq, 0, D, c.peer_w_query)
    g2 = P.sb("c_g2", [128, D], F32)
    P.load(g2, g2[:], c.norm2_gain[0].partition_broadcast(128))
    iof = P.sb("c_iof", [128, 128], F32)
    P.load(iof, iof[:], c.cst["iota128"])
    iob = P.sb("c_iob", [128, 128], BF16)
    P.op("dve", lambda e: e.tensor_copy(out=iob[:], in_=iof[:]), reads=[iof], writes=[iob])
    skT = P.sb("c_skT", [128, 8, 128], BF16)
    P.open_scope()
    skf = P.sb("c_skf", [128, 16, 64], F32)
    P.load(skf, skf[:], c.peer_sub_keys.rearrange("h p k d -> k (h p) d"))
    skb = P.sb("c_skb", [128, 16 * 64], BF16)
    P.op("dve", lambda e: e.tensor_copy(out=skb[:], in_=skf[:].rearrange("k a d -> k (a d)")), reads=[skf], writes=[skb])
    transpose8(c, skb, skT, bank[7])
    P.close_scope()

    if cstage == 2:
        return
    x1s = [P.sb("c_x1s%d" % i, [128, D], F32) for i in range(GT)]
    st = (P.sb("c_ssq", [128, 1], F32), P.sb("c_tmp", [128, 1], F32), P.sb("c_rstd", [128, 1], F32))
    xb = P.sb("c_xb", [128, D], BF16)
    xnT = P.sb("c_xnT", [128, 8, GT * 128], BF16)
    qT = P.sb("c_qT", [128, 8, 128], BF16)
    sc = P.sb("c_sc", [128, 16, 128], F32)
    sc_t = [T("sc_t%d" % i) for i in range(16)]
    v16 = P.sb("c_v16", [128, 16, 16], F32)
    ix = P.sb("c_ix", [128, 16, 16], U32)
    ixf = P.sb("c_ixf", [128, 16, 16], F32)
    cand = P.sb("c_cand", [128, 8, 256], F32)
    cand_t = [T("cand_t%d" % i) for i in range(8)]
    v16b, ixb, c16b, posb = T("v16b"), T("ixb"), T("c16b"), T("posb")
    c16 = P.sb("c_c16", [128, 8, 16], F32)
    pos = P.sb("c_pos", [128, 8, 16], U32)
    posf = P.sb("c_posf", [128, 8, 16], F32)
    pki = P.sb("c_pki", [128, 8, 16], I32)
    pkf = P.sb("c_pkf", [128, 8, 16], F32)
    qkf = P.sb("c_qkf", [128, 8, 16], F32)
    decs = [P.sb("c_dec%d" % i, [128, 2, 16, 16], F32) for i in range(4)]
    abg = [P.sb("c_abg%d" % i, [128, 8, 16], F32) for i in range(3)]
    z8 = P.sb("c_z8", [128, 8], F32)
    abgT = [P.sb("c_abgT%d" % i, [128, GT * 128], F32) for i in range(3)]
    CH = 8
    Ach = [P.sb("c_Ach%d" % i, [128, CH, 128], BF16) for i in range(2)]
    Bch = [P.sb("c_Bch%d" % i, [128, CH, 128], BF16) for i in range(2)]
    Wg = P.sb("c_Wg", [128, 128, GT * 128], BF16)
    NS = 4
    esl = [P.sb("c_esl%d" % i, [128, 2 * D], BF16) for i in range(NS)]
    actb = [P.sb("c_act%d" % i, [128, GT * 128], BF16) for i in range(2)]
    wab = [P.sb("c_wa%d" % i, [128, GT * 128], BF16) for i in range(2)]

    groups = []
    n0 = 0
    while n0 < NTT:
        g = min(GT, NTT - n0)
        groups.append((n0, g))
        n0 += g
    blk_ctr = [0]

    def route_tile(n, ti):
        x_t = x1s[ti]
        src_x1 = c.x if c.phases == "C" else c.out
        P.load(x_t, x_t[:], src_x1[n * 128:(n + 1) * 128, :], dram_reads=[c.x1_t[n]])
        ssq, tmp, rstd = st
        P.op("act", lambda e: e.activation(out=xb[:], in_=x_t[:], func=AF.Square, accum_out=ssq[:]), reads=[x_t], writes=[xb, ssq])
        rms_rstd(c, "n", ssq, D, rstd, tmp)
        P.op("dve", lambda e: e.scalar_tensor_tensor(out=xb[:], in0=x_t[:], scalar=rstd[:], in1=g2[:], op0=ALU.mult, op1=ALU.mult),
             reads=[x_t, rstd, g2], writes=[xb])
        pvx = bank[7][:].bitcast(BF16)
        for k in range(8):
            P.op("pe", lambda e, k=k: e.transpose(out=pvx[:, k * 128:(k + 1) * 128], in_=xb[:, k * 128:(k + 1) * 128], identity=c.identb[:]),
                 reads=[xb, c.identb], writes=[bank[7]] if k == 0 else [], parts=[] if k == 0 else [bank[7]])
        xt1 = xnT[:, :, ti * 128:(ti + 1) * 128]
        P.op("act", lambda e: e.copy(out=xt1, in_=pvx.rearrange("p (k t) -> p k t", t=128)), reads=[bank[7]], writes=[xnT] if ti == 0 else [], parts=[] if ti == 0 else [xnT])
        if cstage == 29:
            return
        for hd in range(8):
            bk = bank[hd % 2]
            for k in range(8):
                P.op("pe", lambda e, k=k, hd=hd, bk=bk: e.matmul(out=bk[:, 0:128], lhsT=wq[:, k, hd * 128:(hd + 1) * 128], rhs=xt1[:, k, :],
                                                                start=(k == 0), stop=(k == 7)),
                     reads=[wq, xnT], writes=[bk] if k == 0 else [], parts=[] if k == 0 else [bk])
            if hd % 2 == 0:
                P.op("act", lambda e, hd=hd, bk=bk: e.copy(out=qT[:, hd, :], in_=bk[:, 0:128]), reads=[bk], writes=[qT] if hd == 0 else [], parts=[] if hd == 0 else [qT])
            else:
                P.op("dve", lambda e, hd=hd, bk=bk: e.tensor_copy(out=qT[:, hd, :], in_=bk[:, 0:128]), reads=[bk], parts=[qT])
        if cstage == 30:
            return
        sc4 = sc[:].rearrange("p (h two) k -> p h two k", two=2)
        for par in range(2):
            for hf in range(2):
                bk = bank[2 + par * 2 + hf]
                for u in range(4):
                    hd = hf * 4 + u
                    P.op("pe", lambda e, u=u, hd=hd, par=par, bk=bk: e.matmul(out=bk[:, u * 128:(u + 1) * 128], lhsT=qT[par * 64:(par + 1) * 64, hd, :],
                                                                          rhs=skT[par * 64:(par + 1) * 64, hd, :], start=True, stop=True),
                         reads=[qT, skT], writes=[bk] if u == 0 else [], parts=[] if u == 0 else [bk])
        for par in range(2):
            for hf in range(2):
                bk = bank[2 + par * 2 + hf]
                first = (par == 0 and hf == 0)
                if hf == 0:
                    P.op("act", lambda e, par=par, hf=hf, bk=bk: e.copy(out=sc4[:, hf * 4:hf * 4 + 4, par, :], in_=bk[:].rearrange("p (a k) -> p a k", k=128)),
                         reads=[bk], writes=[sc_t[(hf * 4 + u_) * 2 + par] for u_ in range(4)])
                else:
                    P.op("dve", lambda e, par=par, hf=hf, bk=bk: e.tensor_copy(out=sc4[:, hf * 4:hf * 4 + 4, par, :], in_=bk[:].rearrange("p (a k) -> p a k", k=128)),
                         reads=[bk], writes=[sc_t[(hf * 4 + u_) * 2 + par] for u_ in range(4)])
        if cstage in (31, 305, 306, 307):
            return
        for hp in range(16):
            P.op("dve", lambda e, hp=hp: e.max(out=v16[:, hp, 0:8], in_=sc[:, hp, :]), reads=[sc_t[hp]], writes=[v16] if hp == 0 else [], parts=[] if hp == 0 else [v16])
        for hp in range(16):
            P.op("dve", lambda e, hp=hp: e.max_index(out=ix[:, hp, 0:8], in_max=v16[:, hp, 0:8], in_values=sc[:, hp, :]), reads=[sc_t[hp], v16],
                 writes=[ix] if hp == 0 else [], parts=[] if hp == 0 else [ix])
        for hp in range(16):
            P.op("dve", lambda e, hp=hp: e.match_replace(out=sc[:, hp, :], in_to_replace=v16[:, hp, 0:8], in_values=sc[:, hp, :], imm_value=-1e30),
                 reads=[sc_t[hp], v16], writes=[sc_t[hp]])
        for hp in range(16):
            P.op("dve", lambda e, hp=hp: e.max(out=v16[:, hp, 8:16], in_=sc[:, hp, :]), reads=[sc_t[hp]], writes=[v16b] if hp == 0 else [], parts=[] if hp == 0 else [v16b])
        for hp in range(16):
            P.op("dve", lambda e, hp=hp: e.max_index(out=ix[:, hp, 8:16], in_max=v16[:, hp, 8:16], in_values=sc[:, hp, :]), reads=[sc_t[hp], v16b],
                 writes=[ixb] if hp == 0 else [], parts=[] if hp == 0 else [ixb])
        if cstage == 32:
            return
        P.op("dve", lambda e: e.tensor_copy(out=ixf[:], in_=ix[:]), reads=[ix, ixb], writes=[ixf])
        vv = v16[:].rearrange("p (h two) k -> p h two k", two=2)
        iv = ixf[:].rearrange("p (h two) k -> p h two k", two=2)
        P.op("dve", lambda e: e.tensor_tensor(out=cand[:].rearrange("p h (a b) -> p h a b", b=16),
                                              in0=vv[:, :, 0, :].unsqueeze(3).to_broadcast([128, 8, 16, 16]),
                                              in1=vv[:, :, 1, :].unsqueeze(2).to_broadcast([128, 8, 16, 16]), op=ALU.add),
             reads=[v16, v16b], writes=cand_t)
        for h in range(8):
            P.op("dve", lambda e, h=h: e.max(out=c16[:, h, 0:8], in_=cand[:, h, :]), reads=[cand_t[h]], writes=[c16] if h == 0 else [], parts=[] if h == 0 else [c16])
        for h in range(8):
            P.op("dve", lambda e, h=h: e.max_index(out=pos[:, h, 0:8], in_max=c16[:, h, 0:8], in_values=cand[:, h, :]), reads=[cand_t[h], c16],
                 writes=[pos] if h == 0 else [], parts=[] if h == 0 else [pos])
        for h in range(8):
            P.op("dve", lambda e, h=h: e.match_replace(out=cand[:, h, :], in_to_replace=c16[:, h, 0:8], in_values=cand[:, h, :], imm_value=-1e30),
                 reads=[cand_t[h], c16], writes=[cand_t[h]])
        for h in range(8):
            P.op("dve", lambda e, h=h: e.max(out=c16[:, h, 8:16], in_=cand[:, h, :]), reads=[cand_t[h]], writes=[c16b] if h == 0 else [], parts=[] if h == 0 else [c16b])
        for h in range(8):
            P.op("dve", lambda e, h=h: e.max_index(out=pos[:, h, 8:16], in_max=c16[:, h, 8:16], in_values=cand[:, h, :]), reads=[cand_t[h], c16b],
                 writes=[posb] if h == 0 else [], parts=[] if h == 0 else [posb])
        if cstage == 33:
            return
        P.op("dve", lambda e: e.tensor_copy(out=posf[:], in_=pos[:]), reads=[pos, posb], writes=[posf])
        P.op("dve", lambda e: e.tensor_scalar(out=pkf[:], in0=posf[:], scalar1=-7.5, scalar2=0.0625, op0=ALU.add, op1=ALU.mult), reads=[posf], writes=[pkf])
        P.op("dve", lambda e: e.tensor_copy(out=pki[:], in_=pkf[:]), reads=[pkf], writes=[pki])
        P.op("dve", lambda e: e.tensor_copy(out=pkf[:], in_=pki[:]), reads=[pki], writes=[pkf])
        P.op("dve", lambda e: e.scalar_tensor_tensor(out=qkf[:], in0=pkf[:], scalar=-16.0, in1=posf[:], op0=ALU.mult, op1=ALU.add), reads=[pkf, posf], writes=[qkf])
        combos = [(hh, which, sel, dst) for hh in range(4) for (which, sel, dst) in ((0, pkf, abg[0]), (1, qkf, abg[1]))]
        for half in range(2):
            sub = combos[half * 4:(half + 1) * 4]
            for di, (hh, which, sel, dst) in enumerate(sub):
                hs = slice(hh * 2, hh * 2 + 2)
                dec = decs[di]
                P.op("dve", lambda e, sel=sel, hs=hs, dec=dec: e.tensor_tensor(out=dec[:], in0=iof[:, 0:16].unsqueeze(1).unsqueeze(1).to_broadcast([128, 2, 16, 16]),
                                                                            in1=sel[:, hs, :].unsqueeze(3).to_broadcast([128, 2, 16, 16]), op=ALU.is_equal),
                     reads=[iof, sel], writes=[dec])
            for di, (hh, which, sel, dst) in enumerate(sub):
                hs = slice(hh * 2, hh * 2 + 2)
                dec = decs[di]
                P.op("pool", lambda e, which=which, hs=hs, dec=dec: e.tensor_tensor(out=dec[:], in0=dec[:], in1=iv[:, hs, which, :].unsqueeze(2).to_broadcast([128, 2, 16, 16]), op=ALU.mult),
                     reads=[dec, ixf], writes=[dec])
            for di, (hh, which, sel, dst) in enumerate(sub):
                hs = slice(hh * 2, hh * 2 + 2)
                dec = decs[di]
                firstw = (hh == 0)
                P.op("dve", lambda e, dst=dst, hs=hs, dec=dec: e.tensor_reduce(out=dst[:, hs, :], in_=dec[:], axis=AX.X, op=ALU.add),
                     reads=[dec], writes=[dst] if firstw else [], parts=[] if firstw else [dst])
        if cstage == 34:
            return
        P.op("dve", lambda e: e.tensor_tensor(out=abg[2][:], in0=c16[:], in1=c16[:, :, 0:1].to_broadcast([128, 8, 16]), op=ALU.subtract), reads=[c16, c16b], writes=[abg[2]])
        P.op("act", lambda e: e.activation(out=abg[2][:], in_=abg[2][:], func=AF.Exp), reads=[abg[2]], writes=[abg[2]])
        P.op("dve", lambda e: e.tensor_reduce(out=z8[:], in_=abg[2][:], axis=AX.X, op=ALU.add), reads=[abg[2]], writes=[z8])
        P.op("dve", lambda e: e.reciprocal(out=z8[:], in_=z8[:]), reads=[z8], writes=[z8])
        P.op("dve", lambda e: e.tensor_tensor(out=abg[2][:], in0=abg[2][:], in1=z8[:].unsqueeze(2).to_broadcast([128, 8, 16]), op=ALU.mult), reads=[abg[2], z8], writes=[abg[2]])
        if cstage == 35:
            return
        for i3 in range(3):
            P.op("pe", lambda e, i3=i3: e.transpose(out=bank[6][:, i3 * 128:(i3 + 1) * 128], in_=abg[i3][:].rearrange("p h k -> p (h k)"), identity=c.identf[:]),
                 reads=[abg[i3], c.identf], writes=[bank[6]] if i3 == 0 else [], parts=[] if i3 == 0 else [bank[6]])
        for i3 in range(3):
            P.op("act", lambda e, i3=i3: e.copy(out=abgT[i3][:, ti * 128:(ti + 1) * 128], in_=bank[6][:, i3 * 128:(i3 + 1) * 128]), reads=[bank[6]], parts=[abgT[i3]])
        if cstage == 36:
            return
        for q8 in range(0, 128, CH):
            chn = (q8 // CH) % 2
            tg8 = ti * 128 + q8
            io_bc = iof[:].unsqueeze(1).to_broadcast([128, CH, 128])
            P.op("dve", lambda e, chn=chn, tg8=tg8, io_bc=io_bc: e.tensor_tensor(out=Ach[chn][:], in0=io_bc,
                                                                             in1=abgT[0][:, tg8:tg8 + CH].unsqueeze(2).to_broadcast([128, CH, 128]), op=ALU.is_equal),
                 reads=[iof, abgT[0]], writes=[Ach[chn]])
            P.op("dve", lambda e, chn=chn, tg8=tg8, io_bc=io_bc: e.tensor_tensor(out=Bch[chn][:], in0=io_bc,
                                                                             in1=abgT[1][:, tg8:tg8 + CH].unsqueeze(2).to_broadcast([128, CH, 128]), op=ALU.is_equal),
                 reads=[iof, abgT[1]], writes=[Bch[chn]])
            P.op("pool", lambda e, chn=chn, tg8=tg8: e.tensor_tensor(out=Bch[chn][:], in0=Bch[chn][:],
                                                                   in1=abgT[2][:, tg8:tg8 + CH].unsqueeze(2).to_broadcast([128, CH, 128]), op=ALU.mult),
                 reads=[Bch[chn], abgT[2]], writes=[Bch[chn]])
            for tq in range(q8, q8 + CH, 4):
                bk = bank[(tq // 4) % 2]
                for t in range(tq, tq + 4):
                    tl = t % CH
                    u = t - tq
                    P.op("pe", lambda e, chn=chn, tl=tl, u=u, bk=bk: e.matmul(out=bk[:, u * 128:(u + 1) * 128], lhsT=Ach[chn][:, tl, :], rhs=Bch[chn][:, tl, :], start=True, stop=True),
                         reads=[Ach[chn], Bch[chn]], writes=[bk] if u == 0 else [], parts=[] if u == 0 else [bk])
                tg0 = ti * 128 + tq
                P.op("act", lambda e, bk=bk, tg0=tg0: e.copy(out=Wg[:, :, tg0:tg0 + 4], in_=bk[:].rearrange("p (t c) -> p c t", c=128)),
                     reads=[bk], parts=[Wg])

    def expert_loop(n0, g):
        W = g * 128
        for cb in range(128):
            sl = blk_ctr[0] % NS
            blk_ctr[0] += 1
            P.load(esl[sl], esl[sl][:], ex_s[cb], dram_reads=[downT_t[cb], up_t[cb // 8]])
            sb_ = bank[6 + cb % 2]
            for k in range(8):
                P.op("pe", lambda e, k=k, sl=sl, sb_=sb_: e.matmul(out=sb_[:, 0:W], lhsT=esl[sl][:, k * 128:(k + 1) * 128], rhs=xnT[:, k, 0:W], start=(k == 0), stop=(k == 7)),
                     reads=[esl[sl], xnT], writes=[sb_] if k == 0 else [], parts=[] if k == 0 else [sb_])
            ab = actb[cb % 2]
            wb = wab[cb % 2]
            P.op("act", lambda e, ab=ab, sb_=sb_: e.activation(out=ab[:, 0:W], in_=sb_[:, 0:W], func=AF.Gelu), reads=[sb_], writes=[ab])
            P.op("dve", lambda e, ab=ab, wb=wb, cb=cb: e.tensor_tensor(out=wb[:, 0:W], in0=ab[:, 0:W], in1=Wg[:, cb, 0:W], op=ALU.mult), reads=[ab, Wg], writes=[wb])
            for tt in range(g):
                for hf in range(2):
                    bk = bank[tt * 2 + hf]
                    P.op("pe", lambda e, tt=tt, hf=hf, bk=bk, wb=wb, sl=sl, cb=cb: e.matmul(out=bk[:], lhsT=wb[:, tt * 128:(tt + 1) * 128], rhs=esl[sl][:, D + hf * 512:D + (hf + 1) * 512],
                                                                                      start=(cb == 0), stop=(cb == 127)),
                         reads=[wb, esl[sl]], writes=[bk] if cb == 0 else [], parts=[] if cb == 0 else [bk])
        for tt in range(g):
            n = n0 + tt
            x_t = x1s[tt]
            for hf in range(2):
                bk = bank[tt * 2 + hf]
                P.op("dve", lambda e, bk=bk, hf=hf, x_t=x_t: e.tensor_tensor(out=x_t[:, hf * 512:(hf + 1) * 512], in0=bk[:], in1=x_t[:, hf * 512:(hf + 1) * 512], op=ALU.add),
                     reads=[bk, x_t], writes=[x_t])
            P.store(c.out[n * 128:(n + 1) * 128, :], x_t, x_t[:], dram_writes=[c.x1_t[n]], final=True)

    for (n0, g) in groups:
        for ti in range(g):
            route_tile(n0 + ti, ti)
        if 3 <= cstage <= 40:
            return
        if cstage == 50:
            continue
        expert_loop(n0, g)


from concourse.bass_utils import run_bass_kernel_spmd

W_NAMES = ["norm1_gain", "w_in", "ml_i_bias", "ml_f_bias", "q_norm_gain", "k_norm_gain", "attn_sinks", "ml_out_norm_gain",
           "w_branch_attn", "w_branch_mlstm", "w_out", "norm2_gain", "peer_w_query", "peer_sub_keys", "peer_down", "peer_up"]
PEER_NAMES = ["norm2_gain", "peer_w_query", "peer_sub_keys", "peer_down", "peer_up"]


def make_in_maps(inputs, NB, NT, ncores, phases="ABC"):
    consts = host_consts()
    S = NT * 128
    maps = []
    shared = {}
    for k in W_NAMES:
        if "C" not in phases and k in PEER_NAMES:
            continue
        shared[k] = np.ascontiguousarray(inputs[k][0])
    for k, v in consts.items():
        shared["c_" + k] = v
    for ci in range(ncores):
        m = dict(shared)
        m["x"] = np.ascontiguousarray(inputs["x"][ci * NB:(ci + 1) * NB, :S]).reshape(NB * S, D)
        m["positions"] = np.ascontiguousarray(inputs["positions"][ci * NB:(ci + 1) * NB, :S]).reshape(NB * S).astype(np.int32)
        maps.append(m)
    return maps


def kernel(**inputs):
    NB, NT, ncores = 2, 32, 8
    nc, _ = build_program(NB, NT, "ABC")
    maps = make_in_maps(inputs, NB, NT, ncores, "ABC")
    res = run_bass_kernel_spmd(nc, maps, core_ids=list(range(ncores)))
    outs = [r["out"].reshape(NB, NT * 128, D) for r in res.results]
    return np.concatenate(outs, axis=0).astype(np.float32)
```

```python
import numpy as np
import concourse.bass as bass
import concourse.mybir as mybir
from contextlib import ExitStack

F32 = mybir.dt.float32
BF16 = mybir.dt.bfloat16
I32 = mybir.dt.int32
U32 = mybir.dt.uint32
ALU = mybir.AluOpType
AF = mybir.ActivationFunctionType
AX = mybir.AxisListType

ENGS = ("pe", "act", "dve", "pool", "sp")


class T:
    __slots__ = ("name", "ap", "writers", "readers", "dsem", "dcount", "last_dma_read")

    def __init__(self, name, ap=None):
        self.name = name
        self.ap = ap
        self.writers = []
        self.readers = []
        self.dsem = None
        self.dcount = 0
        self.last_dma_read = None

    def __getitem__(self, k):
        return self.ap[k]


class Op:
    __slots__ = ("eng", "fn", "seq", "deps", "dma", "dsem", "dval", "signal", "sigval")

    def __init__(self, eng, fn):
        self.eng = eng
        self.fn = fn
        self.seq = None
        self.deps = []
        self.dma = False
        self.dsem = None
        self.dval = 0
        self.signal = False
        self.sigval = 0


class Prog:
    def __init__(self, nc):
        self.nc = nc
        self.stack = ExitStack()
        self.ops = []
        self.per_eng = {e: [] for e in ENGS}
        self.nsem = 0
        self.esem = {}
        for e in ENGS:
            if e != "sp":
                self.esem[e] = self.sem("s_" + e)
        self.stores = []
        self.scopes = []
        self.last_dma = {}

    def open_scope(self):
        self.scopes.append(ExitStack())

    def close_scope(self):
        self.barrier()
        self.scopes.pop().close()

    def barrier(self):
        last_c = []
        for e in ENGS:
            for o in reversed(self.per_eng[e]):
                if not o.dma and o.fn is not None:
                    last_c.append(o)
                    break
        deps = last_c + list(self.last_dma.values())
        for e in ENGS:
            op = Op(e, None)
            op.seq = len(self.per_eng[e])
            op.deps = list(deps)
            self.ops.append(op)
            self.per_eng[e].append(op)

    def sem(self, name):
        self.nsem += 1
        return self.stack.enter_context(self.nc.semaphore(name))

    def sb(self, name, shape, dt):
        stk = self.scopes[-1] if self.scopes else self.stack
        t = stk.enter_context(self.nc.sbuf_tensor(name, list(shape), dt))
        return T(name, t)

    def ps(self, name, shape, dt):
        t = self.stack.enter_context(self.nc.psum_tensor(name, list(shape), dt))
        return T(name, t)

    def dram(self, name, shape, dt, kind="Internal"):
        return self.nc.dram_tensor(name, list(shape), dt, kind=kind).ap()

    def _rec(self, eng, fn, reads, writes, parts=(), dma_tile=None, dma_is_read=False):
        op = Op(eng, fn)
        op.seq = len(self.per_eng[eng])
        deps = []
        for t in reads:
            deps.extend(t.writers)
        for t in writes:
            if t.readers:
                deps.extend(t.readers)
                deps.extend(t.writers)
                t.writers = [op]
                t.readers = []
            else:
                deps.extend(t.writers)
                t.writers = [op]
        for t in parts:
            if t.readers:
                deps.extend(t.readers)
                deps.extend(t.writers)
                t.writers = [op]
                t.readers = []
            else:
                t.writers = t.writers + [op]
        for t in reads:
            t.readers.append(op)
        if dma_tile is not None:
            op.dma = True
            if dma_tile.dsem is None:
                dma_tile.dsem = self.sem("d_" + dma_tile.name)
            if dma_is_read and dma_tile.last_dma_read is not None:
                deps.append(dma_tile.last_dma_read)
            dma_tile.dcount += 1
            op.dsem = dma_tile.dsem
            op.dval = 16 * dma_tile.dcount
            dma_tile.last_dma_read = op if dma_is_read else None
            self.last_dma[id(op.dsem)] = op
        op.deps = [d for d in deps if d is not op]
        self.ops.append(op)
        self.per_eng[eng].append(op)
        return op

    def op(self, eng, fn, reads=(), writes=(), parts=()):
        return self._rec(eng, fn, reads, writes, parts)

    def dma(self, out, in_, reads=(), writes=(), parts=(), sem_tile=None, is_read=False, eng="sp", **kw):
        def fn(e):
            return e.dma_start(out=out, in_=in_, **kw)
        return self._rec(eng, fn, reads, writes, parts, dma_tile=sem_tile, dma_is_read=is_read)

    def load(self, dst_tile, dst_ap, src_ap, part=False, dram_reads=(), eng="sp", **kw):
        if part:
            return self.dma(dst_ap, src_ap, reads=dram_reads, parts=(dst_tile,), sem_tile=dst_tile, eng=eng, **kw)
        return self.dma(dst_ap, src_ap, reads=dram_reads, writes=(dst_tile,), sem_tile=dst_tile, eng=eng, **kw)

    def store(self, dst_ap, src_tile, src_ap, dram_writes=(), dram_parts=(), final=False, eng="sp", **kw):
        o = self.dma(dst_ap, src_ap, reads=(src_tile,), writes=dram_writes, parts=dram_parts,
                     sem_tile=src_tile, is_read=True, eng=eng, **kw)
        if final:
            self.stores.append(o)
        return o

    def emit(self):
        nc = self.nc
        fin = Op("sp", None)
        fin.seq = len(self.per_eng["sp"])
        fin.deps = list(self.stores)
        self.ops.append(fin)
        self.per_eng["sp"].append(fin)

        clock = {e: {f: -1 for f in ENGS} for e in ENGS}
        dclock = {e: {} for e in ENGS}
        waits = {}
        for op in self.ops:
            X = op.eng
            need_e = {}
            need_d = {}
            for d in op.deps:
                if d.dma:
                    key = id(d.dsem)
                    if dclock[X].get(key, 0) >= d.dval:
                        continue
                    cur = need_d.get(key)
                    if cur is None or cur[1] < d.dval:
                        need_d[key] = (d.dsem, d.dval)
                else:
                    Y = d.eng
                    if Y == X and X == "pe":
                        continue
                    if clock[X][Y] >= d.seq:
                        continue
                    if need_e.get(Y, -1) < d.seq:
                        need_e[Y] = d.seq
            wl = []
            for Y, s in need_e.items():
                clock[X][Y] = s
                tgt = self.per_eng[Y][s]
                tgt.signal = True
                wl.append(("e", Y, tgt))
            for key, (sem, val) in need_d.items():
                dclock[X][key] = val
                wl.append(("d", sem, val))
            waits[id(op)] = wl
        for e in ENGS:
            c = 0
            for op in self.per_eng[e]:
                if op.signal and not op.dma:
                    c += 1
                    op.sigval = c
        self.sigmax = {e: max([o.sigval for o in self.per_eng[e]] + [0]) for e in ENGS}
        engobj = {"pe": "tensor", "act": "scalar", "dve": "vector", "pool": "gpsimd", "sp": "sync"}
        with nc.Block() as block:
            for e in ENGS:
                ops_e = self.per_eng[e]
                if not ops_e:
                    continue

                def body(eng, ops_e=ops_e, e=e):
                    for op in ops_e:
                        for w in waits[id(op)]:
                            if w[0] == "e":
                                eng.wait_ge(self.esem[w[1]], w[2].sigval)
                            else:
                                eng.wait_ge(w[1], w[2])
                        if op.fn is None:
                            continue
                        ins = op.fn(eng)
                        if op.dma:
                            ins.then_inc(op.dsem, 16)
                        elif op.signal:
                            ins.then_inc(self.esem[e], 1)

                getattr(block, engobj[e])(body)
        self.stack.close()


D = 1024
IN_W = 6416
EPS = 1e-6
NEG = -30000.0
TWO_PI = 6.283185


def host_consts():
    c = {}
    c["identf"] = np.eye(128, dtype=np.float32)
    k = np.arange(128)[:, None]
    q = np.arange(128)[None, :]
    m_prev = np.where(k > q, 0.0, NEG).astype(np.float32)
    m_cur = np.where(k <= q, 0.0, NEG).astype(np.float32)
    c["amask"] = np.stack([np.tile(m_prev, (1, 4)), np.tile(m_cur, (1, 4))], axis=1).astype(np.float32)
    c["cmask"] = np.tile((k <= q).astype(np.float32), (1, 4))
    invf = (500000.0 ** (-np.arange(0, 16, 2, dtype=np.float32) / 16.0)).astype(np.float32)
    c["invf"] = np.tile((invf / (2 * np.pi)).astype(np.float32)[None, :], (128, 1))
    onesab = np.zeros((128, 2, 128), np.float32)
    onesab[:, 0, 0:64] = 1.0
    onesab[:, 1, 64:128] = 1.0
    c["onesab"] = onesab
    c["iota128"] = np.tile(np.arange(128, dtype=np.float32)[None, :], (128, 1))
    return c


CONST_SHAPES = {"identf": [128, 128], "amask": [128, 2, 512], "cmask": [128, 512], "invf": [128, 8],
                "onesab": [128, 2, 128], "iota128": [128, 128]}


class Ctx:
    pass


def build_program(NB, NT, phases="ABC", dbg=False):
    NTT = NB * NT
    TOK = NTT * 128
    nc = bass.Bass("TRN2", target_bir_lowering=False)
    P = Prog(nc)
    c = Ctx()
    c.nc, c.P, c.NB, c.NT, c.NTT, c.TOK = nc, P, NB, NT, NTT, TOK
    c.phases = phases

    def din(name, shape, dt=F32):
        return nc.dram_tensor(name, list(shape), dt, kind="ExternalInput").ap()

    c.x = din("x", [TOK, D])
    c.pos = din("positions", [TOK], I32)
    c.norm1_gain = din("norm1_gain", [1, D])
    c.w_in = din("w_in", [D, IN_W])
    c.ml_i_bias = din("ml_i_bias", [1, 8])
    c.ml_f_bias = din("ml_f_bias", [1, 8])
    c.q_norm_gain = din("q_norm_gain", [1, 64])
    c.k_norm_gain = din("k_norm_gain", [1, 64])
    c.attn_sinks = din("attn_sinks", [1, 16])
    c.ml_out_norm_gain = din("ml_out_norm_gain", [1, D])
    c.w_branch_attn = din("w_branch_attn", [D, D])
    c.w_branch_mlstm = din("w_branch_mlstm", [D, D])
    c.w_out = din("w_out", [D, D])
    if "C" in phases:
        c.norm2_gain = din("norm2_gain", [1, D])
        c.peer_w_query = din("peer_w_query", [D, D])
        c.peer_sub_keys = din("peer_sub_keys", [8, 2, 128, 64])
        c.peer_down = din("peer_down", [16384, D])
        c.peer_up = din("peer_up", [16384, D])
    c.cst = {k: din("c_" + k, s) for k, s in CONST_SHAPES.items()}
    c.out = nc.dram_tensor("out", [TOK, D], F32, kind="ExternalOutput").ap()
    if dbg:
        c.mixa = nc.dram_tensor("mixa_s", [TOK, D], F32, kind="ExternalOutput").ap()
    else:
        c.mixa = P.dram("mixa_s", [TOK, D], F32)
    c.dbg = dbg
    c.dbg_outs = {}
    c.mixa_t = [T("mixa%d" % i) for i in range(NTT)]
    c.x1_t = [T("x1_%d" % i) for i in range(NTT)]

    c.identf = P.sb("identf", [128, 128], F32)
    c.identb = P.sb("identb", [128, 128], BF16)
    P.load(c.identf, c.identf[:], c.cst["identf"])
    P.op("dve", lambda e: e.tensor_copy(out=c.identb[:], in_=c.identf[:]), reads=[c.identf], writes=[c.identb])
    c.bank = [P.ps("bank%d" % i, [128, 512], F32) for i in range(8)]

    for ph, fn in (("A", phase_a), ("B", phase_b), ("C", phase_c)):
        if ph in phases:
            P.open_scope()
            fn(c)
            P.close_scope()
    P.emit()
    return nc, P


def rms_rstd(c, pfx, ssq, n, rstd, tmp):
    P = c.P
    P.op("dve", lambda e: e.tensor_scalar(out=tmp[:], in0=ssq[:], scalar1=1.0 / n, scalar2=EPS, op0=ALU.mult, op1=ALU.add),
         reads=[ssq], writes=[tmp])
    P.op("act", lambda e: e.activation(out=tmp[:], in_=tmp[:], func=AF.Sqrt), reads=[tmp], writes=[tmp])
    P.op("dve", lambda e: e.reciprocal(out=rstd[:], in_=tmp[:]), reads=[tmp], writes=[rstd])


def norm_and_transpose(c, xs, gain, hb, hT, junk, st, ptr_bank):
    P = c.P
    ssq, tmp, rstd = st
    P.op("act", lambda e: e.activation(out=junk[:], in_=xs[:], func=AF.Square, accum_out=ssq[:]),
         reads=[xs], writes=[junk, ssq])
    rms_rstd(c, "n", ssq, D, rstd, tmp)
    P.op("dve", lambda e: e.scalar_tensor_tensor(out=hb[:], in0=xs[:], scalar=rstd[:], in1=gain[:], op0=ALU.mult, op1=ALU.mult),
         reads=[xs, rstd, gain], writes=[hb])
    transpose8(c, hb, hT, ptr_bank)


def transpose8(c, src, dst, ptr_bank, eng="act"):
    P = c.P
    pv = ptr_bank[:].bitcast(BF16)
    for k in range(8):
        P.op("pe", lambda e, k=k: e.transpose(out=pv[:, k * 128:(k + 1) * 128], in_=src[:, k * 128:(k + 1) * 128], identity=c.identb[:]),
             reads=[src, c.identb], writes=[ptr_bank] if k == 0 else [], parts=[] if k == 0 else [ptr_bank])
    if eng == "act":
        P.op("act", lambda e: e.copy(out=dst[:].rearrange("p k t -> p (k t)"), in_=pv), reads=[ptr_bank], writes=[dst])
    else:
        P.op(eng, lambda e: e.tensor_copy(out=dst[:].rearrange("p k t -> p (k t)"), in_=pv), reads=[ptr_bank], writes=[dst])


def load_w_bf16(c, dst, col0, ncols, src, dcol0=0):
    P = c.P
    for k in range(8):
        P.load(dst, dst[:, k, dcol0:dcol0 + ncols], src[k * 128:(k + 1) * 128, col0:col0 + ncols], part=True, eng="pool",
               max_dma_last_dim=4096)


def rope_tables(c):
    P = c.P
    NTT = c.NTT
    posi = P.sb("posi", [128, NTT], I32)
    P.load(posi, posi[:], c.pos.rearrange("(n p) -> p n", p=128), allow_slow_non_contiguous=True)
    posf = P.sb("posf", [128, NTT], F32)
    P.op("dve", lambda e: e.tensor_copy(out=posf[:], in_=posi[:]), reads=[posi], writes=[posf])
    invf = P.sb("invf", [128, 8], F32)
    P.load(invf, invf[:], c.cst["invf"])
    y = P.sb("rope_y", [128, NTT, 16], F32)
    yi = P.sb("rope_yi", [128, NTT, 16], I32)
    yf = P.sb("rope_yf", [128, NTT, 16], F32)
    cs = P.sb("rope_cs", [128, NTT, 16], F32)
    pb = posf[:].unsqueeze(2).to_broadcast([128, NTT, 8])
    ib = invf[:].unsqueeze(1).to_broadcast([128, NTT, 8])
    P.op("dve", lambda e: e.tensor_tensor(out=y[:, :, 8:16], in0=pb, in1=ib, op=ALU.mult), reads=[posf, invf], writes=[y])
    P.op("dve", lambda e: e.tensor_scalar(out=y[:, :, 0:8], in0=y[:, :, 8:16], scalar1=0.25, scalar2=None, op0=ALU.add),
         reads=[y], writes=[y])
    P.op("dve", lambda e: e.tensor_copy(out=yi[:], in_=y[:]), reads=[y], writes=[yi])
    P.op("dve", lambda e: e.tensor_copy(out=yf[:], in_=yi[:]), reads=[yi], writes=[yf])
    P.op("dve", lambda e: e.tensor_tensor(out=y[:], in0=y[:], in1=yf[:], op=ALU.subtract), reads=[y, yf], writes=[y])
    P.op("act", lambda e: e.activation(out=cs[:], in_=y[:], func=AF.Sin, scale=TWO_PI), reads=[y], writes=[cs])
    return cs


def qk_norm_rope(c, pfx, src, nh, gain, cs_n, outb, tmps):
    P = c.P
    sq, ssq, tmp, rstd, qn, r1, r2 = tmps
    W = nh * 64
    s3 = src[:, 0:W].rearrange("p (h d) -> p h d", d=64)
    P.op("pool", lambda e: e.tensor_tensor(out=sq[:, 0:W], in0=src[:, 0:W], in1=src[:, 0:W], op=ALU.mult), reads=[src], writes=[sq])
    P.op("dve", lambda e: e.tensor_reduce(out=ssq[:, 0:nh], in_=sq[:, 0:W].rearrange("p (h d) -> p h d", d=64), axis=AX.X, op=ALU.add),
         reads=[sq], writes=[ssq])
    P.op("dve", lambda e: e.tensor_scalar(out=tmp[:, 0:nh], in0=ssq[:, 0:nh], scalar1=1.0 / 64, scalar2=EPS, op0=ALU.mult, op1=ALU.add),
         reads=[ssq], writes=[tmp])
    P.op("act", lambda e: e.activation(out=tmp[:, 0:nh], in_=tmp[:, 0:nh], func=AF.Sqrt), reads=[tmp], writes=[tmp])
    P.op("dve", lambda e: e.reciprocal(out=rstd[:, 0:nh], in_=tmp[:, 0:nh]), reads=[tmp], writes=[rstd])
    q3 = qn[:, 0:W].rearrange("p (h d) -> p h d", d=64)
    P.op("dve", lambda e: e.tensor_tensor(out=q3, in0=s3, in1=rstd[:, 0:nh].unsqueeze(2).to_broadcast([128, nh, 64]), op=ALU.mult),
         reads=[src, rstd], writes=[qn])
    P.op("pool", lambda e: e.tensor_tensor(out=q3, in0=q3, in1=gain[:].unsqueeze(1).to_broadcast([128, nh, 64]), op=ALU.mult),
         reads=[qn, gain], writes=[qn])
    P.op("act", lambda e: e.copy(out=outb[:], in_=q3), reads=[qn], writes=[outb])
    cosb = cs_n[:, 0:8].unsqueeze(1).to_broadcast([128, nh, 8])
    sinb = cs_n[:, 8:16].unsqueeze(1).to_broadcast([128, nh, 8])
    a3 = r1[:, 0:nh * 8].rearrange("p (h d) -> p h d", d=8)
    b3 = r2[:, 0:nh * 8].rearrange("p (h d) -> p h d", d=8)
    cst = c.cs
    P.op("dve", lambda e: e.tensor_tensor(out=a3, in0=q3[:, :, 0:8], in1=cosb, op=ALU.mult), reads=[qn, cst], writes=[r1])
    P.op("dve", lambda e: e.tensor_tensor(out=b3, in0=q3[:, :, 8:16], in1=sinb, op=ALU.mult), reads=[qn, cst], writes=[r2])
    P.op("dve", lambda e: e.tensor_tensor(out=outb[:, :, 0:8], in0=a3, in1=b3, op=ALU.subtract), reads=[r1, r2, outb], writes=[outb])
    P.op("dve", lambda e: e.tensor_tensor(out=a3, in0=q3[:, :, 8:16], in1=cosb, op=ALU.mult), reads=[qn, cst], writes=[r1])
    P.op("dve", lambda e: e.tensor_tensor(out=b3, in0=q3[:, :, 0:8], in1=sinb, op=ALU.mult), reads=[qn, cst], writes=[r2])
    P.op("dve", lambda e: e.tensor_tensor(out=outb[:, :, 8:16], in0=a3, in1=b3, op=ALU.add), reads=[r1, r2, outb], writes=[outb])


def phase_a(c):
    P, NB, NT, NTT = c.P, c.NB, c.NT, c.NTT
    bank = c.bank
    c.g1 = P.sb("a_g1", [128, D], F32)
    P.load(c.g1, c.g1[:], c.norm1_gain[0].partition_broadcast(128))
    wA = P.sb("wA", [128, 8, 2304], BF16)
    load_w_bf16(c, wA, 0, 1280, c.w_in, 0)
    load_w_bf16(c, wA, 4368, 1024, c.w_in, 1280)
    wba = P.sb("wba", [128, 8, D], BF16)
    load_w_bf16(c, wba, 0, D, c.w_branch_attn)
    gq = P.sb("gq", [128, 64], F32)
    gk = P.sb("gk", [128, 64], F32)
    P.load(gq, gq[:], c.q_norm_gain[0].partition_broadcast(128))
    P.load(gk, gk[:], c.k_norm_gain[0].partition_broadcast(128))
    sk = P.sb("sk", [128, 16], F32)
    P.load(sk, sk[:], c.attn_sinks[0].partition_broadcast(128))
    sinkp = P.sb("sinkp", [128, 8], F32)
    sk3 = sk[:].rearrange("p (i two) -> p i two", two=2)
    P.op("dve", lambda e: e.tensor_copy(out=sinkp[0:64, :], in_=sk3[0:64, :, 0]), reads=[sk], writes=[sinkp])
    P.op("dve", lambda e: e.tensor_copy(out=sinkp[64:128, :], in_=sk3[64:128, :, 1]), reads=[sk], parts=[sinkp])
    P.op("act", lambda e: e.activation(out=sinkp[:], in_=sinkp[:], func=AF.Exp), reads=[sinkp], writes=[sinkp])
    amaskf = P.sb("amaskf", [128, 2, 512], F32)
    P.load(amaskf, amaskf[:], c.cst["amask"])
    amask = P.sb("amask", [128, 2, 512], BF16)
    P.op("dve", lambda e: e.tensor_copy(out=amask[:], in_=amaskf[:]), reads=[amaskf], writes=[amask])
    onesf = P.sb("onesf", [128, 2, 128], F32)
    P.load(onesf, onesf[:], c.cst["onesab"])
    onesab = P.sb("onesab", [128, 2, 128], BF16)
    P.op("dve", lambda e: e.tensor_copy(out=onesab[:], in_=onesf[:]), reads=[onesf], writes=[onesab])
    c.cs = rope_tables(c)

    xs = [P.sb("a_xs%d" % i, [128, D], F32) for i in range(2)]
    kT = [P.sb("a_kT%d" % i, [128, 2, 128], BF16) for i in range(2)]
    vA = [P.sb("a_vA%d" % i, [128, 2, 128], BF16) for i in range(2)]
    vB = [P.sb("a_vB%d" % i, [128, 2, 128], BF16) for i in range(2)]
    for i in range(2):
        P.op("pool", lambda e, i=i: e.memset(vA[i][:], 0.0), writes=[vA[i]])
        P.op("pool", lambda e, i=i: e.memset(vB[i][:], 0.0), writes=[vB[i]])
    SETS = []
    for si in range(2):
        sx = "s%d_" % si
        junk = P.sb(sx + "a_junk", [128, D], BF16)
        st = (P.sb(sx + "a_ssq", [128, 1], F32), P.sb(sx + "a_tmp", [128, 1], F32), P.sb(sx + "a_rstd", [128, 1], F32))
        hb = P.sb(sx + "a_hb", [128, D], BF16)
        hT = P.sb(sx + "a_hT", [128, 8, 128], BF16)
        qf = P.sb(sx + "a_qf", [128, D], F32)
        kvf = P.sb(sx + "a_kvf", [128, 256], F32)
        sga = P.sb(sx + "a_sga", [128, D], F32)
        tmps = (P.sb(sx + "a_sq", [128, D], F32), P.sb(sx + "a_ssq16", [128, 16], F32), P.sb(sx + "a_tmp16", [128, 16], F32),
                P.sb(sx + "a_rstd16", [128, 16], F32), P.sb(sx + "a_qn", [128, D], F32), P.sb(sx + "a_r1", [128, 128], F32), P.sb(sx + "a_r2", [128, 128], F32))
        qb = P.sb(sx + "a_qb", [128, 16, 64], BF16)
        kb = P.sb(sx + "a_kb", [128, 2, 64], BF16)
        kdup = P.sb(sx + "a_kdup", [128, 2, 2, 64], BF16)
        qT = P.sb(sx + "a_qT", [128, 8, 128], BF16)
        PT = [[[P.sb(sx + "a_PT%d%d%d" % (g, k, h), [128, 4, 128], BF16) for h in range(2)] for k in range(2)] for g in range(2)]
        rden = P.sb(sx + "a_rden", [128, 4, 128], F32)
        attT = P.sb(sx + "a_attT", [128, 8, 128], BF16)

        SETS.append((junk, st, hb, hT, qf, kvf, sga, tmps, qb, kb, kdup, qT, PT, rden, attT))
    mo = [P.sb("a_mo%d" % i, [128, D], F32) for i in range(2)]

    def load_x(n):
        P.load(xs[n % 2], xs[n % 2][:], c.x[n * 128:(n + 1) * 128, :])

    load_x(0)

    def tile(n):
        j = n % NT
        cur = n % 2
        (junk, st, hb, hT, qf, kvf, sga, tmps, qb, kb, kdup, qT, PT, rden, attT) = SETS[n % 2]
        prv = 1 - cur
        if n + 1 < NTT:
            load_x(n + 1)
        x_t = xs[cur]
        norm_and_transpose(c, x_t, c.g1, hb, hT, junk, st, bank[7])
        def proj(bk, col0, ncols):
            for k in range(8):
                P.op("pe", lambda e, k=k: e.matmul(out=bk[:, 0:ncols], lhsT=hT[:, k, :], rhs=wA[:, k, col0:col0 + ncols],
                                                   start=(k == 0), stop=(k == 7)),
                     reads=[hT, wA], writes=[bk] if k == 0 else [], parts=[] if k == 0 else [bk])
        proj(bank[0], 0, 512)
        P.op("dve", lambda e: e.tensor_copy(out=qf[:, 0:512], in_=bank[0][:]), reads=[bank[0]], writes=[qf])
        proj(bank[1], 512, 512)
        P.op("act", lambda e: e.copy(out=qf[:, 512:1024], in_=bank[1][:]), reads=[bank[1]], parts=[qf])
        proj(bank[2], 1024, 256)
        P.op("dve", lambda e: e.tensor_copy(out=kvf[:], in_=bank[2][:, 0:256]), reads=[bank[2]], writes=[kvf])
        proj(bank[3], 1280, 512)
        P.op("act", lambda e: e.activation(out=sga[:, 0:512], in_=bank[3][:], func=AF.Sigmoid), reads=[bank[3]], writes=[sga])
        proj(bank[4], 1792, 512)
        P.op("act", lambda e: e.activation(out=sga[:, 512:1024], in_=bank[4][:], func=AF.Sigmoid), reads=[bank[4]], parts=[sga])
        cs_n = c.cs[:, n, :]
        qk_norm_rope(c, "q", qf, 16, gq, cs_n, qb, tmps)
        qk_norm_rope(c, "k", kvf, 2, gk, cs_n, kb, tmps)
        P.op("pool", lambda e: e.tensor_copy(out=kdup[:, :, 0, :], in_=kb[:]), reads=[kb], writes=[kdup])
        P.op("pool", lambda e: e.tensor_copy(out=kdup[:, :, 1, :], in_=kb[:]), reads=[kb], parts=[kdup])
        v3 = kvf[:, 128:256].rearrange("p (g d) -> p g d", d=64)
        P.op("pool", lambda e: e.tensor_copy(out=vA[cur][:, :, 0:64], in_=v3), reads=[kvf], writes=[vA[cur]])
        P.op("pool", lambda e: e.tensor_copy(out=vB[cur][:, :, 64:128], in_=v3), reads=[kvf], writes=[vB[cur]])
        pv = bank[7][:].bitcast(BF16)
        qflat = qb[:].rearrange("p h d -> p (h d)")
        for k in range(8):
            P.op("pe", lambda e, k=k: e.transpose(out=pv[:, k * 128:(k + 1) * 128], in_=qflat[:, k * 128:(k + 1) * 128], identity=c.identb[:]),
                 reads=[qb, c.identb], writes=[bank[7]] if k == 0 else [], parts=[] if k == 0 else [bank[7]])
        P.op("act", lambda e: e.copy(out=qT[:].rearrange("p k t -> p (k t)"), in_=pv), reads=[bank[7]], writes=[qT])
        pv6 = bank[6][:].bitcast(BF16)
        kflat = kdup[:].rearrange("p g u d -> p (g u d)")
        for g in range(2):
            P.op("pe", lambda e, g=g: e.transpose(out=pv6[:, g * 128:(g + 1) * 128], in_=kflat[:, g * 128:(g + 1) * 128], identity=c.identb[:]),
                 reads=[kdup, c.identb], writes=[bank[6]] if g == 0 else [], parts=[] if g == 0 else [bank[6]])
        P.op("dve", lambda e: e.tensor_copy(out=kT[cur][:].rearrange("p g t -> p (g t)"), in_=pv6[:, 0:256]), reads=[bank[6]], writes=[kT[cur]])
        kbs = [1] if j == 0 else [0, 1]
        bi = 0
        for g in range(2):
            for kk in kbs:
                slot = cur if kk == 1 else prv
                for hh in range(2):
                    bk = bank[bi % 6]
                    bi += 1
                    P.op("pe", lambda e, bk=bk, slot=slot, g=g, hh=hh: e.matmul(
                        out=bk[:], lhsT=kT[slot][hh * 64:(hh + 1) * 64, g, :],
                        rhs=qT[hh * 64:(hh + 1) * 64, 4 * g:4 * g + 4, :], start=True, stop=False),
                        reads=[kT[slot], qT], writes=[bk])
                    P.op("pe", lambda e, bk=bk, kk=kk: e.matmul(out=bk[:], lhsT=c.identb[:], rhs=amask[:, kk, :], start=False, stop=True),
                         reads=[c.identb, amask], parts=[bk])
                    pt = PT[g][kk][hh]
                    P.op("act", lambda e, bk=bk, pt=pt: e.activation(out=pt[:].rearrange("p i t -> p (i t)"), in_=bk[:], func=AF.Exp, scale=0.125),
                         reads=[bk], writes=[pt])
        for g in range(2):
            pav = bank[6]
            pden = bank[7]
            first_av = True
            for p in range(4):
                combos = [(kk, hh) for kk in kbs for hh in range(2)]
                for ci, (kk, hh) in enumerate(combos):
                    slot = cur if kk == 1 else prv
                    vt = vA[slot] if hh == 0 else vB[slot]
                    pt = PT[g][kk][hh]
                    w_first = first_av
                    P.op("pe", lambda e, vt=vt, pt=pt, p=p, g=g, ci=ci, ncmb=len(combos): e.matmul(
                        out=pav[:, p * 128:(p + 1) * 128], lhsT=vt[:, g, :], rhs=pt[:, p, :], start=(ci == 0), stop=(ci == ncmb - 1)),
                        reads=[vt, pt], writes=[pav] if w_first else [], parts=[] if w_first else [pav])
                    P.op("pe", lambda e, pt=pt, p=p, hh=hh, ci=ci, ncmb=len(combos): e.matmul(
                        out=pden[:, p * 128:(p + 1) * 128], lhsT=onesab[:, hh, :], rhs=pt[:, p, :], start=(ci == 0), stop=(ci == ncmb - 1)),
                        reads=[onesab, pt], writes=[pden] if w_first else [], parts=[] if w_first else [pden])
                    first_av = False
            P.op("dve", lambda e, g=g: e.tensor_tensor(out=rden[:], in0=pden[:].rearrange("p (i t) -> p i t", t=128),
                                                       in1=sinkp[:, 4 * g:4 * g + 4].unsqueeze(2).to_broadcast([128, 4, 128]), op=ALU.add),
                 reads=[pden, sinkp], writes=[rden])
            P.op("dve", lambda e: e.reciprocal(out=rden[:], in_=rden[:]), reads=[rden], writes=[rden])
            P.op("dve", lambda e, g=g: e.tensor_tensor(out=attT[:, 4 * g:4 * g + 4, :], in0=pav[:].rearrange("p (i t) -> p i t", t=128),
                                                       in1=rden[:], op=ALU.mult),
                 reads=[pav, rden], writes=[attT] if g == 0 else [], parts=[] if g == 0 else [attT])
        m_t = mo[n % 2]
        for hn in range(2):
            bk = bank[hn]
            for i in range(8):
                P.op("pe", lambda e, bk=bk, i=i, hn=hn: e.matmul(out=bk[:], lhsT=attT[:, i, :], rhs=wba[:, i, hn * 512:(hn + 1) * 512],
                                                               start=(i == 0), stop=(i == 7)),
                     reads=[attT, wba], writes=[bk] if i == 0 else [], parts=[] if i == 0 else [bk])
            P.op("dve", lambda e, bk=bk, hn=hn: e.tensor_tensor(out=m_t[:, hn * 512:(hn + 1) * 512], in0=bk[:], in1=sga[:, hn * 512:(hn + 1) * 512], op=ALU.mult),
                 reads=[bk, sga], writes=[m_t] if hn == 0 else [], parts=[] if hn == 0 else [m_t])
        P.store(c.mixa[n * 128:(n + 1) * 128, :], m_t, m_t[:], dram_writes=[c.mixa_t[n]], final=c.dbg)
        if c.dbg and n == c.dbg - 1:
            for nm, tl, shp, dt in (("qb", qb, [128, 1024], BF16), ("kb", kb, [128, 128], BF16), ("attT", attT, [128, 1024], BF16),
                                    ("qf", qf, [128, 1024], F32), ("sga", sga, [128, 1024], F32), ("hT", hT, [128, 1024], BF16),
                                    ("PT", PT[0][1][0], [128, 512], BF16), ("rden", rden, [128, 512], F32), ("qT", qT, [128, 1024], BF16),
                                    ("kT", kT[cur], [128, 256], BF16), ("kdup", kdup, [128, 256], BF16), ("vA", vA[cur], [128, 256], BF16)):
                d_ap = c.nc.dram_tensor("dbg_" + nm, shp, dt, kind="ExternalOutput").ap()
                flat = tl[:]
                if len(flat.shape) == 3:
                    flat = flat.rearrange("p a b -> p (a b)")
                if len(flat.shape) == 4:
                    flat = flat.rearrange("p a b c -> p (a b c)")
                P.store(d_ap, tl, flat, final=True)

    for n in range(NTT):
        tile(n)


def phase_b(c):
    P, NB, NT, NTT = c.P, c.NB, c.NT, c.NTT
    bank = c.bank
    c.g1 = P.sb("b_g1", [128, D], F32)
    P.load(c.g1, c.g1[:], c.norm1_gain[0].partition_broadcast(128))
    wB = P.sb("wB", [128, 8, 4112], BF16)
    load_w_bf16(c, wB, 1280, 3088, c.w_in, 0)
    load_w_bf16(c, wB, 5392, 1024, c.w_in, 3088)
    wbm = P.sb("wbm", [128, 8, D], BF16)
    load_w_bf16(c, wbm, 0, D, c.w_branch_mlstm)
    wout = P.sb("wout", [128, 8, D], BF16)
    load_w_bf16(c, wout, 0, D, c.w_out)
    mlg = P.sb("mlg", [128, D], F32)
    P.load(mlg, mlg[:], c.ml_out_norm_gain[0].partition_broadcast(128))
    cmask = P.sb("cmask", [128, 512], F32)
    P.load(cmask, cmask[:], c.cst["cmask"])
    bif = P.sb("b_bif", [8, 2], F32)
    P.load(bif, bif[:, 0:1], c.ml_i_bias.rearrange("o h -> h o"), allow_slow_non_contiguous=True)
    P.load(bif, bif[:, 1:2], c.ml_f_bias.rearrange("o h -> h o"), part=True, allow_slow_non_contiguous=True)
    P.op("dve", lambda e: e.tensor_scalar(out=bif[:], in0=bif[:], scalar1=1.0 / 15.0, scalar2=None, op0=ALU.mult), reads=[bif], writes=[bif])

    xs = [P.sb("b_xs%d" % i, [128, D], F32) for i in range(2)]
    ma = [P.sb("b_ma%d" % i, [128, D], F32) for i in range(2)]
    junk = P.sb("b_junk", [128, D], BF16)
    st = (P.sb("b_ssq", [128, 1], F32), P.sb("b_tmp", [128, 1], F32), P.sb("b_rstd", [128, 1], F32))
    hb = P.sb("b_hb", [128, D], BF16)
    hT = P.sb("b_hT", [128, 8, 128], BF16)
    g_ti = P.sb("g_ti", [8, 128], F32)
    g_tf = P.sb("g_tf", [8, 128], F32)
    g_nl = P.sb("g_nl", [8, 128], F32)
    g_cum = P.sb("g_cum", [8, 128], F32)
    g_a = P.sb("g_a", [8, 128], F32)
    g_M = P.sb("g_M", [8, 128], F32)
    g_d2 = P.sb("g_d2", [8, 128], F32)
    g_ones = P.sb("g_ones", [8, 128], F32)
    P.op("pool", lambda e: e.memset(g_ones[:], 1.0), writes=[g_ones])
    g_out = [P.sb("g_out%d" % i, [8, 128], F32) for i in range(5)]
    cumc = [P.sb("g_cumc%d" % b, [8, 1], F32) for b in range(NB)]
    Mc = [P.sb("g_Mc%d" % b, [8, 1], F32) for b in range(NB)]
    g_nM0 = P.sb("g_nM0", [8, 1], F32)
    g_nMe = P.sb("g_nMe", [8, 1], F32)
    g_dd = P.sb("g_dd", [8, 1], F32)
    gtok = P.sb("b_gtok", [128, 5, 8], F32)
    qt = P.sb("b_qt", [128, 512], BF16)
    kt = P.sb("b_kt", [128, 512], BF16)
    khat = P.sb("b_khat", [128, 512], BF16)
    vaug = P.sb("b_vaug", [128, 8, 129], BF16)
    P.op("pool", lambda e: e.memset(vaug[:], 1.0), writes=[vaug])
    sgo = P.sb("b_sgo", [128, D], F32)
    sgm = P.sb("b_sgm", [128, D], F32)
    qtT = P.sb("b_qtT", [64, 8, 128], BF16)
    ktT = P.sb("b_ktT", [64, 8, 128], BF16)
    PTm = P.sb("b_PT", [128, 8, 128], BF16)
    C32 = [P.sb("b_C32_%d" % b, [64, 8, 129], F32) for b in range(NB)]
    Cb = [P.sb("b_Cb_%d" % b, [64, 8, 129], BF16) for b in range(NB)]
    dmax = P.sb("b_dmax", [128, 8], F32)
    rc = P.sb("b_rc", [128, 8], F32)
    hm = P.sb("b_hm", [128, 8, 128], F32)
    hsq = P.sb("b_hsq", [128, 8, 128], F32)
    hs8 = (P.sb("b_hssq", [128, 8], F32), P.sb("b_htmp", [128, 8], F32), P.sb("b_hrstd", [128, 8], F32))
    hn = P.sb("b_hn", [128, D], BF16)
    hnT = P.sb("b_hnT", [128, 8, 128], BF16)
    mx = P.sb("b_mx", [128, D], F32)
    mxb = P.sb("b_mxb", [128, D], BF16)
    mxT = P.sb("b_mxT", [128, 8, 128], BF16)
    xo = [P.sb("b_xo%d" % i, [128, D], F32) for i in range(2)]
    HG = [(0, 3), (3, 6), (6, 8)]

    def load_in(n):
        P.load(xs[n % 2], xs[n % 2][:], c.x[n * 128:(n + 1) * 128, :])
        P.load(ma[n % 2], ma[n % 2][:], c.mixa[n * 128:(n + 1) * 128, :], dram_reads=[c.mixa_t[n]])

    load_in(0)

    def tile(n):
        b = n // NT
        j = n % NT
        if n + 1 < NTT:
            load_in(n + 1)
        x_t = xs[n % 2]
        ma_t = ma[n % 2]
        norm_and_transpose(c, x_t, c.g1, hb, hT, junk, st, bank[7])

        def proj(bk, col0, ncols, M=128, ocol=0):
            for k in range(8):
                P.op("pe", lambda e, k=k: e.matmul(out=bk[0:M, ocol:ocol + ncols], lhsT=hT[:, k, :], rhs=wB[:, k, col0:col0 + ncols],
                                                   start=(k == 0), stop=(k == 7)),
                     reads=[hT, wB], writes=[bk] if (k == 0 and ocol == 0) else [], parts=[] if (k == 0 and ocol == 0) else [bk])
        for gi, col in enumerate((2048, 2056)):
            for k in range(8):
                P.op("pe", lambda e, k=k, gi=gi, col=col: e.matmul(out=bank[6][0:8, gi * 128:(gi + 1) * 128], lhsT=wB[:, k, col:col + 8], rhs=hT[:, k, :],
                                                                    start=(k == 0), stop=(k == 7)),
                     reads=[hT, wB], writes=[bank[6]] if (k == 0 and gi == 0) else [], parts=[] if (k == 0 and gi == 0) else [bank[6]])
        P.op("act", lambda e: e.activation(out=g_ti[:], in_=bank[6][0:8, 0:128], func=AF.Tanh, bias=bif[:, 0:1], scale=1.0 / 15.0),
             reads=[bank[6], bif], writes=[g_ti])
        P.op("act", lambda e: e.activation(out=g_tf[:], in_=bank[6][0:8, 128:256], func=AF.Tanh, bias=bif[:, 1:2], scale=1.0 / 15.0),
             reads=[bank[6], bif], writes=[g_tf])
        P.op("act", lambda e: e.activation(out=g_nl[:], in_=g_tf[:], func=AF.Exp, scale=-15.0), reads=[g_tf], writes=[g_nl])
        P.op("act", lambda e: e.activation(out=g_nl[:], in_=g_nl[:], func=AF.Ln, bias=1.0), reads=[g_nl], writes=[g_nl])
        if j == 0:
            P.op("dve", lambda e: e.tensor_tensor_scan(out=g_cum[:], data0=g_ones[:], data1=g_nl[:], initial=0.0, op0=ALU.mult, op1=ALU.add),
                 reads=[g_ones, g_nl], writes=[g_cum])
        else:
            P.op("dve", lambda e: e.tensor_tensor_scan(out=g_cum[:], data0=g_ones[:], data1=g_nl[:], initial=cumc[b][:], op0=ALU.mult, op1=ALU.add),
                 reads=[g_ones, g_nl, cumc[b]], writes=[g_cum])
        P.op("dve", lambda e: e.scalar_tensor_tensor(out=g_a[:], in0=g_ti[:], scalar=15.0, in1=g_cum[:], op0=ALU.mult, op1=ALU.add),
             reads=[g_ti, g_cum], writes=[g_a])
        if j == 0:
            P.op("dve", lambda e: e.memset(Mc[b][:], 0.0), writes=[Mc[b]])
        P.op("dve", lambda e: e.tensor_tensor_scan(out=g_M[:], data0=g_a[:], data1=g_a[:], initial=Mc[b][:], op0=ALU.max, op1=ALU.max),
             reads=[g_a, Mc[b]], writes=[g_M])
        P.op("dve", lambda e: e.tensor_scalar(out=g_nM0[:], in0=Mc[b][:], scalar1=-1.0, scalar2=None, op0=ALU.mult), reads=[Mc[b]], writes=[g_nM0])
        P.op("dve", lambda e: e.tensor_scalar(out=g_nMe[:], in0=g_M[:, 127:128], scalar1=-1.0, scalar2=None, op0=ALU.mult), reads=[g_M], writes=[g_nMe])
        P.op("dve", lambda e: e.tensor_tensor(out=g_dd[:], in0=Mc[b][:], in1=g_nMe[:], op=ALU.add), reads=[Mc[b], g_nMe], writes=[g_dd])
        P.op("dve", lambda e: e.tensor_tensor(out=g_d2[:], in0=g_cum[:], in1=g_M[:], op=ALU.subtract), reads=[g_cum, g_M], writes=[g_d2])
        P.op("act", lambda e: e.activation(out=g_out[0][:], in_=g_M[:], func=AF.Exp, bias=Mc[b][:], scale=-1.0), reads=[g_M, Mc[b]], writes=[g_out[0]])
        P.op("act", lambda e: e.activation(out=g_out[1][:], in_=g_a[:], func=AF.Exp, bias=g_nM0[:], scale=1.0), reads=[g_a, g_nM0], writes=[g_out[1]])
        P.op("act", lambda e: e.activation(out=g_out[2][:], in_=g_a[:], func=AF.Exp, bias=g_nMe[:], scale=1.0), reads=[g_a, g_nMe], writes=[g_out[2]])
        P.op("act", lambda e: e.activation(out=g_out[3][:], in_=g_a[:], func=AF.Exp, bias=g_dd[:], scale=0.0), reads=[g_a, g_dd], writes=[g_out[3]])
        P.op("act", lambda e: e.activation(out=g_out[4][:], in_=g_d2[:], func=AF.Exp), reads=[g_d2], writes=[g_out[4]])
        P.op("dve", lambda e: e.tensor_copy(out=cumc[b][:], in_=g_cum[:, 127:128]), reads=[g_cum], writes=[cumc[b]])
        P.op("dve", lambda e: e.tensor_copy(out=Mc[b][:], in_=g_M[:, 127:128]), reads=[g_M], writes=[Mc[b]])
        for qi in range(5):
            P.op("pe", lambda e, qi=qi: e.transpose(out=bank[6][:, 256 + qi * 8:256 + (qi + 1) * 8], in_=g_out[qi][:], identity=c.identf[0:8, 0:8]),
                 reads=[g_out[qi], c.identf], writes=[bank[6]] if qi == 0 else [], parts=[] if qi == 0 else [bank[6]])
        P.op("dve", lambda e: e.tensor_copy(out=gtok[:].rearrange("p q h -> p (q h)"), in_=bank[6][:, 256:296]), reads=[bank[6]], writes=[gtok])
        proj(bank[0], 0, 512)
        proj(bank[1], 512, 512)
        P.op("dve", lambda e: e.tensor_tensor(out=qt[:].rearrange("p (h d) -> p h d", d=64), in0=bank[0][:].rearrange("p (h d) -> p h d", d=64),
                                              in1=gtok[:, 0, :].unsqueeze(2).to_broadcast([128, 8, 64]), op=ALU.mult),
             reads=[bank[0], gtok], writes=[qt])
        P.op("dve", lambda e: e.scalar_tensor_tensor(out=kt[:].rearrange("p (h d) -> p h d", d=64), in0=bank[1][:].rearrange("p (h d) -> p h d", d=64),
                                                     scalar=0.125, in1=gtok[:, 1, :].unsqueeze(2).to_broadcast([128, 8, 64]), op0=ALU.mult, op1=ALU.mult),
             reads=[bank[1], gtok], writes=[kt])
        P.op("dve", lambda e: e.scalar_tensor_tensor(out=khat[:].rearrange("p (h d) -> p h d", d=64), in0=bank[1][:].rearrange("p (h d) -> p h d", d=64),
                                                     scalar=0.125, in1=gtok[:, 2, :].unsqueeze(2).to_broadcast([128, 8, 64]), op0=ALU.mult, op1=ALU.mult),
             reads=[bank[1], gtok], writes=[khat])
        for hv in range(2):
            proj(bank[2 + hv], 1024 + hv * 512, 512)
            P.op("act", lambda e, hv=hv: e.copy(out=vaug[:, 4 * hv:4 * hv + 4, 0:128], in_=bank[2 + hv][:].rearrange("p (h d) -> p h d", d=128)),
                 reads=[bank[2 + hv]], writes=[vaug] if hv == 0 else [], parts=[] if hv == 0 else [vaug])
        for hv in range(2):
            proj(bank[4 + hv], 2064 + hv * 512, 512)
            P.op("act", lambda e, hv=hv: e.activation(out=sgo[:, hv * 512:(hv + 1) * 512], in_=bank[4 + hv][:], func=AF.Sigmoid),
                 reads=[bank[4 + hv]], writes=[sgo] if hv == 0 else [], parts=[] if hv == 0 else [sgo])
        for hv in range(2):
            proj(bank[2 + hv], 3088 + hv * 512, 512)
            P.op("act", lambda e, hv=hv: e.activation(out=sgm[:, hv * 512:(hv + 1) * 512], in_=bank[2 + hv][:], func=AF.Sigmoid),
                 reads=[bank[2 + hv]], writes=[sgm] if hv == 0 else [], parts=[] if hv == 0 else [sgm])
        for (src, dstT, bk, eng) in ((qt, qtT, bank[7], "act"), (kt, ktT, bank[6], "dve")):
            pv = bk[:].bitcast(BF16)
            for h in range(8):
                P.op("pe", lambda e, h=h, src=src, pv=pv: e.transpose(out=pv[0:64, h * 128:(h + 1) * 128], in_=src[:, h * 64:(h + 1) * 64], identity=c.identb[:]),
                     reads=[src, c.identb], writes=[bk] if h == 0 else [], parts=[] if h == 0 else [bk])
            if eng == "act":
                P.op("act", lambda e, pv=pv, dstT=dstT: e.copy(out=dstT[:].rearrange("p h t -> p (h t)"), in_=pv[0:64, :]), reads=[bk], writes=[dstT])
            else:
                P.op("dve", lambda e, pv=pv, dstT=dstT: e.tensor_copy(out=dstT[:].rearrange("p h t -> p (h t)"), in_=pv[0:64, :]), reads=[bk], writes=[dstT])
        for hb4 in range(2):
            bk = bank[hb4]
            for hh in range(4):
                h = hb4 * 4 + hh
                P.op("pe", lambda e, h=h, hh=hh, bk=bk: e.matmul(out=bk[:, hh * 128:(hh + 1) * 128], lhsT=ktT[:, h, :], rhs=qtT[:, h, :], start=True, stop=True),
                     reads=[ktT, qtT], writes=[bk] if hh == 0 else [], parts=[] if hh == 0 else [bk])
            P.op("dve", lambda e, bk=bk, hb4=hb4: e.tensor_tensor(out=PTm[:, 4 * hb4:4 * hb4 + 4, :].rearrange("p h t -> p (h t)"), in0=bk[:], in1=cmask[:], op=ALU.mult),
                 reads=[bk, cmask], writes=[PTm] if hb4 == 0 else [], parts=[] if hb4 == 0 else [PTm])
        for gi, (h0, h1) in enumerate(HG):
            bk = bank[2 + gi]
            for h in range(h0, h1):
                o = (h - h0) * 129
                P.op("pe", lambda e, h=h, o=o, bk=bk: e.matmul(out=bk[:, o:o + 129], lhsT=PTm[:, h, :], rhs=vaug[:, h, :], start=True, stop=(j == 0)),
                     reads=[PTm, vaug], writes=[bk] if h == h0 else [], parts=[] if h == h0 else [bk])
                if j > 0:
                    P.op("pe", lambda e, h=h, o=o, bk=bk: e.matmul(out=bk[:, o:o + 129], lhsT=qtT[:, h, :], rhs=Cb[b][:, h, :], start=False, stop=True),
                         reads=[qtT, Cb[b]], parts=[bk])
        for gi, (h0, h1) in enumerate(HG):
            bk = bank[5 + gi]
            for h in range(h0, h1):
                o = (h - h0) * 129
                P.op("pe", lambda e, h=h, o=o, bk=bk: e.matmul(out=bk[0:64, o:o + 129], lhsT=khat[:, h * 64:(h + 1) * 64], rhs=vaug[:, h, :], start=True, stop=True),
                     reads=[khat, vaug], writes=[bk] if h == h0 else [], parts=[] if h == h0 else [bk])
        if j > 0:
            P.op("dve", lambda e: e.tensor_tensor(out=C32[b][:], in0=C32[b][:], in1=gtok[0:64, 3, :].unsqueeze(2).to_broadcast([64, 8, 129]), op=ALU.mult),
                 reads=[C32[b], gtok], writes=[C32[b]])
        for gi, (h0, h1) in enumerate(HG):
            bk = bank[5 + gi]
            nh = h1 - h0
            if j > 0:
                P.op("dve", lambda e, bk=bk, h0=h0, h1=h1, nh=nh: e.tensor_tensor(out=C32[b][:, h0:h1, :], in0=C32[b][:, h0:h1, :],
                                                                                  in1=bk[0:64, 0:nh * 129].rearrange("p (h v) -> p h v", v=129), op=ALU.add),
                     reads=[bk, C32[b]], writes=[C32[b]])
            else:
                P.op("dve", lambda e, bk=bk, h0=h0, h1=h1, nh=nh: e.tensor_copy(out=C32[b][:, h0:h1, :], in_=bk[0:64, 0:nh * 129].rearrange("p (h v) -> p h v", v=129)),
                     reads=[bk], writes=[C32[b]] if gi == 0 else [], parts=[] if gi == 0 else [C32[b]])
        P.op("act", lambda e: e.copy(out=Cb[b][:], in_=C32[b][:]), reads=[C32[b]], writes=[Cb[b]])
        for gi, (h0, h1) in enumerate(HG):
            bk = bank[2 + gi]
            nh = h1 - h0
            v3 = bk[:, 0:nh * 129].rearrange("p (h v) -> p h v", v=129)
            P.op("dve", lambda e, v3=v3, h0=h0, h1=h1: e.tensor_tensor(out=dmax[:, h0:h1].unsqueeze(2), in0=v3[:, :, 128:129], in1=gtok[:, 4, h0:h1].unsqueeze(2), op=ALU.max),
                 reads=[bk, gtok], writes=[dmax])
            P.op("dve", lambda e, v3=v3, h0=h0, h1=h1: e.scalar_tensor_tensor(out=dmax[:, h0:h1].unsqueeze(2), in0=v3[:, :, 128:129], scalar=-1.0, in1=dmax[:, h0:h1].unsqueeze(2), op0=ALU.mult, op1=ALU.max),
                 reads=[bk, dmax], writes=[dmax])
        P.op("dve", lambda e: e.reciprocal(out=rc[:], in_=dmax[:]), reads=[dmax], writes=[rc])
        for gi, (h0, h1) in enumerate(HG):
            bk = bank[2 + gi]
            nh = h1 - h0
            v3 = bk[:, 0:nh * 129].rearrange("p (h v) -> p h v", v=129)
            P.op("dve", lambda e, v3=v3, h0=h0, h1=h1, nh=nh: e.tensor_tensor(out=hm[:, h0:h1, :], in0=v3[:, :, 0:128],
                                                                              in1=rc[:, h0:h1].unsqueeze(2).to_broadcast([128, nh, 128]), op=ALU.mult),
                 reads=[bk, rc], writes=[hm] if gi == 0 else [], parts=[] if gi == 0 else [hm])
        P.op("pool", lambda e: e.tensor_tensor(out=hsq[:], in0=hm[:], in1=hm[:], op=ALU.mult), reads=[hm], writes=[hsq])
        P.op("dve", lambda e: e.tensor_reduce(out=hs8[0][:], in_=hsq[:], axis=AX.X, op=ALU.add), reads=[hsq], writes=[hs8[0]])
        rms_rstd(c, "h", hs8[0], 128, hs8[2], hs8[1])
        P.op("dve", lambda e: e.tensor_tensor(out=hm[:], in0=hm[:], in1=hs8[2][:].unsqueeze(2).to_broadcast([128, 8, 128]), op=ALU.mult),
             reads=[hm, hs8[2]], writes=[hm])
        hmf = hm[:].rearrange("p h v -> p (h v)")
        P.op("pool", lambda e: e.tensor_tensor(out=hmf, in0=hmf, in1=mlg[:], op=ALU.mult), reads=[hm, mlg], writes=[hm])
        P.op("dve", lambda e: e.tensor_tensor(out=hn[:], in0=hmf, in1=sgo[:], op=ALU.mult), reads=[hm, sgo], writes=[hn])
        transpose8(c, hn, hnT, bank[7])
        for hv in range(2):
            bk = bank[hv]
            for i in range(8):
                P.op("pe", lambda e, bk=bk, i=i, hv=hv: e.matmul(out=bk[:], lhsT=hnT[:, i, :], rhs=wbm[:, i, hv * 512:(hv + 1) * 512], start=(i == 0), stop=(i == 7)),
                     reads=[hnT, wbm], writes=[bk] if i == 0 else [], parts=[] if i == 0 else [bk])
            P.op("dve", lambda e, bk=bk, hv=hv: e.tensor_tensor(out=mx[:, hv * 512:(hv + 1) * 512], in0=bk[:], in1=sgm[:, hv * 512:(hv + 1) * 512], op=ALU.mult),
                 reads=[bk, sgm], writes=[mx] if hv == 0 else [], parts=[] if hv == 0 else [mx])
        P.op("pool", lambda e: e.tensor_tensor(out=mxb[:], in0=mx[:], in1=ma_t[:], op=ALU.add), reads=[mx, ma_t], writes=[mxb])
        transpose8(c, mxb, mxT, bank[6], eng="dve")
        xo_t = xo[n % 2]
        for hv in range(2):
            bk = bank[2 + hv]
            for i in range(8):
                P.op("pe", lambda e, bk=bk, i=i, hv=hv: e.matmul(out=bk[:], lhsT=mxT[:, i, :], rhs=wout[:, i, hv * 512:(hv + 1) * 512], start=(i == 0), stop=(i == 7)),
                     reads=[mxT, wout], writes=[bk] if i == 0 else [], parts=[] if i == 0 else [bk])
            P.op("dve", lambda e, bk=bk, hv=hv: e.tensor_tensor(out=xo_t[:, hv * 512:(hv + 1) * 512], in0=bk[:], in1=x_t[:, hv * 512:(hv + 1) * 512], op=ALU.add),
                 reads=[bk, x_t], writes=[xo_t] if hv == 0 else [], parts=[] if hv == 0 else [xo_t])
        P.store(c.out[n * 128:(n + 1) * 128, :], xo_t, xo_t[:], dram_writes=[c.x1_t[n]], final=("C" not in c.phases))

    for n in range(NTT):
        tile(n)


def phase_c(c):
    P, NB, NT, NTT = c.P, c.NB, c.NT, c.NTT
    nc = c.nc
    bank = c.bank
    GT = 3
    ex_s = P.dram("ex_s", [128, 128, 2 * D], BF16)
    downT_s = ex_s[:, :, 0:D]
    up_s = ex_s[:, :, D:2 * D]
    downT_t = [T("downT_t%d" % i) for i in range(128)]
    up_t = [T("up_t%d" % i) for i in range(16)]
    up_v = c.peer_up.rearrange("(i c) d -> c i d", c=128)
    dn_v = c.peer_down.rearrange("(i c) d -> c i d", c=128)
    dummy = T("up_dma_sem")
    for u in range(16):
        P.dma(up_s[u * 8:(u + 1) * 8], up_v[u * 8:(u + 1) * 8], writes=[up_t[u]], sem_tile=dummy, eng="pool", max_dma_last_dim=4096)
    P.open_scope()
    dsrc = [P.sb("c_dsrc%d" % i, [128, D], BF16) for i in range(2)]
    dtr = [P.sb("c_dtr%d" % i, [128, D], BF16) for i in range(2)]
    for cb in range(128):
        sl = cb % 2
        P.load(dsrc[sl], dsrc[sl][:], dn_v[cb], eng="pool", max_dma_last_dim=4096)
        pv = bank[6 + sl][:].bitcast(BF16)
        for k in range(8):
            P.op("pe", lambda e, k=k, sl=sl, pv=pv: e.transpose(out=pv[:, k * 128:(k + 1) * 128], in_=dsrc[sl][:, k * 128:(k + 1) * 128], identity=c.identb[:]),
                 reads=[dsrc[sl], c.identb], writes=[bank[6 + sl]] if k == 0 else [], parts=[] if k == 0 else [bank[6 + sl]])
        if sl == 0:
            P.op("act", lambda e, sl=sl, pv=pv: e.copy(out=dtr[sl][:], in_=pv), reads=[bank[6 + sl]], writes=[dtr[sl]])
        else:
            P.op("dve", lambda e, sl=sl, pv=pv: e.tensor_copy(out=dtr[sl][:], in_=pv), reads=[bank[6 + sl]], writes=[dtr[sl]])
        P.store(downT_s[cb], dtr[sl], dtr[sl][:], dram_writes=[downT_t[cb]])
    P.close_scope()
    cstage = c.dbg if (c.dbg and c.phases == "C") else 99
    if cstage == 1:
        return

    wq = P.sb("c_wq", [128, 8, D], BF16)
    load_w_bf16(c, wq, 0, D, c.peer_w_query)
    g2 = P.sb("c_g2", [128, D], F32)
    P.load(g2, g2[:], c.norm2_gain[0].partition_broadcast(128))
    iof = P.sb("c_iof", [128, 128], F32)
    P.load(iof, iof[:], c.cst["iota128"])
    iob = P.sb("c_iob", [128, 128], BF16)
    P.op("dve", lambda e: e.tensor_copy(out=iob[:], in_=iof[:]), reads=[iof], writes=[iob])
    skT = P.sb("c_skT", [128, 8, 128], BF16)
    P.open_scope()
    skf = P.sb("c_skf", [128, 16, 64], F32)
    P.load(skf, skf[:], c.peer_sub_keys.rearrange("h p k d -> k (h p) d"))
    skb = P.sb("c_skb", [128, 16 * 64], BF16)
    P.op("dve", lambda e: e.tensor_copy(out=skb[:], in_=skf[:].rearrange("k a d -> k (a d)")), reads=[skf], writes=[skb])
    transpose8(c, skb, skT, bank[7])
    P.close_scope()

    if cstage == 2:
        return
    x1s = [P.sb("c_x1s%d" % i, [128, D], F32) for i in range(GT)]
    st = (P.sb("c_ssq", [128, 1], F32), P.sb("c_tmp", [128, 1], F32), P.sb("c_rstd", [128, 1], F32))
    xb = P.sb("c_xb", [128, D], BF16)
    xnT = P.sb("c_xnT", [128, 8, GT * 128], BF16)
    qT = P.sb("c_qT", [128, 8, 128], BF16)
    sc = P.sb("c_sc", [128, 16, 128], F32)
    sc_t = [T("sc_t%d" % i) for i in range(16)]
    v16 = P.sb("c_v16", [128, 16, 16], F32)
    ix = P.sb("c_ix", [128, 16, 16], U32)
    ixf = P.sb("c_ixf", [128, 16, 16], F32)
    cand = P.sb("c_cand", [128, 8, 256], F32)
    cand_t = [T("cand_t%d" % i) for i in range(8)]
    v16b, ixb, c16b, posb = T("v16b"), T("ixb"), T("c16b"), T("posb")
    c16 = P.sb("c_c16", [128, 8, 16], F32)
    pos = P.sb("c_pos", [128, 8, 16], U32)
    posf = P.sb("c_posf", [128, 8, 16], F32)
    pki = P.sb("c_pki", [128, 8, 16], I32)
    pkf = P.sb("c_pkf", [128, 8, 16], F32)
    qkf = P.sb("c_qkf", [128, 8, 16], F32)
    decs = [P.sb("c_dec%d" % i, [128, 2, 16, 16], F32) for i in range(4)]
    abg = [P.sb("c_abg%d" % i, [128, 8, 16], F32) for i in range(3)]
    z8 = P.sb("c_z8", [128, 8], F32)
    abgT = [P.sb("c_abgT%d" % i, [128, GT * 128], F32) for i in range(3)]
    CH = 8
    Ach = [P.sb("c_Ach%d" % i, [128, CH, 128], BF16) for i in range(2)]
    Bch = [P.sb("c_Bch%d" % i, [128, CH, 128], BF16) for i in range(2)]
    Wg = P.sb("c_Wg", [128, 128, GT * 128], BF16)
    NS = 4
    esl = [P.sb("c_esl%d" % i, [128, 2 * D], BF16) for i in range(NS)]
    actb = [P.sb("c_act%d" % i, [128, GT * 128], BF16) for i in range(2)]
    wab = [P.sb("c_wa%d" % i, [128, GT * 128], BF16) for i in range(2)]

    groups = []
    n0 = 0
    while n0 < NTT:
        g = min(GT, NTT - n0)
        groups.append((n0, g))
        n0 += g
    blk_ctr = [0]

    def route_tile(n, ti):
        x_t = x1s[ti]
        src_x1 = c.x if c.phases == "C" else c.out
        P.load(x_t, x_t[:], src_x1[n * 128:(n + 1) * 128, :], dram_reads=[c.x1_t[n]])
        ssq, tmp, rstd = st
        P.op("act", lambda e: e.activation(out=xb[:], in_=x_t[:], func=AF.Square, accum_out=ssq[:]), reads=[x_t], writes=[xb, ssq])
        rms_rstd(c, "n", ssq, D, rstd, tmp)
        P.op("dve", lambda e: e.scalar_tensor_tensor(out=xb[:], in0=x_t[:], scalar=rstd[:], in1=g2[:], op0=ALU.mult, op1=ALU.mult),
             reads=[x_t, rstd, g2], writes=[xb])
        pvx = bank[7][:].bitcast(BF16)
        for k in range(8):
            P.op("pe", lambda e, k=k: e.transpose(out=pvx[:, k * 128:(k + 1) * 128], in_=xb[:, k * 128:(k + 1) * 128], identity=c.identb[:]),
                 reads=[xb, c.identb], writes=[bank[7]] if k == 0 else [], parts=[] if k == 0 else [bank[7]])
        xt1 = xnT[:, :, ti * 128:(ti + 1) * 128]
        P.op("act", lambda e: e.copy(out=xt1, in_=pvx.rearrange("p (k t) -> p k t", t=128)), reads=[bank[7]], writes=[xnT] if ti == 0 else [], parts=[] if ti == 0 else [xnT])
        if cstage == 29:
            return
        for hd in range(8):
            bk = bank[hd % 2]
            for k in range(8):
                P.op("pe", lambda e, k=k, hd=hd, bk=bk: e.matmul(out=bk[:, 0:128], lhsT=wq[:, k, hd * 128:(hd + 1) * 128], rhs=xt1[:, k, :],
                                                                start=(k == 0), stop=(k == 7)),
                     reads=[wq, xnT], writes=[bk] if k == 0 else [], parts=[] if k == 0 else [bk])
            P.op("act", lambda e, hd=hd, bk=bk: e.copy(out=qT[:, hd, :], in_=bk[:, 0:128]), reads=[bk], writes=[qT] if hd == 0 else [], parts=[] if hd == 0 else [qT])
        if cstage == 30:
            return
        sc4 = sc[:].rearrange("p (h two) k -> p h two k", two=2)
        for par in range(2):
            for hf in range(2):
                bk = bank[2 + par * 2 + hf]
                for u in range(4):
                    hd = hf * 4 + u
                    P.op("pe", lambda e, u=u, hd=hd, par=par, bk=bk: e.matmul(out=bk[:, u * 128:(u + 1) * 128], lhsT=qT[par * 64:(par + 1) * 64, hd, :],
                                                                          rhs=skT[par * 64:(par + 1) * 64, hd, :], start=True, stop=True),
                         reads=[qT, skT], writes=[bk] if u == 0 else [], parts=[] if u == 0 else [bk])
        for par in range(2):
            for hf in range(2):
                bk = bank[2 + par * 2 + hf]
                first = (par == 0 and hf == 0)
                if hf == 0:
                    P.op("act", lambda e, par=par, hf=hf, bk=bk: e.copy(out=sc4[:, hf * 4:hf * 4 + 4, par, :], in_=bk[:].rearrange("p (a k) -> p a k", k=128)),
                         reads=[bk], writes=[sc_t[(hf * 4 + u_) * 2 + par] for u_ in range(4)])
                else:
                    P.op("dve", lambda e, par=par, hf=hf, bk=bk: e.tensor_copy(out=sc4[:, hf * 4:hf * 4 + 4, par, :], in_=bk[:].rearrange("p (a k) -> p a k", k=128)),
                         reads=[bk], writes=[sc_t[(hf * 4 + u_) * 2 + par] for u_ in range(4)])
        if cstage in (31, 305, 306, 307):
            return
        for hp in range(16):
            P.op("dve", lambda e, hp=hp: e.max(out=v16[:, hp, 0:8], in_=sc[:, hp, :]), reads=[sc_t[hp]], writes=[v16] if hp == 0 else [], parts=[] if hp == 0 else [v16])
        for hp in range(16):
            P.op("dve", lambda e, hp=hp: e.max_index(out=ix[:, hp, 0:8], in_max=v16[:, hp, 0:8], in_values=sc[:, hp, :]), reads=[sc_t[hp], v16],
                 writes=[ix] if hp == 0 else [], parts=[] if hp == 0 else [ix])
        for hp in range(16):
            P.op("dve", lambda e, hp=hp: e.match_replace(out=sc[:, hp, :], in_to_replace=v16[:, hp, 0:8], in_values=sc[:, hp, :], imm_value=-1e30),
                 reads=[sc_t[hp], v16], writes=[sc_t[hp]])
        for hp in range(16):
            P.op("dve", lambda e, hp=hp: e.max(out=v16[:, hp, 8:16], in_=sc[:, hp, :]), reads=[sc_t[hp]], writes=[v16b] if hp == 0 else [], parts=[] if hp == 0 else [v16b])
        for hp in range(16):
            P.op("dve", lambda e, hp=hp: e.max_index(out=ix[:, hp, 8:16], in_max=v16[:, hp, 8:16], in_values=sc[:, hp, :]), reads=[sc_t[hp], v16b],
                 writes=[ixb] if hp == 0 else [], parts=[] if hp == 0 else [ixb])
        if cstage == 32:
            return
        P.op("dve", lambda e: e.tensor_copy(out=ixf[:], in_=ix[:]), reads=[ix, ixb], writes=[ixf])
        vv = v16[:].rearrange("p (h two) k -> p h two k", two=2)
        iv = ixf[:].rearrange("p (h two) k -> p h two k", two=2)
        P.op("dve", lambda e: e.tensor_tensor(out=cand[:].rearrange("p h (a b) -> p h a b", b=16),
                                              in0=vv[:, :, 0, :].unsqueeze(3).to_broadcast([128, 8, 16, 16]),
                                              in1=vv[:, :, 1, :].unsqueeze(2).to_broadcast([128, 8, 16, 16]), op=ALU.add),
             reads=[v16, v16b], writes=cand_t)
        for h in range(8):
            P.op("dve", lambda e, h=h: e.max(out=c16[:, h, 0:8], in_=cand[:, h, :]), reads=[cand_t[h]], writes=[c16] if h == 0 else [], parts=[] if h == 0 else [c16])
        for h in range(8):
            P.op("dve", lambda e, h=h: e.max_index(out=pos[:, h, 0:8], in_max=c16[:, h, 0:8], in_values=cand[:, h, :]), reads=[cand_t[h], c16],
                 writes=[pos] if h == 0 else [], parts=[] if h == 0 else [pos])
        for h in range(8):
            P.op("dve", lambda e, h=h: e.match_replace(out=cand[:, h, :], in_to_replace=c16[:, h, 0:8], in_values=cand[:, h, :], imm_value=-1e30),
                 reads=[cand_t[h], c16], writes=[cand_t[h]])
        for h in range(8):
            P.op("dve", lambda e, h=h: e.max(out=c16[:, h, 8:16], in_=cand[:, h, :]), reads=[cand_t[h]], writes=[c16b] if h == 0 else [], parts=[] if h == 0 else [c16b])
        for h in range(8):
            P.op("dve", lambda e, h=h: e.max_index(out=pos[:, h, 8:16], in_max=c16[:, h, 8:16], in_values=cand[:, h, :]), reads=[cand_t[h], c16b],
                 writes=[posb] if h == 0 else [], parts=[] if h == 0 else [posb])
        if cstage == 33:
            return
        P.op("dve", lambda e: e.tensor_copy(out=posf[:], in_=pos[:]), reads=[pos, posb], writes=[posf])
        P.op("dve", lambda e: e.tensor_scalar(out=pkf[:], in0=posf[:], scalar1=-7.5, scalar2=0.0625, op0=ALU.add, op1=ALU.mult), reads=[posf], writes=[pkf])
        P.op("dve", lambda e: e.tensor_copy(out=pki[:], in_=pkf[:]), reads=[pkf], writes=[pki])
        P.op("dve", lambda e: e.tensor_copy(out=pkf[:], in_=pki[:]), reads=[pki], writes=[pkf])
        P.op("dve", lambda e: e.scalar_tensor_tensor(out=qkf[:], in0=pkf[:], scalar=-16.0, in1=posf[:], op0=ALU.mult, op1=ALU.add), reads=[pkf, posf], writes=[qkf])
        combos = [(hh, which, sel, dst) for hh in range(4) for (which, sel, dst) in ((0, pkf, abg[0]), (1, qkf, abg[1]))]
        for half in range(2):
            sub = combos[half * 4:(half + 1) * 4]
            for di, (hh, which, sel, dst) in enumerate(sub):
                hs = slice(hh * 2, hh * 2 + 2)
                dec = decs[di]
                P.op("dve", lambda e, sel=sel, hs=hs, dec=dec: e.tensor_tensor(out=dec[:], in0=iof[:, 0:16].unsqueeze(1).unsqueeze(1).to_broadcast([128, 2, 16, 16]),
                                                                            in1=sel[:, hs, :].unsqueeze(3).to_broadcast([128, 2, 16, 16]), op=ALU.is_equal),
                     reads=[iof, sel], writes=[dec])
            for di, (hh, which, sel, dst) in enumerate(sub):
                hs = slice(hh * 2, hh * 2 + 2)
                dec = decs[di]
                P.op("pool", lambda e, which=which, hs=hs, dec=dec: e.tensor_tensor(out=dec[:], in0=dec[:], in1=iv[:, hs, which, :].unsqueeze(2).to_broadcast([128, 2, 16, 16]), op=ALU.mult),
                     reads=[dec, ixf], writes=[dec])
            for di, (hh, which, sel, dst) in enumerate(sub):
                hs = slice(hh * 2, hh * 2 + 2)
                dec = decs[di]
                firstw = (hh == 0)
                P.op("dve", lambda e, dst=dst, hs=hs, dec=dec: e.tensor_reduce(out=dst[:, hs, :], in_=dec[:], axis=AX.X, op=ALU.add),
                     reads=[dec], writes=[dst] if firstw else [], parts=[] if firstw else [dst])
        if cstage == 34:
            return
        P.op("dve", lambda e: e.tensor_tensor(out=abg[2][:], in0=c16[:], in1=c16[:, :, 0:1].to_broadcast([128, 8, 16]), op=ALU.subtract), reads=[c16, c16b], writes=[abg[2]])
        P.op("act", lambda e: e.activation(out=abg[2][:], in_=abg[2][:], func=AF.Exp), reads=[abg[2]], writes=[abg[2]])
        P.op("dve", lambda e: e.tensor_reduce(out=z8[:], in_=abg[2][:], axis=AX.X, op=ALU.add), reads=[abg[2]], writes=[z8])
        P.op("dve", lambda e: e.reciprocal(out=z8[:], in_=z8[:]), reads=[z8], writes=[z8])
        P.op("dve", lambda e: e.tensor_tensor(out=abg[2][:], in0=abg[2][:], in1=z8[:].unsqueeze(2).to_broadcast([128, 8, 16]), op=ALU.mult), reads=[abg[2], z8], writes=[abg[2]])
        if cstage == 35:
            return
        for i3 in range(3):
            P.op("pe", lambda e, i3=i3: e.transpose(out=bank[6][:, i3 * 128:(i3 + 1) * 128], in_=abg[i3][:].rearrange("p h k -> p (h k)"), identity=c.identf[:]),
                 reads=[abg[i3], c.identf], writes=[bank[6]] if i3 == 0 else [], parts=[] if i3 == 0 else [bank[6]])
        for i3 in range(3):
            P.op("act", lambda e, i3=i3: e.copy(out=abgT[i3][:, ti * 128:(ti + 1) * 128], in_=bank[6][:, i3 * 128:(i3 + 1) * 128]), reads=[bank[6]], parts=[abgT[i3]])
        if cstage == 36:
            return
        for q8 in range(0, 128, CH):
            chn = (q8 // CH) % 2
            tg8 = ti * 128 + q8
            io_bc = iof[:].unsqueeze(1).to_broadcast([128, CH, 128])
            P.op("dve", lambda e, chn=chn, tg8=tg8, io_bc=io_bc: e.tensor_tensor(out=Ach[chn][:], in0=io_bc,
                                                                             in1=abgT[0][:, tg8:tg8 + CH].unsqueeze(2).to_broadcast([128, CH, 128]), op=ALU.is_equal),
                 reads=[iof, abgT[0]], writes=[Ach[chn]])
            P.op("dve", lambda e, chn=chn, tg8=tg8, io_bc=io_bc: e.tensor_tensor(out=Bch[chn][:], in0=io_bc,
                                                                             in1=abgT[1][:, tg8:tg8 + CH].unsqueeze(2).to_broadcast([128, CH, 128]), op=ALU.is_equal),
                 reads=[iof, abgT[1]], writes=[Bch[chn]])
            P.op("pool", lambda e, chn=chn, tg8=tg8: e.tensor_tensor(out=Bch[chn][:], in0=Bch[chn][:],
                                                                   in1=abgT[2][:, tg8:tg8 + CH].unsqueeze(2).to_broadcast([128, CH, 128]), op=ALU.mult),
                 reads=[Bch[chn], abgT[2]], writes=[Bch[chn]])
            for tq in range(q8, q8 + CH, 4):
                bk = bank[(tq // 4) % 2]
                for t in range(tq, tq + 4):
                    tl = t % CH
                    u = t - tq
                    P.op("pe", lambda e, chn=chn, tl=tl, u=u, bk=bk: e.matmul(out=bk[:, u * 128:(u + 1) * 128], lhsT=Ach[chn][:, tl, :], rhs=Bch[chn][:, tl, :], start=True, stop=True),
                         reads=[Ach[chn], Bch[chn]], writes=[bk] if u == 0 else [], parts=[] if u == 0 else [bk])
                tg0 = ti * 128 + tq
                P.op("act", lambda e, bk=bk, tg0=tg0: e.copy(out=Wg[:, :, tg0:tg0 + 4], in_=bk[:].rearrange("p (t c) -> p c t", c=128)),
                     reads=[bk], parts=[Wg])

    def expert_loop(n0, g):
        W = g * 128
        slots = {}

        def down(cb):
            sl = blk_ctr[0] % NS
            blk_ctr[0] += 1
            slots[cb] = sl
            P.load(esl[sl], esl[sl][:], ex_s[cb], dram_reads=[downT_t[cb], up_t[cb // 8]])
            sb_ = bank[6 + cb % 2]
            for k in range(8):
                P.op("pe", lambda e, k=k, sl=sl, sb_=sb_: e.matmul(out=sb_[:, 0:W], lhsT=esl[sl][:, k * 128:(k + 1) * 128], rhs=xnT[:, k, 0:W], start=(k == 0), stop=(k == 7)),
                     reads=[esl[sl], xnT], writes=[sb_] if k == 0 else [], parts=[] if k == 0 else [sb_])
            ab = actb[cb % 2]
            wb = wab[cb % 2]
            P.op("act", lambda e, ab=ab, sb_=sb_: e.activation(out=ab[:, 0:W], in_=sb_[:, 0:W], func=AF.Gelu), reads=[sb_], writes=[ab])
            P.op("dve", lambda e, ab=ab, wb=wb, cb=cb: e.tensor_tensor(out=wb[:, 0:W], in0=ab[:, 0:W], in1=Wg[:, cb, 0:W], op=ALU.mult), reads=[ab, Wg], writes=[wb])

        def up(cb):
            sl = slots[cb]
            wb = wab[cb % 2]
            for tt in range(g):
                for hf in range(2):
                    bk = bank[tt * 2 + hf]
                    P.op("pe", lambda e, tt=tt, hf=hf, bk=bk, wb=wb, sl=sl, cb=cb: e.matmul(out=bk[:], lhsT=wb[:, tt * 128:(tt + 1) * 128], rhs=esl[sl][:, D + hf * 512:D + (hf + 1) * 512],
                                                                                      start=(cb == 0), stop=(cb == 127)),
                         reads=[wb, esl[sl]], writes=[bk] if cb == 0 else [], parts=[] if cb == 0 else [bk])

        down(0)
        for cb in range(128):
            if cb + 1 < 128:
                down(cb + 1)
            up(cb)
        for tt in range(g):
            n = n0 + tt
            x_t = x1s[tt]
            for hf in range(2):
                bk = bank[tt * 2 + hf]
                P.op("dve", lambda e, bk=bk, hf=hf, x_t=x_t: e.tensor_tensor(out=x_t[:, hf * 512:(hf + 1) * 512], in0=bk[:], in1=x_t[:, hf * 512:(hf + 1) * 512], op=ALU.add),
                     reads=[bk, x_t], writes=[x_t])
            P.store(c.out[n * 128:(n + 1) * 128, :], x_t, x_t[:], dram_writes=[c.x1_t[n]], final=True)

    for (n0, g) in groups:
        for ti in range(g):
            route_tile(n0 + ti, ti)
        if 3 <= cstage <= 40:
            return
        if cstage == 50:
            continue
        expert_loop(n0, g)


from concourse.bass_utils import run_bass_kernel_spmd

W_NAMES = ["norm1_gain", "w_in", "ml_i_bias", "ml_f_bias", "q_norm_gain", "k_norm_gain", "attn_sinks", "ml_out_norm_gain",
           "w_branch_attn", "w_branch_mlstm", "w_out", "norm2_gain", "peer_w_query", "peer_sub_keys", "peer_down", "peer_up"]
PEER_NAMES = ["norm2_gain", "peer_w_query", "peer_sub_keys", "peer_down", "peer_up"]


def make_in_maps(inputs, NB, NT, ncores, phases="ABC"):
    consts = host_consts()
    S = NT * 128
    maps = []
    shared = {}
    for k in W_NAMES:
        if "C" not in phases and k in PEER_NAMES:
            continue
        shared[k] = np.ascontiguousarray(inputs[k][0])
    for k, v in consts.items():
        shared["c_" + k] = v
    for ci in range(ncores):
        m = dict(shared)
        m["x"] = np.ascontiguousarray(inputs["x"][ci * NB:(ci + 1) * NB, :S]).reshape(NB * S, D)
        m["positions"] = np.ascontiguousarray(inputs["positions"][ci * NB:(ci + 1) * NB, :S]).reshape(NB * S).astype(np.int32)
        maps.append(m)
    return maps


def kernel(**inputs):
    NB, NT, ncores = 2, 32, 8
    nc, _ = build_program(NB, NT, "ABC")
    maps = make_in_maps(inputs, NB, NT, ncores, "ABC")
    res = run_bass_kernel_spmd(nc, maps, core_ids=list(range(ncores)))
    outs = [r["out"].reshape(NB, NT * 128, D) for r in res.results]
    return np.concatenate(outs, axis=0).astype(np.float32)
```

```python
import numpy as np
import concourse.bass as bass
import concourse.mybir as mybir
from contextlib import ExitStack

F32 = mybir.dt.float32
BF16 = mybir.dt.bfloat16
I32 = mybir.dt.int32
U32 = mybir.dt.uint32
ALU = mybir.AluOpType
AF = mybir.ActivationFunctionType
AX = mybir.AxisListType

ENGS = ("pe", "act", "dve", "pool", "sp")


class T:
    __slots__ = ("name", "ap", "writers", "readers", "dsem", "dcount", "last_dma_read")

    def __init__(self, name, ap=None):
        self.name = name
        self.ap = ap
        self.writers = []
        self.readers = []
        self.dsem = None
        self.dcount = 0
        self.last_dma_read = None

    def __getitem__(self, k):
        return self.ap[k]


class Op:
    __slots__ = ("eng", "fn", "seq", "deps", "dma", "dsem", "dval", "signal", "sigval")

    def __init__(self, eng, fn):
        self.eng = eng
        self.fn = fn
        self.seq = None
        self.deps = []
        self.dma = False
        self.dsem = None
        self.dval = 0
        self.signal = False
        self.sigval = 0


class Prog:
    def __init__(self, nc):
        self.nc = nc
        self.stack = ExitStack()
        self.ops = []
        self.per_eng = {e: [] for e in ENGS}
        self.nsem = 0
        self.esem = {}
        for e in ENGS:
            if e != "sp":
                self.esem[e] = self.sem("s_" + e)
        self.stores = []
        self.scopes = []
        self.last_dma = {}

    def open_scope(self):
        self.scopes.append(ExitStack())

    def close_scope(self):
        self.barrier()
        self.scopes.pop().close()

    def barrier(self):
        last_c = []
        for e in ENGS:
            for o in reversed(self.per_eng[e]):
                if not o.dma and o.fn is not None:
                    last_c.append(o)
                    break
        deps = last_c + list(self.last_dma.values())
        for e in ENGS:
            op = Op(e, None)
            op.seq = len(self.per_eng[e])
            op.deps = list(deps)
            self.ops.append(op)
            self.per_eng[e].append(op)

    def sem(self, name):
        self.nsem += 1
        return self.stack.enter_context(self.nc.semaphore(name))

    def sb(self, name, shape, dt):
        stk = self.scopes[-1] if self.scopes else self.stack
        t = stk.enter_context(self.nc.sbuf_tensor(name, list(shape), dt))
        return T(name, t)

    def ps(self, name, shape, dt):
        t = self.stack.enter_context(self.nc.psum_tensor(name, list(shape), dt))
        return T(name, t)

    def dram(self, name, shape, dt, kind="Internal"):
        return self.nc.dram_tensor(name, list(shape), dt, kind=kind).ap()

    def _rec(self, eng, fn, reads, writes, parts=(), dma_tile=None, dma_is_read=False):
        op = Op(eng, fn)
        op.seq = len(self.per_eng[eng])
        deps = []
        for t in reads:
            deps.extend(t.writers)
        for t in writes:
            if t.readers:
                deps.extend(t.readers)
                deps.extend(t.writers)
                t.writers = [op]
                t.readers = []
            else:
                deps.extend(t.writers)
                t.writers = [op]
        for t in parts:
            if t.readers:
                deps.extend(t.readers)
                deps.extend(t.writers)
                t.writers = [op]
                t.readers = []
            else:
                t.writers = t.writers + [op]
        for t in reads:
            t.readers.append(op)
        if dma_tile is not None:
            op.dma = True
            if dma_tile.dsem is None:
                dma_tile.dsem = self.sem("d_" + dma_tile.name)
            if dma_is_read and dma_tile.last_dma_read is not None:
                deps.append(dma_tile.last_dma_read)
            dma_tile.dcount += 1
            op.dsem = dma_tile.dsem
            op.dval = 16 * dma_tile.dcount
            dma_tile.last_dma_read = op if dma_is_read else None
            self.last_dma[id(op.dsem)] = op
        op.deps = [d for d in deps if d is not op]
        self.ops.append(op)
        self.per_eng[eng].append(op)
        return op

    def op(self, eng, fn, reads=(), writes=(), parts=()):
        return self._rec(eng, fn, reads, writes, parts)

    def dma(self, out, in_, reads=(), writes=(), parts=(), sem_tile=None, is_read=False, eng="sp", **kw):
        def fn(e):
            return e.dma_start(out=out, in_=in_, **kw)
        return self._rec(eng, fn, reads, writes, parts, dma_tile=sem_tile, dma_is_read=is_read)

    def load(self, dst_tile, dst_ap, src_ap, part=False, dram_reads=(), eng="sp", **kw):
        if part:
            return self.dma(dst_ap, src_ap, reads=dram_reads, parts=(dst_tile,), sem_tile=dst_tile, eng=eng, **kw)
        return self.dma(dst_ap, src_ap, reads=dram_reads, writes=(dst_tile,), sem_tile=dst_tile, eng=eng, **kw)

    def store(self, dst_ap, src_tile, src_ap, dram_writes=(), dram_parts=(), final=False, eng="sp", **kw):
        o = self.dma(dst_ap, src_ap, reads=(src_tile,), writes=dram_writes, parts=dram_parts,
                     sem_tile=src_tile, is_read=True, eng=eng, **kw)
        if final:
            self.stores.append(o)
        return o

    def emit(self):
        nc = self.nc
        fin = Op("sp", None)
        fin.seq = len(self.per_eng["sp"])
        fin.deps = list(self.stores)
        self.ops.append(fin)
        self.per_eng["sp"].append(fin)

        clock = {e: {f: -1 for f in ENGS} for e in ENGS}
        dclock = {e: {} for e in ENGS}
        waits = {}
        for op in self.ops:
            X = op.eng
            need_e = {}
            need_d = {}
            for d in op.deps:
                if d.dma:
                    key = id(d.dsem)
                    if dclock[X].get(key, 0) >= d.dval:
                        continue
                    cur = need_d.get(key)
                    if cur is None or cur[1] < d.dval:
                        need_d[key] = (d.dsem, d.dval)
                else:
                    Y = d.eng
                    if Y == X and X == "pe":
                        continue
                    if clock[X][Y] >= d.seq:
                        continue
                    if need_e.get(Y, -1) < d.seq:
                        need_e[Y] = d.seq
            wl = []
            for Y, s in need_e.items():
                clock[X][Y] = s
                tgt = self.per_eng[Y][s]
                tgt.signal = True
                wl.append(("e", Y, tgt))
            for key, (sem, val) in need_d.items():
                dclock[X][key] = val
                wl.append(("d", sem, val))
            waits[id(op)] = wl
        for e in ENGS:
            c = 0
            for op in self.per_eng[e]:
                if op.signal and not op.dma:
                    c += 1
                    op.sigval = c
        self.sigmax = {e: max([o.sigval for o in self.per_eng[e]] + [0]) for e in ENGS}
        engobj = {"pe": "tensor", "act": "scalar", "dve": "vector", "pool": "gpsimd", "sp": "sync"}
        with nc.Block() as block:
            for e in ENGS:
                ops_e = self.per_eng[e]
                if not ops_e:
                    continue

                def body(eng, ops_e=ops_e, e=e):
                    for op in ops_e:
                        for w in waits[id(op)]:
                            if w[0] == "e":
                                eng.wait_ge(self.esem[w[1]], w[2].sigval)
                            else:
                                eng.wait_ge(w[1], w[2])
                        if op.fn is None:
                            continue
                        ins = op.fn(eng)
                        if op.dma:
                            ins.then_inc(op.dsem, 16)
                        elif op.signal:
                            ins.then_inc(self.esem[e], 1)

                getattr(block, engobj[e])(body)
        self.stack.close()


D = 1024
IN_W = 6416
EPS = 1e-6
NEG = -30000.0
TWO_PI = 6.283185


def host_consts():
    c = {}
    c["identf"] = np.eye(128, dtype=np.float32)
    k = np.arange(128)[:, None]
    q = np.arange(128)[None, :]
    m_prev = np.where(k > q, 0.0, NEG).astype(np.float32)
    m_cur = np.where(k <= q, 0.0, NEG).astype(np.float32)
    c["amask"] = np.stack([np.tile(m_prev, (1, 4)), np.tile(m_cur, (1, 4))], axis=1).astype(np.float32)
    c["cmask"] = np.tile((k <= q).astype(np.float32), (1, 4))
    invf = (500000.0 ** (-np.arange(0, 16, 2, dtype=np.float32) / 16.0)).astype(np.float32)
    c["invf"] = np.tile((invf / (2 * np.pi)).astype(np.float32)[None, :], (128, 1))
    onesab = np.zeros((128, 2, 128), np.float32)
    onesab[:, 0, 0:64] = 1.0
    onesab[:, 1, 64:128] = 1.0
    c["onesab"] = onesab
    c["iota128"] = np.tile(np.arange(128, dtype=np.float32)[None, :], (128, 1))
    return c


CONST_SHAPES = {"identf": [128, 128], "amask": [128, 2, 512], "cmask": [128, 512], "invf": [128, 8],
                "onesab": [128, 2, 128], "iota128": [128, 128]}


class Ctx:
    pass


def build_program(NB, NT, phases="ABC", dbg=False):
    NTT = NB * NT
    TOK = NTT * 128
    nc = bass.Bass("TRN2", target_bir_lowering=False)
    P = Prog(nc)
    c = Ctx()
    c.nc, c.P, c.NB, c.NT, c.NTT, c.TOK = nc, P, NB, NT, NTT, TOK
    c.phases = phases

    def din(name, shape, dt=F32):
        return nc.dram_tensor(name, list(shape), dt, kind="ExternalInput").ap()

    c.x = din("x", [TOK, D])
    c.pos = din("positions", [TOK], I32)
    c.norm1_gain = din("norm1_gain", [1, D])
    c.w_in = din("w_in", [D, IN_W])
    c.ml_i_bias = din("ml_i_bias", [1, 8])
    c.ml_f_bias = din("ml_f_bias", [1, 8])
    c.q_norm_gain = din("q_norm_gain", [1, 64])
    c.k_norm_gain = din("k_norm_gain", [1, 64])
    c.attn_sinks = din("attn_sinks", [1, 16])
    c.ml_out_norm_gain = din("ml_out_norm_gain", [1, D])
    c.w_branch_attn = din("w_branch_attn", [D, D])
    c.w_branch_mlstm = din("w_branch_mlstm", [D, D])
    c.w_out = din("w_out", [D, D])
    if "C" in phases:
        c.norm2_gain = din("norm2_gain", [1, D])
        c.peer_w_query = din("peer_w_query", [D, D])
        c.peer_sub_keys = din("peer_sub_keys", [8, 2, 128, 64])
        c.peer_down = din("peer_down", [16384, D])
        c.peer_up = din("peer_up", [16384, D])
    c.cst = {k: din("c_" + k, s) for k, s in CONST_SHAPES.items()}
    c.out = nc.dram_tensor("out", [TOK, D], F32, kind="ExternalOutput").ap()
    if dbg:
        c.mixa = nc.dram_tensor("mixa_s", [TOK, D], F32, kind="ExternalOutput").ap()
    else:
        c.mixa = P.dram("mixa_s", [TOK, D], F32)
    c.dbg = dbg
    c.dbg_outs = {}
    c.mixa_t = [T("mixa%d" % i) for i in range(NTT)]
    c.x1_t = [T("x1_%d" % i) for i in range(NTT)]

    c.identf = P.sb("identf", [128, 128], F32)
    c.identb = P.sb("identb", [128, 128], BF16)
    P.load(c.identf, c.identf[:], c.cst["identf"])
    P.op("dve", lambda e: e.tensor_copy(out=c.identb[:], in_=c.identf[:]), reads=[c.identf], writes=[c.identb])
    c.bank = [P.ps("bank%d" % i, [128, 512], F32) for i in range(8)]

    for ph, fn in (("A", phase_a), ("B", phase_b), ("C", phase_c)):
        if ph in phases:
            P.open_scope()
            fn(c)
            P.close_scope()
    P.emit()
    return nc, P


def rms_rstd(c, pfx, ssq, n, rstd, tmp):
    P = c.P
    P.op("dve", lambda e: e.tensor_scalar(out=tmp[:], in0=ssq[:], scalar1=1.0 / n, scalar2=EPS, op0=ALU.mult, op1=ALU.add),
         reads=[ssq], writes=[tmp])
    P.op("act", lambda e: e.activation(out=tmp[:], in_=tmp[:], func=AF.Sqrt), reads=[tmp], writes=[tmp])
    P.op("dve", lambda e: e.reciprocal(out=rstd[:], in_=tmp[:]), reads=[tmp], writes=[rstd])


def norm_and_transpose(c, xs, gain, hb, hT, junk, st, ptr_bank):
    P = c.P
    ssq, tmp, rstd = st
    P.op("act", lambda e: e.activation(out=junk[:], in_=xs[:], func=AF.Square, accum_out=ssq[:]),
         reads=[xs], writes=[junk, ssq])
    rms_rstd(c, "n", ssq, D, rstd, tmp)
    P.op("dve", lambda e: e.scalar_tensor_tensor(out=hb[:], in0=xs[:], scalar=rstd[:], in1=gain[:], op0=ALU.mult, op1=ALU.mult),
         reads=[xs, rstd, gain], writes=[hb])
    transpose8(c, hb, hT, ptr_bank)


def transpose8(c, src, dst, ptr_bank, eng="act"):
    P = c.P
    pv = ptr_bank[:].bitcast(BF16)
    for k in range(8):
        P.op("pe", lambda e, k=k: e.transpose(out=pv[:, k * 128:(k + 1) * 128], in_=src[:, k * 128:(k + 1) * 128], identity=c.identb[:]),
             reads=[src, c.identb], writes=[ptr_bank] if k == 0 else [], parts=[] if k == 0 else [ptr_bank])
    if eng == "act":
        P.op("act", lambda e: e.copy(out=dst[:].rearrange("p k t -> p (k t)"), in_=pv), reads=[ptr_bank], writes=[dst])
    else:
        P.op(eng, lambda e: e.tensor_copy(out=dst[:].rearrange("p k t -> p (k t)"), in_=pv), reads=[ptr_bank], writes=[dst])


def load_w_bf16(c, dst, col0, ncols, src, dcol0=0):
    P = c.P
    for k in range(8):
        P.load(dst, dst[:, k, dcol0:dcol0 + ncols], src[k * 128:(k + 1) * 128, col0:col0 + ncols], part=True, eng="pool",
               max_dma_last_dim=4096)


def rope_tables(c):
    P = c.P
    NTT = c.NTT
    posi = P.sb("posi", [128, NTT], I32)
    P.load(posi, posi[:], c.pos.rearrange("(n p) -> p n", p=128), allow_slow_non_contiguous=True)
    posf = P.sb("posf", [128, NTT], F32)
    P.op("dve", lambda e: e.tensor_copy(out=posf[:], in_=posi[:]), reads=[posi], writes=[posf])
    invf = P.sb("invf", [128, 8], F32)
    P.load(invf, invf[:], c.cst["invf"])
    y = P.sb("rope_y", [128, NTT, 16], F32)
    yi = P.sb("rope_yi", [128, NTT, 16], I32)
    yf = P.sb("rope_yf", [128, NTT, 16], F32)
    cs = P.sb("rope_cs", [128, NTT, 16], F32)
    pb = posf[:].unsqueeze(2).to_broadcast([128, NTT, 8])
    ib = invf[:].unsqueeze(1).to_broadcast([128, NTT, 8])
    P.op("dve", lambda e: e.tensor_tensor(out=y[:, :, 8:16], in0=pb, in1=ib, op=ALU.mult), reads=[posf, invf], writes=[y])
    P.op("dve", lambda e: e.tensor_scalar(out=y[:, :, 0:8], in0=y[:, :, 8:16], scalar1=0.25, scalar2=None, op0=ALU.add),
         reads=[y], writes=[y])
    P.op("dve", lambda e: e.tensor_copy(out=yi[:], in_=y[:]), reads=[y], writes=[yi])
    P.op("dve", lambda e: e.tensor_copy(out=yf[:], in_=yi[:]), reads=[yi], writes=[yf])
    P.op("dve", lambda e: e.tensor_tensor(out=y[:], in0=y[:], in1=yf[:], op=ALU.subtract), reads=[y, yf], writes=[y])
    P.op("act", lambda e: e.activation(out=cs[:], in_=y[:], func=AF.Sin, scale=TWO_PI), reads=[y], writes=[cs])
    return cs


def qk_norm_rope(c, pfx, src, nh, gain, cs_n, outb, tmps):
    P = c.P
    sq, ssq, tmp, rstd, qn, r1, r2 = tmps
    W = nh * 64
    s3 = src[:, 0:W].rearrange("p (h d) -> p h d", d=64)
    P.op("pool", lambda e: e.tensor_tensor(out=sq[:, 0:W], in0=src[:, 0:W], in1=src[:, 0:W], op=ALU.mult), reads=[src], writes=[sq])
    P.op("dve", lambda e: e.tensor_reduce(out=ssq[:, 0:nh], in_=sq[:, 0:W].rearrange("p (h d) -> p h d", d=64), axis=AX.X, op=ALU.add),
         reads=[sq], writes=[ssq])
    P.op("dve", lambda e: e.tensor_scalar(out=tmp[:, 0:nh], in0=ssq[:, 0:nh], scalar1=1.0 / 64, scalar2=EPS, op0=ALU.mult, op1=ALU.add),
         reads=[ssq], writes=[tmp])
    P.op("act", lambda e: e.activation(out=tmp[:, 0:nh], in_=tmp[:, 0:nh], func=AF.Sqrt), reads=[tmp], writes=[tmp])
    P.op("dve", lambda e: e.reciprocal(out=rstd[:, 0:nh], in_=tmp[:, 0:nh]), reads=[tmp], writes=[rstd])
    q3 = qn[:, 0:W].rearrange("p (h d) -> p h d", d=64)
    P.op("dve", lambda e: e.tensor_tensor(out=q3, in0=s3, in1=rstd[:, 0:nh].unsqueeze(2).to_broadcast([128, nh, 64]), op=ALU.mult),
         reads=[src, rstd], writes=[qn])
    P.op("pool", lambda e: e.tensor_tensor(out=q3, in0=q3, in1=gain[:].unsqueeze(1).to_broadcast([128, nh, 64]), op=ALU.mult),
         reads=[qn, gain], writes=[qn])
    P.op("act", lambda e: e.copy(out=outb[:], in_=q3), reads=[qn], writes=[outb])
    cosb = cs_n[:, 0:8].unsqueeze(1).to_broadcast([128, nh, 8])
    sinb = cs_n[:, 8:16].unsqueeze(1).to_broadcast([128, nh, 8])
    a3 = r1[:, 0:nh * 8].rearrange("p (h d) -> p h d", d=8)
    b3 = r2[:, 0:nh * 8].rearrange("p (h d) -> p h d", d=8)
    cst = c.cs
    P.op("dve", lambda e: e.tensor_tensor(out=a3, in0=q3[:, :, 0:8], in1=cosb, op=ALU.mult), reads=[qn, cst], writes=[r1])
    P.op("dve", lambda e: e.tensor_tensor(out=b3, in0=q3[:, :, 8:16], in1=sinb, op=ALU.mult), reads=[qn, cst], writes=[r2])
    P.op("dve", lambda e: e.tensor_tensor(out=outb[:, :, 0:8], in0=a3, in1=b3, op=ALU.subtract), reads=[r1, r2, outb], writes=[outb])
    P.op("dve", lambda e: e.tensor_tensor(out=a3, in0=q3[:, :, 8:16], in1=cosb, op=ALU.mult), reads=[qn, cst], writes=[r1])
    P.op("dve", lambda e: e.tensor_tensor(out=b3, in0=q3[:, :, 0:8], in1=sinb, op=ALU.mult), reads=[qn, cst], writes=[r2])
    P.op("dve", lambda e: e.tensor_tensor(out=outb[:, :, 8:16], in0=a3, in1=b3, op=ALU.add), reads=[r1, r2, outb], writes=[outb])


def phase_a(c):
    P, NB, NT, NTT = c.P, c.NB, c.NT, c.NTT
    bank = c.bank
    c.g1 = P.sb("a_g1", [128, D], F32)
    P.load(c.g1, c.g1[:], c.norm1_gain[0].partition_broadcast(128))
    wA = P.sb("wA", [128, 8, 2304], BF16)
    load_w_bf16(c, wA, 0, 1280, c.w_in, 0)
    load_w_bf16(c, wA, 4368, 1024, c.w_in, 1280)
    wba = P.sb("wba", [128, 8, D], BF16)
    load_w_bf16(c, wba, 0, D, c.w_branch_attn)
    gq = P.sb("gq", [128, 64], F32)
    gk = P.sb("gk", [128, 64], F32)
    P.load(gq, gq[:], c.q_norm_gain[0].partition_broadcast(128))
    P.load(gk, gk[:], c.k_norm_gain[0].partition_broadcast(128))
    sk = P.sb("sk", [128, 16], F32)
    P.load(sk, sk[:], c.attn_sinks[0].partition_broadcast(128))
    sinkp = P.sb("sinkp", [128, 8], F32)
    sk3 = sk[:].rearrange("p (i two) -> p i two", two=2)
    P.op("dve", lambda e: e.tensor_copy(out=sinkp[0:64, :], in_=sk3[0:64, :, 0]), reads=[sk], writes=[sinkp])
    P.op("dve", lambda e: e.tensor_copy(out=sinkp[64:128, :], in_=sk3[64:128, :, 1]), reads=[sk], parts=[sinkp])
    P.op("act", lambda e: e.activation(out=sinkp[:], in_=sinkp[:], func=AF.Exp), reads=[sinkp], writes=[sinkp])
    amaskf = P.sb("amaskf", [128, 2, 512], F32)
    P.load(amaskf, amaskf[:], c.cst["amask"])
    amask = P.sb("amask", [128, 2, 512], BF16)
    P.op("dve", lambda e: e.tensor_copy(out=amask[:], in_=amaskf[:]), reads=[amaskf], writes=[amask])
    onesf = P.sb("onesf", [128, 2, 128], F32)
    P.load(onesf, onesf[:], c.cst["onesab"])
    onesab = P.sb("onesab", [128, 2, 128], BF16)
    P.op("dve", lambda e: e.tensor_copy(out=onesab[:], in_=onesf[:]), reads=[onesf], writes=[onesab])
    c.cs = rope_tables(c)

    xs = [P.sb("a_xs%d" % i, [128, D], F32) for i in range(2)]
    kT = [P.sb("a_kT%d" % i, [128, 2, 128], BF16) for i in range(2)]
    vA = [P.sb("a_vA%d" % i, [128, 2, 128], BF16) for i in range(2)]
    vB = [P.sb("a_vB%d" % i, [128, 2, 128], BF16) for i in range(2)]
    for i in range(2):
        P.op("pool", lambda e, i=i: e.memset(vA[i][:], 0.0), writes=[vA[i]])
        P.op("pool", lambda e, i=i: e.memset(vB[i][:], 0.0), writes=[vB[i]])
    SETS = []
    for si in range(2):
        sx = "s%d_" % si
        junk = P.sb(sx + "a_junk", [128, D], BF16)
        st = (P.sb(sx + "a_ssq", [128, 1], F32), P.sb(sx + "a_tmp", [128, 1], F32), P.sb(sx + "a_rstd", [128, 1], F32))
        hb = P.sb(sx + "a_hb", [128, D], BF16)
        hT = P.sb(sx + "a_hT", [128, 8, 128], BF16)
        qf = P.sb(sx + "a_qf", [128, D], F32)
        kvf = P.sb(sx + "a_kvf", [128, 256], F32)
        sga = P.sb(sx + "a_sga", [128, D], F32)
        tmps = (P.sb(sx + "a_sq", [128, D], F32), P.sb(sx + "a_ssq16", [128, 16], F32), P.sb(sx + "a_tmp16", [128, 16], F32),
                P.sb(sx + "a_rstd16", [128, 16], F32), P.sb(sx + "a_qn", [128, D], F32), P.sb(sx + "a_r1", [128, 128], F32), P.sb(sx + "a_r2", [128, 128], F32))
        qb = P.sb(sx + "a_qb", [128, 16, 64], BF16)
        kb = P.sb(sx + "a_kb", [128, 2, 64], BF16)
        kdup = P.sb(sx + "a_kdup", [128, 2, 2, 64], BF16)
        qT = P.sb(sx + "a_qT", [128, 8, 128], BF16)
        PT = [[[P.sb(sx + "a_PT%d%d%d" % (g, k, h), [128, 4, 128], BF16) for h in range(2)] for k in range(2)] for g in range(2)]
        rden = P.sb(sx + "a_rden", [128, 4, 128], F32)
        attT = P.sb(sx + "a_attT", [128, 8, 128], BF16)

        SETS.append((junk, st, hb, hT, qf, kvf, sga, tmps, qb, kb, kdup, qT, PT, rden, attT))
    mo = [P.sb("a_mo%d" % i, [128, D], F32) for i in range(2)]

    def load_x(n):
        P.load(xs[n % 2], xs[n % 2][:], c.x[n * 128:(n + 1) * 128, :])

    load_x(0)

    def tile(n):
        j = n % NT
        cur = n % 2
        (junk, st, hb, hT, qf, kvf, sga, tmps, qb, kb, kdup, qT, PT, rden, attT) = SETS[n % 2]
        prv = 1 - cur
        if n + 1 < NTT:
            load_x(n + 1)
        x_t = xs[cur]
        norm_and_transpose(c, x_t, c.g1, hb, hT, junk, st, bank[7])
        def proj(bk, col0, ncols):
            for k in range(8):
                P.op("pe", lambda e, k=k: e.matmul(out=bk[:, 0:ncols], lhsT=hT[:, k, :], rhs=wA[:, k, col0:col0 + ncols],
                                                   start=(k == 0), stop=(k == 7)),
                     reads=[hT, wA], writes=[bk] if k == 0 else [], parts=[] if k == 0 else [bk])
        proj(bank[0], 0, 512)
        P.op("dve", lambda e: e.tensor_copy(out=qf[:, 0:512], in_=bank[0][:]), reads=[bank[0]], writes=[qf])
        proj(bank[1], 512, 512)
        P.op("act", lambda e: e.copy(out=qf[:, 512:1024], in_=bank[1][:]), reads=[bank[1]], parts=[qf])
        proj(bank[2], 1024, 256)
        P.op("dve", lambda e: e.tensor_copy(out=kvf[:], in_=bank[2][:, 0:256]), reads=[bank[2]], writes=[kvf])
        proj(bank[3], 1280, 512)
        P.op("act", lambda e: e.activation(out=sga[:, 0:512], in_=bank[3][:], func=AF.Sigmoid), reads=[bank[3]], writes=[sga])
        proj(bank[4], 1792, 512)
        P.op("act", lambda e: e.activation(out=sga[:, 512:1024], in_=bank[4][:], func=AF.Sigmoid), reads=[bank[4]], parts=[sga])
        cs_n = c.cs[:, n, :]
        qk_norm_rope(c, "q", qf, 16, gq, cs_n, qb, tmps)
        qk_norm_rope(c, "k", kvf, 2, gk, cs_n, kb, tmps)
        P.op("pool", lambda e: e.tensor_copy(out=kdup[:, :, 0, :], in_=kb[:]), reads=[kb], writes=[kdup])
        P.op("pool", lambda e: e.tensor_copy(out=kdup[:, :, 1, :], in_=kb[:]), reads=[kb], parts=[kdup])
        v3 = kvf[:, 128:256].rearrange("p (g d) -> p g d", d=64)
        P.op("pool", lambda e: e.tensor_copy(out=vA[cur][:, :, 0:64], in_=v3), reads=[kvf], writes=[vA[cur]])
        P.op("pool", lambda e: e.tensor_copy(out=vB[cur][:, :, 64:128], in_=v3), reads=[kvf], writes=[vB[cur]])
        pv = bank[7][:].bitcast(BF16)
        qflat = qb[:].rearrange("p h d -> p (h d)")
        for k in range(8):
            P.op("pe", lambda e, k=k: e.transpose(out=pv[:, k * 128:(k + 1) * 128], in_=qflat[:, k * 128:(k + 1) * 128], identity=c.identb[:]),
                 reads=[qb, c.identb], writes=[bank[7]] if k == 0 else [], parts=[] if k == 0 else [bank[7]])
        P.op("act", lambda e: e.copy(out=qT[:].rearrange("p k t -> p (k t)"), in_=pv), reads=[bank[7]], writes=[qT])
        pv6 = bank[6][:].bitcast(BF16)
        kflat = kdup[:].rearrange("p g u d -> p (g u d)")
        for g in range(2):
            P.op("pe", lambda e, g=g: e.transpose(out=pv6[:, g * 128:(g + 1) * 128], in_=kflat[:, g * 128:(g + 1) * 128], identity=c.identb[:]),
                 reads=[kdup, c.identb], writes=[bank[6]] if g == 0 else [], parts=[] if g == 0 else [bank[6]])
        P.op("dve", lambda e: e.tensor_copy(out=kT[cur][:].rearrange("p g t -> p (g t)"), in_=pv6[:, 0:256]), reads=[bank[6]], writes=[kT[cur]])
        kbs = [1] if j == 0 else [0, 1]
        bi = 0
        for g in range(2):
            for kk in kbs:
                slot = cur if kk == 1 else prv
                for hh in range(2):
                    bk = bank[bi % 6]
                    bi += 1
                    P.op("pe", lambda e, bk=bk, slot=slot, g=g, hh=hh: e.matmul(
                        out=bk[:], lhsT=kT[slot][hh * 64:(hh + 1) * 64, g, :],
                        rhs=qT[hh * 64:(hh + 1) * 64, 4 * g:4 * g + 4, :], start=True, stop=False),
                        reads=[kT[slot], qT], writes=[bk])
                    P.op("pe", lambda e, bk=bk, kk=kk: e.matmul(out=bk[:], lhsT=c.identb[:], rhs=amask[:, kk, :], start=False, stop=True),
                         reads=[c.identb, amask], parts=[bk])
                    pt = PT[g][kk][hh]
                    P.op("act", lambda e, bk=bk, pt=pt: e.activation(out=pt[:].rearrange("p i t -> p (i t)"), in_=bk[:], func=AF.Exp, scale=0.125),
                         reads=[bk], writes=[pt])
        for g in range(2):
            pav = bank[6]
            pden = bank[7]
            first_av = True
            for p in range(4):
                combos = [(kk, hh) for kk in kbs for hh in range(2)]
                for ci, (kk, hh) in enumerate(combos):
                    slot = cur if kk == 1 else prv
                    vt = vA[slot] if hh == 0 else vB[slot]
                    pt = PT[g][kk][hh]
                    w_first = first_av
                    P.op("pe", lambda e, vt=vt, pt=pt, p=p, g=g, ci=ci, ncmb=len(combos): e.matmul(
                        out=pav[:, p * 128:(p + 1) * 128], lhsT=vt[:, g, :], rhs=pt[:, p, :], start=(ci == 0), stop=(ci == ncmb - 1)),
                        reads=[vt, pt], writes=[pav] if w_first else [], parts=[] if w_first else [pav])
                    P.op("pe", lambda e, pt=pt, p=p, hh=hh, ci=ci, ncmb=len(combos): e.matmul(
                        out=pden[:, p * 128:(p + 1) * 128], lhsT=onesab[:, hh, :], rhs=pt[:, p, :], start=(ci == 0), stop=(ci == ncmb - 1)),
                        reads=[onesab, pt], writes=[pden] if w_first else [], parts=[] if w_first else [pden])
                    first_av = False
            P.op("dve", lambda e, g=g: e.tensor_tensor(out=rden[:], in0=pden[:].rearrange("p (i t) -> p i t", t=128),
                                                       in1=sinkp[:, 4 * g:4 * g + 4].unsqueeze(2).to_broadcast([128, 4, 128]), op=ALU.add),
                 reads=[pden, sinkp], writes=[rden])
            P.op("dve", lambda e: e.reciprocal(out=rden[:], in_=rden[:]), reads=[rden], writes=[rden])
            P.op("dve", lambda e, g=g: e.tensor_tensor(out=attT[:, 4 * g:4 * g + 4, :], in0=pav[:].rearrange("p (i t) -> p i t", t=128),
                                                       in1=rden[:], op=ALU.mult),
                 reads=[pav, rden], writes=[attT] if g == 0 else [], parts=[] if g == 0 else [attT])
        m_t = mo[n % 2]
        for hn in range(2):
            bk = bank[hn]
            for i in range(8):
                P.op("pe", lambda e, bk=bk, i=i, hn=hn: e.matmul(out=bk[:], lhsT=attT[:, i, :], rhs=wba[:, i, hn * 512:(hn + 1) * 512],
                                                               start=(i == 0), stop=(i == 7)),
                     reads=[attT, wba], writes=[bk] if i == 0 else [], parts=[] if i == 0 else [bk])
            P.op("dve", lambda e, bk=bk, hn=hn: e.tensor_tensor(out=m_t[:, hn * 512:(hn + 1) * 512], in0=bk[:], in1=sga[:, hn * 512:(hn + 1) * 512], op=ALU.mult),
                 reads=[bk, sga], writes=[m_t] if hn == 0 else [], parts=[] if hn == 0 else [m_t])
        P.store(c.mixa[n * 128:(n + 1) * 128, :], m_t, m_t[:], dram_writes=[c.mixa_t[n]], final=c.dbg)
        if c.dbg and n == c.dbg - 1:
            for nm, tl, shp, dt in (("qb", qb, [128, 1024], BF16), ("kb", kb, [128, 128], BF16), ("attT", attT, [128, 1024], BF16),
                                    ("qf", qf, [128, 1024], F32), ("sga", sga, [128, 1024], F32), ("hT", hT, [128, 1024], BF16),
                                    ("PT", PT[0][1][0], [128, 512], BF16), ("rden", rden, [128, 512], F32), ("qT", qT, [128, 1024], BF16),
                                    ("kT", kT[cur], [128, 256], BF16), ("kdup", kdup, [128, 256], BF16), ("vA", vA[cur], [128, 256], BF16)):
                d_ap = c.nc.dram_tensor("dbg_" + nm, shp, dt, kind="ExternalOutput").ap()
                flat = tl[:]
                if len(flat.shape) == 3:
                    flat = flat.rearrange("p a b -> p (a b)")
                if len(flat.shape) == 4:
                    flat = flat.rearrange("p a b c -> p (a b c)")
                P.store(d_ap, tl, flat, final=True)

    for n in range(NTT):
        tile(n)


def phase_b(c):
    P, NB, NT, NTT = c.P, c.NB, c.NT, c.NTT
    bank = c.bank
    c.g1 = P.sb("b_g1", [128, D], F32)
    P.load(c.g1, c.g1[:], c.norm1_gain[0].partition_broadcast(128))
    wB = P.sb("wB", [128, 8, 4112], BF16)
    load_w_bf16(c, wB, 1280, 3088, c.w_in, 0)
    load_w_bf16(c, wB, 5392, 1024, c.w_in, 3088)
    wbm = P.sb("wbm", [128, 8, D], BF16)
    load_w_bf16(c, wbm, 0, D, c.w_branch_mlstm)
    wout = P.sb("wout", [128, 8, D], BF16)
    load_w_bf16(c, wout, 0, D, c.w_out)
    mlg = P.sb("mlg", [128, D], F32)
    P.load(mlg, mlg[:], c.ml_out_norm_gain[0].partition_broadcast(128))
    cmask = P.sb("cmask", [128, 512], F32)
    P.load(cmask, cmask[:], c.cst["cmask"])
    bif = P.sb("b_bif", [8, 2], F32)
    P.load(bif, bif[:, 0:1], c.ml_i_bias.rearrange("o h -> h o"), allow_slow_non_contiguous=True)
    P.load(bif, bif[:, 1:2], c.ml_f_bias.rearrange("o h -> h o"), part=True, allow_slow_non_contiguous=True)
    P.op("dve", lambda e: e.tensor_scalar(out=bif[:], in0=bif[:], scalar1=1.0 / 15.0, scalar2=None, op0=ALU.mult), reads=[bif], writes=[bif])

    xs = [P.sb("b_xs%d" % i, [128, D], F32) for i in range(2)]
    ma = [P.sb("b_ma%d" % i, [128, D], F32) for i in range(2)]
    junk = P.sb("b_junk", [128, D], BF16)
    st = (P.sb("b_ssq", [128, 1], F32), P.sb("b_tmp", [128, 1], F32), P.sb("b_rstd", [128, 1], F32))
    hb = P.sb("b_hb", [128, D], BF16)
    hT = P.sb("b_hT", [128, 8, 128], BF16)
    g_ti = P.sb("g_ti", [8, 128], F32)
    g_tf = P.sb("g_tf", [8, 128], F32)
    g_nl = P.sb("g_nl", [8, 128], F32)
    g_cum = P.sb("g_cum", [8, 128], F32)
    g_a = P.sb("g_a", [8, 128], F32)
    g_M = P.sb("g_M", [8, 128], F32)
    g_d2 = P.sb("g_d2", [8, 128], F32)
    g_ones = P.sb("g_ones", [8, 128], F32)
    P.op("pool", lambda e: e.memset(g_ones[:], 1.0), writes=[g_ones])
    g_out = [P.sb("g_out%d" % i, [8, 128], F32) for i in range(5)]
    cumc = [P.sb("g_cumc%d" % b, [8, 1], F32) for b in range(NB)]
    Mc = [P.sb("g_Mc%d" % b, [8, 1], F32) for b in range(NB)]
    g_nM0 = P.sb("g_nM0", [8, 1], F32)
    g_nMe = P.sb("g_nMe", [8, 1], F32)
    g_dd = P.sb("g_dd", [8, 1], F32)
    gtok = P.sb("b_gtok", [128, 5, 8], F32)
    qt = P.sb("b_qt", [128, 512], BF16)
    kt = P.sb("b_kt", [128, 512], BF16)
    khat = P.sb("b_khat", [128, 512], BF16)
    vaug = P.sb("b_vaug", [128, 8, 129], BF16)
    P.op("pool", lambda e: e.memset(vaug[:], 1.0), writes=[vaug])
    sgo = P.sb("b_sgo", [128, D], F32)
    sgm = P.sb("b_sgm", [128, D], F32)
    qtT = P.sb("b_qtT", [64, 8, 128], BF16)
    ktT = P.sb("b_ktT", [64, 8, 128], BF16)
    PTm = P.sb("b_PT", [128, 8, 128], BF16)
    C32 = [P.sb("b_C32_%d" % b, [64, 8, 129], F32) for b in range(NB)]
    Cb = [P.sb("b_Cb_%d" % b, [64, 8, 129], BF16) for b in range(NB)]
    dmax = P.sb("b_dmax", [128, 8], F32)
    rc = P.sb("b_rc", [128, 8], F32)
    hm = P.sb("b_hm", [128, 8, 128], F32)
    hsq = P.sb("b_hsq", [128, 8, 128], F32)
    hs8 = (P.sb("b_hssq", [128, 8], F32), P.sb("b_htmp", [128, 8], F32), P.sb("b_hrstd", [128, 8], F32))
    hn = P.sb("b_hn", [128, D], BF16)
    hnT = P.sb("b_hnT", [128, 8, 128], BF16)
    mx = P.sb("b_mx", [128, D], F32)
    mxb = P.sb("b_mxb", [128, D], BF16)
    mxT = P.sb("b_mxT", [128, 8, 128], BF16)
    xo = [P.sb("b_xo%d" % i, [128, D], F32) for i in range(2)]
    HG = [(0, 3), (3, 6), (6, 8)]

    def load_in(n):
        P.load(xs[n % 2], xs[n % 2][:], c.x[n * 128:(n + 1) * 128, :])
        P.load(ma[n % 2], ma[n % 2][:], c.mixa[n * 128:(n + 1) * 128, :], dram_reads=[c.mixa_t[n]])

    load_in(0)

    def tile(n):
        b = n // NT
        j = n % NT
        if n + 1 < NTT:
            load_in(n + 1)
        x_t = xs[n % 2]
        ma_t = ma[n % 2]
        norm_and_transpose(c, x_t, c.g1, hb, hT, junk, st, bank[7])

        def proj(bk, col0, ncols, M=128, ocol=0):
            for k in range(8):
                P.op("pe", lambda e, k=k: e.matmul(out=bk[0:M, ocol:ocol + ncols], lhsT=hT[:, k, :], rhs=wB[:, k, col0:col0 + ncols],
                                                   start=(k == 0), stop=(k == 7)),
                     reads=[hT, wB], writes=[bk] if (k == 0 and ocol == 0) else [], parts=[] if (k == 0 and ocol == 0) else [bk])
        for gi, col in enumerate((2048, 2056)):
            for k in range(8):
                P.op("pe", lambda e, k=k, gi=gi, col=col: e.matmul(out=bank[6][0:8, gi * 128:(gi + 1) * 128], lhsT=wB[:, k, col:col + 8], rhs=hT[:, k, :],
                                                                    start=(k == 0), stop=(k == 7)),
                     reads=[hT, wB], writes=[bank[6]] if (k == 0 and gi == 0) else [], parts=[] if (k == 0 and gi == 0) else [bank[6]])
        P.op("act", lambda e: e.activation(out=g_ti[:], in_=bank[6][0:8, 0:128], func=AF.Tanh, bias=bif[:, 0:1], scale=1.0 / 15.0),
             reads=[bank[6], bif], writes=[g_ti])
        P.op("act", lambda e: e.activation(out=g_tf[:], in_=bank[6][0:8, 128:256], func=AF.Tanh, bias=bif[:, 1:2], scale=1.0 / 15.0),
             reads=[bank[6], bif], writes=[g_tf])
        P.op("act", lambda e: e.activation(out=g_nl[:], in_=g_tf[:], func=AF.Exp, scale=-15.0), reads=[g_tf], writes=[g_nl])
        P.op("act", lambda e: e.activation(out=g_nl[:], in_=g_nl[:], func=AF.Ln, bias=1.0), reads=[g_nl], writes=[g_nl])
        if j == 0:
            P.op("dve", lambda e: e.tensor_tensor_scan(out=g_cum[:], data0=g_ones[:], data1=g_nl[:], initial=0.0, op0=ALU.mult, op1=ALU.add),
                 reads=[g_ones, g_nl], writes=[g_cum])
        else:
            P.op("dve", lambda e: e.tensor_tensor_scan(out=g_cum[:], data0=g_ones[:], data1=g_nl[:], initial=cumc[b][:], op0=ALU.mult, op1=ALU.add),
                 reads=[g_ones, g_nl, cumc[b]], writes=[g_cum])
        P.op("dve", lambda e: e.scalar_tensor_tensor(out=g_a[:], in0=g_ti[:], scalar=15.0, in1=g_cum[:], op0=ALU.mult, op1=ALU.add),
             reads=[g_ti, g_cum], writes=[g_a])
        if j == 0:
            P.op("dve", lambda e: e.memset(Mc[b][:], 0.0), writes=[Mc[b]])
        P.op("dve", lambda e: e.tensor_tensor_scan(out=g_M[:], data0=g_a[:], data1=g_a[:], initial=Mc[b][:], op0=ALU.max, op1=ALU.max),
             reads=[g_a, Mc[b]], writes=[g_M])
        P.op("dve", lambda e: e.tensor_scalar(out=g_nM0[:], in0=Mc[b][:], scalar1=-1.0, scalar2=None, op0=ALU.mult), reads=[Mc[b]], writes=[g_nM0])
        P.op("dve", lambda e: e.tensor_scalar(out=g_nMe[:], in0=g_M[:, 127:128], scalar1=-1.0, scalar2=None, op0=ALU.mult), reads=[g_M], writes=[g_nMe])
        P.op("dve", lambda e: e.tensor_tensor(out=g_dd[:], in0=Mc[b][:], in1=g_nMe[:], op=ALU.add), reads=[Mc[b], g_nMe], writes=[g_dd])
        P.op("dve", lambda e: e.tensor_tensor(out=g_d2[:], in0=g_cum[:], in1=g_M[:], op=ALU.subtract), reads=[g_cum, g_M], writes=[g_d2])
        P.op("act", lambda e: e.activation(out=g_out[0][:], in_=g_M[:], func=AF.Exp, bias=Mc[b][:], scale=-1.0), reads=[g_M, Mc[b]], writes=[g_out[0]])
        P.op("act", lambda e: e.activation(out=g_out[1][:], in_=g_a[:], func=AF.Exp, bias=g_nM0[:], scale=1.0), reads=[g_a, g_nM0], writes=[g_out[1]])
        P.op("act", lambda e: e.activation(out=g_out[2][:], in_=g_a[:], func=AF.Exp, bias=g_nMe[:], scale=1.0), reads=[g_a, g_nMe], writes=[g_out[2]])
        P.op("act", lambda e: e.activation(out=g_out[3][:], in_=g_a[:], func=AF.Exp, bias=g_dd[:], scale=0.0), reads=[g_a, g_dd], writes=[g_out[3]])
        P.op("act", lambda e: e.activation(out=g_out[4][:], in_=g_d2[:], func=AF.Exp), reads=[g_d2], writes=[g_out[4]])
        P.op("dve", lambda e: e.tensor_copy(out=cumc[b][:], in_=g_cum[:, 127:128]), reads=[g_cum], writes=[cumc[b]])
        P.op("dve", lambda e: e.tensor_copy(out=Mc[b][:], in_=g_M[:, 127:128]), reads=[g_M], writes=[Mc[b]])
        for qi in range(5):
            P.op("pe", lambda e, qi=qi: e.transpose(out=bank[6][:, 256 + qi * 8:256 + (qi + 1) * 8], in_=g_out[qi][:], identity=c.identf[0:8, 0:8]),
                 reads=[g_out[qi], c.identf], writes=[bank[6]] if qi == 0 else [], parts=[] if qi == 0 else [bank[6]])
        P.op("dve", lambda e: e.tensor_copy(out=gtok[:].rearrange("p q h -> p (q h)"), in_=bank[6][:, 256:296]), reads=[bank[6]], writes=[gtok])
        proj(bank[0], 0, 512)
        proj(bank[1], 512, 512)
        P.op("dve", lambda e: e.tensor_tensor(out=qt[:].rearrange("p (h d) -> p h d", d=64), in0=bank[0][:].rearrange("p (h d) -> p h d", d=64),
                                              in1=gtok[:, 0, :].unsqueeze(2).to_broadcast([128, 8, 64]), op=ALU.mult),
             reads=[bank[0], gtok], writes=[qt])
        P.op("dve", lambda e: e.scalar_tensor_tensor(out=kt[:].rearrange("p (h d) -> p h d", d=64), in0=bank[1][:].rearrange("p (h d) -> p h d", d=64),
                                                     scalar=0.125, in1=gtok[:, 1, :].unsqueeze(2).to_broadcast([128, 8, 64]), op0=ALU.mult, op1=ALU.mult),
             reads=[bank[1], gtok], writes=[kt])
        P.op("dve", lambda e: e.scalar_tensor_tensor(out=khat[:].rearrange("p (h d) -> p h d", d=64), in0=bank[1][:].rearrange("p (h d) -> p h d", d=64),
                                                     scalar=0.125, in1=gtok[:, 2, :].unsqueeze(2).to_broadcast([128, 8, 64]), op0=ALU.mult, op1=ALU.mult),
             reads=[bank[1], gtok], writes=[khat])
        for hv in range(2):
            proj(bank[2 + hv], 1024 + hv * 512, 512)
            P.op("act", lambda e, hv=hv: e.copy(out=vaug[:, 4 * hv:4 * hv + 4, 0:128], in_=bank[2 + hv][:].rearrange("p (h d) -> p h d", d=128)),
                 reads=[bank[2 + hv]], writes=[vaug] if hv == 0 else [], parts=[] if hv == 0 else [vaug])
        for hv in range(2):
            proj(bank[4 + hv], 2064 + hv * 512, 512)
            P.op("act", lambda e, hv=hv: e.activation(out=sgo[:, hv * 512:(hv + 1) * 512], in_=bank[4 + hv][:], func=AF.Sigmoid),
                 reads=[bank[4 + hv]], writes=[sgo] if hv == 0 else [], parts=[] if hv == 0 else [sgo])
        for hv in range(2):
            proj(bank[2 + hv], 3088 + hv * 512, 512)
            P.op("act", lambda e, hv=hv: e.activation(out=sgm[:, hv * 512:(hv + 1) * 512], in_=bank[2 + hv][:], func=AF.Sigmoid),
                 reads=[bank[2 + hv]], writes=[sgm] if hv == 0 else [], parts=[] if hv == 0 else [sgm])
        for (src, dstT, bk, eng) in ((qt, qtT, bank[7], "act"), (kt, ktT, bank[6], "dve")):
            pv = bk[:].bitcast(BF16)
            for h in range(8):
                P.op("pe", lambda e, h=h, src=src, pv=pv: e.transpose(out=pv[0:64, h * 128:(h + 1) * 128], in_=src[:, h * 64:(h + 1) * 64], identity=c.identb[:]),
                     reads=[src, c.identb], writes=[bk] if h == 0 else [], parts=[] if h == 0 else [bk])
            if eng == "act":
                P.op("act", lambda e, pv=pv, dstT=dstT: e.copy(out=dstT[:].rearrange("p h t -> p (h t)"), in_=pv[0:64, :]), reads=[bk], writes=[dstT])
            else:
                P.op("dve", lambda e, pv=pv, dstT=dstT: e.tensor_copy(out=dstT[:].rearrange("p h t -> p (h t)"), in_=pv[0:64, :]), reads=[bk], writes=[dstT])
        for hb4 in range(2):
            bk = bank[hb4]
            for hh in range(4):
                h = hb4 * 4 + hh
                P.op("pe", lambda e, h=h, hh=hh, bk=bk: e.matmul(out=bk[:, hh * 128:(hh + 1) * 128], lhsT=ktT[:, h, :], rhs=qtT[:, h, :], start=True, stop=True),
                     reads=[ktT, qtT], writes=[bk] if hh == 0 else [], parts=[] if hh == 0 else [bk])
            P.op("dve", lambda e, bk=bk, hb4=hb4: e.tensor_tensor(out=PTm[:, 4 * hb4:4 * hb4 + 4, :].rearrange("p h t -> p (h t)"), in0=bk[:], in1=cmask[:], op=ALU.mult),
                 reads=[bk, cmask], writes=[PTm] if hb4 == 0 else [], parts=[] if hb4 == 0 else [PTm])
        for gi, (h0, h1) in enumerate(HG):
            bk = bank[2 + gi]
            for h in range(h0, h1):
                o = (h - h0) * 129
                P.op("pe", lambda e, h=h, o=o, bk=bk: e.matmul(out=bk[:, o:o + 129], lhsT=PTm[:, h, :], rhs=vaug[:, h, :], start=True, stop=(j == 0)),
                     reads=[PTm, vaug], writes=[bk] if h == h0 else [], parts=[] if h == h0 else [bk])
                if j > 0:
                    P.op("pe", lambda e, h=h, o=o, bk=bk: e.matmul(out=bk[:, o:o + 129], lhsT=qtT[:, h, :], rhs=Cb[b][:, h, :], start=False, stop=True),
                         reads=[qtT, Cb[b]], parts=[bk])
        for gi, (h0, h1) in enumerate(HG):
            bk = bank[5 + gi]
            for h in range(h0, h1):
                o = (h - h0) * 129
                P.op("pe", lambda e, h=h, o=o, bk=bk: e.matmul(out=bk[0:64, o:o + 129], lhsT=khat[:, h * 64:(h + 1) * 64], rhs=vaug[:, h, :], start=True, stop=True),
                     reads=[khat, vaug], writes=[bk] if h == h0 else [], parts=[] if h == h0 else [bk])
        if j > 0:
            P.op("dve", lambda e: e.tensor_tensor(out=C32[b][:], in0=C32[b][:], in1=gtok[0:64, 3, :].unsqueeze(2).to_broadcast([64, 8, 129]), op=ALU.mult),
                 reads=[C32[b], gtok], writes=[C32[b]])
        for gi, (h0, h1) in enumerate(HG):
            bk = bank[5 + gi]
            nh = h1 - h0
            if j > 0:
                P.op("dve", lambda e, bk=bk, h0=h0, h1=h1, nh=nh: e.tensor_tensor(out=C32[b][:, h0:h1, :], in0=C32[b][:, h0:h1, :],
                                                                                  in1=bk[0:64, 0:nh * 129].rearrange("p (h v) -> p h v", v=129), op=ALU.add),
                     reads=[bk, C32[b]], writes=[C32[b]])
            else:
                P.op("dve", lambda e, bk=bk, h0=h0, h1=h1, nh=nh: e.tensor_copy(out=C32[b][:, h0:h1, :], in_=bk[0:64, 0:nh * 129].rearrange("p (h v) -> p h v", v=129)),
                     reads=[bk], writes=[C32[b]] if gi == 0 else [], parts=[] if gi == 0 else [C32[b]])
        P.op("act", lambda e: e.copy(out=Cb[b][:], in_=C32[b][:]), reads=[C32[b]], writes=[Cb[b]])
        for gi, (h0, h1) in enumerate(HG):
            bk = bank[2 + gi]
            nh = h1 - h0
            v3 = bk[:, 0:nh * 129].rearrange("p (h v) -> p h v", v=129)
            P.op("dve", lambda e, v3=v3, h0=h0, h1=h1: e.tensor_tensor(out=dmax[:, h0:h1].unsqueeze(2), in0=v3[:, :, 128:129], in1=gtok[:, 4, h0:h1].unsqueeze(2), op=ALU.max),
                 reads=[bk, gtok], writes=[dmax])
            P.op("dve", lambda e, v3=v3, h0=h0, h1=h1: e.scalar_tensor_tensor(out=dmax[:, h0:h1].unsqueeze(2), in0=v3[:, :, 128:129], scalar=-1.0, in1=dmax[:, h0:h1].unsqueeze(2), op0=ALU.mult, op1=ALU.max),
                 reads=[bk, dmax], writes=[dmax])
        P.op("dve", lambda e: e.reciprocal(out=rc[:], in_=dmax[:]), reads=[dmax], writes=[rc])
        for gi, (h0, h1) in enumerate(HG):
            bk = bank[2 + gi]
            nh = h1 - h0
            v3 = bk[:, 0:nh * 129].rearrange("p (h v) -> p h v", v=129)
            P.op("dve", lambda e, v3=v3, h0=h0, h1=h1, nh=nh: e.tensor_tensor(out=hm[:, h0:h1, :], in0=v3[:, :, 0:128],
                                                                              in1=rc[:, h0:h1].unsqueeze(2).to_broadcast([128, nh, 128]), op=ALU.mult),
                 reads=[bk, rc], writes=[hm] if gi == 0 else [], parts=[] if gi == 0 else [hm])
        P.op("pool", lambda e: e.tensor_tensor(out=hsq[:], in0=hm[:], in1=hm[:], op=ALU.mult), reads=[hm], writes=[hsq])
        P.op("dve", lambda e: e.tensor_reduce(out=hs8[0][:], in_=hsq[:], axis=AX.X, op=ALU.add), reads=[hsq], writes=[hs8[0]])
        rms_rstd(c, "h", hs8[0], 128, hs8[2], hs8[1])
        P.op("dve", lambda e: e.tensor_tensor(out=hm[:], in0=hm[:], in1=hs8[2][:].unsqueeze(2).to_broadcast([128, 8, 128]), op=ALU.mult),
             reads=[hm, hs8[2]], writes=[hm])
        hmf = hm[:].rearrange("p h v -> p (h v)")
        P.op("pool", lambda e: e.tensor_tensor(out=hmf, in0=hmf, in1=mlg[:], op=ALU.mult), reads=[hm, mlg], writes=[hm])
        P.op("dve", lambda e: e.tensor_tensor(out=hn[:], in0=hmf, in1=sgo[:], op=ALU.mult), reads=[hm, sgo], writes=[hn])
        transpose8(c, hn, hnT, bank[7])
        for hv in range(2):
            bk = bank[hv]
            for i in range(8):
                P.op("pe", lambda e, bk=bk, i=i, hv=hv: e.matmul(out=bk[:], lhsT=hnT[:, i, :], rhs=wbm[:, i, hv * 512:(hv + 1) * 512], start=(i == 0), stop=(i == 7)),
                     reads=[hnT, wbm], writes=[bk] if i == 0 else [], parts=[] if i == 0 else [bk])
            P.op("dve", lambda e, bk=bk, hv=hv: e.tensor_tensor(out=mx[:, hv * 512:(hv + 1) * 512], in0=bk[:], in1=sgm[:, hv * 512:(hv + 1) * 512], op=ALU.mult),
                 reads=[bk, sgm], writes=[mx] if hv == 0 else [], parts=[] if hv == 0 else [mx])
        P.op("pool", lambda e: e.tensor_tensor(out=mxb[:], in0=mx[:], in1=ma_t[:], op=ALU.add), reads=[mx, ma_t], writes=[mxb])
        transpose8(c, mxb, mxT, bank[6], eng="dve")
        xo_t = xo[n % 2]
        for hv in range(2):
            bk = bank[2 + hv]
            for i in range(8):
                P.op("pe", lambda e, bk=bk, i=i, hv=hv: e.matmul(out=bk[:], lhsT=mxT[:, i, :], rhs=wout[:, i, hv * 512:(hv + 1) * 512], start=(i == 0), stop=(i == 7)),
                     reads=[mxT, wout], writes=[bk] if i == 0 else [], parts=[] if i == 0 else [bk])
            P.op("dve", lambda e, bk=bk, hv=hv: e.tensor_tensor(out=xo_t[:, hv * 512:(hv + 1) * 512], in0=bk[:], in1=x_t[:, hv * 512:(hv + 1) * 512], op=ALU.add),
                 reads=[bk, x_t], writes=[xo_t] if hv == 0 else [], parts=[] if hv == 0 else [xo_t])
        P.store(c.out[n * 128:(n + 1) * 128, :], xo_t, xo_t[:], dram_writes=[c.x1_t[n]], final=("C" not in c.phases))

    for n in range(NTT):
        tile(n)


def phase_c(c):
    P, NB, NT, NTT = c.P, c.NB, c.NT, c.NTT
    nc = c.nc
    bank = c.bank
    GT = 3
    ex_s = P.dram("ex_s", [128, 128, 2 * D], BF16)
    downT_s = ex_s[:, :, 0:D]
    up_s = ex_s[:, :, D:2 * D]
    downT_t = [T("downT_t%d" % i) for i in range(128)]
    up_t = [T("up_t%d" % i) for i in range(16)]
    up_v = c.peer_up.rearrange("(i c) d -> c i d", c=128)
    dn_v = c.peer_down.rearrange("(i c) d -> c i d", c=128)
    dummy = T("up_dma_sem")
    for u in range(16):
        P.dma(up_s[u * 8:(u + 1) * 8], up_v[u * 8:(u + 1) * 8], writes=[up_t[u]], sem_tile=dummy, eng="pool", max_dma_last_dim=4096)
    P.open_scope()
    dsrc = [P.sb("c_dsrc%d" % i, [128, D], BF16) for i in range(2)]
    dtr = [P.sb("c_dtr%d" % i, [128, D], BF16) for i in range(2)]
    for cb in range(128):
        sl = cb % 2
        P.load(dsrc[sl], dsrc[sl][:], dn_v[cb], eng="pool", max_dma_last_dim=4096)
        pv = bank[6 + sl][:].bitcast(BF16)
        for k in range(8):
            P.op("pe", lambda e, k=k, sl=sl, pv=pv: e.transpose(out=pv[:, k * 128:(k + 1) * 128], in_=dsrc[sl][:, k * 128:(k + 1) * 128], identity=c.identb[:]),
                 reads=[dsrc[sl], c.identb], writes=[bank[6 + sl]] if k == 0 else [], parts=[] if k == 0 else [bank[6 + sl]])
        if sl == 0:
            P.op("act", lambda e, sl=sl, pv=pv: e.copy(out=dtr[sl][:], in_=pv), reads=[bank[6 + sl]], writes=[dtr[sl]])
        else:
            P.op("dve", lambda e, sl=sl, pv=pv: e.tensor_copy(out=dtr[sl][:], in_=pv), reads=[bank[6 + sl]], writes=[dtr[sl]])
        P.store(downT_s[cb], dtr[sl], dtr[sl][:], dram_writes=[downT_t[cb]])
    P.close_scope()
    cstage = c.dbg if (c.dbg and c.phases == "C") else 99
    if cstage == 1:
        return

    wq = P.sb("c_wq", [128, 8, D], BF16)
    load_w_bf16(c, wq, 0, D, c.peer_w_query)
    g2 = P.sb("c_g2", [128, D], F32)
    P.load(g2, g2[:], c.norm2_gain[0].partition_broadcast(128))
    iof = P.sb("c_iof", [128, 128], F32)
    P.load(iof, iof[:], c.cst["iota128"])
    iob = P.sb("c_iob", [128, 128], BF16)
    P.op("dve", lambda e: e.tensor_copy(out=iob[:], in_=iof[:]), reads=[iof], writes=[iob])
    skT = P.sb("c_skT", [128, 8, 128], BF16)
    P.open_scope()
    skf = P.sb("c_skf", [128, 16, 64], F32)
    P.load(skf, skf[:], c.peer_sub_keys.rearrange("h p k d -> k (h p) d"))
    skb = P.sb("c_skb", [128, 16 * 64], BF16)
    P.op("dve", lambda e: e.tensor_copy(out=skb[:], in_=skf[:].rearrange("k a d -> k (a d)")), reads=[skf], writes=[skb])
    transpose8(c, skb, skT, bank[7])
    P.close_scope()

    if cstage == 2:
        return
    x1s = [P.sb("c_x1s%d" % i, [128, D], F32) for i in range(GT)]
    st = (P.sb("c_ssq", [128, 1], F32), P.sb("c_tmp", [128, 1], F32), P.sb("c_rstd", [128, 1], F32))
    xb = P.sb("c_xb", [128, D], BF16)
    xnT = P.sb("c_xnT", [128, 8, GT * 128], BF16)
    qT = P.sb("c_qT", [128, 8, 128], BF16)
    scs = [P.sb("c_sc%d" % i, [128, 16, 128], F32) for i in range(2)]
    sc_ts = [[T("sc_t%d_%d" % (b_, i)) for i in range(16)] for b_ in range(2)]
    v16 = P.sb("c_v16", [128, 16, 16], F32)
    ix = P.sb("c_ix", [128, 16, 16], U32)
    ixf = P.sb("c_ixf", [128, 16, 16], F32)
    v16b, ixb, c16b, posb = T("v16b"), T("ixb"), T("c16b"), T("posb")
    c16 = P.sb("c_c16", [128, 8, 16], F32)
    pos = P.sb("c_pos", [128, 8, 16], U32)
    posf = P.sb("c_posf", [128, 8, 16], F32)
    pki = P.sb("c_pki", [128, 8, 16], I32)
    pkf = P.sb("c_pkf", [128, 8, 16], F32)
    qkf = P.sb("c_qkf", [128, 8, 16], F32)
    decs = [P.sb("c_dec%d" % i, [128, 2, 16, 16], F32) for i in range(4)]
    abg = [P.sb("c_abg%d" % i, [128, 8, 16], F32) for i in range(3)]
    z8 = P.sb("c_z8", [128, 8], F32)
    abgT = [P.sb("c_abgT%d" % i, [128, GT * 128], F32) for i in range(3)]
    CH = 8
    Ach = [P.sb("c_Ach%d" % i, [128, CH, 128], BF16) for i in range(2)]
    Bch = [P.sb("c_Bch%d" % i, [128, CH, 128], BF16) for i in range(2)]
    Wg = P.sb("c_Wg", [128, 128, GT * 128], BF16)
    NS = 4
    esl = [P.sb("c_esl%d" % i, [128, 2 * D], BF16) for i in range(NS)]
    actb = [P.sb("c_act%d" % i, [128, GT * 128], BF16) for i in range(2)]
    wab = [P.sb("c_wa%d" % i, [128, GT * 128], BF16) for i in range(2)]

    groups = []
    n0 = 0
    while n0 < NTT:
        g = min(GT, NTT - n0)
        groups.append((n0, g))
        n0 += g
    blk_ctr = [0]

    def route_front(n, ti, buf):
        sc = scs[buf]
        sc_t = sc_ts[buf]
        x_t = x1s[ti]
        src_x1 = c.x if c.phases == "C" else c.out
        P.load(x_t, x_t[:], src_x1[n * 128:(n + 1) * 128, :], dram_reads=[c.x1_t[n]])
        ssq, tmp, rstd = st
        P.op("act", lambda e: e.activation(out=xb[:], in_=x_t[:], func=AF.Square, accum_out=ssq[:]), reads=[x_t], writes=[xb, ssq])
        rms_rstd(c, "n", ssq, D, rstd, tmp)
        P.op("dve", lambda e: e.scalar_tensor_tensor(out=xb[:], in0=x_t[:], scalar=rstd[:], in1=g2[:], op0=ALU.mult, op1=ALU.mult),
             reads=[x_t, rstd, g2], writes=[xb])
        pvx = bank[7][:].bitcast(BF16)
        for k in range(8):
            P.op("pe", lambda e, k=k: e.transpose(out=pvx[:, k * 128:(k + 1) * 128], in_=xb[:, k * 128:(k + 1) * 128], identity=c.identb[:]),
                 reads=[xb, c.identb], writes=[bank[7]] if k == 0 else [], parts=[] if k == 0 else [bank[7]])
        xt1 = xnT[:, :, ti * 128:(ti + 1) * 128]
        P.op("act", lambda e: e.copy(out=xt1, in_=pvx.rearrange("p (k t) -> p k t", t=128)), reads=[bank[7]], writes=[xnT] if ti == 0 else [], parts=[] if ti == 0 else [xnT])
        if cstage == 29:
            return
        for hd in range(8):
            bk = bank[hd % 2]
            for k in range(8):
                P.op("pe", lambda e, k=k, hd=hd, bk=bk: e.matmul(out=bk[:, 0:128], lhsT=wq[:, k, hd * 128:(hd + 1) * 128], rhs=xt1[:, k, :],
                                                                start=(k == 0), stop=(k == 7)),
                     reads=[wq, xnT], writes=[bk] if k == 0 else [], parts=[] if k == 0 else [bk])
            P.op("act", lambda e, hd=hd, bk=bk: e.copy(out=qT[:, hd, :], in_=bk[:, 0:128]), reads=[bk], writes=[qT] if hd == 0 else [], parts=[] if hd == 0 else [qT])
        if cstage == 30:
            return
        sc4 = sc[:].rearrange("p (h two) k -> p h two k", two=2)
        for par in range(2):
            for hf in range(2):
                bk = bank[2 + par * 2 + hf]
                for u in range(4):
                    hd = hf * 4 + u
                    P.op("pe", lambda e, u=u, hd=hd, par=par, bk=bk: e.matmul(out=bk[:, u * 128:(u + 1) * 128], lhsT=qT[par * 64:(par + 1) * 64, hd, :],
                                                                          rhs=skT[par * 64:(par + 1) * 64, hd, :], start=True, stop=True),
                         reads=[qT, skT], writes=[bk] if u == 0 else [], parts=[] if u == 0 else [bk])
        for par in range(2):
            for hf in range(2):
                bk = bank[2 + par * 2 + hf]
                first = (par == 0 and hf == 0)
                if hf == 0:
                    P.op("act", lambda e, par=par, hf=hf, bk=bk: e.copy(out=sc4[:, hf * 4:hf * 4 + 4, par, :], in_=bk[:].rearrange("p (a k) -> p a k", k=128)),
                         reads=[bk], writes=[sc_t[(hf * 4 + u_) * 2 + par] for u_ in range(4)])
                else:
                    P.op("act", lambda e, par=par, hf=hf, bk=bk: e.copy(out=sc4[:, hf * 4:hf * 4 + 4, par, :], in_=bk[:].rearrange("p (a k) -> p a k", k=128)),
                         reads=[bk], writes=[sc_t[(hf * 4 + u_) * 2 + par] for u_ in range(4)])
        if cstage in (31, 305, 306, 307):
            return

    def route_back(n, ti, buf):
        sc = scs[buf]
        sc_t = sc_ts[buf]
        cand = sc[:].rearrange("p a k -> p (a k)").rearrange("p (h c) -> p h c", c=256)
        cand_t = [[sc_t[2 * h_], sc_t[2 * h_ + 1]] for h_ in range(8)]
        for hp in range(16):
            P.op("dve", lambda e, hp=hp: e.max(out=v16[:, hp, 0:8], in_=sc[:, hp, :]), reads=[sc_t[hp]], writes=[v16] if hp == 0 else [], parts=[] if hp == 0 else [v16])
        for hp in range(16):
            P.op("dve", lambda e, hp=hp: e.max_index(out=ix[:, hp, 0:8], in_max=v16[:, hp, 0:8], in_values=sc[:, hp, :]), reads=[sc_t[hp], v16],
                 writes=[ix] if hp == 0 else [], parts=[] if hp == 0 else [ix])
        for hp in range(16):
            P.op("dve", lambda e, hp=hp: e.match_replace(out=sc[:, hp, :], in_to_replace=v16[:, hp, 0:8], in_values=sc[:, hp, :], imm_value=-1e30),
                 reads=[sc_t[hp], v16], writes=[sc_t[hp]])
        for hp in range(16):
            P.op("dve", lambda e, hp=hp: e.max(out=v16[:, hp, 8:16], in_=sc[:, hp, :]), reads=[sc_t[hp]], writes=[v16b] if hp == 0 else [], parts=[] if hp == 0 else [v16b])
        for hp in range(16):
            P.op("dve", lambda e, hp=hp: e.max_index(out=ix[:, hp, 8:16], in_max=v16[:, hp, 8:16], in_values=sc[:, hp, :]), reads=[sc_t[hp], v16b],
                 writes=[ixb] if hp == 0 else [], parts=[] if hp == 0 else [ixb])
        if cstage == 32:
            return
        P.op("dve", lambda e: e.tensor_copy(out=ixf[:], in_=ix[:]), reads=[ix, ixb], writes=[ixf])
        vv = v16[:].rearrange("p (h two) k -> p h two k", two=2)
        iv = ixf[:].rearrange("p (h two) k -> p h two k", two=2)
        P.op("dve", lambda e: e.tensor_tensor(out=cand.rearrange("p h (a b) -> p h a b", b=16),
                                              in0=vv[:, :, 0, :].unsqueeze(3).to_broadcast([128, 8, 16, 16]),
                                              in1=vv[:, :, 1, :].unsqueeze(2).to_broadcast([128, 8, 16, 16]), op=ALU.add),
             reads=[v16, v16b], writes=list(sc_t))
        for h in range(8):
            P.op("dve", lambda e, h=h: e.max(out=c16[:, h, 0:8], in_=cand[:, h, :]), reads=cand_t[h], writes=[c16] if h == 0 else [], parts=[] if h == 0 else [c16])
        for h in range(8):
            P.op("dve", lambda e, h=h: e.max_index(out=pos[:, h, 0:8], in_max=c16[:, h, 0:8], in_values=cand[:, h, :]), reads=cand_t[h] + [c16],
                 writes=[pos] if h == 0 else [], parts=[] if h == 0 else [pos])
        for h in range(8):
            P.op("dve", lambda e, h=h: e.match_replace(out=cand[:, h, :], in_to_replace=c16[:, h, 0:8], in_values=cand[:, h, :], imm_value=-1e30),
                 reads=cand_t[h] + [c16], writes=cand_t[h])
        for h in range(8):
            P.op("dve", lambda e, h=h: e.max(out=c16[:, h, 8:16], in_=cand[:, h, :]), reads=cand_t[h], writes=[c16b] if h == 0 else [], parts=[] if h == 0 else [c16b])
        for h in range(8):
            P.op("dve", lambda e, h=h: e.max_index(out=pos[:, h, 8:16], in_max=c16[:, h, 8:16], in_values=cand[:, h, :]), reads=cand_t[h] + [c16b],
                 writes=[posb] if h == 0 else [], parts=[] if h == 0 else [posb])
        if cstage == 33:
            return
        P.op("dve", lambda e: e.tensor_copy(out=posf[:], in_=pos[:]), reads=[pos, posb], writes=[posf])
        P.op("dve", lambda e: e.tensor_scalar(out=pkf[:], in0=posf[:], scalar1=-7.5, scalar2=0.0625, op0=ALU.add, op1=ALU.mult), reads=[posf], writes=[pkf])
        P.op("dve", lambda e: e.tensor_copy(out=pki[:], in_=pkf[:]), reads=[pkf], writes=[pki])
        P.op("dve", lambda e: e.tensor_copy(out=pkf[:], in_=pki[:]), reads=[pki], writes=[pkf])
        P.op("dve", lambda e: e.scalar_tensor_tensor(out=qkf[:], in0=pkf[:], scalar=-16.0, in1=posf[:], op0=ALU.mult, op1=ALU.add), reads=[pkf, posf], writes=[qkf])
        combos = [(hh, which, sel, dst) for hh in range(4) for (which, sel, dst) in ((0, pkf, abg[0]), (1, qkf, abg[1]))]
        for half in range(2):
            sub = combos[half * 4:(half + 1) * 4]
            for di, (hh, which, sel, dst) in enumerate(sub):
                hs = slice(hh * 2, hh * 2 + 2)
                dec = decs[di]
                P.op("dve", lambda e, sel=sel, hs=hs, dec=dec: e.tensor_tensor(out=dec[:], in0=iof[:, 0:16].unsqueeze(1).unsqueeze(1).to_broadcast([128, 2, 16, 16]),
                                                                            in1=sel[:, hs, :].unsqueeze(3).to_broadcast([128, 2, 16, 16]), op=ALU.is_equal),
                     reads=[iof, sel], writes=[dec])
            for di, (hh, which, sel, dst) in enumerate(sub):
                hs = slice(hh * 2, hh * 2 + 2)
                dec = decs[di]
                P.op("pool", lambda e, which=which, hs=hs, dec=dec: e.tensor_tensor(out=dec[:], in0=dec[:], in1=iv[:, hs, which, :].unsqueeze(2).to_broadcast([128, 2, 16, 16]), op=ALU.mult),
                     reads=[dec, ixf], writes=[dec])
            for di, (hh, which, sel, dst) in enumerate(sub):
                hs = slice(hh * 2, hh * 2 + 2)
                dec = decs[di]
                firstw = (hh == 0)
                P.op("dve", lambda e, dst=dst, hs=hs, dec=dec: e.tensor_reduce(out=dst[:, hs, :], in_=dec[:], axis=AX.X, op=ALU.add),
                     reads=[dec], writes=[dst] if firstw else [], parts=[] if firstw else [dst])
        if cstage == 34:
            return
        P.op("dve", lambda e: e.tensor_tensor(out=abg[2][:], in0=c16[:], in1=c16[:, :, 0:1].to_broadcast([128, 8, 16]), op=ALU.subtract), reads=[c16, c16b], writes=[abg[2]])
        P.op("act", lambda e: e.activation(out=abg[2][:], in_=abg[2][:], func=AF.Exp), reads=[abg[2]], writes=[abg[2]])
        P.op("dve", lambda e: e.tensor_reduce(out=z8[:], in_=abg[2][:], axis=AX.X, op=ALU.add), reads=[abg[2]], writes=[z8])
        P.op("dve", lambda e: e.reciprocal(out=z8[:], in_=z8[:]), reads=[z8], writes=[z8])
        P.op("dve", lambda e: e.tensor_tensor(out=abg[2][:], in0=abg[2][:], in1=z8[:].unsqueeze(2).to_broadcast([128, 8, 16]), op=ALU.mult), reads=[abg[2], z8], writes=[abg[2]])
        if cstage == 35:
            return
        for i3 in range(3):
            P.op("pe", lambda e, i3=i3: e.transpose(out=bank[6][:, i3 * 128:(i3 + 1) * 128], in_=abg[i3][:].rearrange("p h k -> p (h k)"), identity=c.identf[:]),
                 reads=[abg[i3], c.identf], writes=[bank[6]] if i3 == 0 else [], parts=[] if i3 == 0 else [bank[6]])
        for i3 in range(3):
            P.op("act", lambda e, i3=i3: e.copy(out=abgT[i3][:, ti * 128:(ti + 1) * 128], in_=bank[6][:, i3 * 128:(i3 + 1) * 128]), reads=[bank[6]], parts=[abgT[i3]])
        if cstage == 36:
            return
        for q8 in range(0, 128, CH):
            chn = (q8 // CH) % 2
            tg8 = ti * 128 + q8
            io_bc = iof[:].unsqueeze(1).to_broadcast([128, CH, 128])
            P.op("dve", lambda e, chn=chn, tg8=tg8, io_bc=io_bc: e.tensor_tensor(out=Ach[chn][:], in0=io_bc,
                                                                             in1=abgT[0][:, tg8:tg8 + CH].unsqueeze(2).to_broadcast([128, CH, 128]), op=ALU.is_equal),
                 reads=[iof, abgT[0]], writes=[Ach[chn]])
            P.op("dve", lambda e, chn=chn, tg8=tg8, io_bc=io_bc: e.tensor_tensor(out=Bch[chn][:], in0=io_bc,
                                                                             in1=abgT[1][:, tg8:tg8 + CH].unsqueeze(2).to_broadcast([128, CH, 128]), op=ALU.is_equal),
                 reads=[iof, abgT[1]], writes=[Bch[chn]])
            P.op("pool", lambda e, chn=chn, tg8=tg8: e.tensor_tensor(out=Bch[chn][:], in0=Bch[chn][:],
                                                                   in1=abgT[2][:, tg8:tg8 + CH].unsqueeze(2).to_broadcast([128, CH, 128]), op=ALU.mult),
                 reads=[Bch[chn], abgT[2]], writes=[Bch[chn]])
            for tq in range(q8, q8 + CH, 4):
                bk = bank[(tq // 4) % 2]
                for t in range(tq, tq + 4):
                    tl = t % CH
                    u = t - tq
                    P.op("pe", lambda e, chn=chn, tl=tl, u=u, bk=bk: e.matmul(out=bk[:, u * 128:(u + 1) * 128], lhsT=Ach[chn][:, tl, :], rhs=Bch[chn][:, tl, :], start=True, stop=True),
                         reads=[Ach[chn], Bch[chn]], writes=[bk] if u == 0 else [], parts=[] if u == 0 else [bk])
                tg0 = ti * 128 + tq
                P.op("act", lambda e, bk=bk, tg0=tg0: e.copy(out=Wg[:, :, tg0:tg0 + 4], in_=bk[:].rearrange("p (t c) -> p c t", c=128)),
                     reads=[bk], parts=[Wg])

    def expert_loop(n0, g):
        W = g * 128
        slots = {}

        def down(cb):
            sl = blk_ctr[0] % NS
            blk_ctr[0] += 1
            slots[cb] = sl
            P.load(esl[sl], esl[sl][:], ex_s[cb], dram_reads=[downT_t[cb], up_t[cb // 8]])
            sb_ = bank[6 + cb % 2]
            for k in range(8):
                P.op("pe", lambda e, k=k, sl=sl, sb_=sb_: e.matmul(out=sb_[:, 0:W], lhsT=esl[sl][:, k * 128:(k + 1) * 128], rhs=xnT[:, k, 0:W], start=(k == 0), stop=(k == 7)),
                     reads=[esl[sl], xnT], writes=[sb_] if k == 0 else [], parts=[] if k == 0 else [sb_])
            ab = actb[cb % 2]
            wb = wab[cb % 2]
            P.op("act", lambda e, ab=ab, sb_=sb_: e.activation(out=ab[:, 0:W], in_=sb_[:, 0:W], func=AF.Gelu), reads=[sb_], writes=[ab])
            P.op("dve", lambda e, ab=ab, wb=wb, cb=cb: e.tensor_tensor(out=wb[:, 0:W], in0=ab[:, 0:W], in1=Wg[:, cb, 0:W], op=ALU.mult), reads=[ab, Wg], writes=[wb])

        def up(cb):
            sl = slots[cb]
            wb = wab[cb % 2]
            for tt in range(g):
                for hf in range(2):
                    bk = bank[tt * 2 + hf]
                    P.op("pe", lambda e, tt=tt, hf=hf, bk=bk, wb=wb, sl=sl, cb=cb: e.matmul(out=bk[:], lhsT=wb[:, tt * 128:(tt + 1) * 128], rhs=esl[sl][:, D + hf * 512:D + (hf + 1) * 512],
                                                                                      start=(cb == 0), stop=(cb == 127)),
                         reads=[wb, esl[sl]], writes=[bk] if cb == 0 else [], parts=[] if cb == 0 else [bk])

        down(0)
        for cb in range(128):
            if cb + 1 < 128:
                down(cb + 1)
            up(cb)
        for tt in range(g):
            n = n0 + tt
            x_t = x1s[tt]
            for hf in range(2):
                bk = bank[tt * 2 + hf]
                P.op("dve", lambda e, bk=bk, hf=hf, x_t=x_t: e.tensor_tensor(out=x_t[:, hf * 512:(hf + 1) * 512], in0=bk[:], in1=x_t[:, hf * 512:(hf + 1) * 512], op=ALU.add),
                     reads=[bk, x_t], writes=[x_t])
            P.store(c.out[n * 128:(n + 1) * 128, :], x_t, x_t[:], dram_writes=[c.x1_t[n]], final=True)

    for (n0, g) in groups:
        route_front(n0, 0, 0)
        for ti in range(g):
            if ti + 1 < g:
                route_front(n0 + ti + 1, ti + 1, (ti + 1) % 2)
            route_back(n0 + ti, ti, ti % 2)
        if 3 <= cstage <= 40:
            return
        if cstage == 50:
            continue
        expert_loop(n0, g)


from concourse.bass_utils import run_bass_kernel_spmd

W_NAMES = ["norm1_gain", "w_in", "ml_i_bias", "ml_f_bias", "q_norm_gain", "k_norm_gain", "attn_sinks", "ml_out_norm_gain",
           "w_branch_attn", "w_branch_mlstm", "w_out", "norm2_gain", "peer_w_query", "peer_sub_keys", "peer_down", "peer_up"]
PEER_NAMES = ["norm2_gain", "peer_w_query", "peer_sub_keys", "peer_down", "peer_up"]


def make_in_maps(inputs, NB, NT, ncores, phases="ABC"):
    consts = host_consts()
    S = NT * 128
    maps = []
    shared = {}
    for k in W_NAMES:
        if "C" not in phases and k in PEER_NAMES:
            continue
        shared[k] = np.ascontiguousarray(inputs[k][0])
    for k, v in consts.items():
        shared["c_" + k] = v
    for ci in range(ncores):
        m = dict(shared)
        m["x"] = np.ascontiguousarray(inputs["x"][ci * NB:(ci + 1) * NB, :S]).reshape(NB * S, D)
        m["positions"] = np.ascontiguousarray(inputs["positions"][ci * NB:(ci + 1) * NB, :S]).reshape(NB * S).astype(np.int32)
        maps.append(m)
    return maps


def kernel(**inputs):
    NB, NT, ncores = 2, 32, 8
    nc, _ = build_program(NB, NT, "ABC")
    maps = make_in_maps(inputs, NB, NT, ncores, "ABC")
    res = run_bass_kernel_spmd(nc, maps, core_ids=list(range(ncores)))
    outs = [r["out"].reshape(NB, NT * 128, D) for r in res.results]
    return np.concatenate(outs, axis=0).astype(np.float32)
```
